# Optimizing a Trainium2 kernel written in Bass

```python
import math
import jax, jax.numpy as jnp
from jax import lax
import numpy as np

D_MODEL = 1024
BATCH = 2
SEQ = 8192
DEPTH = 2

P_DIM = 256
N_BRANCH = 4
W_MIX = D_MODEL // 4
HEAD_DIM = 64
N_HEADS = W_MIX // HEAD_DIM
DIFF_DH = HEAD_DIM // 2
ROPE_THETA = 500000.0
ROPE_DIMS = DIFF_DH // 4
Q_BLOCK = 128
RWKV_LORA_W = 64
RWKV_LORA_A = 64
RWKV_LORA_G = 128
RWKV_GN_EPS = 64e-5
DIFF_LN_EPS = 1e-5
GDN_CONV = 4
GDN_CHUNK = 64
FFN_DENSE = 2816
N_EXPERTS = 8
TOP_K = 2
FFN_EXPERT = 3584
MOE_BLOCK = 256
NORM_EPS = 1e-6
N_DENSE = (DEPTH + 1) // 2
N_MOE = DEPTH // 2

RWKV_COLS = 3 * W_MIX + RWKV_LORA_W + RWKV_LORA_A + RWKV_LORA_G
DIFF_COLS = 3 * W_MIX
FOX_COLS = 3 * W_MIX + N_HEADS
GDN_COLS = 4 * W_MIX + 2 * N_HEADS
GATE_COLS = N_BRANCH * D_MODEL
IN_COLS = RWKV_COLS + DIFF_COLS + FOX_COLS + GDN_COLS + GATE_COLS

kernel_name = "hybrid_rwkv7_diffattn_fox_gdn_moe"


def _split(t, sizes):
    return jnp.split(t, [int(c) for c in np.cumsum(sizes)[:-1]], axis=-1)


def rmsnorm(x, g, eps=NORM_EPS):
    xf = x.astype(jnp.float32)
    y = xf * lax.rsqrt(jnp.mean(xf * xf, axis=-1, keepdims=True) + eps)
    return (y * g.astype(jnp.float32)).astype(x.dtype)


def l2norm(x, eps=1e-6):
    xf = x.astype(jnp.float32)
    return xf * lax.rsqrt(jnp.sum(xf * xf, axis=-1, keepdims=True) + eps)


def causal_mask(i, seq):
    qpos = i * Q_BLOCK + jnp.arange(Q_BLOCK)
    return qpos[:, None] >= jnp.arange(seq)[None, :]


def sweep_query_blocks(block_fn, seq):
    out = lax.map(block_fn, jnp.arange(seq // Q_BLOCK))
    nb, b, qb, h, d = out.shape
    return out.transpose(1, 0, 2, 3, 4).reshape(b, nb * qb, h, d)


def partial_rope(x, pos):
    half = ROPE_DIMS // 2
    inv = ROPE_THETA ** (-jnp.arange(half, dtype=jnp.float32) * 2.0 / ROPE_DIMS)
    ang = pos.astype(jnp.float32)[:, None] * inv[None, :]
    shape = (1, pos.shape[0]) + (1,) * (x.ndim - 3) + (half,)
    cos = jnp.cos(ang).reshape(shape)
    sin = jnp.sin(ang).reshape(shape)
    xf = x.astype(jnp.float32)
    x1, x2, rest = xf[..., :half], xf[..., half:ROPE_DIMS], xf[..., ROPE_DIMS:]
    return jnp.concatenate([x1 * cos - x2 * sin, x2 * cos + x1 * sin, rest], axis=-1).astype(x.dtype)


def causal_conv(u, w):
    seq = u.shape[1]
    k = w.shape[-1]
    up = jnp.pad(u, ((0, 0), (k - 1, 0), (0, 0)))
    return sum(up[:, j:j + seq, :] * w[:, j] for j in range(k))


def rwkv7_scan(r, w, k, v, kk, a):
    b, s, h, n = r.shape

    def step(state, inp):
        r_t, w_t, k_t, v_t, kk_t, a_t = inp
        sa = jnp.einsum('bhvk,bhk->bhv', state, -kk_t)
        state = (state * w_t[:, :, None, :]
                 + sa[..., None] * (kk_t * a_t)[:, :, None, :]
                 + v_t[..., None] * k_t[:, :, None, :])
        return state, jnp.einsum('bhvk,bhk->bhv', state, r_t)

    xs = tuple(t.transpose(1, 0, 2, 3) for t in (r, w, k, v, kk, a))
    _, o = lax.scan(step, jnp.zeros((b, h, n, n), jnp.float32), xs)
    return o.transpose(1, 0, 2, 3)


def rwkv7_branch(u, mu, w0, w2, a0, a2, g2, k_k, k_a, r_k, ln_w, ln_b):
    b, s, _ = u.shape
    u = u.astype(jnp.float32)
    u_prev = jnp.pad(u, ((0, 0), (1, 0), (0, 0)))[:, :s]
    xm = u + (u_prev - u) * mu
    r, k, v, xw, xa, xg = _split(xm, [W_MIX, W_MIX, W_MIX, RWKV_LORA_W, RWKV_LORA_A, RWKV_LORA_G])
    logw = -jax.nn.softplus(-(w0 + jnp.tanh(xw) @ w2)) - 0.5
    decay = jnp.exp(-jnp.exp(logw))
    a = jax.nn.sigmoid(a0 + xa @ a2)
    g = jax.nn.sigmoid(xg) @ g2
    heads = lambda t: t.reshape(b, s, N_HEADS, HEAD_DIM)
    kk = l2norm(heads(k * k_k))
    k = k * (1.0 + (a - 1.0) * k_a)
    r_h, k_h, v_h, w_h, a_h = map(heads, (r, k, v, decay, a))
    o = rwkv7_scan(r_h, w_h, k_h, v_h, kk, a_h)
    mean = jnp.mean(o, axis=-1, keepdims=True)
    var = jnp.mean((o - mean) ** 2, axis=-1, keepdims=True)
    o = ((o - mean) * lax.rsqrt(var + RWKV_GN_EPS)).reshape(b, s, W_MIX) * ln_w + ln_b
    bonus = jnp.sum(r_h * k_h * r_k, axis=-1, keepdims=True) * v_h
    return (o + bonus.reshape(b, s, W_MIX)) * g


def diff_branch(u, pos, lam_p, subln_w, layer_idx):
    b, s, _ = u.shape
    q, k, v = _split(u, [W_MIX, W_MIX, W_MIX])
    q = partial_rope(q.reshape(b, s, N_HEADS, 2, DIFF_DH), pos)
    k = partial_rope(k.reshape(b, s, N_HEADS, 2, DIFF_DH), pos)
    v = v.reshape(b, s, N_HEADS, HEAD_DIM)
    lam_init = 0.8 - 0.6 * math.exp(-0.3 * layer_idx)
    lp = lam_p.astype(jnp.float32)
    lam = jnp.exp(jnp.sum(lp[0] * lp[1])) - jnp.exp(jnp.sum(lp[2] * lp[3])) + lam_init
    scale = DIFF_DH ** -0.5

    def blk(i):
        qs = lax.dynamic_slice_in_dim(q, i * Q_BLOCK, Q_BLOCK, axis=1)
        sc = jnp.einsum('bqhcd,bkhcd->bhcqk', qs, k).astype(jnp.float32) * scale
        sc = jnp.where(causal_mask(i, s)[None, None, None], sc, -jnp.inf)
        pm = jax.nn.softmax(sc, axis=-1)
        pd = pm[:, :, 0] - lam * pm[:, :, 1]
        return jnp.einsum('bhqk,bkhd->bqhd', pd.astype(v.dtype), v)

    o = sweep_query_blocks(blk, s)
    o = rmsnorm(o, subln_w, DIFF_LN_EPS) * (1.0 - lam_init)
    return o.reshape(b, s, W_MIX)


def fox_branch(u, f_bias):
    b, s, _ = u.shape
    q, k, v, fl = _split(u, [W_MIX, W_MIX, W_MIX, N_HEADS])
    q = q.reshape(b, s, N_HEADS, HEAD_DIM)
    k = k.reshape(b, s, N_HEADS, HEAD_DIM)
    v = v.reshape(b, s, N_HEADS, HEAD_DIM)
    logf = jax.nn.log_sigmoid(fl.astype(jnp.float32) + f_bias.astype(jnp.float32))
    c = jnp.cumsum(logf, axis=1).transpose(0, 2, 1)
    scale = HEAD_DIM ** -0.5

    def blk(i):
        qs = lax.dynamic_slice_in_dim(q, i * Q_BLOCK, Q_BLOCK, axis=1)
        cq = lax.dynamic_slice_in_dim(c, i * Q_BLOCK, Q_BLOCK, axis=2)
        sc = jnp.einsum('bqhd,bkhd->bhqk', qs, k).astype(jnp.float32) * scale
        sc = sc + (cq[..., :, None] - c[:, :, None, :])
        sc = jnp.where(causal_mask(i, s)[None, None], sc, -jnp.inf)
        pm = jax.nn.softmax(sc, axis=-1)
        return jnp.einsum('bhqk,bkhd->bqhd', pm.astype(v.dtype), v)

    return sweep_query_blocks(blk, s).reshape(b, s, W_MIX)


def gated_delta_chunked(q, k, v, g, beta):
    b, s, h, dk = q.shape
    dv = v.shape[-1]
    c = GDN_CHUNK
    nc = s // c
    chunks = lambda t: t.reshape(b, nc, c, h, t.shape[-1]).transpose(1, 0, 3, 2, 4)
    q, k, v = chunks(q), chunks(k), chunks(v)
    g = g.reshape(b, nc, c, h).transpose(1, 0, 3, 2)
    beta = beta.reshape(b, nc, c, h).transpose(1, 0, 3, 2)
    gam = jnp.cumsum(g, axis=-1)
    idx = jnp.arange(c)
    incl = idx[:, None] >= idx[None, :]
    strict = idx[:, None] > idx[None, :]
    decay = jnp.exp(jnp.where(incl, gam[..., :, None] - gam[..., None, :], -jnp.inf))
    kk = jnp.einsum('nbhik,nbhjk->nbhij', k, k)
    a_mat = jnp.where(strict, beta[..., :, None] * decay * kk, 0.0) + jnp.eye(c, dtype=jnp.float32)
    rhs = jnp.concatenate([beta[..., None] * v, (beta * jnp.exp(gam))[..., None] * k], axis=-1)
    sol = lax.linalg.triangular_solve(a_mat, rhs, left_side=True, lower=True, unit_diagonal=True)
    u0, wm = sol[..., :dv], sol[..., dv:]
    qk = jnp.einsum('nbhik,nbhjk->nbhij', q, k) * decay

    def step(state, xs):
        qc, kc, u0c, wc, qkc, gc = xs
        uc = u0c - jnp.einsum('bhck,bhkv->bhcv', wc, state)
        o = jnp.exp(gc)[..., None] * jnp.einsum('bhck,bhkv->bhcv', qc, state) + jnp.einsum('bhij,bhjv->bhiv', qkc, uc)
        gl = gc[..., -1:]
        state = jnp.exp(gl)[..., None] * state + jnp.einsum('bhjk,bhjv->bhkv', kc * jnp.exp(gl - gc)[..., None], uc)
        return state, o

    _, o = lax.scan(step, jnp.zeros((b, h, dk, dv), jnp.float32), (q, k, u0, wm, qk, gam))
    return o.transpose(1, 0, 3, 2, 4).reshape(b, s, h, dv)


def gdn_branch(u, conv_w, a_log, dt_bias, norm_w):
    b, s, _ = u.shape
    qkv, b_l, a_l, gate = _split(u, [3 * W_MIX, N_HEADS, N_HEADS, W_MIX])
    qkv = jax.nn.silu(causal_conv(qkv, conv_w))
    q, k, v = _split(qkv, [W_MIX, W_MIX, W_MIX])
    heads = lambda t: t.reshape(b, s, N_HEADS, HEAD_DIM)
    q = l2norm(heads(q)) * (HEAD_DIM ** -0.5)
    k = l2norm(heads(k))
    v = heads(v).astype(jnp.float32)
    beta = jax.nn.sigmoid(b_l.astype(jnp.float32))
    g = -jnp.exp(a_log.astype(jnp.float32)) * jax.nn.softplus(a_l.astype(jnp.float32) + dt_bias)
    o = gated_delta_chunked(q, k, v, g, beta)
    o = rmsnorm(o, norm_w) * jax.nn.silu(heads(gate).astype(jnp.float32))
    return o.reshape(b, s, W_MIX)


def hybrid_mixer(h, pos, layer_idx, w_in, w_bo, w_out,
                 rwkv_mu, rwkv_w0, rwkv_w2, rwkv_a0, rwkv_a2, rwkv_g2, rwkv_kk, rwkv_ka, rwkv_rk,
                 rwkv_ln_w, rwkv_ln_b, diff_lam, diff_subln, fox_fbias,
                 gdn_conv, gdn_a_log, gdn_dt_bias, gdn_norm):
    b, s, _ = h.shape
    u = h @ w_in
    u_a, u_b, u_c, u_d, u_g = _split(u, [RWKV_COLS, DIFF_COLS, FOX_COLS, GDN_COLS, GATE_COLS])
    y_a = rwkv7_branch(u_a, rwkv_mu, rwkv_w0, rwkv_w2, rwkv_a0, rwkv_a2, rwkv_g2,
                       rwkv_kk, rwkv_ka, rwkv_rk, rwkv_ln_w, rwkv_ln_b).astype(h.dtype)
    y_b = diff_branch(u_b, pos, diff_lam, diff_subln, layer_idx).astype(h.dtype)
    y_c = fox_branch(u_c, fox_fbias).astype(h.dtype)
    y_d = gdn_branch(u_d, gdn_conv, gdn_a_log, gdn_dt_bias, gdn_norm).astype(h.dtype)
    ys = jnp.stack([y_a, y_b, y_c, y_d], axis=2)
    proj = jnp.einsum('bsnw,nwd->bsnd', ys, w_bo)
    gates = jax.nn.sigmoid(u_g.reshape(b, s, N_BRANCH, D_MODEL))
    return jnp.sum(gates * proj, axis=2) @ w_out


def swiglu(h, wg, wu, wd):
    return (jax.nn.silu(h @ wg) * (h @ wu)) @ wd


def moe_swiglu(h, router, wg, wu, wd):
    b, s, d = h.shape
    n = b * s
    xf = h.reshape(n, d)
    logits = (xf @ router).astype(jnp.float32)
    top_l, top_i = lax.top_k(logits, TOP_K)
    gate = jax.nn.softmax(top_l, axis=-1)
    na = n * TOP_K
    e = top_i.reshape(na)
    tok = jnp.arange(na) // TOP_K
    order = jnp.argsort(e)
    e_s, tok_s, w_s = e[order], tok[order], gate.reshape(na)[order]
    counts = jnp.zeros((N_EXPERTS,), jnp.int32).at[e].add(1)
    pcounts = (counts + MOE_BLOCK - 1) // MOE_BLOCK * MOE_BLOCK
    offs = jnp.cumsum(counts) - counts
    pends = jnp.cumsum(pcounts)
    poffs = pends - pcounts
    dest = poffs[e_s] + (jnp.arange(na) - offs[e_s])
    nb = (na + MOE_BLOCK - 1) // MOE_BLOCK + N_EXPERTS
    xbuf = jnp.zeros((nb * MOE_BLOCK, d), xf.dtype).at[dest].set(xf[tok_s])
    starts = jnp.arange(nb) * MOE_BLOCK
    blk_e = jnp.minimum(jnp.sum(pends[None, :] <= starts[:, None], axis=1), N_EXPERTS - 1)

    def run(args):
        xb, eb = args
        return (jax.nn.silu(xb @ wg[eb]) * (xb @ wu[eb])) @ wd[eb]

    ybuf = lax.map(run, (xbuf.reshape(nb, MOE_BLOCK, d), blk_e)).reshape(nb * MOE_BLOCK, d)
    y = jax.ops.segment_sum(ybuf[dest] * w_s[:, None].astype(xf.dtype), tok_s, num_segments=n)
    return y.reshape(b, s, d)


def setup_inputs(seed: int = 0) -> dict:
    key = jax.random.key(seed)
    ks = jax.random.split(key, 36)
    nrm = lambda i, shape, sc: sc * jax.random.normal(ks[i], shape, jnp.float32)
    uni = lambda i, shape, lo, hi: jax.random.uniform(ks[i], shape, jnp.float32, lo, hi)
    dt = jnp.exp(uni(24, (DEPTH, N_HEADS), math.log(1e-3), math.log(1e-1)))
    return {
        "x": nrm(0, (BATCH, SEQ, D_MODEL), 1.0),
        "p": nrm(1, (DEPTH, BATCH, SEQ, P_DIM), 1.0),
        "norm_mix": 1.0 + nrm(2, (DEPTH, D_MODEL), 0.02),
        "norm_ffn": 1.0 + nrm(3, (DEPTH, D_MODEL), 0.02),
        "norm_ple": 1.0 + nrm(4, (DEPTH, D_MODEL), 0.02),
        "w_in": nrm(5, (DEPTH, D_MODEL, IN_COLS), D_MODEL ** -0.5),
        "w_bo": nrm(6, (DEPTH, N_BRANCH, W_MIX, D_MODEL), W_MIX ** -0.5),
        "w_out": nrm(7, (DEPTH, D_MODEL, D_MODEL), D_MODEL ** -0.5),
        "rwkv_mu": uni(8, (DEPTH, RWKV_COLS), 0.0, 1.0),
        "rwkv_w0": uni(9, (DEPTH, W_MIX), -4.0, 0.0),
        "rwkv_w2": nrm(10, (DEPTH, RWKV_LORA_W, W_MIX), 0.5 * RWKV_LORA_W ** -0.5),
        "rwkv_a0": nrm(11, (DEPTH, W_MIX), 0.1),
        "rwkv_a2": nrm(12, (DEPTH, RWKV_LORA_A, W_MIX), RWKV_LORA_A ** -0.5),
        "rwkv_g2": nrm(13, (DEPTH, RWKV_LORA_G, W_MIX), RWKV_LORA_G ** -0.5),
        "rwkv_kk": 0.85 + nrm(14, (DEPTH, W_MIX), 0.05),
        "rwkv_ka": 1.0 + nrm(15, (DEPTH, W_MIX), 0.05),
        "rwkv_rk": nrm(16, (DEPTH, N_HEADS, HEAD_DIM), 0.1),
        "rwkv_ln_w": 1.0 + nrm(17, (DEPTH, W_MIX), 0.02),
        "rwkv_ln_b": nrm(18, (DEPTH, W_MIX), 0.02),
        "diff_lam": nrm(19, (DEPTH, 4, DIFF_DH), 0.1),
        "diff_subln": 1.0 + nrm(20, (DEPTH, HEAD_DIM), 0.02),
        "fox_fbias": 3.0 + nrm(21, (DEPTH, N_HEADS), 0.5),
        "gdn_conv": nrm(22, (DEPTH, 3 * W_MIX, GDN_CONV), GDN_CONV ** -0.5),
        "gdn_a_log": jnp.log(uni(23, (DEPTH, N_HEADS), 1.0, 16.0)),
        "gdn_dt_bias": dt + jnp.log(-jnp.expm1(-dt)),
        "gdn_norm": 1.0 + nrm(25, (DEPTH, HEAD_DIM), 0.02),
        "ffn_w_gate": nrm(26, (N_DENSE, D_MODEL, FFN_DENSE), D_MODEL ** -0.5),
        "ffn_w_up": nrm(27, (N_DENSE, D_MODEL, FFN_DENSE), D_MODEL ** -0.5),
        "ffn_w_down": nrm(28, (N_DENSE, FFN_DENSE, D_MODEL), FFN_DENSE ** -0.5),
        "moe_router": nrm(29, (N_MOE, D_MODEL, N_EXPERTS), D_MODEL ** -0.5),
        "moe_w_gate": nrm(30, (N_MOE, N_EXPERTS, D_MODEL, FFN_EXPERT), D_MODEL ** -0.5),
        "moe_w_up": nrm(31, (N_MOE, N_EXPERTS, D_MODEL, FFN_EXPERT), D_MODEL ** -0.5),
        "moe_w_down": nrm(32, (N_MOE, N_EXPERTS, FFN_EXPERT, D_MODEL), FFN_EXPERT ** -0.5),
        "ple_proj": nrm(33, (DEPTH, P_DIM, D_MODEL), P_DIM ** -0.5),
        "ple_gate": nrm(34, (DEPTH, D_MODEL, D_MODEL), D_MODEL ** -0.5),
        "final_norm": 1.0 + nrm(35, (D_MODEL,), 0.02),
    }


def reference(x, p, norm_mix, norm_ffn, norm_ple, w_in, w_bo, w_out,
              rwkv_mu, rwkv_w0, rwkv_w2, rwkv_a0, rwkv_a2, rwkv_g2, rwkv_kk, rwkv_ka, rwkv_rk,
              rwkv_ln_w, rwkv_ln_b, diff_lam, diff_subln, fox_fbias,
              gdn_conv, gdn_a_log, gdn_dt_bias, gdn_norm,
              ffn_w_gate, ffn_w_up, ffn_w_down,
              moe_router, moe_w_gate, moe_w_up, moe_w_down,
              ple_proj, ple_gate, final_norm):
    pos = jnp.arange(x.shape[1])
    for i in range(DEPTH):
        h = rmsnorm(x, norm_mix[i])
        x = x + hybrid_mixer(h, pos, i, w_in[i], w_bo[i], w_out[i],
                             rwkv_mu[i], rwkv_w0[i], rwkv_w2[i], rwkv_a0[i], rwkv_a2[i], rwkv_g2[i],
                             rwkv_kk[i], rwkv_ka[i], rwkv_rk[i], rwkv_ln_w[i], rwkv_ln_b[i],
                             diff_lam[i], diff_subln[i], fox_fbias[i],
                             gdn_conv[i], gdn_a_log[i], gdn_dt_bias[i], gdn_norm[i])
        h = rmsnorm(x, norm_ffn[i])
        j = i // 2
        if i % 2 == 0:
            x = x + swiglu(h, ffn_w_gate[j], ffn_w_up[j], ffn_w_down[j])
        else:
            x = x + moe_swiglu(h, moe_router[j], moe_w_gate[j], moe_w_up[j], moe_w_down[j])
        h = rmsnorm(x, norm_ple[i])
        x = x + jax.nn.sigmoid(h @ ple_gate[i]) * (p[i] @ ple_proj[i])
    return rmsnorm(x, final_norm)
```

```python
import math
import contextlib
import numpy as np
import concourse.bass as bass
import concourse.mybir as mybir
from concourse.bass_utils import run_bass_kernel_spmd


F32 = mybir.dt.float32
BF16 = mybir.dt.bfloat16
AF = mybir.ActivationFunctionType
ALU = mybir.AluOpType
AX = mybir.AxisListType

COMPUTE = ("tensor", "vector", "scalar", "gpsimd")
QUEUES = ("sync", "gpsimd", "scalar")


class Chan:
    def __init__(self, sem):
        self.sem = sem
        self.n = 0
        self.T = 0
        self.issue_waited = 0


class Prog:
    def __init__(self, nc, n_chan=12):
        self.nc = nc
        self.es = contextlib.ExitStack()
        self.ops = {e: [] for e in ("tensor", "vector", "scalar", "gpsimd", "sync")}
        self.cnt = {e: 0 for e in COMPUTE}
        self.sem = {e: self.es.enter_context(nc.semaphore("s_" + e)) for e in COMPUTE}
        self.chans = [Chan(self.es.enter_context(nc.semaphore("c%d" % i))) for i in range(n_chan)]
        self.rr = 0
        self.last_w = {}
        self.readers = {}
        self.known = {e: {} for e in self.ops}
        self.tensors = {}
        self.pes = None
        self.phase_id = 0
        self.flush_id = 0
        self.qcache = {}

    def sb(self, name, shape, dt=F32):
        es = self.pes if self.pes is not None else self.es
        t = es.enter_context(self.nc.sbuf_tensor("sb%d_" % self.phase_id + name, list(shape), dt))
        return t

    def phase_begin(self):
        self.phase_id += 1
        self.pes = contextlib.ExitStack()

    def phase_end(self):
        self.barrier()
        self.flush()
        self.pes.close()
        self.pes = None

    def barrier(self):
        for e in self.ops:
            waits = []
            kn = self.known[e]
            for e2 in COMPUTE:
                v = self.cnt[e2]
                if v and kn.get(("eng", e2), 0) < v:
                    kn[("eng", e2)] = v
                    waits.append((self.sem[e2], v))
            for ci, ch in enumerate(self.chans):
                if ch.n and kn.get(("chan", ci), 0) < ch.n:
                    kn[("chan", ci)] = ch.n
                    waits.append((ch.sem, 16 * ch.n))
                ch.T = ch.n
                ch.issue_waited = ch.n
            if waits:
                self.ops[e].append((None, waits, None))
        self.last_w = {}
        self.readers = {}

    def flush(self):
        nc = self.nc
        self.flush_id += 1
        with nc.Block() as block:
            def mk(engname):
                def body(e):
                    for fn, waits, inc in self.ops[engname]:
                        for s, v in waits:
                            e.wait_ge(s, v)
                        if fn is not None:
                            ins = fn(e)
                            if inc is not None:
                                ins.then_inc(inc[0], inc[1])
                return body
            block.sync(mk("sync"))
            block.tensor(mk("tensor"))
            block.vector(mk("vector"))
            block.scalar(mk("scalar"))
            block.gpsimd(mk("gpsimd"))
        self.ops = {e: [] for e in self.ops}

    def ps(self, name, shape, dt=F32):
        t = self.es.enter_context(self.nc.psum_tensor("ps_" + name, list(shape), dt))
        return t

    def _dep_waits(self, eng, reads, writes):
        deps = []
        for k in reads:
            w = self.last_w.get(k)
            if w is not None:
                deps.append(w)
        relax = getattr(self, "relax_same_engine", False)
        for k in writes:
            w = self.last_w.get(k)
            if w is not None and not (relax and w[0] == "eng" and w[1] == eng):
                deps.append(w)
            for rd in self.readers.get(k, ()):
                if relax and rd[0] == "eng" and rd[1] == eng:
                    continue
                deps.append(rd)
        waits = {}
        for d in deps:
            if d[0] == "eng":
                _, e2, n = d
                if e2 == eng and eng == "tensor":
                    continue
                key = ("eng", e2)
                waits[key] = max(waits.get(key, 0), n)
            else:
                _, ci = d
                ch = self.chans[ci]
                key = ("chan", ci)
                waits[key] = max(waits.get(key, 0), ch.n)
                ch.T = max(ch.T, ch.n)
        out = []
        kn = self.known[eng]
        for key, v in waits.items():
            if kn.get(key, 0) >= v:
                continue
            kn[key] = v
            if key[0] == "eng":
                out.append((self.sem[key[1]], v))
            else:
                out.append((self.chans[key[1]].sem, 16 * v))
        return out

    def _record(self, tag, reads, writes):
        for k in reads:
            self.readers.setdefault(k, []).append(tag)
        for k in writes:
            self.last_w[k] = tag
            self.readers[k] = []

    def op(self, eng, fn, reads=(), writes=()):
        waits = self._dep_waits(eng, reads, writes)
        self.cnt[eng] += 1
        n = self.cnt[eng]
        self.ops[eng].append((fn, waits, (self.sem[eng], 1)))
        self._record(("eng", eng, n), reads, writes)

    def dma(self, out, in_, reads=(), writes=(), q="sync", chan=None, **kw):
        if chan is None:
            chan = self.rr
            self.rr = (self.rr + 1) % len(self.chans)
        ch = self.chans[chan]
        waits = self._dep_waits(q, reads, writes)
        if ch.T > ch.issue_waited:
            kn = self.known[q]
            key = ("chan", chan)
            if kn.get(key, 0) < ch.T:
                kn[key] = ch.T
                waits.append((ch.sem, 16 * ch.T))
            ch.issue_waited = ch.T
        ch.n += 1
        def fn(e, out=out, in_=in_, kw=kw):
            o = out(e) if callable(out) else out
            i = in_(e) if callable(in_) else in_
            try:
                return e.dma_start(out=o, in_=i, **kw)
            except Exception:
                print("DMA FAIL out=", o, " in=", i)
                raise
        self.ops[q].append((fn, waits, (ch.sem, 16)))
        self._record(("chan", chan), reads, writes)

    def qid(self, e):
        key = (self.flush_id, id(e))
        if key not in self.qcache:
            self.qcache[key] = e.snap(e.partition_id() % 4, min_val=0, max_val=3)
        return self.qcache[key]

    def collective(self, kind, groups, src, dst, reads=(), writes=(), block=True):
        if not hasattr(self, "cc_sem"):
            self.cc_sem = self.es.enter_context(self.nc.semaphore("cc_sem")); self.cc_n = 0
        waits = self._dep_waits("gpsimd", reads, writes)
        self.cc_n += 1
        n = self.cc_n
        self.ops["gpsimd"].append((lambda e: e.collective_compute(kind, ALU.bypass, replica_groups=groups, ins=[src], outs=[dst]), waits, (self.cc_sem, 1)))
        if block:
            self.collective_wait(n)
        return n

    def collective_wait(self, n=None):
        n = self.cc_n if n is None else n
        for e in self.ops:
            self.ops[e].append((None, [(self.cc_sem, n)], None))

    def wait_all_dma(self, eng="sync"):
        waits = []
        for ch in self.chans:
            if ch.n:
                waits.append((ch.sem, 16 * ch.n))
        self.ops[eng].append((None, waits, None))

    def emit(self):
        self.flush()
        self.es.close()


S = 8192
D = 1024
NTB = S // 512
NT = S // 128

def colsel(h):
    cm = []
    r0 = 0; dq = 1024; dk = 1280; dv = 1536
    fq = 1792; fk = 2048; fv = 2304; ffl = 2560
    gq = 2564; gk = 2820; gv = 3076; gb = 3332; ga = 3336; gg = 3340
    hs = np.arange(64) + h * 64

    def swap(base):
        idx = base + hs
        sw = idx.copy()
        for c in range(2):
            for d in range(4):
                sw[c * 32 + d] = idx[c * 32 + d + 4]
                sw[c * 32 + d + 4] = idx[c * 32 + d]
        return sw
    cm.append(("qd", dq + hs)); cm.append(("qds", swap(dq)))
    cm.append(("kd", dk + hs)); cm.append(("kds", swap(dk)))
    cm.append(("qf", fq + hs)); cm.append(("kf", fk + hs))
    cm.append(("fl", np.array([ffl + h])))
    cm.append(("rr", 0 + hs)); cm.append(("rk", 256 + hs)); cm.append(("rv", 512 + hs))
    cm.append(("xw", 768 + np.arange(64))); cm.append(("xa", 832 + np.arange(64)))
    cm.append(("xg", 896 + np.arange(128)))
    cm.append(("gq", gq + hs)); cm.append(("gk", gk + hs)); cm.append(("gv", gv + hs))
    cm.append(("gb", np.full(128, gb + h))); cm.append(("ga", np.full(128, ga + h)))
    names = {}
    idx = []
    o = 0
    for n, ix in cm:
        names[n] = (o, len(ix)); o += len(ix); idx.append(ix)
    tm = [("vd", dv + hs), ("vf", fv + hs), ("ggt", gg + hs)]
    tnames = {}; tidx = []; o = 0
    for n, ix in tm:
        tnames[n] = (o, len(ix)); o += len(ix); tidx.append(ix)
    return names, np.concatenate(idx), tnames, np.concatenate(tidx)


def rope_tables():
    pos = np.arange(S, dtype=np.float32)
    inv = (500000.0 ** (-np.arange(4, dtype=np.float32) * 2.0 / 8)).astype(np.float32)
    ang = pos[None, :] * inv[:, None]
    cos = np.cos(ang).astype(np.float32); sin = np.sin(ang).astype(np.float32)
    ct = np.ones((64, S), np.float32); st = np.zeros((64, S), np.float32)
    for c in range(2):
        for d in range(4):
            ct[c * 32 + d] = cos[d]; ct[c * 32 + d + 4] = cos[d]
            st[c * 32 + d] = -sin[d]; st[c * 32 + d + 4] = sin[d]
    return np.stack([ct, st])


class KA:
    def __init__(self, layer_idx, do=("diff", "fox", "rwkv", "gdn")):
        self.layer_idx = layer_idx
        self.do = do
        self.names, _, self.tnames, _ = colsel(0)
        self.NCc = sum(n for _, n in self.names.values())
        self.NCv = sum(n for _, n in self.tnames.values())

    def declare(self, nc, sfx=""):
        NCc, NCv = self.NCc, self.NCv
        I = {}
        def inp(name, shape):
            I[name] = nc.dram_tensor(name + sfx, list(shape), F32, kind="ExternalInput").ap()
        inp("wc", [D, NCc]); inp("wv", [D, NCv]); inp("gm", [128, 8])
        inp("dlam", [1, 128]); inp("dsub", [1, 64]); inp("fbias", [1, 1])
        inp("rp", [128, 16]); inp("w2h", [64, 64]); inp("a2h", [64, 64]); inp("g2h", [128, 64]); inp("rln", [2, 64])
        inp("gp", [128, 16]); inp("gnorm", [1, 64])
        self.I = I
        self.UT = nc.dram_tensor("ut" + sfx, [NCc, S], F32).ap()
        self.UV = nc.dram_tensor("uv" + sfx, [S, NCv], F32).ap()
        self.sfx = sfx

    def emit(self, nc, P, shared, xsrc, rope, Ydst):
        self.nc = nc; self.P = P
        self.pb = shared["pb"]; self.ident = shared["ident"]
        self.mask = shared["mask"]; self.BTi = shared["BTi"]; self.BTe = shared["BTe"]; self.BTeT = shared["BTeT"]; self.ones64 = shared["ones64"]
        self.I["x"] = xsrc; self.I["rope"] = rope
        self.Y = Ydst
        P.phase_begin(); self.phase0(); P.phase_end()
        if "diff" in self.do or "fox" in self.do:
            P.phase_begin()
            if "diff" in self.do:
                self.diff()
            if "fox" in self.do:
                self.fox()
            P.phase_end()
            for nm in ("qT", "stg", "on"):
                if hasattr(self, nm):
                    delattr(self, nm)
        gens = []
        if "rwkv" in self.do or "gdn" in self.do:
            P.phase_begin()
            if "rwkv" in self.do:
                gens.append(self.rwkv())
            if "gdn" in self.do:
                gens.append(self.gdn())
            while gens:
                for g in list(gens):
                    try:
                        next(g)
                    except StopIteration:
                        gens.remove(g)
            P.phase_end()

    @staticmethod
    def make_shared(nc, P):
        sh = {}
        sh["pb"] = [P.ps("pb%d" % i, [128, 512]) for i in range(8)]
        ident = P.sb("ident", [128, 128]); sh["ident"] = ident
        P.op("gpsimd", lambda e: e.memset(ident[:], 1.0), writes=["ident"])
        P.op("gpsimd", lambda e: e.affine_select(out=ident[:], in_=ident[:], compare_op=ALU.is_equal,
                                                   fill=0.0, base=0, pattern=[[-1, 128]], channel_multiplier=1),
             reads=["ident"], writes=["ident"])
        tmp = KA(0); tmp.P = P; tmp.nc = nc
        tmp.attn_common_masks()
        sh["mask"] = tmp.mask; sh["BTi"] = tmp.BTi; sh["BTe"] = tmp.BTe; sh["BTeT"] = tmp.BTeT; sh["ones64"] = tmp.ones64
        return sh

    def build(self):
        nc = bass.Bass("TRN2", target_bir_lowering=False)
        self.declare(nc)
        xsrc = nc.dram_tensor("x", [S, D], F32, kind="ExternalInput").ap()
        rope = nc.dram_tensor("rope", [2, 64, S], F32, kind="ExternalInput").ap()
        dbg = getattr(self, "dbg", False)
        Y = nc.dram_tensor("y", [S, 4, 64], F32, kind="ExternalOutput").ap()
        P = Prog(nc, n_chan=16)
        P.relax_same_engine = getattr(self, 'relax', False)
        sh = KA.make_shared(nc, P)
        self.emit(nc, P, sh, xsrc, rope, Y)
        P.wait_all_dma("sync")
        P.emit()
        return nc

    def tt(self, eng, out, in0, in1, op, r, w):
        self.P.op(eng, lambda e: e.tensor_tensor(out=out, in0=in0, in1=in1, op=op), reads=r, writes=w)

    def ts(self, eng, out, in0, s1, op0, r, w, s2=None, op1=None):
        if op1 is None:
            self.P.op(eng, lambda e: e.tensor_scalar(out=out, in0=in0, scalar1=s1, scalar2=None, op0=op0), reads=r, writes=w)
        else:
            self.P.op(eng, lambda e: e.tensor_scalar(out=out, in0=in0, scalar1=s1, scalar2=s2, op0=op0, op1=op1), reads=r, writes=w)

    def stt(self, eng, out, in0, sc, in1, op0, op1, r, w):
        eng = "vector"
        self.P.op(eng, lambda e: e.scalar_tensor_tensor(out=out, in0=in0, scalar=sc, in1=in1, op0=op0, op1=op1), reads=r, writes=w)

    def act(self, out, in_, func, r, w, **kw):
        self.P.op("scalar", lambda e: e.activation(out=out, in_=in_, func=func, **kw), reads=r, writes=w)

    def cp(self, eng, out, in_, r, w):
        if eng == "scalar":
            self.P.op(eng, lambda e: e.copy(out=out, in_=in_), reads=r, writes=w)
        else:
            self.P.op(eng, lambda e: e.tensor_copy(out=out, in_=in_), reads=r, writes=w)

    def mm(self, out, lhsT, rhs, r, w, start=True, stop=True):
        self.P.op("tensor", lambda e: e.matmul(out, lhsT=lhsT, rhs=rhs, start=start, stop=stop), reads=r, writes=w)

    def tr(self, out, in_, r, w, n=128):
        self.P.op("tensor", lambda e: e.transpose(out=out, in_=in_, identity=self.ident[0:n, 0:n]), reads=list(r) + ["ident"], writes=w)

    def ms(self, eng, out, val, w):
        self.P.op(eng, lambda e: e.memset(out, val), writes=w)

    def phase0(self):
        P, nc, I = self.P, self.nc, self.I
        NCc, NCv = self.NCc, self.NCv
        gm = P.sb("gm", [128, 8])
        P.dma(gm[:], I["gm"], writes=["gm"])
        wcb = P.sb("wcb", [128, 8, NCc], BF16)
        wvb = P.sb("wvb", [128, 8, NCv], BF16)
        wst = [P.sb("wst%d" % i, [128, 1408]) for i in range(2)]
        k = 0
        for (src, dst, n, dkey) in ((I["wc"], wcb, NCc, "wcb"), (I["wv"], wvb, NCv, "wvb")):
            for c in range(8):
                st = wst[k % 2]; key = "wst%d" % (k % 2); k += 1
                P.dma(st[:, 0:n], src[c * 128:(c + 1) * 128, :], writes=[key])
                P.op("vector", lambda e, st=st, dst=dst, c=c, n=n: e.tensor_scalar(
                    out=dst[:, c, :], in0=st[:, 0:n], scalar1=gm[:, c:c + 1], scalar2=None, op0=ALU.mult),
                    reads=[key, "gm"], writes=[dkey])
        groups = []
        o = 0
        while o < NCc:
            n = min(128, NCc - o); groups.append((o, n)); o += n
        xt = [P.sb("xt%d" % i, [128, D]) for i in range(2)]
        sq = P.sb("sqj", [128, D])
        ss = [P.sb("ss%d" % i, [128, 1]) for i in range(2)]
        hT = [P.sb("hT%d" % i, [128, 8, 512], BF16) for i in range(2)]
        og = [P.sb("og%d" % i, [128, 512]) for i in range(3)]
        ov = [P.sb("ov%d" % i, [128, NCv]) for i in range(2)]
        pT = [self.pb[0], self.pb[1]]
        gi = 0; xi = 0; vi = 0
        wckey = "wcb"; wvkey = "wvb"
        for tb in range(NTB):
            h = hT[tb % 2]; hk = "hT%d" % (tb % 2)
            for st in range(4):
                t0 = tb * 512 + st * 128
                x = xt[xi % 2]; xk = "xt%d" % (xi % 2); s_ = ss[xi % 2]; sk = "ss%d" % (xi % 2); xi += 1
                P.dma(x[:], (I["x"](t0) if callable(I["x"]) else I["x"][t0:t0 + 128, :]), writes=[xk], q="sync")
                P.op("scalar", lambda e, x=x, s_=s_: e.activation(out=sq[:], in_=x[:], func=AF.Square, accum_out=s_[:]),
                     reads=[xk], writes=["sqj", sk])
                P.op("scalar", lambda e, s_=s_: e.activation(out=s_[:], in_=s_[:], func=AF.Sqrt, scale=1.0 / D, bias=1e-6),
                     reads=[sk], writes=[sk])
                P.op("vector", lambda e, s_=s_: e.reciprocal(out=s_[:], in_=s_[:]), reads=[sk], writes=[sk])
                P.op("vector", lambda e, x=x, s_=s_: e.tensor_scalar(out=x[:], in0=x[:], scalar1=s_[:, 0:1], scalar2=None, op0=ALU.mult),
                     reads=[xk, sk], writes=[xk])
                for half in range(2):
                    pt = pT[half]; pk = "pb%d" % half
                    for c4 in range(4):
                        c = half * 4 + c4
                        P.op("tensor", lambda e, pt=pt, c4=c4, c=c, x=x: e.transpose(
                            out=pt[:, c4 * 128:(c4 + 1) * 128], in_=x[:, c * 128:(c + 1) * 128], identity=self.ident[:]),
                            reads=[xk, "ident"], writes=[pk])
                    eng = "scalar" if half == 0 else "vector"
                    if eng == "scalar":
                        P.op("scalar", lambda e, pt=pt, h=h, half=half, st=st: e.copy(
                            out=h[:, half * 4:(half + 1) * 4, st * 128:(st + 1) * 128],
                            in_=pt[:].rearrange("p (c t) -> p c t", c=4)), reads=[pk], writes=[hk])
                    else:
                        P.op("vector", lambda e, pt=pt, h=h, half=half, st=st: e.tensor_copy(
                            out=h[:, half * 4:(half + 1) * 4, st * 128:(st + 1) * 128],
                            in_=pt[:].rearrange("p (c t) -> p c t", c=4)), reads=[pk], writes=[hk])
            for (o, n) in groups:
                pg = self.pb[2 + gi % 2]; pk = "pb%d" % (2 + gi % 2)
                ob = og[gi % 3]; ok = "og%d" % (gi % 3); gi += 1
                for c in range(8):
                    P.op("tensor", lambda e, pg=pg, c=c, o=o, n=n, h=h: e.matmul(
                        pg[0:n, :], lhsT=wcb[:, c, o:o + n], rhs=h[:, c, :], start=(c == 0), stop=(c == 7)),
                        reads=[wckey, hk], writes=[pk])
                eng = "scalar" if gi % 2 == 0 else "vector"
                if eng == "scalar":
                    P.op("scalar", lambda e, pg=pg, ob=ob, n=n: e.copy(out=ob[0:n, :], in_=pg[0:n, :]), reads=[pk], writes=[ok])
                else:
                    P.op("vector", lambda e, pg=pg, ob=ob, n=n: e.tensor_copy(out=ob[0:n, :], in_=pg[0:n, :]), reads=[pk], writes=[ok])
                P.dma(self.UT[o:o + n, tb * 512:(tb + 1) * 512], ob[0:n, :], reads=[ok], writes=["UT"], q="sync")
            for st in range(4):
                t0 = tb * 512 + st * 128
                pv = self.pb[4 + vi % 2]; pk = "pb%d" % (4 + vi % 2)
                ob = ov[vi % 2]; ok = "ov%d" % (vi % 2); vi += 1
                for c in range(8):
                    P.op("tensor", lambda e, pv=pv, c=c, h=h, st=st: e.matmul(
                        pv[:, 0:NCv], lhsT=h[:, c, st * 128:(st + 1) * 128], rhs=wvb[:, c, :], start=(c == 0), stop=(c == 7)),
                        reads=[wvkey, hk], writes=[pk])
                P.op("vector", lambda e, pv=pv, ob=ob: e.tensor_copy(out=ob[:], in_=pv[:, 0:NCv]), reads=[pk], writes=[ok])
                P.dma(self.UV[t0:t0 + 128, :], ob[:], reads=[ok], writes=["UV"], q="sync")

    def attn_common_masks(self):
        if hasattr(self, "mask"):
            return
        P = self.P
        self.chunk_masks()
        self.mask = P.sb("amask", [128, 4, 512], BF16)
        P.op("gpsimd", lambda e: e.memset(self.mask[:], 1.0), writes=["amask"])
        for r in range(4):
            P.op("gpsimd", lambda e, r=r: e.affine_select(out=self.mask[:, r, :], in_=self.mask[:, r, :], compare_op=ALU.is_ge,
                                                        fill=0.0, base=-r * 128, pattern=[[1, 512]], channel_multiplier=-1),
                 reads=["amask"], writes=["amask"])

    def attn_loop(self, name, ncomp, kq_fn, bias_fn, scale, Vaug, vkey, rd_keys, epilogue):
        P = self.P
        self.attn_common_masks()
        PT = [P.sb("%s_pt%d" % (name, i), [128, 512], BF16) for i in range(3)]
        stb = [self.pb[0], self.pb[1], self.pb[2]]
        ob = [self.pb[4], self.pb[5]]
        n = 0
        for i in range(NTB):
            pairs = [(c, j) for c in range(ncomp) for j in range(4 * i + 4)]
            def issue_S(idx, n):
                c, j = pairs[idx]
                lhsT, rhs = kq_fn(c, j, i)
                sp = stb[n % 3]; sk = "pb%d" % (n % 3)
                P.op("tensor", lambda e, sp=sp, lhsT=lhsT, rhs=rhs: e.matmul(sp[:], lhsT=lhsT, rhs=rhs, start=True, stop=True),
                     reads=rd_keys, writes=[sk])
            issue_S(0, n)
            for idx, (c, j) in enumerate(pairs):
                if idx + 1 < len(pairs):
                    issue_S(idx + 1, n + 1)
                sp = stb[n % 3]; sk = "pb%d" % (n % 3)
                pt = PT[n % 3]; ptk = "%s_pt%d" % (name, n % 3)
                b = bias_fn(j)
                if b is None:
                    P.op("scalar", lambda e, sp=sp, pt=pt: e.activation(out=pt[:], in_=sp[:], func=AF.Exp, scale=scale),
                         reads=[sk], writes=[ptk])
                else:
                    P.op("scalar", lambda e, sp=sp, pt=pt, b=b: e.activation(out=pt[:], in_=sp[:], func=AF.Exp, scale=scale, bias=b),
                         reads=[sk, name + "_bias"], writes=[ptk])
                r = j - 4 * i
                if r >= 0:
                    P.op("vector", lambda e, pt=pt, r=r: e.tensor_tensor(out=pt[:], in0=pt[:], in1=self.mask[:, r, :], op=ALU.mult),
                         reads=[ptk, "amask"], writes=[ptk])
                o = ob[c]; okey = "pb%d" % (4 + c)
                for s in range(4):
                    P.op("tensor", lambda e, o=o, pt=pt, s=s, j=j, last=(j == 4 * i + 3): e.matmul(
                        o[:, s * 65:(s + 1) * 65], lhsT=pt[:, s * 128:(s + 1) * 128], rhs=Vaug[:, j, :],
                        start=(j == 0 and s == 0), stop=(last and s == 3)), reads=[ptk, vkey], writes=[okey])
                n += 1
            epilogue(i, ob)

    def load_qk(self, dst, dkey, src_name, swap_name=None, rope=None, extra_scale=None):
        P = self.P
        o, n = self.names[src_name]
        W = 2048
        for b in range(S // W):
            a = self.stg[0]; P.dma(a[0:64, :], self.UT[o:o + 64, b * W:(b + 1) * W], reads=["UT"], writes=["stg0"])
            if swap_name is not None:
                o2, _ = self.names[swap_name]
                a2 = self.stg[1]; P.dma(a2[0:64, :], self.UT[o2:o2 + 64, b * W:(b + 1) * W], reads=["UT"], writes=["stg1"])
                cs = self.stg[2]; P.dma(cs[0:64, :], self.I["rope"][0, :, b * W:(b + 1) * W], writes=["stg2"])
                sn = self.stg[3]; P.dma(sn[0:64, :], self.I["rope"][1, :, b * W:(b + 1) * W], writes=["stg3"])
                P.op("vector", lambda e, a=a, cs=cs: e.tensor_tensor(out=a[0:64, :], in0=a[0:64, :], in1=cs[0:64, :], op=ALU.mult),
                     reads=["stg0", "stg2"], writes=["stg0"])
                P.op("gpsimd", lambda e, a2=a2, sn=sn: e.tensor_tensor(out=a2[0:64, :], in0=a2[0:64, :], in1=sn[0:64, :], op=ALU.mult),
                     reads=["stg1", "stg3"], writes=["stg1"])
                P.op("vector", lambda e, a=a, a2=a2, b=b: e.tensor_tensor(out=dst[0:64, b * W:(b + 1) * W], in0=a[0:64, :], in1=a2[0:64, :], op=ALU.add),
                     reads=["stg0", "stg1"], writes=[dkey])
            else:
                if extra_scale is None:
                    P.op("vector", lambda e, a=a, b=b: e.tensor_copy(out=dst[0:64, b * W:(b + 1) * W], in_=a[0:64, :]),
                         reads=["stg0"], writes=[dkey])
                else:
                    P.op("vector", lambda e, a=a, b=b: e.tensor_scalar(out=dst[0:64, b * W:(b + 1) * W], in0=a[0:64, :],
                                                                         scalar1=extra_scale, scalar2=None, op0=ALU.mult),
                         reads=["stg0"], writes=[dkey])

    def load_v(self, Vaug, vkey, tname):
        P = self.P
        o, n = self.tnames[tname]
        P.op("gpsimd", lambda e: e.memset(Vaug[:, :, 64:65], 1.0), writes=[vkey])
        for b in range(S // 2048):
            a = self.stg[0]
            P.dma(a[:, 0:16 * 64].rearrange("p (t d) -> p t d", d=64),
                  self.UV[b * 2048:(b + 1) * 2048, o:o + 64].rearrange("(t p) d -> p t d", p=128), reads=["UV"], writes=["stg0"])
            P.op("vector", lambda e, a=a, b=b: e.tensor_copy(out=Vaug[:, b * 16:(b + 1) * 16, 0:64],
                                                             in_=a[:, 0:16 * 64].rearrange("p (t d) -> p t d", d=64)),
                 reads=["stg0"], writes=[vkey])

    def attn_alloc(self):
        if hasattr(self, "qT"):
            return
        P = self.P
        self.stg = [P.sb("stg%d" % i, [128, 2048]) for i in range(4)]
        self.qT = P.sb("qT", [128, S], BF16)
        self.kT = P.sb("kT", [128, S], BF16)
        self.Vaug = P.sb("Vaug", [128, NT, 65], BF16)
        self.ostage = [P.sb("ostage%d" % i, [128, 4, 64]) for i in range(2)]
        self.osc = P.sb("osc", [128, 16])
        self.otmp = [P.sb("otmp%d" % i, [128, 512]) for i in range(2)]
        self.on = 0

    def diff(self):
        P, I = self.P, self.I
        self.attn_alloc()
        qT, kT, Vaug = self.qT, self.kT, self.Vaug
        self.load_qk(qT, "qT", "qd", "qds", rope=True)
        self.load_qk(kT, "kT", "kd", "kds", rope=True)
        self.load_v(Vaug, "Vaug", "vd")
        lam_init = 0.8 - 0.6 * math.exp(-0.3 * self.layer_idx)
        lt = P.sb("lamt", [128, 128]); lp = P.sb("lamp", [128, 64]); ls = P.sb("lams", [128, 2]); nl = P.sb("neglam", [128, 1])
        P.dma(lt[:], I["dlam"].partition_broadcast(128), writes=["lamt"])
        P.op("vector", lambda e: e.tensor_tensor(out=lp[:].rearrange("p (a d) -> p a d", a=2),
                                                 in0=lt[:].rearrange("p (a b d) -> p a b d", a=2, b=2)[:, :, 0, :],
                                                 in1=lt[:].rearrange("p (a b d) -> p a b d", a=2, b=2)[:, :, 1, :], op=ALU.mult),
             reads=["lamt"], writes=["lamp"])
        P.op("vector", lambda e: e.reduce_sum(out=ls[:], in_=lp[:].rearrange("p (a d) -> p a d", a=2), axis=AX.X), reads=["lamp"], writes=["lams"])
        P.op("scalar", lambda e: e.activation(out=ls[:], in_=ls[:], func=AF.Exp), reads=["lams"], writes=["lams"])
        P.op("vector", lambda e: e.tensor_tensor(out=nl[:], in0=ls[:, 1:2], in1=ls[:, 0:1], op=ALU.subtract), reads=["lams"], writes=["neglam"])
        P.op("vector", lambda e: e.tensor_scalar(out=nl[:], in0=nl[:], scalar1=-lam_init, scalar2=None, op0=ALU.add), reads=["neglam"], writes=["neglam"])
        sub = P.sb("dsub", [128, 64])
        P.dma(sub[:], I["dsub"].partition_broadcast(128), writes=["dsub"])
        P.op("vector", lambda e: e.tensor_scalar(out=sub[:], in0=sub[:], scalar1=(1.0 - lam_init), scalar2=None, op0=ALU.mult), reads=["dsub"], writes=["dsub"])
        scale = 32 ** -0.5

        def kq(c, j, i):
            return kT[c * 32:(c + 1) * 32, j * 128:(j + 1) * 128], qT[c * 32:(c + 1) * 32, i * 512:(i + 1) * 512]

        def epi(i, ob):
            osc = self.osc
            o0 = ob[0][:, 0:260].rearrange("p (s d) -> p s d", d=65)
            o1 = ob[1][:, 0:260].rearrange("p (s d) -> p s d", d=65)
            og = self.ostage[self.on % 2]; ogk = "ostage%d" % (self.on % 2); self.on += 1
            P.op("vector", lambda e: e.reciprocal(out=osc[:, 0:4], in_=o0[:, :, 64]), reads=["pb4"], writes=["osc"])
            P.op("vector", lambda e: e.reciprocal(out=osc[:, 4:8], in_=o1[:, :, 64]), reads=["pb5"], writes=["osc"])
            P.op("vector", lambda e: e.tensor_scalar(out=osc[:, 4:8], in0=osc[:, 4:8], scalar1=nl[:, 0:1], scalar2=None, op0=ALU.mult),
                 reads=["osc", "neglam"], writes=["osc"])
            for s in range(4):
                P.op("vector", lambda e, s=s: e.tensor_scalar(out=og[:, s, :], in0=o0[:, s, 0:64], scalar1=osc[:, s:s + 1], scalar2=None, op0=ALU.mult),
                     reads=["pb4", "osc"], writes=[ogk])
                P.op("vector", lambda e, s=s: e.scalar_tensor_tensor(out=og[:, s, :], in0=o1[:, s, 0:64], scalar=osc[:, 4 + s:5 + s], in1=og[:, s, :],
                                                                      op0=ALU.mult, op1=ALU.add), reads=["pb5", "osc", ogk], writes=[ogk])
                P.op("scalar", lambda e, s=s: e.activation(out=self.stg[3][:, 0:64], in_=og[:, s, :], func=AF.Square, accum_out=osc[:, 8 + s:9 + s]),
                     reads=[ogk], writes=["stg3", "osc"])
            P.op("scalar", lambda e: e.activation(out=osc[:, 8:12], in_=osc[:, 8:12], func=AF.Sqrt, scale=1.0 / 64, bias=1e-5), reads=["osc"], writes=["osc"])
            P.op("vector", lambda e: e.reciprocal(out=osc[:, 8:12], in_=osc[:, 8:12]), reads=["osc"], writes=["osc"])
            for s in range(4):
                P.op("vector", lambda e, s=s: e.scalar_tensor_tensor(out=og[:, s, :], in0=og[:, s, :], scalar=osc[:, 8 + s:9 + s], in1=sub[:],
                                                                      op0=ALU.mult, op1=ALU.mult), reads=[ogk, "osc", "dsub"], writes=[ogk])
            P.dma(self.Y[i * 512:(i + 1) * 512, 1, :].rearrange("(s p) d -> p s d", p=128), og[:], reads=[ogk], writes=["Y1"], q="sync")

        self.attn_loop("diff", 2, kq, lambda j: None, scale, Vaug, "Vaug", ["qT", "kT"], epi)

    def fox(self):
        P, I = self.P, self.I
        self.attn_alloc()
        qT, kT, Vaug = self.qT, self.kT, self.Vaug
        scale = 64 ** -0.5
        self.load_qk(qT, "qT", "qf", extra_scale=scale)
        self.load_qk(kT, "kT", "kf")
        self.load_v(Vaug, "Vaug", "vf")
        o, _ = self.names["fl"]
        W = 2048
        z = self.stg[0]; t1 = self.stg[1]; crow = self.stg[2]; tmp = self.stg[3]
        one = P.sb("fone", [1, W]); fb = P.sb("ffb", [1, 1]); cp = P.sb("fcp", [1, 3, W], BF16)
        carry = P.sb("fcarry", [1, 1]); onesb = P.sb("fonesb", [1, W], BF16)
        negc = P.sb("fnegc", [128, NT]); one1 = P.sb("fone1", [1, 1])
        P.dma(fb[:], I["fbias"], writes=["ffb"])
        P.op("gpsimd", lambda e: e.memset(one[:], 1.0), writes=["fone"])
        P.op("gpsimd", lambda e: e.memset(one1[:], -1.0), writes=["fone1"])
        P.op("gpsimd", lambda e: e.memset(carry[:], 0.0), writes=["fcarry"])
        P.op("vector", lambda e: e.tensor_copy(out=onesb[:], in_=one[:]), reads=["fone"], writes=["fonesb"])
        pc = self.pb[6]
        for ch in range(S // W):
            zz = z[0:1, :]; tt = t1[0:1, :]; cc = crow[0:1, :]; mm = tmp[0:1, :]
            P.dma(zz, self.UT[o:o + 1, ch * W:(ch + 1) * W], reads=["UT"], writes=["stg0"])
            P.op("vector", lambda e, zz=zz: e.tensor_scalar(out=zz, in0=zz, scalar1=fb[:, 0:1], scalar2=None, op0=ALU.add), reads=["stg0", "ffb"], writes=["stg0"])
            P.op("scalar", lambda e, zz=zz, tt=tt: e.activation(out=tt, in_=zz, func=AF.Abs), reads=["stg0"], writes=["stg1"])
            P.op("scalar", lambda e, tt=tt: e.activation(out=tt, in_=tt, func=AF.Exp, scale=-1.0), reads=["stg1"], writes=["stg1"])
            P.op("scalar", lambda e, tt=tt: e.activation(out=tt, in_=tt, func=AF.Ln, bias=1.0), reads=["stg1"], writes=["stg1"])
            P.op("vector", lambda e, zz=zz: e.tensor_scalar(out=zz, in0=zz, scalar1=0.0, scalar2=None, op0=ALU.min), reads=["stg0"], writes=["stg0"])
            P.op("vector", lambda e, zz=zz, tt=tt: e.tensor_tensor(out=zz, in0=zz, in1=tt, op=ALU.subtract), reads=["stg0", "stg1"], writes=["stg0"])
            P.op("vector", lambda e, zz=zz, cc=cc: e.tensor_tensor_scan(out=cc, data0=one[:], data1=zz, initial=carry[:, 0:1], op0=ALU.mult, op1=ALU.add),
                 reads=["fone", "stg0", "fcarry"], writes=["stg2"])
            P.op("vector", lambda e, cc=cc: e.tensor_copy(out=carry[:], in_=cc[:, W - 1:W]), reads=["stg2"], writes=["fcarry"])
            P.op("vector", lambda e, cc=cc: e.tensor_copy(out=cp[:, 0, :], in_=cc), reads=["stg2"], writes=["fcp"])
            P.op("vector", lambda e, cc=cc, mm=mm: e.tensor_tensor(out=mm, in0=cc, in1=cp[:, 0, :], op=ALU.subtract), reads=["stg2", "fcp"], writes=["stg3"])
            P.op("vector", lambda e, mm=mm: e.tensor_copy(out=cp[:, 1, :], in_=mm), reads=["stg3"], writes=["fcp"])
            P.op("vector", lambda e, mm=mm: e.tensor_tensor(out=mm, in0=mm, in1=cp[:, 1, :], op=ALU.subtract), reads=["stg3", "fcp"], writes=["stg3"])
            P.op("vector", lambda e, mm=mm: e.tensor_copy(out=cp[:, 2, :], in_=mm), reads=["stg3"], writes=["fcp"])
            for r in range(3):
                P.dma(qT[64 + r:65 + r, ch * W:(ch + 1) * W], cp[:, r, :], reads=["fcp"], writes=["qT"])
                P.dma(kT[64 + r:65 + r, ch * W:(ch + 1) * W], onesb[:], reads=["fonesb"], writes=["kT"])
            for t in range(W // 128):
                tg = ch * (W // 128) + t
                P.op("tensor", lambda e, t=t, tg=tg, cc=cc: e.matmul(pc[:, tg:tg + 1], lhsT=cc[0:1, t * 128:(t + 1) * 128], rhs=one1[0:1, 0:1], start=True, stop=True),
                     reads=["stg2", "fone1"], writes=["pb6"])
        P.op("vector", lambda e: e.tensor_copy(out=negc[:], in_=pc[:, 0:NT]), reads=["pb6"], writes=["fox_bias"])

        def kq(c, j, i):
            return kT[0:67, j * 128:(j + 1) * 128], qT[0:67, i * 512:(i + 1) * 512]

        def epi(i, ob):
            osc = self.osc
            o0 = ob[0][:, 0:260].rearrange("p (s d) -> p s d", d=65)
            og = self.ostage[self.on % 2]; ogk = "ostage%d" % (self.on % 2); self.on += 1
            P.op("vector", lambda e: e.reciprocal(out=osc[:, 0:4], in_=o0[:, :, 64]), reads=["pb4"], writes=["osc"])
            for s in range(4):
                P.op("vector", lambda e, s=s: e.tensor_scalar(out=og[:, s, :], in0=o0[:, s, 0:64], scalar1=osc[:, s:s + 1], scalar2=None, op0=ALU.mult),
                     reads=["pb4", "osc"], writes=[ogk])
            P.dma(self.Y[i * 512:(i + 1) * 512, 2, :].rearrange("(s p) d -> p s d", p=128), og[:], reads=[ogk], writes=["Y2"], q="sync")

        self.attn_loop("fox", 1, kq, lambda j: negc[:, j:j + 1], 1.0, Vaug, "Vaug", ["qT", "kT"], epi)


    def chunk_masks(self):
        P = self.P
        self.BTi = P.sb("BTi", [128, 128]); self.BTe = P.sb("BTe", [128, 128]); self.BTeT = P.sb("BTeT", [128, 128])
        self.ones64 = P.sb("ones64", [128, 64])
        self.ms("gpsimd", self.ones64[:], 1.0, ["ones64"])
        for (t, key, op, pat, cm, zb) in ((self.BTi, "BTi", ALU.is_ge, [[1, 128]], -1, (0, 64)),
                                          (self.BTe, "BTe", ALU.is_gt, [[1, 128]], -1, (0, 64)),
                                          (self.BTeT, "BTeT", ALU.is_gt, [[-1, 128]], 1, (64, 0))):
            self.ms("gpsimd", t[:], 1.0, [key])
            P.op("gpsimd", lambda e, t=t, op=op, pat=pat, cm=cm: e.affine_select(out=t[:], in_=t[:], compare_op=op, fill=0.0, base=0,
                                                                              pattern=pat, channel_multiplier=cm), reads=[key], writes=[key])
            self.ms("gpsimd", t[zb[0]:zb[0] + 64, zb[1]:zb[1] + 64], 0.0, [key])

    def nm_alloc(self, pfx):
        P = self.P
        if not hasattr(self, "nmN") or not isinstance(self.nmN, dict):
            self.nmN = {}; self.nmNT = {}; self.nmX = {}; self._nm_result = {}
        self.nmN[pfx] = [P.sb(pfx + "nmN%d" % i, [128, 128]) for i in range(2)]
        self.nmNT[pfx] = [P.sb(pfx + "nmNT%d" % i, [128, 128]) for i in range(2)]
        self.nmX[pfx] = P.sb(pfx + "nmXb", [128, 128])

    def rwkv(self):
        P, I = self.P, self.I
        N = 512
        names = self.names
        rp = P.sb("rp", [128, 32]); w2h = P.sb("w2h", [64, 64]); a2h = P.sb("a2h", [64, 64]); g2h = P.sb("g2h", [128, 64])
        lnw = P.sb("lnw", [128, 64]); lnb = P.sb("lnb", [128, 64])
        P.dma(rp[:, 0:16], I["rp"], writes=["rp"])
        P.dma(w2h[:], I["w2h"], writes=["w2h"]); P.dma(a2h[:], I["a2h"], writes=["a2h"]); P.dma(g2h[:], I["g2h"], writes=["g2h"])
        P.dma(lnw[:], I["rln"][0:1, :].partition_broadcast(128), writes=["lnw"])
        P.dma(lnb[:], I["rln"][1:2, :].partition_broadcast(128), writes=["lnb"])
        self.ts("vector", rp[:, 16:22], rp[:, 0:6], -1.0, ALU.mult, ["rp"], ["rp"], s2=1.0, op1=ALU.add)
        self.ts("vector", rp[:, 22:23], rp[:, 9:10], -1.0, ALU.mult, ["rp"], ["rp"], s2=1.0, op1=ALU.add)
        MU = {"rr": 0, "rk": 1, "rv": 2, "xw": 3, "xa": 4, "xg": 5}
        self.nm_alloc("rw_")
        inb = {nm: [P.sb("rin_%s%d" % (nm, i), [128 if nm == "xg" else 64, N + 1]) for i in range(2)] for nm in MU}
        def T64(nm):
            return P.sb("rw_" + nm, [64, N])
        r = T64("r"); k0 = T64("k0"); v = T64("v"); tw = T64("tw"); xa = T64("xa"); xg = P.sb("rw_xg", [128, N])
        ld = T64("ld"); a = T64("a"); kkr = T64("kkr"); kk = T64("kk"); k = T64("k"); al = T64("al")
        PI = T64("PI"); PE = T64("PE"); PV = T64("PV"); rt = T64("rt"); bt = T64("bt"); at = T64("at"); kt = T64("kt")
        tmp = T64("tmp"); prod = T64("prod")
        tok = P.sb("rw_tok", [128, 5, 64])
        AT = P.sb("rw_AT", [128, 4, 128])
        A0 = P.sb("rw_nmA", [128, 128])
        X0 = P.sb("rw_nmXa", [128, 128])
        RT = P.sb("rw_RT", [64, 128]); MT = P.sb("rw_MT", [64, 64]); H = P.sb("rw_H", [64, 64])
        Tst = [P.sb("rw_T%d" % i, [64, 64]) for i in range(2)]
        oo = P.sb("rw_oo", [128, 64]); o2 = P.sb("rw_o2", [128, 64]); sc = P.sb("rw_sc", [128, 8])
        yst = [P.sb("rw_y%d" % i, [128, 64]) for i in range(2)]
        pb = self.pb
        self.ms("vector", Tst[0][:], 0.0, ["rw_T0"])
        ti = 0; yi = 0
        for tb in range(NTB):
            t0 = tb * N
            cur = {}
            for nm in MU:
                o, n = names[nm]
                buf = inb[nm][tb % 2]; key = "rin_%s%d" % (nm, tb % 2)
                if tb == 0:
                    self.ms("gpsimd", buf[0:n, 0:1], 0.0, [key])
                    P.dma(buf[0:n, 1:N + 1], self.UT[o:o + n, 0:N], reads=["UT"], writes=[key])
                else:
                    P.dma(buf[0:n, :], self.UT[o:o + n, t0 - 1:t0 + N], reads=["UT"], writes=[key])
                cur[nm] = (buf, key, n)
            for nm, dst, dk, eng in (("rr", r, "rw_r", "vector"), ("rk", k0, "rw_k0", "gpsimd"), ("rv", v, "rw_v", "vector"),
                                      ("xw", tw, "rw_tw", "gpsimd"), ("xa", xa, "rw_xa", "vector"), ("xg", xg, "rw_xg", "gpsimd")):
                buf, key, n = cur[nm]; c = MU[nm]
                self.ts(eng, dst[0:n, :], buf[0:n, 1:N + 1], rp[0:n, 16 + c:17 + c], ALU.mult, [key, "rp"], [dk])
                self.stt(eng, dst[0:n, :], buf[0:n, 0:N], rp[0:n, c:c + 1], dst[0:n, :], ALU.mult, ALU.add, [key, "rp", dk], [dk])
            yield
            self.act(tw[:], tw[:], AF.Tanh, ["rw_tw"], ["rw_tw"])
            self.mm(pb[0][0:64, :], w2h[:], tw[:], ["w2h", "rw_tw"], ["pb0"])
            self.act(ld[:], pb[0][0:64, :], AF.Sigmoid, ["pb0", "rp"], ["rw_ld"], bias=rp[0:64, 6:7])
            self.ts("vector", ld[:], ld[:], -math.exp(-0.5), ALU.mult, ["rw_ld"], ["rw_ld"])
            self.mm(pb[1][0:64, :], a2h[:], xa[:], ["a2h", "rw_xa"], ["pb1"])
            self.act(a[:], pb[1][0:64, :], AF.Sigmoid, ["pb1", "rp"], ["rw_a"], bias=rp[0:64, 7:8])
            self.act(xg[:], xg[:], AF.Sigmoid, ["rw_xg"], ["rw_xg"])
            yield
            self.ts("vector", kkr[:], k0[:], rp[0:64, 8:9], ALU.mult, ["rw_k0", "rp"], ["rw_kkr"])
            self.tt("gpsimd", tmp[:], kkr[:], kkr[:], ALU.mult, ["rw_kkr"], ["rw_tmp"])
            self.mm(pb[0][0:64, :], self.ones64[0:64, :], tmp[:], ["ones64", "rw_tmp"], ["pb0"])
            self.act(tmp[:], pb[0][0:64, :], AF.Sqrt, ["pb0"], ["rw_tmp"], bias=1e-6)
            P.op("vector", lambda e: e.reciprocal(out=tmp[:], in_=tmp[:]), reads=["rw_tmp"], writes=["rw_tmp"])
            self.tt("vector", kk[:], kkr[:], tmp[:], ALU.mult, ["rw_kkr", "rw_tmp"], ["rw_kk"])
            self.ts("gpsimd", tmp[:], a[:], rp[0:64, 9:10], ALU.mult, ["rw_a", "rp"], ["rw_tmp"], s2=rp[0:64, 22:23], op1=ALU.add)
            self.tt("gpsimd", k[:], k0[:], tmp[:], ALU.mult, ["rw_k0", "rw_tmp"], ["rw_k"])
            self.tt("vector", al[:], kk[:], a[:], ALU.mult, ["rw_kk", "rw_a"], ["rw_al"])
            self.stt("gpsimd", prod[:], r[:], rp[0:64, 10:11], k[:], ALU.mult, ALU.mult, ["rw_r", "rp", "rw_k"], ["rw_prod"])
            yield
            for st in range(4):
                sl = slice(st * 128, (st + 1) * 128)
                self.tr(pb[4][:, 256:320], ld[:, sl], ["rw_ld"], ["pb4"], n=64)
                self.cp("vector", tok[:, 4, :], pb[4][:, 256:320], ["pb4"], ["rw_tok"])
                self.mm(pb[2][0:64, sl], tok[:, 4, :], self.BTi[:], ["rw_tok", "BTi"], ["pb2"])
                self.mm(pb[3][0:64, sl], tok[:, 4, :], self.BTe[:], ["rw_tok", "BTe"], ["pb3"])
            self.act(PI[:], pb[2][0:64, :], AF.Exp, ["pb2"], ["rw_PI"])
            self.act(PV[:], pb[2][0:64, :], AF.Exp, ["pb2"], ["rw_PV"], scale=-1.0)
            self.act(PE[:], pb[3][0:64, :], AF.Exp, ["pb3"], ["rw_PE"])
            self.tt("vector", rt[:], r[:], PI[:], ALU.mult, ["rw_r", "rw_PI"], ["rw_rt"])
            self.stt("gpsimd", bt[:], kk[:], -1.0, PE[:], ALU.mult, ALU.mult, ["rw_kk", "rw_PE"], ["rw_bt"])
            self.tt("vector", at[:], al[:], PV[:], ALU.mult, ["rw_al", "rw_PV"], ["rw_at"])
            self.tt("gpsimd", kt[:], k[:], PV[:], ALU.mult, ["rw_k", "rw_PV"], ["rw_kt"])
            yield
            for st in range(4):
                sl = slice(st * 128, (st + 1) * 128)
                tk0 = t0 + st * 128
                for q, (src, sk) in enumerate(((v, "rw_v"), (at, "rw_at"), (kt, "rw_kt"), (bt, "rw_bt"))):
                    self.tr(pb[4][:, q * 64:(q + 1) * 64], src[:, sl], [sk], ["pb4"], n=64)
                self.cp("scalar", tok[:, 0:4, :], pb[4][:, 0:256].rearrange("p (q d) -> p q d", q=4), ["pb4"], ["rw_tok"])
                self.mm(pb[5][:, 0:128], at[:, sl], bt[:, sl], ["rw_at", "rw_bt"], ["pb5"])
                self.mm(pb[5][:, 128:256], at[:, sl], rt[:, sl], ["rw_at", "rw_rt"], ["pb5"])
                self.mm(pb[5][:, 256:384], kt[:, sl], bt[:, sl], ["rw_kt", "rw_bt"], ["pb5"])
                self.mm(pb[5][:, 384:512], kt[:, sl], rt[:, sl], ["rw_kt", "rw_rt"], ["pb5"])
                self.mm(pb[7][:, 256:384], bt[:, sl], at[:, sl], ["rw_at", "rw_bt"], ["pb7"])
                p5 = pb[5][:].rearrange("p (q t) -> p q t", q=4)
                self.tt("vector", AT[:, 0, :], p5[:, 0, :], self.BTe[:], ALU.mult, ["pb5", "BTe"], ["rw_AT0"])
                self.tt("vector", AT[:, 1, :], p5[:, 1, :], self.BTi[:], ALU.mult, ["pb5", "BTi"], ["rw_AT1"])
                self.tt("vector", AT[:, 2, :], p5[:, 2, :], self.BTe[:], ALU.mult, ["pb5", "BTe"], ["rw_AT2"])
                self.tt("vector", AT[:, 3, :], p5[:, 3, :], self.BTi[:], ALU.mult, ["pb5", "BTi"], ["rw_AT3"])
                self.tt("vector", A0[:], pb[7][:, 256:384], self.BTeT[:], ALU.mult, ["pb7", "BTeT"], ["rw_nmA"])
                yield
                self.mm(pb[7][:, 0:64], AT[:, 2, :], tok[:, 0, :], ["rw_AT2", "rw_tok"], ["pb7"])
                self.cp("gpsimd", X0[:, 0:64], tok[:, 3, :], ["rw_tok"], ["rw_nmXa"])
                self.cp("scalar", X0[:, 64:128], pb[7][:, 0:64], ["pb7"], ["rw_nmXa"])
                yield
                yield from self.neumann_gen("rw_", X0, A0[:], "rw_nmA", AT[:, 0, :], "rw_AT0", pb[7][:, 384:512], "pb7")
                X, Xk = self._nm_result["rw_"]
                self.mm(pb[7][0:64, 64:192], X[:, 0:64], AT[:, 1, :], [Xk, "rw_AT1"], ["pb7"])
                self.tt("vector", RT[:], pb[7][0:64, 64:192], rt[:, sl], ALU.add, ["pb7", "rw_rt"], ["rw_RT"])
                self.mm(pb[0][:, 0:64], AT[:, 1, :], X[:, 64:128], ["rw_AT1", Xk], ["pb0"], start=True, stop=False)
                self.mm(pb[0][:, 0:64], AT[:, 3, :], tok[:, 0, :], ["rw_AT3", "rw_tok"], ["pb0"], start=False, stop=False)
                for c in range(2):
                    cs = slice(c * 64, (c + 1) * 64)
                    Tc = Tst[ti % 2]; Tk = "rw_T%d" % (ti % 2); Tn = Tst[(ti + 1) % 2]; Tnk = "rw_T%d" % ((ti + 1) % 2); ti += 1
                    self.mm(pb[0][cs, 0:64], RT[:, cs], Tc[:], ["rw_RT", Tk], ["pb0"], start=False, stop=True)
                    self.mm(pb[1][0:64, 0:64], X[cs, 0:64], tok[cs, 1, :], [Xk, "rw_tok"], ["pb1"])
                    self.tt("vector", MT[:], pb[1][0:64, 0:64], self.ident[0:64, 0:64], ALU.add, ["pb1", "ident"], ["rw_MT"])
                    self.mm(pb[3][0:64, 0:64], tok[cs, 1, :], X[cs, 64:128], ["rw_tok", Xk], ["pb3"], start=True, stop=False)
                    self.mm(pb[3][0:64, 0:64], tok[cs, 2, :], tok[cs, 0, :], ["rw_tok"], ["pb3"], start=False, stop=True)
                    pc_col = PI[:, st * 128 + c * 64 + 63:st * 128 + c * 64 + 64]
                    self.ts("gpsimd" if False else "vector", H[:], pb[3][0:64, 0:64], pc_col, ALU.mult, ["pb3", "rw_PI"], ["rw_H"])
                    self.mm(pb[2][0:64, 0:64], MT[:], Tc[:], ["rw_MT", Tk], ["pb2"])
                    self.stt("vector", Tn[:], pb[2][0:64, 0:64], pc_col, H[:], ALU.mult, ALU.add, ["pb2", "rw_PI", "rw_H"], [Tnk])
                yield
                self.cp("scalar", oo[:], pb[0][:, 0:64], ["pb0"], ["rw_oo"])
                P.op("vector", lambda e: e.reduce_sum(out=sc[:, 0:1], in_=oo[:], axis=AX.X), reads=["rw_oo"], writes=["rw_sc"])
                self.ts("vector", sc[:, 0:1], sc[:, 0:1], -1.0 / 64, ALU.mult, ["rw_sc"], ["rw_sc"])
                self.ts("vector", oo[:], oo[:], sc[:, 0:1], ALU.add, ["rw_oo", "rw_sc"], ["rw_oo"])
                self.act(o2[:], oo[:], AF.Square, ["rw_oo"], ["rw_o2", "rw_sc"], accum_out=sc[:, 1:2])
                self.act(sc[:, 1:2], sc[:, 1:2], AF.Sqrt, ["rw_sc"], ["rw_sc"], scale=1.0 / 64, bias=64e-5)
                P.op("vector", lambda e: e.reciprocal(out=sc[:, 1:2], in_=sc[:, 1:2]), reads=["rw_sc"], writes=["rw_sc"])
                self.stt("vector", oo[:], oo[:], sc[:, 1:2], lnw[:], ALU.mult, ALU.mult, ["rw_oo", "rw_sc", "lnw"], ["rw_oo"])
                self.tt("gpsimd", oo[:], oo[:], lnb[:], ALU.add, ["rw_oo", "lnb"], ["rw_oo"])
                self.mm(pb[1][:, 64:65], prod[:, sl], self.ones64[0:64, 0:1], ["rw_prod", "ones64"], ["pb1"])
                self.mm(pb[1][:, 128:192], xg[:, sl], g2h[:], ["rw_xg", "g2h"], ["pb1"])
                self.cp("scalar", sc[:, 2:3], pb[1][:, 64:65], ["pb1"], ["rw_sc"])
                self.stt("vector", oo[:], tok[:, 0, :], sc[:, 2:3], oo[:], ALU.mult, ALU.add, ["rw_tok", "rw_sc", "rw_oo"], ["rw_oo"])
                y = yst[yi % 2]; yk = "rw_y%d" % (yi % 2); yi += 1
                self.tt("vector", y[:], pb[1][:, 128:192], oo[:], ALU.mult, ["rw_oo", "pb1"], [yk])
                P.dma(self.Y[tk0:tk0 + 128, 0, :], y[:], reads=[yk], writes=["Y0"], q="sync")
                yield

    def gdn(self):
        P, I = self.P, self.I
        N = 512
        names = self.names; pb = self.pb
        gp = P.sb("gp", [128, 16]); gnw = P.sb("gnw", [128, 64])
        P.dma(gp[:], I["gp"], writes=["gp"])
        P.dma(gnw[:], I["gnorm"].partition_broadcast(128), writes=["gnw"])
        self.act(gp[:, 10:11], gp[:, 8:9], AF.Exp, ["gp"], ["gp"])
        self.ts("vector", gp[:, 10:11], gp[:, 10:11], -1.0, ALU.mult, ["gp"], ["gp"])
        self.nm_alloc("gd_")
        qin = [P.sb("gd_qin%d" % i, [64, N + 3]) for i in range(2)]
        kvin = [P.sb("gd_kvin%d" % i, [128, N + 3]) for i in range(2)]
        bin_ = [P.sb("gd_bin%d" % i, [128, N]) for i in range(2)]
        ain = [P.sb("gd_ain%d" % i, [128, N]) for i in range(2)]
        q = P.sb("gd_q", [64, N]); kv = P.sb("gd_kv", [128, N]); beta = P.sb("gd_beta", [128, N]); gb_ = P.sb("gd_g", [128, N])
        t1 = P.sb("gd_t1", [128, N]); t2 = P.sb("gd_t2", [128, N])
        gtok = P.sb("gd_gtok", [128, 128]); gam = P.sb("gd_gam", [128, 128]); egam = P.sb("gd_egam", [128, 128]); gamt = P.sb("gd_gamt", [128, 1])
        BT = P.sb("gd_BT", [128, 128]); kb = P.sb("gd_kb", [64, 128]); qe = P.sb("gd_qe", [64, 128])
        DT = P.sb("gd_DT", [128, 128]); Dm = P.sb("gd_D", [128, 128])
        NT = P.sb("gd_NT", [128, 128]); Nm = P.sb("gd_N", [128, 128]); QK = P.sb("gd_QK", [128, 128])
        X0 = P.sb("gd_nmXa", [128, 128])
        ek = P.sb("gd_ek", [64, 128]); KhT = P.sb("gd_KhT", [64, 128]); Kh = P.sb("gd_Kh", [128, 64])
        RT = P.sb("gd_RT", [64, 128]); MT = P.sb("gd_MT", [64, 64]); H = P.sb("gd_H", [64, 64])
        Tst = [P.sb("gd_T%d" % i, [64, 64]) for i in range(2)]
        oo = P.sb("gd_oo", [128, 64]); o2 = P.sb("gd_o2", [128, 64]); sc = P.sb("gd_sc", [128, 4])
        gate = [P.sb("gd_gate%d" % i, [128, 64]) for i in range(2)]
        yst = [P.sb("gd_y%d" % i, [128, 64]) for i in range(2)]
        self.ms("vector", Tst[0][:], 0.0, ["gd_T0"])
        ti = 0; yi = 0
        oq, _ = names["gq"]; ok_, _ = names["gk"]; ov_, _ = names["gv"]; ob_, _ = names["gb"]; oa_, _ = names["ga"]
        ogg, _ = self.tnames["ggt"]
        for tb in range(NTB):
            t0 = tb * N
            qi = qin[tb % 2]; qk_ = "gd_qin%d" % (tb % 2); kvi = kvin[tb % 2]; kvk = "gd_kvin%d" % (tb % 2)
            bi = bin_[tb % 2]; bk = "gd_bin%d" % (tb % 2); ai = ain[tb % 2]; ak = "gd_ain%d" % (tb % 2)
            if tb == 0:
                self.ms("gpsimd", qi[:, 0:3], 0.0, [qk_]); self.ms("gpsimd", kvi[:, 0:3], 0.0, [kvk])
                P.dma(qi[:, 3:N + 3], self.UT[oq:oq + 64, 0:N], reads=["UT"], writes=[qk_])
                P.dma(kvi[0:64, 3:N + 3], self.UT[ok_:ok_ + 64, 0:N], reads=["UT"], writes=[kvk])
                P.dma(kvi[64:128, 3:N + 3], self.UT[ov_:ov_ + 64, 0:N], reads=["UT"], writes=[kvk])
            else:
                P.dma(qi[:, :], self.UT[oq:oq + 64, t0 - 3:t0 + N], reads=["UT"], writes=[qk_])
                P.dma(kvi[0:64, :], self.UT[ok_:ok_ + 64, t0 - 3:t0 + N], reads=["UT"], writes=[kvk])
                P.dma(kvi[64:128, :], self.UT[ov_:ov_ + 64, t0 - 3:t0 + N], reads=["UT"], writes=[kvk])
            P.dma(bi[:], self.UT[ob_:ob_ + 128, t0:t0 + N], reads=["UT"], writes=[bk])
            P.dma(ai[:], self.UT[oa_:oa_ + 128, t0:t0 + N], reads=["UT"], writes=[ak])
            for (src, sk, dst, dk, n, wc0) in ((qi, qk_, q, "gd_q", 64, 0), (kvi, kvk, kv, "gd_kv", 128, 4)):
                self.ts("vector", dst[0:n, :], src[0:n, 3:N + 3], gp[0:n, wc0 + 3:wc0 + 4], ALU.mult, [sk, "gp"], [dk])
                for j in range(3):
                    self.stt("vector", dst[0:n, :], src[0:n, j:N + j], gp[0:n, wc0 + j:wc0 + j + 1], dst[0:n, :], ALU.mult, ALU.add, [sk, "gp", dk], [dk])
                self.act(dst[0:n, :], dst[0:n, :], AF.Silu, [dk], [dk])
            yield
            for (dst, dk, mul) in ((q, "gd_q", 64 ** -0.5), (kv, "gd_kv", 1.0)):
                self.tt("gpsimd", t1[0:64, :], dst[0:64, :], dst[0:64, :], ALU.mult, [dk], ["gd_t1"])
                self.mm(pb[1][0:64, :], self.ones64[0:64, :], t1[0:64, :], ["ones64", "gd_t1"], ["pb1"])
                self.act(t1[0:64, :], pb[1][0:64, :], AF.Sqrt, ["pb1"], ["gd_t1"], bias=1e-6)
                P.op("vector", lambda e: e.reciprocal(out=t1[0:64, :], in_=t1[0:64, :]), reads=["gd_t1"], writes=["gd_t1"])
                self.stt("vector", dst[0:64, :], dst[0:64, :], mul, t1[0:64, :], ALU.mult, ALU.mult, [dk, "gd_t1"], [dk])
            yield
            self.act(beta[:], bi[:], AF.Sigmoid, [bk], ["gd_beta"])
            self.ts("vector", t1[:], ai[:], gp[:, 9:10], ALU.add, [ak, "gp"], ["gd_t1"])
            self.act(t2[:], t1[:], AF.Abs, ["gd_t1"], ["gd_t2"])
            self.act(t2[:], t2[:], AF.Exp, ["gd_t2"], ["gd_t2"], scale=-1.0)
            self.act(t2[:], t2[:], AF.Ln, ["gd_t2"], ["gd_t2"], bias=1.0)
            self.ts("vector", t1[:], t1[:], 0.0, ALU.max, ["gd_t1"], ["gd_t1"])
            self.tt("vector", t1[:], t1[:], t2[:], ALU.add, ["gd_t1", "gd_t2"], ["gd_t1"])
            self.ts("vector", gb_[:], t1[:], gp[:, 10:11], ALU.mult, ["gd_t1", "gp"], ["gd_g"])
            for st in range(4):
                sl = slice(st * 128, (st + 1) * 128)
                tk0 = t0 + st * 128
                gt = gate[yi % 2]; gtk = "gd_gate%d" % (yi % 2)
                P.dma(gt[:], self.UV[tk0:tk0 + 128, ogg:ogg + 64], reads=["UV"], writes=[gtk])
                self.act(gt[:], gt[:], AF.Silu, [gtk], [gtk])
                self.tr(pb[4][:, 0:128], gb_[:, sl], ["gd_g"], ["pb4"], n=128)
                self.cp("vector", gtok[:], pb[4][:, 0:128], ["pb4"], ["gd_gtok"])
                self.mm(pb[4][:, 128:256], gtok[:], self.BTi[:], ["gd_gtok", "BTi"], ["pb4"])
                self.mm(pb[4][:, 256:257], self.BTi[:], gtok[:, 0:1], ["gd_gtok", "BTi"], ["pb4"])
                self.cp("vector", gam[:], pb[4][:, 128:256], ["pb4"], ["gd_gam"])
                self.cp("vector", gamt[:], pb[4][:, 256:257], ["pb4"], ["gd_gamt"])
                self.act(egam[:], pb[4][:, 128:256], AF.Exp, ["pb4"], ["gd_egam"])
                yield
                self.tt("vector", kb[:], kv[0:64, sl], beta[0:64, sl], ALU.mult, ["gd_kv", "gd_beta"], ["gd_kb"])
                self.tt("gpsimd", BT[0:64, :], kb[:], egam[0:64, :], ALU.mult, ["gd_kb", "gd_egam"], ["gd_BT"])
                self.tt("gpsimd", BT[64:128, :], kv[64:128, sl], beta[64:128, sl], ALU.mult, ["gd_kv", "gd_beta"], ["gd_BT"])
                self.tt("gpsimd", qe[:], q[:, sl], egam[0:64, :], ALU.mult, ["gd_q", "gd_egam"], ["gd_qe"])
                self.ts("vector", DT[:], gam[:], gamt[:, 0:1], ALU.subtract, ["gd_gam", "gd_gamt"], ["gd_DT"], s2=0.0, op1=ALU.min)
                self.act(DT[:], DT[:], AF.Exp, ["gd_DT"], ["gd_DT"])
                self.ts("vector", Dm[:], gam[:], gamt[:, 0:1], ALU.subtract, ["gd_gam", "gd_gamt"], ["gd_D"], s2=0.0, op1=ALU.max)
                self.act(Dm[:], Dm[:], AF.Exp, ["gd_D"], ["gd_D"], scale=-1.0)
                yield
                self.mm(pb[5][:, 0:128], kv[0:64, sl], kb[:], ["gd_kv", "gd_kb"], ["pb5"])
                self.mm(pb[5][:, 128:256], kv[0:64, sl], q[:, sl], ["gd_kv", "gd_q"], ["pb5"])
                self.mm(pb[5][:, 256:384], kb[:], kv[0:64, sl], ["gd_kv", "gd_kb"], ["pb5"])
                self.stt("vector", NT[:], pb[5][:, 0:128], -1.0, DT[:], ALU.mult, ALU.mult, ["pb5", "gd_DT"], ["gd_NT"])
                self.tt("gpsimd", NT[:], NT[:], self.BTe[:], ALU.mult, ["gd_NT", "BTe"], ["gd_NT"])
                self.tt("vector", QK[:], pb[5][:, 128:256], DT[:], ALU.mult, ["pb5", "gd_DT"], ["gd_QK"])
                self.tt("gpsimd", QK[:], QK[:], self.BTi[:], ALU.mult, ["gd_QK", "BTi"], ["gd_QK"])
                self.stt("vector", Nm[:], pb[5][:, 256:384], -1.0, Dm[:], ALU.mult, ALU.mult, ["pb5", "gd_D"], ["gd_N"])
                self.tt("gpsimd", Nm[:], Nm[:], self.BTeT[:], ALU.mult, ["gd_N", "BTeT"], ["gd_N"])
                yield
                self.tr(pb[7][:, 0:128], BT[:], ["gd_BT"], ["pb7"], n=128)
                self.cp("scalar", X0[:], pb[7][:, 0:128], ["pb7"], ["gd_nmXa"])
                yield
                yield from self.neumann_gen("gd_", X0, Nm[:], "gd_N", NT[:], "gd_NT", pb[6][:, 128:256], "pb6")
                X, Xk = self._nm_result["gd_"]
                self.mm(pb[7][0:64, 128:256], X[:, 0:64], QK[:], [Xk, "gd_QK"], ["pb7"])
                self.stt("vector", RT[:], pb[7][0:64, 128:256], -1.0, qe[:], ALU.mult, ALU.add, ["pb7", "gd_qe"], ["gd_RT"])
                yield
                for c in range(2):
                    cs = slice(c * 64, (c + 1) * 64)
                    self.act(ek[:, cs], gam[0:64, cs], AF.Exp, ["gd_gam"], ["gd_ek"], scale=-1.0, bias=gam[0:64, c * 64 + 63:c * 64 + 64])
                self.tt("vector", KhT[:], kv[0:64, sl], ek[:], ALU.mult, ["gd_kv", "gd_ek"], ["gd_KhT"])
                self.tr(pb[7][:, 256:320], KhT[:], ["gd_KhT"], ["pb7"], n=64)
                self.cp("scalar", Kh[:], pb[7][:, 256:320], ["pb7"], ["gd_Kh"])
                self.mm(pb[6][:, 0:64], QK[:], X[:, 64:128], ["gd_QK", Xk], ["pb6"], start=True, stop=False)
                for c in range(2):
                    cs = slice(c * 64, (c + 1) * 64)
                    Tc = Tst[ti % 2]; Tk = "gd_T%d" % (ti % 2); Tn = Tst[(ti + 1) % 2]; Tnk = "gd_T%d" % ((ti + 1) % 2); ti += 1
                    self.mm(pb[6][cs, 0:64], RT[:, cs], Tc[:], ["gd_RT", Tk], ["pb6"], start=False, stop=True)
                    self.mm(pb[1][0:64, 0:64], X[cs, 0:64], Kh[cs, :], [Xk, "gd_Kh"], ["pb1"])
                    egl = egam[0:64, c * 64 + 63:c * 64 + 64]
                    self.stt("vector", MT[:], self.ident[0:64, 0:64], egl, pb[1][0:64, 0:64], ALU.mult, ALU.subtract, ["ident", "gd_egam", "pb1"], ["gd_MT"])
                    self.mm(pb[3][0:64, 0:64], Kh[cs, :], X[cs, 64:128], ["gd_Kh", Xk], ["pb3"])
                    self.cp("scalar", H[:], pb[3][0:64, 0:64], ["pb3"], ["gd_H"])
                    self.mm(pb[2][0:64, 0:64], MT[:], Tc[:], ["gd_MT", Tk], ["pb2"])
                    self.tt("vector", Tn[:], pb[2][0:64, 0:64], H[:], ALU.add, ["pb2", "gd_H"], [Tnk])
                yield
                self.cp("scalar", oo[:], pb[6][:, 0:64], ["pb6"], ["gd_oo"])
                self.act(o2[:], oo[:], AF.Square, ["gd_oo"], ["gd_o2", "gd_sc"], accum_out=sc[:, 0:1])
                self.act(sc[:, 0:1], sc[:, 0:1], AF.Sqrt, ["gd_sc"], ["gd_sc"], scale=1.0 / 64, bias=1e-6)
                P.op("vector", lambda e: e.reciprocal(out=sc[:, 0:1], in_=sc[:, 0:1]), reads=["gd_sc"], writes=["gd_sc"])
                self.stt("vector", oo[:], oo[:], sc[:, 0:1], gnw[:], ALU.mult, ALU.mult, ["gd_oo", "gd_sc", "gnw"], ["gd_oo"])
                y = yst[yi % 2]; yk = "gd_y%d" % (yi % 2); yi += 1
                self.tt("vector", y[:], oo[:], gt[:], ALU.mult, ["gd_oo", gtk], [yk])
                P.dma(self.Y[tk0:tk0 + 128, 3, :], y[:], reads=[yk], writes=["Y3"], q="sync")
                yield

    def neumann_gen(self, pfx, X0, n0, n0k, n0t, n0tk, pX, pXk):
        Ns = self.nmN[pfx]; NTs = self.nmNT[pfx]; Xs = [X0, self.nmX[pfx]]
        Nk = [pfx + "nmN0", pfx + "nmN1"]; NTk = [pfx + "nmNT0", pfx + "nmNT1"]; Xk = [pfx + "nmXa", pfx + "nmXb"]
        pT = self.pb[2]; pN = self.pb[3]
        curN, curNT, curNk, curNTk = n0, n0t, n0k, n0tk
        xi = 0
        nr = 6
        for i in range(nr):
            self.mm(pX, curNT, Xs[xi][:], [curNTk, Xk[xi]], [pXk])
            self.tt("vector", Xs[1 - xi][:], pX, Xs[xi][:], ALU.add, [Xk[xi], pXk], [Xk[1 - xi]])
            xi = 1 - xi
            if i < nr - 1:
                self.mm(pT[:, 0:128], curN, curNT, [curNk, curNTk], ["pb2"])
                self.mm(pN[:, 0:128], curNT, curN, [curNk, curNTk], ["pb3"])
                self.cp("scalar", NTs[i % 2][:], pT[:, 0:128], ["pb2"], [NTk[i % 2]])
                self.cp("vector", Ns[i % 2][:], pN[:, 0:128], ["pb3"], [Nk[i % 2]])
                curN, curNT, curNk, curNTk = Ns[i % 2][:], NTs[i % 2][:], Nk[i % 2], NTk[i % 2]
            yield
        self._nm_result[pfx] = (Xs[xi], Xk[xi])


def make_inputs(inp, layer, core):
    b, h = core // 4, core % 4
    names, cidx, tnames, tidx = colsel(h)
    w = inp["w_in"][layer]
    d = {
        "wc": np.ascontiguousarray(w[:, cidx]),
        "wv": np.ascontiguousarray(w[:, tidx]),
        "gm": np.ascontiguousarray(inp["norm_mix"][layer].reshape(8, 128).T),
        "rope": rope_tables(),
        "dlam": np.ascontiguousarray(inp["diff_lam"][layer].reshape(1, 128)),
        "dsub": np.ascontiguousarray(inp["diff_subln"][layer].reshape(1, 64)),
        "fbias": np.ascontiguousarray(inp["fox_fbias"][layer][h].reshape(1, 1)),
    }
    hs = slice(h * 64, (h + 1) * 64)
    rp = np.zeros((128, 16), np.float32)
    mu = inp["rwkv_mu"][layer]
    rp[0:64, 0] = mu[0 + h * 64:0 + h * 64 + 64]; rp[0:64, 1] = mu[256 + h * 64:256 + h * 64 + 64]; rp[0:64, 2] = mu[512 + h * 64:512 + h * 64 + 64]
    rp[0:64, 3] = mu[768:832]; rp[0:64, 4] = mu[832:896]; rp[0:128, 5] = mu[896:1024]
    rp[0:64, 6] = inp["rwkv_w0"][layer][hs]; rp[0:64, 7] = inp["rwkv_a0"][layer][hs]
    rp[0:64, 8] = inp["rwkv_kk"][layer][hs]; rp[0:64, 9] = inp["rwkv_ka"][layer][hs]; rp[0:64, 10] = inp["rwkv_rk"][layer][h]
    d["rp"] = rp
    d["w2h"] = np.ascontiguousarray(inp["rwkv_w2"][layer][:, hs]); d["a2h"] = np.ascontiguousarray(inp["rwkv_a2"][layer][:, hs])
    d["g2h"] = np.ascontiguousarray(inp["rwkv_g2"][layer][:, hs])
    d["rln"] = np.ascontiguousarray(np.stack([inp["rwkv_ln_w"][layer][hs], inp["rwkv_ln_b"][layer][hs]]))
    gp = np.zeros((128, 16), np.float32)
    cw = inp["gdn_conv"][layer]
    gp[0:64, 0:4] = cw[h * 64:(h + 1) * 64]; gp[0:64, 4:8] = cw[256 + h * 64:256 + (h + 1) * 64]; gp[64:128, 4:8] = cw[512 + h * 64:512 + (h + 1) * 64]
    gp[:, 8] = inp["gdn_a_log"][layer][h]; gp[:, 9] = inp["gdn_dt_bias"][layer][h]
    d["gp"] = gp; d["gnorm"] = np.ascontiguousarray(inp["gdn_norm"][layer].reshape(1, 64))
    return d


D = 1024


class KB:
    def __init__(self, layer_idx, ntok=2048, tb=1024, moe=False, final=False, F=None, NE=8):
        self.layer_idx = layer_idx; self.ntok = ntok; self.tb = tb; self.moe = moe; self.final = final
        self.F = F if F is not None else (3584 if moe else 2816)
        self.NE = NE if moe else 1
        self.sbw = min(512, tb)

    def tt(self, eng, out, in0, in1, op, r, w):
        self.P.op(eng, lambda e: e.tensor_tensor(out=out, in0=in0, in1=in1, op=op), reads=r, writes=w)

    def ts(self, eng, out, in0, s1, op0, r, w, s2=None, op1=None):
        if op1 is None:
            self.P.op(eng, lambda e: e.tensor_scalar(out=out, in0=in0, scalar1=s1, scalar2=None, op0=op0), reads=r, writes=w)
        else:
            self.P.op(eng, lambda e: e.tensor_scalar(out=out, in0=in0, scalar1=s1, scalar2=s2, op0=op0, op1=op1), reads=r, writes=w)

    def stt(self, out, in0, sc, in1, op0, op1, r, w):
        self.P.op("vector", lambda e: e.scalar_tensor_tensor(out=out, in0=in0, scalar=sc, in1=in1, op0=op0, op1=op1), reads=r, writes=w)

    def act(self, out, in_, func, r, w, **kw):
        self.P.op("scalar", lambda e: e.activation(out=out, in_=in_, func=func, **kw), reads=r, writes=w)

    def cp(self, eng, out, in_, r, w):
        if eng == "scalar":
            self.P.op(eng, lambda e: e.copy(out=out, in_=in_), reads=r, writes=w)
        else:
            self.P.op(eng, lambda e: e.tensor_copy(out=out, in_=in_), reads=r, writes=w)

    def mm(self, out, lhsT, rhs, r, w, start=True, stop=True):
        self.P.op("tensor", lambda e: e.matmul(out, lhsT=lhsT, rhs=rhs, start=start, stop=stop), reads=r, writes=w)

    def tr(self, out, in_, r, w):
        self.P.op("tensor", lambda e: e.transpose(out=out, in_=in_, identity=self.ident[:]), reads=list(r) + ["ident"], writes=w)

    def declare(self, nc, sfx="", fused=False):
        NT_, F, NE = self.ntok, self.F, self.NE
        I = {}
        def inp(name, shape):
            I[name] = nc.dram_tensor(name + sfx, list(shape), F32, kind="ExternalInput").ap()
        if not fused:
            inp("x", [NT_, D]); inp("ysT", [8, 128, NT_])
        inp("pT", [2, 128, NT_])
        inp("wgate", [D, 4 * D]); inp("wbo", [4, 256, D]); inp("wout", [D, D])
        inp("norms", [128, 24])
        if self.final:
            inp("fnorm", [1, D])
        inp("fwg", [NE, D, F]); inp("fwu", [NE, D, F]); inp("fwd", [NE, F, D])
        if self.moe:
            inp("router", [D, 8])
        inp("plegate", [D, D]); inp("pleproj", [256, D])
        self.I = I
        self.fused = fused

    def emit(self, nc, P, pb, ident, xsrc_fn, out_ap, ysrc_fn=None, after_block=None):
        self.nc = nc; self.P = P; self.pb = pb; self.ident = ident
        self.xsrc_fn = xsrc_fn; self.ysrc_fn = ysrc_fn
        self.O = out_ap
        I = self.I
        self.norms = P.sb("norms", [128, 24])
        P.dma(self.norms[:], I["norms"], writes=["norms"])
        if self.final:
            self.fn = P.sb("fnorm", [128, D])
            P.dma(self.fn[:], I["fnorm"].partition_broadcast(128), writes=["fnorm"])
        TBt = self.tb // 128
        self.x = P.sb("x", [128, TBt, D])
        self.hT = P.sb("hT", [128, 8, self.tb], BF16)
        self.h32 = P.sb("h32", [128, 8, 128])
        self.sq = P.sb("sqj", [128, D]); self.ssv = P.sb("ssv", [128, 2])
        self.xn = [P.sb("xn%d" % i, [128, D]) for i in range(2)]
        self.wst = [P.sb("wst%d" % i, [128, 4096]) for i in range(3)]
        self.wbf = [P.sb("wbf%d" % i, [128, 4096], BF16) for i in range(4)]
        self.wi = 0; self.bi = 0; self.ci = 0
        for blk in range(self.ntok // self.tb):
            self.block(blk)
            if after_block is not None:
                after_block(blk)

    def build(self):
        nc = bass.Bass("TRN2", target_bir_lowering=False)
        self.declare(nc)
        I = self.I
        O = nc.dram_tensor("out", [self.ntok, D], F32, kind="ExternalOutput").ap()
        P = Prog(nc, n_chan=16)
        pb = [P.ps("pb%d" % i, [128, 512]) for i in range(8)]
        ident = P.sb("ident", [128, 128])
        P.op("gpsimd", lambda e: e.memset(ident[:], 1.0), writes=["ident"])
        P.op("gpsimd", lambda e: e.affine_select(out=ident[:], in_=ident[:], compare_op=ALU.is_equal,
                                                   fill=0.0, base=0, pattern=[[-1, 128]], channel_multiplier=1),
             reads=["ident"], writes=["ident"])
        P.phase_begin()
        self.emit(nc, P, pb, ident, lambda e, r0, n: I["x"][r0:r0 + n, :], O)
        P.phase_end()
        P.wait_all_dma("sync")
        P.emit()
        return nc

    def load_w(self, dst_view_fn, src_ap_list, nfree):
        P = self.P
        st = self.wst[self.wi % 3]; sk = "wst%d" % (self.wi % 3); self.wi += 1
        bf = self.wbf[self.bi % 4]; bk = "wbf%d" % (self.bi % 4); self.bi += 1
        for (src, view) in src_ap_list:
            P.dma(view(st), src, writes=[sk])
        eng = ("vector", "scalar")[self.ci % 2]; self.ci += 1
        self.cp(eng, bf[:, 0:nfree], st[:, 0:nfree], [sk], [bk])
        return bf, bk

    def norm_to_hT(self, ncol0, want32=None):
        P = self.P
        TBt = self.tb // 128
        for t in range(TBt):
            xn = self.xn[t % 2]; xk = "xn%d" % (t % 2)
            self.act(self.sq[:], self.x[:, t, :], AF.Square, ["x"], ["sqj", "ssv"], accum_out=self.ssv[:, 0:1])
            self.act(self.ssv[:, 0:1], self.ssv[:, 0:1], AF.Sqrt, ["ssv"], ["ssv"], scale=1.0 / D, bias=1e-6)
            P.op("vector", lambda e: e.reciprocal(out=self.ssv[:, 1:2], in_=self.ssv[:, 0:1]), reads=["ssv"], writes=["ssv"])
            self.ts("vector", xn[:], self.x[:, t, :], self.ssv[:, 1:2], ALU.mult, ["x", "ssv"], [xk])
            for half in range(2):
                pt = self.pb[half]; pk = "pb%d" % half
                for c4 in range(4):
                    c = half * 4 + c4
                    self.tr(pt[:, c4 * 128:(c4 + 1) * 128], xn[:, c * 128:(c + 1) * 128], [xk], [pk])
                gsl = self.norms[:, ncol0 + half * 4:ncol0 + half * 4 + 4]
                self.tt("vector", self.hT[:, half * 4:(half + 1) * 4, t * 128:(t + 1) * 128],
                        pt[:].rearrange("p (c t) -> p c t", c=4), gsl.unsqueeze(2).to_broadcast([128, 4, 128]), ALU.mult,
                        [pk, "norms"], ["hT"])
                if want32 is not None and want32 == t:
                    self.tt("vector", self.h32[:, half * 4:(half + 1) * 4, :],
                            pt[:].rearrange("p (c t) -> p c t", c=4), gsl.unsqueeze(2).to_broadcast([128, 4, 128]), ALU.mult,
                            [pk, "norms"], ["h32"])
            if want32 is not None and want32 == "all":
                pass

    def block(self, blk):
        P, I = self.P, self.I
        tb = self.tb; TBt = tb // 128; t0 = blk * tb; sbw = self.sbw; NSB = tb // sbw
        pb = self.pb
        x = self.x
        if blk > 0:
            P.barrier()
        for t in range(TBt):
            P.dma(x[:, t, :], (lambda e, r0=t0 + t * 128: self.xsrc_fn(e, r0, 128)), writes=["x"])
        self.norm_to_hT(0)
        if not hasattr(self, "yT"):
            self.yT = P.sb("yTact", [128, 8, tb], BF16); self.mT = P.sb("mT", [128, 8, tb], BF16)
            self.ystg = P.sb("ystg", [128, tb]); self.sg = [P.sb("sg%d" % i, [128, 512]) for i in range(2)]
            self.macc = P.sb("macc", [128, 512]); self.mtmp = P.sb("mtmp", [128, 512])
        yT, mT = self.yT, self.mT
        if self.ysrc_fn is None:
            for q in range(8):
                P.dma(self.ystg[:], I["ysT"][q, :, t0:t0 + tb], writes=["ystg"])
                self.cp("gpsimd", yT[:, q, :], self.ystg[:], ["ystg"], ["yT"])
        else:
            for t in range(TBt):
                yt = self.xn[t % 2]; ytk = "xn%d" % (t % 2)
                for r in range(4):
                    P.dma(yt[:].rearrange("p (b h c) -> p b h c", b=4, h=4)[:, :, r, :],
                          (lambda e, r=r, r0=t0 + t * 128: self.ysrc_fn(e, r, r0, 128).rearrange("p (b c) -> p b c", b=4)), writes=[ytk])
                for half in range(2):
                    pt = self.pb[half]; pk = "pb%d" % half
                    for c4 in range(4):
                        q = half * 4 + c4
                        self.tr(pt[:, c4 * 128:(c4 + 1) * 128], yt[:, q * 128:(q + 1) * 128], [ytk], [pk])
                    self.cp("vector" if half else "scalar", yT[:, half * 4:(half + 1) * 4, t * 128:(t + 1) * 128],
                            pt[:].rearrange("p (c t) -> p c t", c=4), [pk], ["yT"])
        si = 0
        for j in range(8):
            wg, wgk = self.load_w(None, [(I["wgate"][:, b * 1024 + j * 128:b * 1024 + (j + 1) * 128].rearrange("(c p) n -> p c n", p=128),
                                          (lambda st, b=b: st[:, 0:4096].rearrange("p (c b n) -> p c b n", c=8, b=4)[:, :, b, :])) for b in range(4)], 4096)
            wb, wbk = self.load_w(None, [(I["wbo"][:, :, j * 128:(j + 1) * 128].rearrange("b (c2 p) n -> p (b c2) n", p=128),
                                          (lambda st: st[:, 0:1024].rearrange("p (q n) -> p q n", q=8)))], 1024)
            for sb_ in range(NSB):
                ts_ = slice(sb_ * sbw, (sb_ + 1) * sbw)
                for b in range(4):
                    for c in range(8):
                        self.mm(pb[2][:, 0:sbw], wg[:, c * 512 + b * 128:c * 512 + (b + 1) * 128], self.hT[:, c, ts_], [wgk, "hT"], ["pb2"],
                                start=(c == 0), stop=(c == 7))
                    for c2 in range(2):
                        self.mm(pb[3][:, 0:sbw], wb[:, (b * 2 + c2) * 128:(b * 2 + c2 + 1) * 128], yT[:, b * 2 + c2, ts_], [wbk, "yT"], ["pb3"],
                                start=(c2 == 0), stop=(c2 == 1))
                    sg = self.sg[si % 2]; sgk = "sg%d" % (si % 2); si += 1
                    self.act(sg[:, 0:sbw], pb[2][:, 0:sbw], AF.Sigmoid, ["pb2"], [sgk])
                    if b == 0:
                        self.tt("vector", self.macc[:, 0:sbw], pb[3][:, 0:sbw], sg[:, 0:sbw], ALU.mult, ["pb3", sgk], ["macc"])
                    else:
                        self.tt("vector", self.mtmp[:, 0:sbw], pb[3][:, 0:sbw], sg[:, 0:sbw], ALU.mult, ["pb3", sgk], ["mtmp"])
                        if b < 3:
                            self.tt("gpsimd", self.macc[:, 0:sbw], self.macc[:, 0:sbw], self.mtmp[:, 0:sbw], ALU.add, ["macc", "mtmp"], ["macc"])
                        else:
                            self.tt("gpsimd", mT[:, j, ts_], self.macc[:, 0:sbw], self.mtmp[:, 0:sbw], ALU.add, ["macc", "mtmp"], ["mT"])
        for hc in range(2):
            wo, wok = self.load_w(None, [(I["wout"][:, hc * 512:(hc + 1) * 512].rearrange("(c p) n -> p c n", p=128),
                                          (lambda st: st[:, 0:4096].rearrange("p (c n) -> p c n", c=8)))], 4096)
            for t in range(TBt):
                pz = pb[4 + t % 2]; pzk = "pb%d" % (4 + t % 2)
                for c in range(8):
                    self.mm(pz[:, :], mT[:, c, t * 128:(t + 1) * 128], wo[:, c * 512:(c + 1) * 512], ["mT", wok], [pzk], start=(c == 0), stop=(c == 7))
                self.tt("vector", x[:, t, hc * 512:(hc + 1) * 512], pz[:, :], x[:, t, hc * 512:(hc + 1) * 512], ALU.add, [pzk, "x"], ["x"])
        P.barrier()
        F = self.F; NF = F // 128
        if not hasattr(self, "actT"):
            self.actT = self.yT
            self.gs = [P.sb("gs%d" % i, [128, 512]) for i in range(2)]
            if self.moe:
                self.rt = P.sb("router", [128, 8, 8]); self.gw = P.sb("gatew", [128, TBt, 8])
                self.r1 = P.sb("r1", [128, 8]); self.r2 = P.sb("r2", [128, 8]); self.rm = P.sb("rm", [128, 4])
                self.m1 = P.sb("rmask1", [128, 8]); self.m2 = P.sb("rmask2", [128, 8])
                P.dma(self.rt[:], I["router"].rearrange("(c p) e -> p c e", p=128), writes=["router"])
        actT = self.actT
        if self.moe:
            for t in range(TBt):
                self.norm_to_hT_tile32(t)
                for c in range(8):
                    self.mm(pb[6][:, 0:8], self.h32[:, c, :], self.rt[:, c, :], ["h32", "router"], ["pb6"], start=(c == 0), stop=(c == 7))
                r1, r2, rm, m1, m2, gw = self.r1, self.r2, self.rm, self.m1, self.m2, self.gw
                self.cp("vector", r1[:], pb[6][:, 0:8], ["pb6"], ["r1"])
                P.op("vector", lambda e: e.reduce_max(out=rm[:, 0:1], in_=r1[:], axis=AX.X), reads=["r1"], writes=["rm"])
                self.ts("vector", m1[:], r1[:], rm[:, 0:1], ALU.is_equal, ["r1", "rm"], ["rmask1"])
                self.stt(r2[:], m1[:], -1e30, r1[:], ALU.mult, ALU.add, ["rmask1", "r1"], ["r2"])
                P.op("vector", lambda e: e.reduce_max(out=rm[:, 1:2], in_=r2[:], axis=AX.X), reads=["r2"], writes=["rm"])
                self.ts("vector", m2[:], r2[:], rm[:, 1:2], ALU.is_equal, ["r2", "rm"], ["rmask2"])
                self.tt("vector", rm[:, 2:3], rm[:, 1:2], rm[:, 0:1], ALU.subtract, ["rm"], ["rm"])
                self.act(rm[:, 2:3], rm[:, 2:3], AF.Exp, ["rm"], ["rm"])
                self.ts("vector", rm[:, 2:3], rm[:, 2:3], 1.0, ALU.add, ["rm"], ["rm"])
                P.op("vector", lambda e: e.reciprocal(out=rm[:, 2:3], in_=rm[:, 2:3]), reads=["rm"], writes=["rm"])
                self.ts("vector", rm[:, 3:4], rm[:, 2:3], -1.0, ALU.mult, ["rm"], ["rm"], s2=1.0, op1=ALU.add)
                self.ts("vector", m1[:], m1[:], rm[:, 2:3], ALU.mult, ["rmask1", "rm"], ["rmask1"])
                self.stt(gw[:, t, :], m2[:], rm[:, 3:4], m1[:], ALU.mult, ALU.add, ["rmask2", "rm", "rmask1"], ["gatew"])
        self.norm_to_hT(8)
        gi = 0
        for e in range(self.NE):
            for f0 in range(0, NF, 8):
                nf = min(8, NF - f0)
                for g0 in range(0, nf, 4):
                    ng = min(4, nf - g0)
                    fa = f0 + g0
                    wg_, wgk_ = self.load_w(None, [(I["fwg"][e, :, fa * 128:(fa + ng) * 128].rearrange("(c p) n -> p c n", p=128),
                                                    (lambda st, ng=ng: st[:, 0:4096].rearrange("p (c n) -> p c n", c=8)[:, :, 0:ng * 128]))], 4096)
                    wu_, wuk_ = self.load_w(None, [(I["fwu"][e, :, fa * 128:(fa + ng) * 128].rearrange("(c p) n -> p c n", p=128),
                                                    (lambda st, ng=ng: st[:, 0:4096].rearrange("p (c n) -> p c n", c=8)[:, :, 0:ng * 128]))], 4096)
                    for ii in range(ng):
                        i = g0 + ii
                        for sb_ in range(NSB):
                            ts_ = slice(sb_ * sbw, (sb_ + 1) * sbw)
                            for c in range(8):
                                self.mm(pb[2][:, 0:sbw], wg_[:, c * 512 + ii * 128:c * 512 + (ii + 1) * 128], self.hT[:, c, ts_], [wgk_, "hT"], ["pb2"], start=(c == 0), stop=(c == 7))
                            for c in range(8):
                                self.mm(pb[3][:, 0:sbw], wu_[:, c * 512 + ii * 128:c * 512 + (ii + 1) * 128], self.hT[:, c, ts_], [wuk_, "hT"], ["pb3"], start=(c == 0), stop=(c == 7))
                            gs = self.gs[gi % 2]; gsk = "gs%d" % (gi % 2); gi += 1
                            self.act(gs[:, 0:sbw], pb[2][:, 0:sbw], AF.Silu, ["pb2"], [gsk])
                            self.tt("vector", actT[:, i, ts_], pb[3][:, 0:sbw], gs[:, 0:sbw], ALU.mult, ["pb3", gsk], ["actT"])
                for hc in range(2):
                    wd, wdk = self.load_w(None, [(I["fwd"][e, f0 * 128:(f0 + nf) * 128, hc * 512:(hc + 1) * 512].rearrange("(i p) n -> p i n", p=128),
                                                  (lambda st, nf=nf: st[:, 0:nf * 512].rearrange("p (i n) -> p i n", n=512)))], nf * 512)
                    for t in range(TBt):
                        pz = pb[4 + t % 2]; pzk = "pb%d" % (4 + t % 2)
                        for i in range(nf):
                            self.mm(pz[:, :], actT[:, i, t * 128:(t + 1) * 128], wd[:, i * 512:(i + 1) * 512], ["actT", wdk], [pzk],
                                    start=(i == 0), stop=(i == nf - 1))
                        xs = x[:, t, hc * 512:(hc + 1) * 512]
                        if self.moe:
                            self.stt(xs, pz[:, :], self.gw[:, t, e:e + 1], xs, ALU.mult, ALU.add, [pzk, "gatew", "x"], ["x"])
                        else:
                            self.tt("vector", xs, pz[:, :], xs, ALU.add, [pzk, "x"], ["x"])
        self.norm_to_hT(16)
        if not hasattr(self, "pTt"):
            self.pTt = P.sb("pTt", [128, 2, tb], BF16)
        for c2 in range(2):
            P.dma(self.ystg[:], I["pT"][c2, :, t0:t0 + tb], writes=["ystg"])
            self.cp("gpsimd", self.pTt[:, c2, :], self.ystg[:], ["ystg"], ["pTt"])
        for hc in range(2):
            wpg, wpgk = self.load_w(None, [(I["plegate"][:, hc * 512:(hc + 1) * 512].rearrange("(c p) n -> p c n", p=128),
                                            (lambda st: st[:, 0:4096].rearrange("p (c n) -> p c n", c=8)))], 4096)
            wpp, wppk = self.load_w(None, [(I["pleproj"][:, hc * 512:(hc + 1) * 512].rearrange("(c2 p) n -> p c2 n", p=128),
                                            (lambda st: st[:, 0:1024].rearrange("p (c2 n) -> p c2 n", c2=2)))], 1024)
            for t in range(TBt):
                for c in range(8):
                    self.mm(pb[2][:, :], self.hT[:, c, t * 128:(t + 1) * 128], wpg[:, c * 512:(c + 1) * 512], ["hT", wpgk], ["pb2"], start=(c == 0), stop=(c == 7))
                for c2 in range(2):
                    self.mm(pb[3][:, :], self.pTt[:, c2, t * 128:(t + 1) * 128], wpp[:, c2 * 512:(c2 + 1) * 512], ["pTt", wppk], ["pb3"], start=(c2 == 0), stop=(c2 == 1))
                gs = self.gs[gi % 2]; gsk = "gs%d" % (gi % 2); gi += 1
                self.act(gs[:], pb[2][:, :], AF.Sigmoid, ["pb2"], [gsk])
                self.tt("vector", gs[:], pb[3][:, :], gs[:], ALU.mult, ["pb3", gsk], [gsk])
                xs = x[:, t, hc * 512:(hc + 1) * 512]
                self.tt("gpsimd", xs, xs, gs[:], ALU.add, ["x", gsk], ["x"])
        for t in range(TBt):
            if self.final:
                xn = self.xn[t % 2]; xk = "xn%d" % (t % 2)
                self.act(self.sq[:], x[:, t, :], AF.Square, ["x"], ["sqj", "ssv"], accum_out=self.ssv[:, 0:1])
                self.act(self.ssv[:, 0:1], self.ssv[:, 0:1], AF.Sqrt, ["ssv"], ["ssv"], scale=1.0 / D, bias=1e-6)
                P.op("vector", lambda e: e.reciprocal(out=self.ssv[:, 1:2], in_=self.ssv[:, 0:1]), reads=["ssv"], writes=["ssv"])
                self.stt(xn[:], x[:, t, :], self.ssv[:, 1:2], self.fn[:], ALU.mult, ALU.mult, ["x", "ssv", "fnorm"], [xk])
                P.dma(self.O[t0 + t * 128:t0 + (t + 1) * 128, :], xn[:], reads=[xk], writes=["O"])
            else:
                P.dma(self.O[t0 + t * 128:t0 + (t + 1) * 128, :], x[:, t, :], reads=["x"], writes=["O"])

    def norm_to_hT_tile32(self, t):
        P = self.P
        xn = self.xn[t % 2]; xk = "xn%d" % (t % 2)
        self.act(self.sq[:], self.x[:, t, :], AF.Square, ["x"], ["sqj", "ssv"], accum_out=self.ssv[:, 0:1])
        self.act(self.ssv[:, 0:1], self.ssv[:, 0:1], AF.Sqrt, ["ssv"], ["ssv"], scale=1.0 / D, bias=1e-6)
        P.op("vector", lambda e: e.reciprocal(out=self.ssv[:, 1:2], in_=self.ssv[:, 0:1]), reads=["ssv"], writes=["ssv"])
        self.ts("vector", xn[:], self.x[:, t, :], self.ssv[:, 1:2], ALU.mult, ["x", "ssv"], [xk])
        for half in range(2):
            pt = self.pb[half]; pk = "pb%d" % half
            for c4 in range(4):
                c = half * 4 + c4
                self.tr(pt[:, c4 * 128:(c4 + 1) * 128], xn[:, c * 128:(c + 1) * 128], [xk], [pk])
            gsl = self.norms[:, 8 + half * 4:8 + half * 4 + 4]
            self.tt("vector", self.h32[:, half * 4:(half + 1) * 4, :],
                    pt[:].rearrange("p (c t) -> p c t", c=4), gsl.unsqueeze(2).to_broadcast([128, 4, 128]), ALU.mult,
                    [pk, "norms"], ["h32"])


def make_inputs_b(inp, layer, core, ys_full, x_full, ntok=2048, final=None, fused=False, sfx=""):
    sl = slice(core * ntok, (core + 1) * ntok)
    if not fused:
        xs = x_full.reshape(-1, D)[sl]
        ys = ys_full.reshape(-1, 4, 2, 128)[sl]
        ysT = np.ascontiguousarray(ys.transpose(1, 2, 3, 0).reshape(8, 128, ntok))
    p = inp["p"][layer].reshape(-1, 2, 128)[sl]
    pT = np.ascontiguousarray(p.transpose(1, 2, 0))
    moe = (layer % 2 == 1); j = layer // 2
    norms = np.concatenate([inp["norm_mix"][layer].reshape(8, 128).T, inp["norm_ffn"][layer].reshape(8, 128).T,
                            inp["norm_ple"][layer].reshape(8, 128).T], axis=1)
    if final is None:
        final = (layer == 1)
    d = {
        "pT": pT,
        "wgate": np.ascontiguousarray(inp["w_in"][layer][:, 3596:7692]), "wbo": inp["w_bo"][layer], "wout": inp["w_out"][layer],
        "norms": np.ascontiguousarray(norms.astype(np.float32)),
        "plegate": inp["ple_gate"][layer], "pleproj": inp["ple_proj"][layer],
    }
    if not fused:
        d["x"] = np.ascontiguousarray(xs); d["ysT"] = ysT
    if final:
        d["fnorm"] = inp["final_norm"].reshape(1, D)
    if moe:
        d["fwg"] = inp["moe_w_gate"][j]; d["fwu"] = inp["moe_w_up"][j]; d["fwd"] = inp["moe_w_down"][j]; d["router"] = inp["moe_router"][j]
    else:
        d["fwg"] = inp["ffn_w_gate"][j][None]; d["fwu"] = inp["ffn_w_up"][j][None]; d["fwd"] = inp["ffn_w_down"][j][None]
    return {k + sfx: v for k, v in d.items()}


def build_fused(do=("diff", "fox", "rwkv", "gdn"), nlayers=2):
    S_ = S
    nc = bass.Bass("TRN2", target_bir_lowering=False)
    ntok = S_ // 4; tb = min(1024, ntok)
    CRY = S_ // 8
    CRX = max(128, ntok // 8)
    NCHX = ntok // CRX
    G16 = CRY // 16
    kas = [KA(l, do=do) for l in range(nlayers)]
    kbs = [KB(l, ntok=ntok, tb=tb, moe=(l % 2 == 1), final=(l == 1)) for l in range(nlayers)]
    x_in = nc.dram_tensor("x", [S_, D], F32, kind="ExternalInput").ap()
    rope = nc.dram_tensor("rope", [2, 64, S_], F32, kind="ExternalInput").ap()
    for l in range(nlayers):
        kas[l].declare(nc, sfx="_a%d" % l)
        kbs[l].declare(nc, sfx="_b%d" % l, fused=True)
    out = nc.dram_tensor("out", [ntok, D], F32, kind="ExternalOutput").ap()
    ybuf = [nc.dram_tensor("ybuf%d" % l, [S_, 256], F32).ap() for l in range(nlayers)]
    yg = [nc.dram_tensor("yg%d" % l, [4 * S_, 256], F32).ap() for l in range(nlayers)]
    xq = nc.dram_tensor("xq", [ntok, D], F32).ap()
    xgc = nc.dram_tensor("xgc", [S_, D], F32).ap()
    xq0 = nc.dram_tensor("xq0", [ntok, D], F32).ap()
    ysel = nc.dram_tensor("ysel", [4 * ntok, 256], F32).ap()
    P = Prog(nc, n_chan=16)
    sh = KA.make_shared(nc, P)
    groups = [[0, 1, 2, 3], [4, 5, 6, 7]]

    def xg_rows(t0):
        r = t0 // ntok; w = t0 % ntok; k = w // CRX; i = w % CRX
        row = (k * 4 + r) * CRX + i
        return xgc[row:row + 128, :]

    for l in range(nlayers):
        xsrc = x_in if l == 0 else xg_rows
        kas[l].emit(nc, P, sh, xsrc, rope, ybuf[l].rearrange("s (b c) -> s b c", b=4))
        for k in range(8):
            P.collective("AllGather", groups, ybuf[l][k * CRY:(k + 1) * CRY, :], yg[l][k * 4 * CRY:(k + 1) * 4 * CRY, :])
        P.phase_begin()
        ygv = yg[l].rearrange("(a b) c -> a (b c)", b=16)
        yselv = ysel.rearrange("(r a b) c -> r a (b c)", r=4, b=16)
        for j in range(2):
            P.dma(yselv[:, j * G16:(j + 1) * G16, :],
                  (lambda e, j=j, ygv=ygv: ygv[bass.ds(P.qid(e) * (8 * G16) + j * 4 * G16, 4 * G16), :].rearrange("(r a) c -> r a c", r=4)),
                  writes=["ysel"])
        if l == 0:
            P.dma(xq0, (lambda e: x_in[bass.ds(P.qid(e) * ntok, ntok), :]), writes=["xq0"])
        P.phase_end()
        P.phase_begin()
        if l == 0:
            xfn = lambda e, r0, n: xq0[r0:r0 + n, :]
        else:
            xfn = lambda e, r0, n: xq[r0:r0 + n, :]
        yfn = lambda e, r, r0, n: ysel[r * ntok + r0:r * ntok + r0 + n, :]
        def after_block(blk, l=l):
            if l < nlayers - 1:
                for k in range(blk * tb // CRX, (blk + 1) * tb // CRX):
                    P.collective("AllGather", groups, xq[k * CRX:(k + 1) * CRX, :], xgc[k * 4 * CRX:(k + 1) * 4 * CRX, :],
                                 reads=["O"], block=False)
        kbs[l].emit(nc, P, sh["pb"], sh["ident"], xfn, (xq if l < nlayers - 1 else out), yfn, after_block=after_block)
        P.phase_end()
        if l < nlayers - 1:
            P.collective_wait()
    P.wait_all_dma("sync")
    P.emit()
    return nc


def make_inputs_fused(inp, core, nlayers=2):
    b, h = core // 4, core % 4
    ntok = S // 4
    m = {"x": np.ascontiguousarray(inp["x"][b]), "rope": rope_tables()}
    for l in range(nlayers):
        a = make_inputs(inp, l, core)
        for k_, v in a.items():
            if k_ != "rope":
                m[k_ + "_a%d" % l] = v
        d = make_inputs_b(inp, l, 0, None, None, ntok=ntok, fused=True, sfx="_b%d" % l)
        p = inp["p"][l][b].reshape(S, 2, 128)[h * ntok:(h + 1) * ntok]
        d["pT_b%d" % l] = np.ascontiguousarray(p.transpose(1, 2, 0))
        m.update(d)
    return m


_CACHE = {}


def kernel(**inputs):
    inp = {k: np.asarray(v) for k, v in inputs.items()}
    if "nc" not in _CACHE:
        _CACHE["nc"] = build_fused()
    cores = list(range(8))
    maps = [make_inputs_fused(inp, core) for core in cores]
    res = run_bass_kernel_spmd(_CACHE["nc"], maps, core_ids=cores)
    out = np.concatenate([res.results[c]["out"] for c in cores], axis=0)
    return out.reshape(2, S, 1024).astype(np.float32)
```

```python
import math
import contextlib
import numpy as np
import concourse.bass as bass
import concourse.mybir as mybir
from concourse.bass_utils import run_bass_kernel_spmd


F32 = mybir.dt.float32
BF16 = mybir.dt.bfloat16
AF = mybir.ActivationFunctionType
ALU = mybir.AluOpType
AX = mybir.AxisListType

COMPUTE = ("tensor", "vector", "scalar", "gpsimd")
QUEUES = ("sync", "gpsimd", "scalar")


class Chan:
    def __init__(self, sem):
        self.sem = sem
        self.n = 0
        self.T = 0
        self.issue_waited = 0


class Prog:
    def __init__(self, nc, n_chan=12):
        self.nc = nc
        self.es = contextlib.ExitStack()
        self.ops = {e: [] for e in ("tensor", "vector", "scalar", "gpsimd", "sync")}
        self.cnt = {e: 0 for e in COMPUTE}
        self.sem = {e: self.es.enter_context(nc.semaphore("s_" + e)) for e in COMPUTE}
        self.chans = [Chan(self.es.enter_context(nc.semaphore("c%d" % i))) for i in range(n_chan)]
        self.rr = 0
        self.last_w = {}
        self.readers = {}
        self.known = {e: {} for e in self.ops}
        self.tensors = {}
        self.pes = None
        self.phase_id = 0
        self.flush_id = 0
        self.qcache = {}

    def sb(self, name, shape, dt=F32):
        es = self.pes if self.pes is not None else self.es
        t = es.enter_context(self.nc.sbuf_tensor("sb%d_" % self.phase_id + name, list(shape), dt))
        return t

    def phase_begin(self):
        self.phase_id += 1
        self.pes = contextlib.ExitStack()

    def phase_end(self):
        self.barrier()
        self.flush()
        self.pes.close()
        self.pes = None

    def barrier(self):
        for e in self.ops:
            waits = []
            kn = self.known[e]
            for e2 in COMPUTE:
                v = self.cnt[e2]
                if v and kn.get(("eng", e2), 0) < v:
                    kn[("eng", e2)] = v
                    waits.append((self.sem[e2], v))
            for ci, ch in enumerate(self.chans):
                if ch.n and kn.get(("chan", ci), 0) < ch.n:
                    kn[("chan", ci)] = ch.n
                    waits.append((ch.sem, 16 * ch.n))
                ch.T = ch.n
                ch.issue_waited = ch.n
            if waits:
                self.ops[e].append((None, waits, None))
        self.last_w = {}
        self.readers = {}

    def flush(self):
        nc = self.nc
        self.flush_id += 1
        with nc.Block() as block:
            def mk(engname):
                def body(e):
                    for fn, waits, inc in self.ops[engname]:
                        for s, v in waits:
                            e.wait_ge(s, v)
                        if fn is not None:
                            ins = fn(e)
                            if inc is not None:
                                ins.then_inc(inc[0], inc[1])
                return body
            block.sync(mk("sync"))
            block.tensor(mk("tensor"))
            block.vector(mk("vector"))
            block.scalar(mk("scalar"))
            block.gpsimd(mk("gpsimd"))
        self.ops = {e: [] for e in self.ops}

    def ps(self, name, shape, dt=F32):
        t = self.es.enter_context(self.nc.psum_tensor("ps_" + name, list(shape), dt))
        return t

    def _dep_waits(self, eng, reads, writes):
        deps = []
        for k in reads:
            w = self.last_w.get(k)
            if w is not None:
                deps.append(w)
        relax = getattr(self, "relax_same_engine", False)
        for k in writes:
            w = self.last_w.get(k)
            if w is not None and not (relax and w[0] == "eng" and w[1] == eng):
                deps.append(w)
            for rd in self.readers.get(k, ()):
                if relax and rd[0] == "eng" and rd[1] == eng:
                    continue
                deps.append(rd)
        waits = {}
        for d in deps:
            if d[0] == "eng":
                _, e2, n = d
                if e2 == eng and eng == "tensor":
                    continue
                key = ("eng", e2)
                waits[key] = max(waits.get(key, 0), n)
            else:
                _, ci = d
                ch = self.chans[ci]
                key = ("chan", ci)
                waits[key] = max(waits.get(key, 0), ch.n)
                ch.T = max(ch.T, ch.n)
        out = []
        kn = self.known[eng]
        for key, v in waits.items():
            if kn.get(key, 0) >= v:
                continue
            kn[key] = v
            if key[0] == "eng":
                out.append((self.sem[key[1]], v))
            else:
                out.append((self.chans[key[1]].sem, 16 * v))
        return out

    def _record(self, tag, reads, writes):
        for k in reads:
            self.readers.setdefault(k, []).append(tag)
        for k in writes:
            self.last_w[k] = tag
            self.readers[k] = []

    def op(self, eng, fn, reads=(), writes=()):
        waits = self._dep_waits(eng, reads, writes)
        self.cnt[eng] += 1
        n = self.cnt[eng]
        self.ops[eng].append((fn, waits, (self.sem[eng], 1)))
        self._record(("eng", eng, n), reads, writes)

    def dma(self, out, in_, reads=(), writes=(), q="sync", chan=None, **kw):
        if chan is None:
            chan = self.rr
            self.rr = (self.rr + 1) % len(self.chans)
        ch = self.chans[chan]
        waits = self._dep_waits(q, reads, writes)
        if ch.T > ch.issue_waited:
            kn = self.known[q]
            key = ("chan", chan)
            if kn.get(key, 0) < ch.T:
                kn[key] = ch.T
                waits.append((ch.sem, 16 * ch.T))
            ch.issue_waited = ch.T
        ch.n += 1
        def fn(e, out=out, in_=in_, kw=kw):
            o = out(e) if callable(out) else out
            i = in_(e) if callable(in_) else in_
            try:
                return e.dma_start(out=o, in_=i, **kw)
            except Exception:
                print("DMA FAIL out=", o, " in=", i)
                raise
        self.ops[q].append((fn, waits, (ch.sem, 16)))
        self._record(("chan", chan), reads, writes)

    def qid(self, e):
        key = (self.flush_id, id(e))
        if key not in self.qcache:
            self.qcache[key] = e.snap(e.partition_id() % 4, min_val=0, max_val=3)
        return self.qcache[key]

    def collective(self, kind, groups, src, dst, reads=(), writes=(), block=True):
        if not hasattr(self, "cc_sem"):
            self.cc_sem = self.es.enter_context(self.nc.semaphore("cc_sem")); self.cc_n = 0
        waits = self._dep_waits("gpsimd", reads, writes)
        self.cc_n += 1
        n = self.cc_n
        self.ops["gpsimd"].append((lambda e: e.collective_compute(kind, ALU.bypass, replica_groups=groups, ins=[src], outs=[dst]), waits, (self.cc_sem, 1)))
        if block:
            self.collective_wait(n)
        return n

    def collective_wait(self, n=None):
        n = self.cc_n if n is None else n
        for e in self.ops:
            self.ops[e].append((None, [(self.cc_sem, n)], None))

    def wait_all_dma(self, eng="sync"):
        waits = []
        for ch in self.chans:
            if ch.n:
                waits.append((ch.sem, 16 * ch.n))
        self.ops[eng].append((None, waits, None))

    def emit(self):
        self.flush()
        self.es.close()


S = 8192
D = 1024
NTB = S // 512
NT = S // 128

def colsel(h):
    cm = []
    r0 = 0; dq = 1024; dk = 1280; dv = 1536
    fq = 1792; fk = 2048; fv = 2304; ffl = 2560
    gq = 2564; gk = 2820; gv = 3076; gb = 3332; ga = 3336; gg = 3340
    hs = np.arange(64) + h * 64

    def swap(base):
        idx = base + hs
        sw = idx.copy()
        for c in range(2):
            for d in range(4):
                sw[c * 32 + d] = idx[c * 32 + d + 4]
                sw[c * 32 + d + 4] = idx[c * 32 + d]
        return sw
    cm.append(("qd", dq + hs)); cm.append(("qds", swap(dq)))
    cm.append(("kd", dk + hs)); cm.append(("kds", swap(dk)))
    cm.append(("qf", fq + hs)); cm.append(("kf", fk + hs))
    cm.append(("fl", np.array([ffl + h])))
    cm.append(("rr", 0 + hs)); cm.append(("rk", 256 + hs)); cm.append(("rv", 512 + hs))
    cm.append(("xw", 768 + np.arange(64))); cm.append(("xa", 832 + np.arange(64)))
    cm.append(("xg", 896 + np.arange(128)))
    cm.append(("gq", gq + hs)); cm.append(("gk", gk + hs)); cm.append(("gv", gv + hs))
    cm.append(("gb", np.full(128, gb + h))); cm.append(("ga", np.full(128, ga + h)))
    names = {}
    idx = []
    o = 0
    for n, ix in cm:
        names[n] = (o, len(ix)); o += len(ix); idx.append(ix)
    tm = [("vd", dv + hs), ("vf", fv + hs), ("ggt", gg + hs)]
    tnames = {}; tidx = []; o = 0
    for n, ix in tm:
        tnames[n] = (o, len(ix)); o += len(ix); tidx.append(ix)
    return names, np.concatenate(idx), tnames, np.concatenate(tidx)


def rope_tables():
    pos = np.arange(S, dtype=np.float32)
    inv = (500000.0 ** (-np.arange(4, dtype=np.float32) * 2.0 / 8)).astype(np.float32)
    ang = pos[None, :] * inv[:, None]
    cos = np.cos(ang).astype(np.float32); sin = np.sin(ang).astype(np.float32)
    ct = np.ones((64, S), np.float32); st = np.zeros((64, S), np.float32)
    for c in range(2):
        for d in range(4):
            ct[c * 32 + d] = cos[d]; ct[c * 32 + d + 4] = cos[d]
            st[c * 32 + d] = -sin[d]; st[c * 32 + d + 4] = sin[d]
    return np.stack([ct, st])


class KA:
    def __init__(self, layer_idx, do=("diff", "fox", "rwkv", "gdn")):
        self.layer_idx = layer_idx
        self.do = do
        self.names, _, self.tnames, _ = colsel(0)
        self.NCc = sum(n for _, n in self.names.values())
        self.NCv = sum(n for _, n in self.tnames.values())

    def declare(self, nc, sfx=""):
        NCc, NCv = self.NCc, self.NCv
        I = {}
        def inp(name, shape):
            I[name] = nc.dram_tensor(name + sfx, list(shape), F32, kind="ExternalInput").ap()
        inp("wc", [D, NCc]); inp("wv", [D, NCv]); inp("gm", [128, 8])
        inp("dlam", [1, 128]); inp("dsub", [1, 64]); inp("fbias", [1, 1])
        inp("rp", [128, 16]); inp("w2h", [64, 64]); inp("a2h", [64, 64]); inp("g2h", [128, 64]); inp("rln", [2, 64])
        inp("gp", [128, 16]); inp("gnorm", [1, 64])
        self.I = I
        self.UT = nc.dram_tensor("ut" + sfx, [NCc, S], F32).ap()
        self.UV = nc.dram_tensor("uv" + sfx, [S, NCv], F32).ap()
        self.sfx = sfx

    def Ybr(self, br):
        if isinstance(self.Y, dict):
            return self.Y[br]
        return self.Y[:, br, :]

    def emit(self, nc, P, shared, xsrc, rope, Ydst, after_attn=None):
        self.nc = nc; self.P = P
        self.pb = shared["pb"]; self.ident = shared["ident"]
        self.mask = shared["mask"]; self.BTi = shared["BTi"]; self.BTe = shared["BTe"]; self.BTeT = shared["BTeT"]; self.ones64 = shared["ones64"]
        self.I["x"] = xsrc; self.I["rope"] = rope
        self.Y = Ydst
        P.phase_begin(); self.phase0(); P.phase_end()
        if "diff" in self.do or "fox" in self.do:
            P.phase_begin()
            if "diff" in self.do:
                self.diff()
            if "fox" in self.do:
                self.fox()
            P.phase_end()
            for nm in ("qT", "stg", "on"):
                if hasattr(self, nm):
                    delattr(self, nm)
        if after_attn is not None:
            after_attn()
        gens = []
        if "rwkv" in self.do or "gdn" in self.do:
            P.phase_begin()
            if "rwkv" in self.do:
                gens.append(self.rwkv())
            if "gdn" in self.do:
                gens.append(self.gdn())
            while gens:
                for g in list(gens):
                    try:
                        next(g)
                    except StopIteration:
                        gens.remove(g)
            P.phase_end()

    @staticmethod
    def make_shared(nc, P):
        sh = {}
        sh["pb"] = [P.ps("pb%d" % i, [128, 512]) for i in range(8)]
        ident = P.sb("ident", [128, 128]); sh["ident"] = ident
        P.op("gpsimd", lambda e: e.memset(ident[:], 1.0), writes=["ident"])
        P.op("gpsimd", lambda e: e.affine_select(out=ident[:], in_=ident[:], compare_op=ALU.is_equal,
                                                   fill=0.0, base=0, pattern=[[-1, 128]], channel_multiplier=1),
             reads=["ident"], writes=["ident"])
        tmp = KA(0); tmp.P = P; tmp.nc = nc
        tmp.attn_common_masks()
        sh["mask"] = tmp.mask; sh["BTi"] = tmp.BTi; sh["BTe"] = tmp.BTe; sh["BTeT"] = tmp.BTeT; sh["ones64"] = tmp.ones64
        return sh

    def build(self):
        nc = bass.Bass("TRN2", target_bir_lowering=False)
        self.declare(nc)
        xsrc = nc.dram_tensor("x", [S, D], F32, kind="ExternalInput").ap()
        rope = nc.dram_tensor("rope", [2, 64, S], F32, kind="ExternalInput").ap()
        dbg = getattr(self, "dbg", False)
        Y = nc.dram_tensor("y", [S, 4, 64], F32, kind="ExternalOutput").ap()
        P = Prog(nc, n_chan=16)
        P.relax_same_engine = getattr(self, 'relax', False)
        sh = KA.make_shared(nc, P)
        self.emit(nc, P, sh, xsrc, rope, Y)
        P.wait_all_dma("sync")
        P.emit()
        return nc

    def tt(self, eng, out, in0, in1, op, r, w):
        self.P.op(eng, lambda e: e.tensor_tensor(out=out, in0=in0, in1=in1, op=op), reads=r, writes=w)

    def ts(self, eng, out, in0, s1, op0, r, w, s2=None, op1=None):
        if op1 is None:
            self.P.op(eng, lambda e: e.tensor_scalar(out=out, in0=in0, scalar1=s1, scalar2=None, op0=op0), reads=r, writes=w)
        else:
            self.P.op(eng, lambda e: e.tensor_scalar(out=out, in0=in0, scalar1=s1, scalar2=s2, op0=op0, op1=op1), reads=r, writes=w)

    def stt(self, eng, out, in0, sc, in1, op0, op1, r, w):
        eng = "vector"
        self.P.op(eng, lambda e: e.scalar_tensor_tensor(out=out, in0=in0, scalar=sc, in1=in1, op0=op0, op1=op1), reads=r, writes=w)

    def act(self, out, in_, func, r, w, **kw):
        self.P.op("scalar", lambda e: e.activation(out=out, in_=in_, func=func, **kw), reads=r, writes=w)

    def cp(self, eng, out, in_, r, w):
        if eng == "scalar":
            self.P.op(eng, lambda e: e.copy(out=out, in_=in_), reads=r, writes=w)
        else:
            self.P.op(eng, lambda e: e.tensor_copy(out=out, in_=in_), reads=r, writes=w)

    def mm(self, out, lhsT, rhs, r, w, start=True, stop=True):
        self.P.op("tensor", lambda e: e.matmul(out, lhsT=lhsT, rhs=rhs, start=start, stop=stop), reads=r, writes=w)

    def tr(self, out, in_, r, w, n=128):
        self.P.op("tensor", lambda e: e.transpose(out=out, in_=in_, identity=self.ident[0:n, 0:n]), reads=list(r) + ["ident"], writes=w)

    def ms(self, eng, out, val, w):
        self.P.op(eng, lambda e: e.memset(out, val), writes=w)

    def phase0(self):
        P, nc, I = self.P, self.nc, self.I
        NCc, NCv = self.NCc, self.NCv
        gm = P.sb("gm", [128, 8])
        P.dma(gm[:], I["gm"], writes=["gm"])
        wcb = P.sb("wcb", [128, 8, NCc], BF16)
        wvb = P.sb("wvb", [128, 8, NCv], BF16)
        wst = [P.sb("wst%d" % i, [128, 1408]) for i in range(2)]
        k = 0
        for (src, dst, n, dkey) in ((I["wc"], wcb, NCc, "wcb"), (I["wv"], wvb, NCv, "wvb")):
            for c in range(8):
                st = wst[k % 2]; key = "wst%d" % (k % 2); k += 1
                P.dma(st[:, 0:n], src[c * 128:(c + 1) * 128, :], writes=[key])
                P.op("vector", lambda e, st=st, dst=dst, c=c, n=n: e.tensor_scalar(
                    out=dst[:, c, :], in0=st[:, 0:n], scalar1=gm[:, c:c + 1], scalar2=None, op0=ALU.mult),
                    reads=[key, "gm"], writes=[dkey])
        groups = []
        o = 0
        while o < NCc:
            n = min(128, NCc - o); groups.append((o, n)); o += n
        xt = [P.sb("xt%d" % i, [128, D]) for i in range(2)]
        sq = P.sb("sqj", [128, D])
        ss = [P.sb("ss%d" % i, [128, 1]) for i in range(2)]
        hT = [P.sb("hT%d" % i, [128, 8, 512], BF16) for i in range(2)]
        og = [P.sb("og%d" % i, [128, 512]) for i in range(3)]
        ov = [P.sb("ov%d" % i, [128, NCv]) for i in range(2)]
        pT = [self.pb[0], self.pb[1]]
        gi = 0; xi = 0; vi = 0
        wckey = "wcb"; wvkey = "wvb"
        for tb in range(NTB):
            h = hT[tb % 2]; hk = "hT%d" % (tb % 2)
            for st in range(4):
                t0 = tb * 512 + st * 128
                x = xt[xi % 2]; xk = "xt%d" % (xi % 2); s_ = ss[xi % 2]; sk = "ss%d" % (xi % 2); xi += 1
                P.dma(x[:], (I["x"](t0) if callable(I["x"]) else I["x"][t0:t0 + 128, :]), writes=[xk], q="sync")
                P.op("scalar", lambda e, x=x, s_=s_: e.activation(out=sq[:], in_=x[:], func=AF.Square, accum_out=s_[:]),
                     reads=[xk], writes=["sqj", sk])
                P.op("scalar", lambda e, s_=s_: e.activation(out=s_[:], in_=s_[:], func=AF.Sqrt, scale=1.0 / D, bias=1e-6),
                     reads=[sk], writes=[sk])
                P.op("vector", lambda e, s_=s_: e.reciprocal(out=s_[:], in_=s_[:]), reads=[sk], writes=[sk])
                P.op("vector", lambda e, x=x, s_=s_: e.tensor_scalar(out=x[:], in0=x[:], scalar1=s_[:, 0:1], scalar2=None, op0=ALU.mult),
                     reads=[xk, sk], writes=[xk])
                for half in range(2):
                    pt = pT[half]; pk = "pb%d" % half
                    for c4 in range(4):
                        c = half * 4 + c4
                        P.op("tensor", lambda e, pt=pt, c4=c4, c=c, x=x: e.transpose(
                            out=pt[:, c4 * 128:(c4 + 1) * 128], in_=x[:, c * 128:(c + 1) * 128], identity=self.ident[:]),
                            reads=[xk, "ident"], writes=[pk])
                    eng = "scalar" if half == 0 else "vector"
                    if eng == "scalar":
                        P.op("scalar", lambda e, pt=pt, h=h, half=half, st=st: e.copy(
                            out=h[:, half * 4:(half + 1) * 4, st * 128:(st + 1) * 128],
                            in_=pt[:].rearrange("p (c t) -> p c t", c=4)), reads=[pk], writes=[hk])
                    else:
                        P.op("vector", lambda e, pt=pt, h=h, half=half, st=st: e.tensor_copy(
                            out=h[:, half * 4:(half + 1) * 4, st * 128:(st + 1) * 128],
                            in_=pt[:].rearrange("p (c t) -> p c t", c=4)), reads=[pk], writes=[hk])
            for (o, n) in groups:
                pg = self.pb[2 + gi % 2]; pk = "pb%d" % (2 + gi % 2)
                ob = og[gi % 3]; ok = "og%d" % (gi % 3); gi += 1
                for c in range(8):
                    P.op("tensor", lambda e, pg=pg, c=c, o=o, n=n, h=h: e.matmul(
                        pg[0:n, :], lhsT=wcb[:, c, o:o + n], rhs=h[:, c, :], start=(c == 0), stop=(c == 7)),
                        reads=[wckey, hk], writes=[pk])
                eng = "scalar" if gi % 2 == 0 else "vector"
                if eng == "scalar":
                    P.op("scalar", lambda e, pg=pg, ob=ob, n=n: e.copy(out=ob[0:n, :], in_=pg[0:n, :]), reads=[pk], writes=[ok])
                else:
                    P.op("vector", lambda e, pg=pg, ob=ob, n=n: e.tensor_copy(out=ob[0:n, :], in_=pg[0:n, :]), reads=[pk], writes=[ok])
                P.dma(self.UT[o:o + n, tb * 512:(tb + 1) * 512], ob[0:n, :], reads=[ok], writes=["UT"], q="sync")
            for st in range(4):
                t0 = tb * 512 + st * 128
                pv = self.pb[4 + vi % 2]; pk = "pb%d" % (4 + vi % 2)
                ob = ov[vi % 2]; ok = "ov%d" % (vi % 2); vi += 1
                for c in range(8):
                    P.op("tensor", lambda e, pv=pv, c=c, h=h, st=st: e.matmul(
                        pv[:, 0:NCv], lhsT=h[:, c, st * 128:(st + 1) * 128], rhs=wvb[:, c, :], start=(c == 0), stop=(c == 7)),
                        reads=[wvkey, hk], writes=[pk])
                P.op("vector", lambda e, pv=pv, ob=ob: e.tensor_copy(out=ob[:], in_=pv[:, 0:NCv]), reads=[pk], writes=[ok])
                P.dma(self.UV[t0:t0 + 128, :], ob[:], reads=[ok], writes=["UV"], q="sync")

    def attn_common_masks(self):
        if hasattr(self, "mask"):
            return
        P = self.P
        self.chunk_masks()
        self.mask = P.sb("amask", [128, 4, 512], BF16)
        P.op("gpsimd", lambda e: e.memset(self.mask[:], 1.0), writes=["amask"])
        for r in range(4):
            P.op("gpsimd", lambda e, r=r: e.affine_select(out=self.mask[:, r, :], in_=self.mask[:, r, :], compare_op=ALU.is_ge,
                                                        fill=0.0, base=-r * 128, pattern=[[1, 512]], channel_multiplier=-1),
                 reads=["amask"], writes=["amask"])

    def attn_loop(self, name, ncomp, kq_fn, bias_fn, scale, Vaug, vkey, rd_keys, epilogue):
        P = self.P
        self.attn_common_masks()
        PT = [P.sb("%s_pt%d" % (name, i), [128, 512], BF16) for i in range(3)]
        stb = [self.pb[0], self.pb[1], self.pb[2]]
        ob = [self.pb[4], self.pb[5]]
        n = 0
        for i in range(NTB):
            pairs = [(c, j) for c in range(ncomp) for j in range(4 * i + 4)]
            def issue_S(idx, n):
                c, j = pairs[idx]
                lhsT, rhs = kq_fn(c, j, i)
                sp = stb[n % 3]; sk = "pb%d" % (n % 3)
                P.op("tensor", lambda e, sp=sp, lhsT=lhsT, rhs=rhs: e.matmul(sp[:], lhsT=lhsT, rhs=rhs, start=True, stop=True),
                     reads=rd_keys, writes=[sk])
            issue_S(0, n)
            for idx, (c, j) in enumerate(pairs):
                if idx + 1 < len(pairs):
                    issue_S(idx + 1, n + 1)
                sp = stb[n % 3]; sk = "pb%d" % (n % 3)
                pt = PT[n % 3]; ptk = "%s_pt%d" % (name, n % 3)
                b = bias_fn(j)
                if b is None:
                    P.op("scalar", lambda e, sp=sp, pt=pt: e.activation(out=pt[:], in_=sp[:], func=AF.Exp, scale=scale),
                         reads=[sk], writes=[ptk])
                else:
                    P.op("scalar", lambda e, sp=sp, pt=pt, b=b: e.activation(out=pt[:], in_=sp[:], func=AF.Exp, scale=scale, bias=b),
                         reads=[sk, name + "_bias"], writes=[ptk])
                r = j - 4 * i
                if r >= 0:
                    P.op("vector", lambda e, pt=pt, r=r: e.tensor_tensor(out=pt[:], in0=pt[:], in1=self.mask[:, r, :], op=ALU.mult),
                         reads=[ptk, "amask"], writes=[ptk])
                o = ob[c]; okey = "pb%d" % (4 + c)
                for s in range(4):
                    P.op("tensor", lambda e, o=o, pt=pt, s=s, j=j, last=(j == 4 * i + 3): e.matmul(
                        o[:, s * 65:(s + 1) * 65], lhsT=pt[:, s * 128:(s + 1) * 128], rhs=Vaug[:, j, :],
                        start=(j == 0 and s == 0), stop=(last and s == 3)), reads=[ptk, vkey], writes=[okey])
                n += 1
            epilogue(i, ob)

    def load_qk(self, dst, dkey, src_name, swap_name=None, rope=None, extra_scale=None):
        P = self.P
        o, n = self.names[src_name]
        W = 2048
        for b in range(S // W):
            a = self.stg[0]; P.dma(a[0:64, :], self.UT[o:o + 64, b * W:(b + 1) * W], reads=["UT"], writes=["stg0"])
            if swap_name is not None:
                o2, _ = self.names[swap_name]
                a2 = self.stg[1]; P.dma(a2[0:64, :], self.UT[o2:o2 + 64, b * W:(b + 1) * W], reads=["UT"], writes=["stg1"])
                cs = self.stg[2]; P.dma(cs[0:64, :], self.I["rope"][0, :, b * W:(b + 1) * W], writes=["stg2"])
                sn = self.stg[3]; P.dma(sn[0:64, :], self.I["rope"][1, :, b * W:(b + 1) * W], writes=["stg3"])
                P.op("vector", lambda e, a=a, cs=cs: e.tensor_tensor(out=a[0:64, :], in0=a[0:64, :], in1=cs[0:64, :], op=ALU.mult),
                     reads=["stg0", "stg2"], writes=["stg0"])
                P.op("gpsimd", lambda e, a2=a2, sn=sn: e.tensor_tensor(out=a2[0:64, :], in0=a2[0:64, :], in1=sn[0:64, :], op=ALU.mult),
                     reads=["stg1", "stg3"], writes=["stg1"])
                P.op("vector", lambda e, a=a, a2=a2, b=b: e.tensor_tensor(out=dst[0:64, b * W:(b + 1) * W], in0=a[0:64, :], in1=a2[0:64, :], op=ALU.add),
                     reads=["stg0", "stg1"], writes=[dkey])
            else:
                if extra_scale is None:
                    P.op("vector", lambda e, a=a, b=b: e.tensor_copy(out=dst[0:64, b * W:(b + 1) * W], in_=a[0:64, :]),
                         reads=["stg0"], writes=[dkey])
                else:
                    P.op("vector", lambda e, a=a, b=b: e.tensor_scalar(out=dst[0:64, b * W:(b + 1) * W], in0=a[0:64, :],
                                                                         scalar1=extra_scale, scalar2=None, op0=ALU.mult),
                         reads=["stg0"], writes=[dkey])

    def load_v(self, Vaug, vkey, tname):
        P = self.P
        o, n = self.tnames[tname]
        P.op("gpsimd", lambda e: e.memset(Vaug[:, :, 64:65], 1.0), writes=[vkey])
        for b in range(S // 2048):
            a = self.stg[0]
            P.dma(a[:, 0:16 * 64].rearrange("p (t d) -> p t d", d=64),
                  self.UV[b * 2048:(b + 1) * 2048, o:o + 64].rearrange("(t p) d -> p t d", p=128), reads=["UV"], writes=["stg0"])
            P.op("vector", lambda e, a=a, b=b: e.tensor_copy(out=Vaug[:, b * 16:(b + 1) * 16, 0:64],
                                                             in_=a[:, 0:16 * 64].rearrange("p (t d) -> p t d", d=64)),
                 reads=["stg0"], writes=[vkey])

    def attn_alloc(self):
        if hasattr(self, "qT"):
            return
        P = self.P
        self.stg = [P.sb("stg%d" % i, [128, 2048]) for i in range(4)]
        self.qT = P.sb("qT", [128, S], BF16)
        self.kT = P.sb("kT", [128, S], BF16)
        self.Vaug = P.sb("Vaug", [128, NT, 65], BF16)
        self.ostage = [P.sb("ostage%d" % i, [128, 4, 64]) for i in range(2)]
        self.osc = P.sb("osc", [128, 16])
        self.otmp = [P.sb("otmp%d" % i, [128, 512]) for i in range(2)]
        self.on = 0

    def diff(self):
        P, I = self.P, self.I
        self.attn_alloc()
        qT, kT, Vaug = self.qT, self.kT, self.Vaug
        self.load_qk(qT, "qT", "qd", "qds", rope=True)
        self.load_qk(kT, "kT", "kd", "kds", rope=True)
        self.load_v(Vaug, "Vaug", "vd")
        lam_init = 0.8 - 0.6 * math.exp(-0.3 * self.layer_idx)
        lt = P.sb("lamt", [128, 128]); lp = P.sb("lamp", [128, 64]); ls = P.sb("lams", [128, 2]); nl = P.sb("neglam", [128, 1])
        P.dma(lt[:], I["dlam"].partition_broadcast(128), writes=["lamt"])
        P.op("vector", lambda e: e.tensor_tensor(out=lp[:].rearrange("p (a d) -> p a d", a=2),
                                                 in0=lt[:].rearrange("p (a b d) -> p a b d", a=2, b=2)[:, :, 0, :],
                                                 in1=lt[:].rearrange("p (a b d) -> p a b d", a=2, b=2)[:, :, 1, :], op=ALU.mult),
             reads=["lamt"], writes=["lamp"])
        P.op("vector", lambda e: e.reduce_sum(out=ls[:], in_=lp[:].rearrange("p (a d) -> p a d", a=2), axis=AX.X), reads=["lamp"], writes=["lams"])
        P.op("scalar", lambda e: e.activation(out=ls[:], in_=ls[:], func=AF.Exp), reads=["lams"], writes=["lams"])
        P.op("vector", lambda e: e.tensor_tensor(out=nl[:], in0=ls[:, 1:2], in1=ls[:, 0:1], op=ALU.subtract), reads=["lams"], writes=["neglam"])
        P.op("vector", lambda e: e.tensor_scalar(out=nl[:], in0=nl[:], scalar1=-lam_init, scalar2=None, op0=ALU.add), reads=["neglam"], writes=["neglam"])
        sub = P.sb("dsub", [128, 64])
        P.dma(sub[:], I["dsub"].partition_broadcast(128), writes=["dsub"])
        P.op("vector", lambda e: e.tensor_scalar(out=sub[:], in0=sub[:], scalar1=(1.0 - lam_init), scalar2=None, op0=ALU.mult), reads=["dsub"], writes=["dsub"])
        scale = 32 ** -0.5

        def kq(c, j, i):
            return kT[c * 32:(c + 1) * 32, j * 128:(j + 1) * 128], qT[c * 32:(c + 1) * 32, i * 512:(i + 1) * 512]

        def epi(i, ob):
            osc = self.osc
            o0 = ob[0][:, 0:260].rearrange("p (s d) -> p s d", d=65)
            o1 = ob[1][:, 0:260].rearrange("p (s d) -> p s d", d=65)
            og = self.ostage[self.on % 2]; ogk = "ostage%d" % (self.on % 2); self.on += 1
            P.op("vector", lambda e: e.reciprocal(out=osc[:, 0:4], in_=o0[:, :, 64]), reads=["pb4"], writes=["osc"])
            P.op("vector", lambda e: e.reciprocal(out=osc[:, 4:8], in_=o1[:, :, 64]), reads=["pb5"], writes=["osc"])
            P.op("vector", lambda e: e.tensor_scalar(out=osc[:, 4:8], in0=osc[:, 4:8], scalar1=nl[:, 0:1], scalar2=None, op0=ALU.mult),
                 reads=["osc", "neglam"], writes=["osc"])
            for s in range(4):
                P.op("vector", lambda e, s=s: e.tensor_scalar(out=og[:, s, :], in0=o0[:, s, 0:64], scalar1=osc[:, s:s + 1], scalar2=None, op0=ALU.mult),
                     reads=["pb4", "osc"], writes=[ogk])
                P.op("vector", lambda e, s=s: e.scalar_tensor_tensor(out=og[:, s, :], in0=o1[:, s, 0:64], scalar=osc[:, 4 + s:5 + s], in1=og[:, s, :],
                                                                      op0=ALU.mult, op1=ALU.add), reads=["pb5", "osc", ogk], writes=[ogk])
                P.op("scalar", lambda e, s=s: e.activation(out=self.stg[3][:, 0:64], in_=og[:, s, :], func=AF.Square, accum_out=osc[:, 8 + s:9 + s]),
                     reads=[ogk], writes=["stg3", "osc"])
            P.op("scalar", lambda e: e.activation(out=osc[:, 8:12], in_=osc[:, 8:12], func=AF.Sqrt, scale=1.0 / 64, bias=1e-5), reads=["osc"], writes=["osc"])
            P.op("vector", lambda e: e.reciprocal(out=osc[:, 8:12], in_=osc[:, 8:12]), reads=["osc"], writes=["osc"])
            for s in range(4):
                P.op("vector", lambda e, s=s: e.scalar_tensor_tensor(out=og[:, s, :], in0=og[:, s, :], scalar=osc[:, 8 + s:9 + s], in1=sub[:],
                                                                      op0=ALU.mult, op1=ALU.mult), reads=[ogk, "osc", "dsub"], writes=[ogk])
            P.dma(self.Ybr(1)[i * 512:(i + 1) * 512, :].rearrange("(s p) d -> p s d", p=128), og[:], reads=[ogk], writes=["Y1"], q="sync")

        self.attn_loop("diff", 2, kq, lambda j: None, scale, Vaug, "Vaug", ["qT", "kT"], epi)

    def fox(self):
        P, I = self.P, self.I
        self.attn_alloc()
        qT, kT, Vaug = self.qT, self.kT, self.Vaug
        scale = 64 ** -0.5
        self.load_qk(qT, "qT", "qf", extra_scale=scale)
        self.load_qk(kT, "kT", "kf")
        self.load_v(Vaug, "Vaug", "vf")
        o, _ = self.names["fl"]
        W = 2048
        z = self.stg[0]; t1 = self.stg[1]; crow = self.stg[2]; tmp = self.stg[3]
        one = P.sb("fone", [1, W]); fb = P.sb("ffb", [1, 1]); cp = P.sb("fcp", [1, 3, W], BF16)
        carry = P.sb("fcarry", [1, 1]); onesb = P.sb("fonesb", [1, W], BF16)
        negc = P.sb("fnegc", [128, NT]); one1 = P.sb("fone1", [1, 1])
        P.dma(fb[:], I["fbias"], writes=["ffb"])
        P.op("gpsimd", lambda e: e.memset(one[:], 1.0), writes=["fone"])
        P.op("gpsimd", lambda e: e.memset(one1[:], -1.0), writes=["fone1"])
        P.op("gpsimd", lambda e: e.memset(carry[:], 0.0), writes=["fcarry"])
        P.op("vector", lambda e: e.tensor_copy(out=onesb[:], in_=one[:]), reads=["fone"], writes=["fonesb"])
        pc = self.pb[6]
        for ch in range(S // W):
            zz = z[0:1, :]; tt = t1[0:1, :]; cc = crow[0:1, :]; mm = tmp[0:1, :]
            P.dma(zz, self.UT[o:o + 1, ch * W:(ch + 1) * W], reads=["UT"], writes=["stg0"])
            P.op("vector", lambda e, zz=zz: e.tensor_scalar(out=zz, in0=zz, scalar1=fb[:, 0:1], scalar2=None, op0=ALU.add), reads=["stg0", "ffb"], writes=["stg0"])
            P.op("scalar", lambda e, zz=zz, tt=tt: e.activation(out=tt, in_=zz, func=AF.Abs), reads=["stg0"], writes=["stg1"])
            P.op("scalar", lambda e, tt=tt: e.activation(out=tt, in_=tt, func=AF.Exp, scale=-1.0), reads=["stg1"], writes=["stg1"])
            P.op("scalar", lambda e, tt=tt: e.activation(out=tt, in_=tt, func=AF.Ln, bias=1.0), reads=["stg1"], writes=["stg1"])
            P.op("vector", lambda e, zz=zz: e.tensor_scalar(out=zz, in0=zz, scalar1=0.0, scalar2=None, op0=ALU.min), reads=["stg0"], writes=["stg0"])
            P.op("vector", lambda e, zz=zz, tt=tt: e.tensor_tensor(out=zz, in0=zz, in1=tt, op=ALU.subtract), reads=["stg0", "stg1"], writes=["stg0"])
            P.op("vector", lambda e, zz=zz, cc=cc: e.tensor_tensor_scan(out=cc, data0=one[:], data1=zz, initial=carry[:, 0:1], op0=ALU.mult, op1=ALU.add),
                 reads=["fone", "stg0", "fcarry"], writes=["stg2"])
            P.op("vector", lambda e, cc=cc: e.tensor_copy(out=carry[:], in_=cc[:, W - 1:W]), reads=["stg2"], writes=["fcarry"])
            P.op("vector", lambda e, cc=cc: e.tensor_copy(out=cp[:, 0, :], in_=cc), reads=["stg2"], writes=["fcp"])
            P.op("vector", lambda e, cc=cc, mm=mm: e.tensor_tensor(out=mm, in0=cc, in1=cp[:, 0, :], op=ALU.subtract), reads=["stg2", "fcp"], writes=["stg3"])
            P.op("vector", lambda e, mm=mm: e.tensor_copy(out=cp[:, 1, :], in_=mm), reads=["stg3"], writes=["fcp"])
            P.op("vector", lambda e, mm=mm: e.tensor_tensor(out=mm, in0=mm, in1=cp[:, 1, :], op=ALU.subtract), reads=["stg3", "fcp"], writes=["stg3"])
            P.op("vector", lambda e, mm=mm: e.tensor_copy(out=cp[:, 2, :], in_=mm), reads=["stg3"], writes=["fcp"])
            for r in range(3):
                P.dma(qT[64 + r:65 + r, ch * W:(ch + 1) * W], cp[:, r, :], reads=["fcp"], writes=["qT"])
                P.dma(kT[64 + r:65 + r, ch * W:(ch + 1) * W], onesb[:], reads=["fonesb"], writes=["kT"])
            for t in range(W // 128):
                tg = ch * (W // 128) + t
                P.op("tensor", lambda e, t=t, tg=tg, cc=cc: e.matmul(pc[:, tg:tg + 1], lhsT=cc[0:1, t * 128:(t + 1) * 128], rhs=one1[0:1, 0:1], start=True, stop=True),
                     reads=["stg2", "fone1"], writes=["pb6"])
        P.op("vector", lambda e: e.tensor_copy(out=negc[:], in_=pc[:, 0:NT]), reads=["pb6"], writes=["fox_bias"])

        def kq(c, j, i):
            return kT[0:67, j * 128:(j + 1) * 128], qT[0:67, i * 512:(i + 1) * 512]

        def epi(i, ob):
            osc = self.osc
            o0 = ob[0][:, 0:260].rearrange("p (s d) -> p s d", d=65)
            og = self.ostage[self.on % 2]; ogk = "ostage%d" % (self.on % 2); self.on += 1
            P.op("vector", lambda e: e.reciprocal(out=osc[:, 0:4], in_=o0[:, :, 64]), reads=["pb4"], writes=["osc"])
            for s in range(4):
                P.op("vector", lambda e, s=s: e.tensor_scalar(out=og[:, s, :], in0=o0[:, s, 0:64], scalar1=osc[:, s:s + 1], scalar2=None, op0=ALU.mult),
                     reads=["pb4", "osc"], writes=[ogk])
            P.dma(self.Ybr(2)[i * 512:(i + 1) * 512, :].rearrange("(s p) d -> p s d", p=128), og[:], reads=[ogk], writes=["Y2"], q="sync")

        self.attn_loop("fox", 1, kq, lambda j: negc[:, j:j + 1], 1.0, Vaug, "Vaug", ["qT", "kT"], epi)


    def chunk_masks(self):
        P = self.P
        self.BTi = P.sb("BTi", [128, 128]); self.BTe = P.sb("BTe", [128, 128]); self.BTeT = P.sb("BTeT", [128, 128])
        self.ones64 = P.sb("ones64", [128, 64])
        self.ms("gpsimd", self.ones64[:], 1.0, ["ones64"])
        for (t, key, op, pat, cm, zb) in ((self.BTi, "BTi", ALU.is_ge, [[1, 128]], -1, (0, 64)),
                                          (self.BTe, "BTe", ALU.is_gt, [[1, 128]], -1, (0, 64)),
                                          (self.BTeT, "BTeT", ALU.is_gt, [[-1, 128]], 1, (64, 0))):
            self.ms("gpsimd", t[:], 1.0, [key])
            P.op("gpsimd", lambda e, t=t, op=op, pat=pat, cm=cm: e.affine_select(out=t[:], in_=t[:], compare_op=op, fill=0.0, base=0,
                                                                              pattern=pat, channel_multiplier=cm), reads=[key], writes=[key])
            self.ms("gpsimd", t[zb[0]:zb[0] + 64, zb[1]:zb[1] + 64], 0.0, [key])

    def nm_alloc(self, pfx):
        P = self.P
        if not hasattr(self, "nmN") or not isinstance(self.nmN, dict):
            self.nmN = {}; self.nmNT = {}; self.nmX = {}; self._nm_result = {}
        self.nmN[pfx] = [P.sb(pfx + "nmN%d" % i, [128, 128]) for i in range(2)]
        self.nmNT[pfx] = [P.sb(pfx + "nmNT%d" % i, [128, 128]) for i in range(2)]
        self.nmX[pfx] = P.sb(pfx + "nmXb", [128, 128])

    def rwkv(self):
        P, I = self.P, self.I
        N = 512
        names = self.names
        rp = P.sb("rp", [128, 32]); w2h = P.sb("w2h", [64, 64]); a2h = P.sb("a2h", [64, 64]); g2h = P.sb("g2h", [128, 64])
        lnw = P.sb("lnw", [128, 64]); lnb = P.sb("lnb", [128, 64])
        P.dma(rp[:, 0:16], I["rp"], writes=["rp"])
        P.dma(w2h[:], I["w2h"], writes=["w2h"]); P.dma(a2h[:], I["a2h"], writes=["a2h"]); P.dma(g2h[:], I["g2h"], writes=["g2h"])
        P.dma(lnw[:], I["rln"][0:1, :].partition_broadcast(128), writes=["lnw"])
        P.dma(lnb[:], I["rln"][1:2, :].partition_broadcast(128), writes=["lnb"])
        self.ts("vector", rp[:, 16:22], rp[:, 0:6], -1.0, ALU.mult, ["rp"], ["rp"], s2=1.0, op1=ALU.add)
        self.ts("vector", rp[:, 22:23], rp[:, 9:10], -1.0, ALU.mult, ["rp"], ["rp"], s2=1.0, op1=ALU.add)
        MU = {"rr": 0, "rk": 1, "rv": 2, "xw": 3, "xa": 4, "xg": 5}
        self.nm_alloc("rw_")
        inb = {nm: [P.sb("rin_%s%d" % (nm, i), [128 if nm == "xg" else 64, N + 1]) for i in range(2)] for nm in MU}
        def T64(nm):
            return P.sb("rw_" + nm, [64, N])
        r = T64("r"); k0 = T64("k0"); v = T64("v"); tw = T64("tw"); xa = T64("xa"); xg = P.sb("rw_xg", [128, N])
        ld = T64("ld"); a = T64("a"); kkr = T64("kkr"); kk = T64("kk"); k = T64("k"); al = T64("al")
        PI = T64("PI"); PE = T64("PE"); PV = T64("PV"); rt = T64("rt"); bt = T64("bt"); at = T64("at"); kt = T64("kt")
        tmp = T64("tmp"); prod = T64("prod")
        tok = P.sb("rw_tok", [128, 5, 64])
        AT = P.sb("rw_AT", [128, 4, 128])
        A0 = P.sb("rw_nmA", [128, 128])
        X0 = P.sb("rw_nmXa", [128, 128])
        RT = P.sb("rw_RT", [64, 128]); MT = P.sb("rw_MT", [64, 64]); H = P.sb("rw_H", [64, 64])
        Tst = [P.sb("rw_T%d" % i, [64, 64]) for i in range(2)]
        oo = P.sb("rw_oo", [128, 64]); o2 = P.sb("rw_o2", [128, 64]); sc = P.sb("rw_sc", [128, 8])
        yst = [P.sb("rw_y%d" % i, [128, 64]) for i in range(2)]
        pb = self.pb
        self.ms("vector", Tst[0][:], 0.0, ["rw_T0"])
        ti = 0; yi = 0
        for tb in range(NTB):
            t0 = tb * N
            cur = {}
            for nm in MU:
                o, n = names[nm]
                buf = inb[nm][tb % 2]; key = "rin_%s%d" % (nm, tb % 2)
                if tb == 0:
                    self.ms("gpsimd", buf[0:n, 0:1], 0.0, [key])
                    P.dma(buf[0:n, 1:N + 1], self.UT[o:o + n, 0:N], reads=["UT"], writes=[key])
                else:
                    P.dma(buf[0:n, :], self.UT[o:o + n, t0 - 1:t0 + N], reads=["UT"], writes=[key])
                cur[nm] = (buf, key, n)
            for nm, dst, dk, eng in (("rr", r, "rw_r", "vector"), ("rk", k0, "rw_k0", "gpsimd"), ("rv", v, "rw_v", "vector"),
                                      ("xw", tw, "rw_tw", "gpsimd"), ("xa", xa, "rw_xa", "vector"), ("xg", xg, "rw_xg", "gpsimd")):
                buf, key, n = cur[nm]; c = MU[nm]
                self.ts(eng, dst[0:n, :], buf[0:n, 1:N + 1], rp[0:n, 16 + c:17 + c], ALU.mult, [key, "rp"], [dk])
                self.stt(eng, dst[0:n, :], buf[0:n, 0:N], rp[0:n, c:c + 1], dst[0:n, :], ALU.mult, ALU.add, [key, "rp", dk], [dk])
            yield
            self.act(tw[:], tw[:], AF.Tanh, ["rw_tw"], ["rw_tw"])
            self.mm(pb[0][0:64, :], w2h[:], tw[:], ["w2h", "rw_tw"], ["pb0"])
            self.act(ld[:], pb[0][0:64, :], AF.Sigmoid, ["pb0", "rp"], ["rw_ld"], bias=rp[0:64, 6:7])
            self.ts("vector", ld[:], ld[:], -math.exp(-0.5), ALU.mult, ["rw_ld"], ["rw_ld"])
            self.mm(pb[1][0:64, :], a2h[:], xa[:], ["a2h", "rw_xa"], ["pb1"])
            self.act(a[:], pb[1][0:64, :], AF.Sigmoid, ["pb1", "rp"], ["rw_a"], bias=rp[0:64, 7:8])
            self.act(xg[:], xg[:], AF.Sigmoid, ["rw_xg"], ["rw_xg"])
            yield
            self.ts("vector", kkr[:], k0[:], rp[0:64, 8:9], ALU.mult, ["rw_k0", "rp"], ["rw_kkr"])
            self.tt("gpsimd", tmp[:], kkr[:], kkr[:], ALU.mult, ["rw_kkr"], ["rw_tmp"])
            self.mm(pb[0][0:64, :], self.ones64[0:64, :], tmp[:], ["ones64", "rw_tmp"], ["pb0"])
            self.act(tmp[:], pb[0][0:64, :], AF.Sqrt, ["pb0"], ["rw_tmp"], bias=1e-6)
            P.op("vector", lambda e: e.reciprocal(out=tmp[:], in_=tmp[:]), reads=["rw_tmp"], writes=["rw_tmp"])
            self.tt("vector", kk[:], kkr[:], tmp[:], ALU.mult, ["rw_kkr", "rw_tmp"], ["rw_kk"])
            self.ts("gpsimd", tmp[:], a[:], rp[0:64, 9:10], ALU.mult, ["rw_a", "rp"], ["rw_tmp"], s2=rp[0:64, 22:23], op1=ALU.add)
            self.tt("gpsimd", k[:], k0[:], tmp[:], ALU.mult, ["rw_k0", "rw_tmp"], ["rw_k"])
            self.tt("vector", al[:], kk[:], a[:], ALU.mult, ["rw_kk", "rw_a"], ["rw_al"])
            self.stt("gpsimd", prod[:], r[:], rp[0:64, 10:11], k[:], ALU.mult, ALU.mult, ["rw_r", "rp", "rw_k"], ["rw_prod"])
            yield
            for st in range(4):
                sl = slice(st * 128, (st + 1) * 128)
                self.tr(pb[4][:, 256:320], ld[:, sl], ["rw_ld"], ["pb4"], n=64)
                self.cp("vector", tok[:, 4, :], pb[4][:, 256:320], ["pb4"], ["rw_tok"])
                self.mm(pb[2][0:64, sl], tok[:, 4, :], self.BTi[:], ["rw_tok", "BTi"], ["pb2"])
                self.mm(pb[3][0:64, sl], tok[:, 4, :], self.BTe[:], ["rw_tok", "BTe"], ["pb3"])
            self.act(PI[:], pb[2][0:64, :], AF.Exp, ["pb2"], ["rw_PI"])
            self.act(PV[:], pb[2][0:64, :], AF.Exp, ["pb2"], ["rw_PV"], scale=-1.0)
            self.act(PE[:], pb[3][0:64, :], AF.Exp, ["pb3"], ["rw_PE"])
            self.tt("vector", rt[:], r[:], PI[:], ALU.mult, ["rw_r", "rw_PI"], ["rw_rt"])
            self.stt("gpsimd", bt[:], kk[:], -1.0, PE[:], ALU.mult, ALU.mult, ["rw_kk", "rw_PE"], ["rw_bt"])
            self.tt("vector", at[:], al[:], PV[:], ALU.mult, ["rw_al", "rw_PV"], ["rw_at"])
            self.tt("gpsimd", kt[:], k[:], PV[:], ALU.mult, ["rw_k", "rw_PV"], ["rw_kt"])
            yield
            for st in range(4):
                sl = slice(st * 128, (st + 1) * 128)
                tk0 = t0 + st * 128
                for q, (src, sk) in enumerate(((v, "rw_v"), (at, "rw_at"), (kt, "rw_kt"), (bt, "rw_bt"))):
                    self.tr(pb[4][:, q * 64:(q + 1) * 64], src[:, sl], [sk], ["pb4"], n=64)
                self.cp("scalar", tok[:, 0:4, :], pb[4][:, 0:256].rearrange("p (q d) -> p q d", q=4), ["pb4"], ["rw_tok"])
                self.mm(pb[5][:, 0:128], at[:, sl], bt[:, sl], ["rw_at", "rw_bt"], ["pb5"])
                self.mm(pb[5][:, 128:256], at[:, sl], rt[:, sl], ["rw_at", "rw_rt"], ["pb5"])
                self.mm(pb[5][:, 256:384], kt[:, sl], bt[:, sl], ["rw_kt", "rw_bt"], ["pb5"])
                self.mm(pb[5][:, 384:512], kt[:, sl], rt[:, sl], ["rw_kt", "rw_rt"], ["pb5"])
                self.mm(pb[7][:, 256:384], bt[:, sl], at[:, sl], ["rw_at", "rw_bt"], ["pb7"])
                p5 = pb[5][:].rearrange("p (q t) -> p q t", q=4)
                self.tt("vector", AT[:, 0, :], p5[:, 0, :], self.BTe[:], ALU.mult, ["pb5", "BTe"], ["rw_AT0"])
                self.tt("vector", AT[:, 1, :], p5[:, 1, :], self.BTi[:], ALU.mult, ["pb5", "BTi"], ["rw_AT1"])
                self.tt("vector", AT[:, 2, :], p5[:, 2, :], self.BTe[:], ALU.mult, ["pb5", "BTe"], ["rw_AT2"])
                self.tt("vector", AT[:, 3, :], p5[:, 3, :], self.BTi[:], ALU.mult, ["pb5", "BTi"], ["rw_AT3"])
                self.tt("vector", A0[:], pb[7][:, 256:384], self.BTeT[:], ALU.mult, ["pb7", "BTeT"], ["rw_nmA"])
                yield
                self.mm(pb[7][:, 0:64], AT[:, 2, :], tok[:, 0, :], ["rw_AT2", "rw_tok"], ["pb7"])
                self.cp("gpsimd", X0[:, 0:64], tok[:, 3, :], ["rw_tok"], ["rw_nmXa"])
                self.cp("scalar", X0[:, 64:128], pb[7][:, 0:64], ["pb7"], ["rw_nmXa"])
                yield
                yield from self.neumann_gen("rw_", X0, A0[:], "rw_nmA", AT[:, 0, :], "rw_AT0", pb[7][:, 384:512], "pb7")
                X, Xk = self._nm_result["rw_"]
                self.mm(pb[7][0:64, 64:192], X[:, 0:64], AT[:, 1, :], [Xk, "rw_AT1"], ["pb7"])
                self.tt("vector", RT[:], pb[7][0:64, 64:192], rt[:, sl], ALU.add, ["pb7", "rw_rt"], ["rw_RT"])
                self.mm(pb[0][:, 0:64], AT[:, 1, :], X[:, 64:128], ["rw_AT1", Xk], ["pb0"], start=True, stop=False)
                self.mm(pb[0][:, 0:64], AT[:, 3, :], tok[:, 0, :], ["rw_AT3", "rw_tok"], ["pb0"], start=False, stop=False)
                for c in range(2):
                    cs = slice(c * 64, (c + 1) * 64)
                    Tc = Tst[ti % 2]; Tk = "rw_T%d" % (ti % 2); Tn = Tst[(ti + 1) % 2]; Tnk = "rw_T%d" % ((ti + 1) % 2); ti += 1
                    self.mm(pb[0][cs, 0:64], RT[:, cs], Tc[:], ["rw_RT", Tk], ["pb0"], start=False, stop=True)
                    self.mm(pb[1][0:64, 0:64], X[cs, 0:64], tok[cs, 1, :], [Xk, "rw_tok"], ["pb1"])
                    self.tt("vector", MT[:], pb[1][0:64, 0:64], self.ident[0:64, 0:64], ALU.add, ["pb1", "ident"], ["rw_MT"])
                    self.mm(pb[3][0:64, 0:64], tok[cs, 1, :], X[cs, 64:128], ["rw_tok", Xk], ["pb3"], start=True, stop=False)
                    self.mm(pb[3][0:64, 0:64], tok[cs, 2, :], tok[cs, 0, :], ["rw_tok"], ["pb3"], start=False, stop=True)
                    pc_col = PI[:, st * 128 + c * 64 + 63:st * 128 + c * 64 + 64]
                    self.ts("gpsimd" if False else "vector", H[:], pb[3][0:64, 0:64], pc_col, ALU.mult, ["pb3", "rw_PI"], ["rw_H"])
                    self.mm(pb[2][0:64, 0:64], MT[:], Tc[:], ["rw_MT", Tk], ["pb2"])
                    self.stt("vector", Tn[:], pb[2][0:64, 0:64], pc_col, H[:], ALU.mult, ALU.add, ["pb2", "rw_PI", "rw_H"], [Tnk])
                yield
                self.cp("scalar", oo[:], pb[0][:, 0:64], ["pb0"], ["rw_oo"])
                P.op("vector", lambda e: e.reduce_sum(out=sc[:, 0:1], in_=oo[:], axis=AX.X), reads=["rw_oo"], writes=["rw_sc"])
                self.ts("vector", sc[:, 0:1], sc[:, 0:1], -1.0 / 64, ALU.mult, ["rw_sc"], ["rw_sc"])
                self.ts("vector", oo[:], oo[:], sc[:, 0:1], ALU.add, ["rw_oo", "rw_sc"], ["rw_oo"])
                self.act(o2[:], oo[:], AF.Square, ["rw_oo"], ["rw_o2", "rw_sc"], accum_out=sc[:, 1:2])
                self.act(sc[:, 1:2], sc[:, 1:2], AF.Sqrt, ["rw_sc"], ["rw_sc"], scale=1.0 / 64, bias=64e-5)
                P.op("vector", lambda e: e.reciprocal(out=sc[:, 1:2], in_=sc[:, 1:2]), reads=["rw_sc"], writes=["rw_sc"])
                self.stt("vector", oo[:], oo[:], sc[:, 1:2], lnw[:], ALU.mult, ALU.mult, ["rw_oo", "rw_sc", "lnw"], ["rw_oo"])
                self.tt("gpsimd", oo[:], oo[:], lnb[:], ALU.add, ["rw_oo", "lnb"], ["rw_oo"])
                self.mm(pb[1][:, 64:65], prod[:, sl], self.ones64[0:64, 0:1], ["rw_prod", "ones64"], ["pb1"])
                self.mm(pb[1][:, 128:192], xg[:, sl], g2h[:], ["rw_xg", "g2h"], ["pb1"])
                self.cp("scalar", sc[:, 2:3], pb[1][:, 64:65], ["pb1"], ["rw_sc"])
                self.stt("vector", oo[:], tok[:, 0, :], sc[:, 2:3], oo[:], ALU.mult, ALU.add, ["rw_tok", "rw_sc", "rw_oo"], ["rw_oo"])
                y = yst[yi % 2]; yk = "rw_y%d" % (yi % 2); yi += 1
                self.tt("vector", y[:], pb[1][:, 128:192], oo[:], ALU.mult, ["rw_oo", "pb1"], [yk])
                P.dma(self.Ybr(0)[tk0:tk0 + 128, :], y[:], reads=[yk], writes=["Y0"], q="sync")
                yield

    def gdn(self):
        P, I = self.P, self.I
        N = 512
        names = self.names; pb = self.pb
        gp = P.sb("gp", [128, 16]); gnw = P.sb("gnw", [128, 64])
        P.dma(gp[:], I["gp"], writes=["gp"])
        P.dma(gnw[:], I["gnorm"].partition_broadcast(128), writes=["gnw"])
        self.act(gp[:, 10:11], gp[:, 8:9], AF.Exp, ["gp"], ["gp"])
        self.ts("vector", gp[:, 10:11], gp[:, 10:11], -1.0, ALU.mult, ["gp"], ["gp"])
        self.nm_alloc("gd_")
        qin = [P.sb("gd_qin%d" % i, [64, N + 3]) for i in range(2)]
        kvin = [P.sb("gd_kvin%d" % i, [128, N + 3]) for i in range(2)]
        bin_ = [P.sb("gd_bin%d" % i, [128, N]) for i in range(2)]
        ain = [P.sb("gd_ain%d" % i, [128, N]) for i in range(2)]
        q = P.sb("gd_q", [64, N]); kv = P.sb("gd_kv", [128, N]); beta = P.sb("gd_beta", [128, N]); gb_ = P.sb("gd_g", [128, N])
        t1 = P.sb("gd_t1", [128, N]); t2 = P.sb("gd_t2", [128, N])
        gtok = P.sb("gd_gtok", [128, 128]); gam = P.sb("gd_gam", [128, 128]); egam = P.sb("gd_egam", [128, 128]); gamt = P.sb("gd_gamt", [128, 1])
        BT = P.sb("gd_BT", [128, 128]); kb = P.sb("gd_kb", [64, 128]); qe = P.sb("gd_qe", [64, 128])
        DT = P.sb("gd_DT", [128, 128]); Dm = P.sb("gd_D", [128, 128])
        NT = P.sb("gd_NT", [128, 128]); Nm = P.sb("gd_N", [128, 128]); QK = P.sb("gd_QK", [128, 128])
        X0 = P.sb("gd_nmXa", [128, 128])
        ek = P.sb("gd_ek", [64, 128]); KhT = P.sb("gd_KhT", [64, 128]); Kh = P.sb("gd_Kh", [128, 64])
        RT = P.sb("gd_RT", [64, 128]); MT = P.sb("gd_MT", [64, 64]); H = P.sb("gd_H", [64, 64])
        Tst = [P.sb("gd_T%d" % i, [64, 64]) for i in range(2)]
        oo = P.sb("gd_oo", [128, 64]); o2 = P.sb("gd_o2", [128, 64]); sc = P.sb("gd_sc", [128, 4])
        gate = [P.sb("gd_gate%d" % i, [128, 64]) for i in range(2)]
        yst = [P.sb("gd_y%d" % i, [128, 64]) for i in range(2)]
        self.ms("vector", Tst[0][:], 0.0, ["gd_T0"])
        ti = 0; yi = 0
        oq, _ = names["gq"]; ok_, _ = names["gk"]; ov_, _ = names["gv"]; ob_, _ = names["gb"]; oa_, _ = names["ga"]
        ogg, _ = self.tnames["ggt"]
        for tb in range(NTB):
            t0 = tb * N
            qi = qin[tb % 2]; qk_ = "gd_qin%d" % (tb % 2); kvi = kvin[tb % 2]; kvk = "gd_kvin%d" % (tb % 2)
            bi = bin_[tb % 2]; bk = "gd_bin%d" % (tb % 2); ai = ain[tb % 2]; ak = "gd_ain%d" % (tb % 2)
            if tb == 0:
                self.ms("gpsimd", qi[:, 0:3], 0.0, [qk_]); self.ms("gpsimd", kvi[:, 0:3], 0.0, [kvk])
                P.dma(qi[:, 3:N + 3], self.UT[oq:oq + 64, 0:N], reads=["UT"], writes=[qk_])
                P.dma(kvi[0:64, 3:N + 3], self.UT[ok_:ok_ + 64, 0:N], reads=["UT"], writes=[kvk])
                P.dma(kvi[64:128, 3:N + 3], self.UT[ov_:ov_ + 64, 0:N], reads=["UT"], writes=[kvk])
            else:
                P.dma(qi[:, :], self.UT[oq:oq + 64, t0 - 3:t0 + N], reads=["UT"], writes=[qk_])
                P.dma(kvi[0:64, :], self.UT[ok_:ok_ + 64, t0 - 3:t0 + N], reads=["UT"], writes=[kvk])
                P.dma(kvi[64:128, :], self.UT[ov_:ov_ + 64, t0 - 3:t0 + N], reads=["UT"], writes=[kvk])
            P.dma(bi[:], self.UT[ob_:ob_ + 128, t0:t0 + N], reads=["UT"], writes=[bk])
            P.dma(ai[:], self.UT[oa_:oa_ + 128, t0:t0 + N], reads=["UT"], writes=[ak])
            for (src, sk, dst, dk, n, wc0) in ((qi, qk_, q, "gd_q", 64, 0), (kvi, kvk, kv, "gd_kv", 128, 4)):
                self.ts("vector", dst[0:n, :], src[0:n, 3:N + 3], gp[0:n, wc0 + 3:wc0 + 4], ALU.mult, [sk, "gp"], [dk])
                for j in range(3):
                    self.stt("vector", dst[0:n, :], src[0:n, j:N + j], gp[0:n, wc0 + j:wc0 + j + 1], dst[0:n, :], ALU.mult, ALU.add, [sk, "gp", dk], [dk])
                self.act(dst[0:n, :], dst[0:n, :], AF.Silu, [dk], [dk])
            yield
            for (dst, dk, mul) in ((q, "gd_q", 64 ** -0.5), (kv, "gd_kv", 1.0)):
                self.tt("gpsimd", t1[0:64, :], dst[0:64, :], dst[0:64, :], ALU.mult, [dk], ["gd_t1"])
                self.mm(pb[1][0:64, :], self.ones64[0:64, :], t1[0:64, :], ["ones64", "gd_t1"], ["pb1"])
                self.act(t1[0:64, :], pb[1][0:64, :], AF.Sqrt, ["pb1"], ["gd_t1"], bias=1e-6)
                P.op("vector", lambda e: e.reciprocal(out=t1[0:64, :], in_=t1[0:64, :]), reads=["gd_t1"], writes=["gd_t1"])
                self.stt("vector", dst[0:64, :], dst[0:64, :], mul, t1[0:64, :], ALU.mult, ALU.mult, [dk, "gd_t1"], [dk])
            yield
            self.act(beta[:], bi[:], AF.Sigmoid, [bk], ["gd_beta"])
            self.ts("vector", t1[:], ai[:], gp[:, 9:10], ALU.add, [ak, "gp"], ["gd_t1"])
            self.act(t2[:], t1[:], AF.Abs, ["gd_t1"], ["gd_t2"])
            self.act(t2[:], t2[:], AF.Exp, ["gd_t2"], ["gd_t2"], scale=-1.0)
            self.act(t2[:], t2[:], AF.Ln, ["gd_t2"], ["gd_t2"], bias=1.0)
            self.ts("vector", t1[:], t1[:], 0.0, ALU.max, ["gd_t1"], ["gd_t1"])
            self.tt("vector", t1[:], t1[:], t2[:], ALU.add, ["gd_t1", "gd_t2"], ["gd_t1"])
            self.ts("vector", gb_[:], t1[:], gp[:, 10:11], ALU.mult, ["gd_t1", "gp"], ["gd_g"])
            for st in range(4):
                sl = slice(st * 128, (st + 1) * 128)
                tk0 = t0 + st * 128
                gt = gate[yi % 2]; gtk = "gd_gate%d" % (yi % 2)
                P.dma(gt[:], self.UV[tk0:tk0 + 128, ogg:ogg + 64], reads=["UV"], writes=[gtk])
                self.act(gt[:], gt[:], AF.Silu, [gtk], [gtk])
                self.tr(pb[4][:, 0:128], gb_[:, sl], ["gd_g"], ["pb4"], n=128)
                self.cp("vector", gtok[:], pb[4][:, 0:128], ["pb4"], ["gd_gtok"])
                self.mm(pb[4][:, 128:256], gtok[:], self.BTi[:], ["gd_gtok", "BTi"], ["pb4"])
                self.mm(pb[4][:, 256:257], self.BTi[:], gtok[:, 0:1], ["gd_gtok", "BTi"], ["pb4"])
                self.cp("vector", gam[:], pb[4][:, 128:256], ["pb4"], ["gd_gam"])
                self.cp("vector", gamt[:], pb[4][:, 256:257], ["pb4"], ["gd_gamt"])
                self.act(egam[:], pb[4][:, 128:256], AF.Exp, ["pb4"], ["gd_egam"])
                yield
                self.tt("vector", kb[:], kv[0:64, sl], beta[0:64, sl], ALU.mult, ["gd_kv", "gd_beta"], ["gd_kb"])
                self.tt("gpsimd", BT[0:64, :], kb[:], egam[0:64, :], ALU.mult, ["gd_kb", "gd_egam"], ["gd_BT"])
                self.tt("gpsimd", BT[64:128, :], kv[64:128, sl], beta[64:128, sl], ALU.mult, ["gd_kv", "gd_beta"], ["gd_BT"])
                self.tt("gpsimd", qe[:], q[:, sl], egam[0:64, :], ALU.mult, ["gd_q", "gd_egam"], ["gd_qe"])
                self.ts("vector", DT[:], gam[:], gamt[:, 0:1], ALU.subtract, ["gd_gam", "gd_gamt"], ["gd_DT"], s2=0.0, op1=ALU.min)
                self.act(DT[:], DT[:], AF.Exp, ["gd_DT"], ["gd_DT"])
                self.ts("vector", Dm[:], gam[:], gamt[:, 0:1], ALU.subtract, ["gd_gam", "gd_gamt"], ["gd_D"], s2=0.0, op1=ALU.max)
                self.act(Dm[:], Dm[:], AF.Exp, ["gd_D"], ["gd_D"], scale=-1.0)
                yield
                self.mm(pb[5][:, 0:128], kv[0:64, sl], kb[:], ["gd_kv", "gd_kb"], ["pb5"])
                self.mm(pb[5][:, 128:256], kv[0:64, sl], q[:, sl], ["gd_kv", "gd_q"], ["pb5"])
                self.mm(pb[5][:, 256:384], kb[:], kv[0:64, sl], ["gd_kv", "gd_kb"], ["pb5"])
                self.stt("vector", NT[:], pb[5][:, 0:128], -1.0, DT[:], ALU.mult, ALU.mult, ["pb5", "gd_DT"], ["gd_NT"])
                self.tt("gpsimd", NT[:], NT[:], self.BTe[:], ALU.mult, ["gd_NT", "BTe"], ["gd_NT"])
                self.tt("vector", QK[:], pb[5][:, 128:256], DT[:], ALU.mult, ["pb5", "gd_DT"], ["gd_QK"])
                self.tt("gpsimd", QK[:], QK[:], self.BTi[:], ALU.mult, ["gd_QK", "BTi"], ["gd_QK"])
                self.stt("vector", Nm[:], pb[5][:, 256:384], -1.0, Dm[:], ALU.mult, ALU.mult, ["pb5", "gd_D"], ["gd_N"])
                self.tt("gpsimd", Nm[:], Nm[:], self.BTeT[:], ALU.mult, ["gd_N", "BTeT"], ["gd_N"])
                yield
                self.tr(pb[7][:, 0:128], BT[:], ["gd_BT"], ["pb7"], n=128)
                self.cp("scalar", X0[:], pb[7][:, 0:128], ["pb7"], ["gd_nmXa"])
                yield
                yield from self.neumann_gen("gd_", X0, Nm[:], "gd_N", NT[:], "gd_NT", pb[6][:, 128:256], "pb6")
                X, Xk = self._nm_result["gd_"]
                self.mm(pb[7][0:64, 128:256], X[:, 0:64], QK[:], [Xk, "gd_QK"], ["pb7"])
                self.stt("vector", RT[:], pb[7][0:64, 128:256], -1.0, qe[:], ALU.mult, ALU.add, ["pb7", "gd_qe"], ["gd_RT"])
                yield
                for c in range(2):
                    cs = slice(c * 64, (c + 1) * 64)
                    self.act(ek[:, cs], gam[0:64, cs], AF.Exp, ["gd_gam"], ["gd_ek"], scale=-1.0, bias=gam[0:64, c * 64 + 63:c * 64 + 64])
                self.tt("vector", KhT[:], kv[0:64, sl], ek[:], ALU.mult, ["gd_kv", "gd_ek"], ["gd_KhT"])
                self.tr(pb[7][:, 256:320], KhT[:], ["gd_KhT"], ["pb7"], n=64)
                self.cp("scalar", Kh[:], pb[7][:, 256:320], ["pb7"], ["gd_Kh"])
                self.mm(pb[6][:, 0:64], QK[:], X[:, 64:128], ["gd_QK", Xk], ["pb6"], start=True, stop=False)
                for c in range(2):
                    cs = slice(c * 64, (c + 1) * 64)
                    Tc = Tst[ti % 2]; Tk = "gd_T%d" % (ti % 2); Tn = Tst[(ti + 1) % 2]; Tnk = "gd_T%d" % ((ti + 1) % 2); ti += 1
                    self.mm(pb[6][cs, 0:64], RT[:, cs], Tc[:], ["gd_RT", Tk], ["pb6"], start=False, stop=True)
                    self.mm(pb[1][0:64, 0:64], X[cs, 0:64], Kh[cs, :], [Xk, "gd_Kh"], ["pb1"])
                    egl = egam[0:64, c * 64 + 63:c * 64 + 64]
                    self.stt("vector", MT[:], self.ident[0:64, 0:64], egl, pb[1][0:64, 0:64], ALU.mult, ALU.subtract, ["ident", "gd_egam", "pb1"], ["gd_MT"])
                    self.mm(pb[3][0:64, 0:64], Kh[cs, :], X[cs, 64:128], ["gd_Kh", Xk], ["pb3"])
                    self.cp("scalar", H[:], pb[3][0:64, 0:64], ["pb3"], ["gd_H"])
                    self.mm(pb[2][0:64, 0:64], MT[:], Tc[:], ["gd_MT", Tk], ["pb2"])
                    self.tt("vector", Tn[:], pb[2][0:64, 0:64], H[:], ALU.add, ["pb2", "gd_H"], [Tnk])
                yield
                self.cp("scalar", oo[:], pb[6][:, 0:64], ["pb6"], ["gd_oo"])
                self.act(o2[:], oo[:], AF.Square, ["gd_oo"], ["gd_o2", "gd_sc"], accum_out=sc[:, 0:1])
                self.act(sc[:, 0:1], sc[:, 0:1], AF.Sqrt, ["gd_sc"], ["gd_sc"], scale=1.0 / 64, bias=1e-6)
                P.op("vector", lambda e: e.reciprocal(out=sc[:, 0:1], in_=sc[:, 0:1]), reads=["gd_sc"], writes=["gd_sc"])
                self.stt("vector", oo[:], oo[:], sc[:, 0:1], gnw[:], ALU.mult, ALU.mult, ["gd_oo", "gd_sc", "gnw"], ["gd_oo"])
                y = yst[yi % 2]; yk = "gd_y%d" % (yi % 2); yi += 1
                self.tt("vector", y[:], oo[:], gt[:], ALU.mult, ["gd_oo", gtk], [yk])
                P.dma(self.Ybr(3)[tk0:tk0 + 128, :], y[:], reads=[yk], writes=["Y3"], q="sync")
                yield

    def neumann_gen(self, pfx, X0, n0, n0k, n0t, n0tk, pX, pXk):
        Ns = self.nmN[pfx]; NTs = self.nmNT[pfx]; Xs = [X0, self.nmX[pfx]]
        Nk = [pfx + "nmN0", pfx + "nmN1"]; NTk = [pfx + "nmNT0", pfx + "nmNT1"]; Xk = [pfx + "nmXa", pfx + "nmXb"]
        pT = self.pb[2]; pN = self.pb[3]
        curN, curNT, curNk, curNTk = n0, n0t, n0k, n0tk
        xi = 0
        nr = 6
        for i in range(nr):
            self.mm(pX, curNT, Xs[xi][:], [curNTk, Xk[xi]], [pXk])
            self.tt("vector", Xs[1 - xi][:], pX, Xs[xi][:], ALU.add, [Xk[xi], pXk], [Xk[1 - xi]])
            xi = 1 - xi
            if i < nr - 1:
                self.mm(pT[:, 0:128], curN, curNT, [curNk, curNTk], ["pb2"])
                self.mm(pN[:, 0:128], curNT, curN, [curNk, curNTk], ["pb3"])
                self.cp("scalar", NTs[i % 2][:], pT[:, 0:128], ["pb2"], [NTk[i % 2]])
                self.cp("vector", Ns[i % 2][:], pN[:, 0:128], ["pb3"], [Nk[i % 2]])
                curN, curNT, curNk, curNTk = Ns[i % 2][:], NTs[i % 2][:], Nk[i % 2], NTk[i % 2]
            yield
        self._nm_result[pfx] = (Xs[xi], Xk[xi])


def make_inputs(inp, layer, core):
    b, h = core // 4, core % 4
    names, cidx, tnames, tidx = colsel(h)
    w = inp["w_in"][layer]
    d = {
        "wc": np.ascontiguousarray(w[:, cidx]),
        "wv": np.ascontiguousarray(w[:, tidx]),
        "gm": np.ascontiguousarray(inp["norm_mix"][layer].reshape(8, 128).T),
        "rope": rope_tables(),
        "dlam": np.ascontiguousarray(inp["diff_lam"][layer].reshape(1, 128)),
        "dsub": np.ascontiguousarray(inp["diff_subln"][layer].reshape(1, 64)),
        "fbias": np.ascontiguousarray(inp["fox_fbias"][layer][h].reshape(1, 1)),
    }
    hs = slice(h * 64, (h + 1) * 64)
    rp = np.zeros((128, 16), np.float32)
    mu = inp["rwkv_mu"][layer]
    rp[0:64, 0] = mu[0 + h * 64:0 + h * 64 + 64]; rp[0:64, 1] = mu[256 + h * 64:256 + h * 64 + 64]; rp[0:64, 2] = mu[512 + h * 64:512 + h * 64 + 64]
    rp[0:64, 3] = mu[768:832]; rp[0:64, 4] = mu[832:896]; rp[0:128, 5] = mu[896:1024]
    rp[0:64, 6] = inp["rwkv_w0"][layer][hs]; rp[0:64, 7] = inp["rwkv_a0"][layer][hs]
    rp[0:64, 8] = inp["rwkv_kk"][layer][hs]; rp[0:64, 9] = inp["rwkv_ka"][layer][hs]; rp[0:64, 10] = inp["rwkv_rk"][layer][h]
    d["rp"] = rp
    d["w2h"] = np.ascontiguousarray(inp["rwkv_w2"][layer][:, hs]); d["a2h"] = np.ascontiguousarray(inp["rwkv_a2"][layer][:, hs])
    d["g2h"] = np.ascontiguousarray(inp["rwkv_g2"][layer][:, hs])
    d["rln"] = np.ascontiguousarray(np.stack([inp["rwkv_ln_w"][layer][hs], inp["rwkv_ln_b"][layer][hs]]))
    gp = np.zeros((128, 16), np.float32)
    cw = inp["gdn_conv"][layer]
    gp[0:64, 0:4] = cw[h * 64:(h + 1) * 64]; gp[0:64, 4:8] = cw[256 + h * 64:256 + (h + 1) * 64]; gp[64:128, 4:8] = cw[512 + h * 64:512 + (h + 1) * 64]
    gp[:, 8] = inp["gdn_a_log"][layer][h]; gp[:, 9] = inp["gdn_dt_bias"][layer][h]
    d["gp"] = gp; d["gnorm"] = np.ascontiguousarray(inp["gdn_norm"][layer].reshape(1, 64))
    return d


D = 1024


class KB:
    def __init__(self, layer_idx, ntok=2048, tb=1024, moe=False, final=False, F=None, NE=8):
        self.layer_idx = layer_idx; self.ntok = ntok; self.tb = tb; self.moe = moe; self.final = final
        self.F = F if F is not None else (3584 if moe else 2816)
        self.NE = NE if moe else 1
        self.sbw = min(512, tb)

    def tt(self, eng, out, in0, in1, op, r, w):
        self.P.op(eng, lambda e: e.tensor_tensor(out=out, in0=in0, in1=in1, op=op), reads=r, writes=w)

    def ts(self, eng, out, in0, s1, op0, r, w, s2=None, op1=None):
        if op1 is None:
            self.P.op(eng, lambda e: e.tensor_scalar(out=out, in0=in0, scalar1=s1, scalar2=None, op0=op0), reads=r, writes=w)
        else:
            self.P.op(eng, lambda e: e.tensor_scalar(out=out, in0=in0, scalar1=s1, scalar2=s2, op0=op0, op1=op1), reads=r, writes=w)

    def stt(self, out, in0, sc, in1, op0, op1, r, w):
        self.P.op("vector", lambda e: e.scalar_tensor_tensor(out=out, in0=in0, scalar=sc, in1=in1, op0=op0, op1=op1), reads=r, writes=w)

    def act(self, out, in_, func, r, w, **kw):
        self.P.op("scalar", lambda e: e.activation(out=out, in_=in_, func=func, **kw), reads=r, writes=w)

    def cp(self, eng, out, in_, r, w):
        if eng == "scalar":
            self.P.op(eng, lambda e: e.copy(out=out, in_=in_), reads=r, writes=w)
        else:
            self.P.op(eng, lambda e: e.tensor_copy(out=out, in_=in_), reads=r, writes=w)

    def mm(self, out, lhsT, rhs, r, w, start=True, stop=True):
        self.P.op("tensor", lambda e: e.matmul(out, lhsT=lhsT, rhs=rhs, start=start, stop=stop), reads=r, writes=w)

    def tr(self, out, in_, r, w):
        self.P.op("tensor", lambda e: e.transpose(out=out, in_=in_, identity=self.ident[:]), reads=list(r) + ["ident"], writes=w)

    def declare(self, nc, sfx="", fused=False):
        NT_, F, NE = self.ntok, self.F, self.NE
        I = {}
        def inp(name, shape):
            I[name] = nc.dram_tensor(name + sfx, list(shape), F32, kind="ExternalInput").ap()
        if not fused:
            inp("x", [NT_, D]); inp("ysT", [8, 128, NT_])
        inp("pT", [2, 128, NT_])
        inp("wgate", [D, 4 * D]); inp("wbo", [4, 256, D]); inp("wout", [D, D])
        inp("norms", [128, 24])
        if self.final:
            inp("fnorm", [1, D])
        inp("fwg", [NE, D, F]); inp("fwu", [NE, D, F]); inp("fwd", [NE, F, D])
        if self.moe:
            inp("router", [D, 8])
        inp("plegate", [D, D]); inp("pleproj", [256, D])
        self.I = I
        self.fused = fused

    def emit(self, nc, P, pb, ident, xsrc_fn, out_ap, ysrc_fn=None, after_block=None):
        self.nc = nc; self.P = P; self.pb = pb; self.ident = ident
        self.xsrc_fn = xsrc_fn; self.ysrc_fn = ysrc_fn
        self.O = out_ap
        I = self.I
        self.norms = P.sb("norms", [128, 24])
        P.dma(self.norms[:], I["norms"], writes=["norms"])
        if self.final:
            self.fn = P.sb("fnorm", [128, D])
            P.dma(self.fn[:], I["fnorm"].partition_broadcast(128), writes=["fnorm"])
        TBt = self.tb // 128
        self.x = P.sb("x", [128, TBt, D])
        self.hT = P.sb("hT", [128, 8, self.tb], BF16)
        self.h32 = P.sb("h32", [128, 8, 128])
        self.sq = P.sb("sqj", [128, D]); self.ssv = P.sb("ssv", [128, 2])
        self.xn = [P.sb("xn%d" % i, [128, D]) for i in range(2)]
        self.wst = [P.sb("wst%d" % i, [128, 4096]) for i in range(3)]
        self.wbf = [P.sb("wbf%d" % i, [128, 4096], BF16) for i in range(4)]
        self.wi = 0; self.bi = 0; self.ci = 0
        for blk in range(self.ntok // self.tb):
            self.block(blk)
            if after_block is not None:
                after_block(blk)

    def build(self):
        nc = bass.Bass("TRN2", target_bir_lowering=False)
        self.declare(nc)
        I = self.I
        O = nc.dram_tensor("out", [self.ntok, D], F32, kind="ExternalOutput").ap()
        P = Prog(nc, n_chan=16)
        pb = [P.ps("pb%d" % i, [128, 512]) for i in range(8)]
        ident = P.sb("ident", [128, 128])
        P.op("gpsimd", lambda e: e.memset(ident[:], 1.0), writes=["ident"])
        P.op("gpsimd", lambda e: e.affine_select(out=ident[:], in_=ident[:], compare_op=ALU.is_equal,
                                                   fill=0.0, base=0, pattern=[[-1, 128]], channel_multiplier=1),
             reads=["ident"], writes=["ident"])
        P.phase_begin()
        self.emit(nc, P, pb, ident, lambda e, r0, n: I["x"][r0:r0 + n, :], O)
        P.phase_end()
        P.wait_all_dma("sync")
        P.emit()
        return nc

    def load_w(self, dst_view_fn, src_ap_list, nfree):
        P = self.P
        st = self.wst[self.wi % 3]; sk = "wst%d" % (self.wi % 3); self.wi += 1
        bf = self.wbf[self.bi % 4]; bk = "wbf%d" % (self.bi % 4); self.bi += 1
        for (src, view) in src_ap_list:
            P.dma(view(st), src, writes=[sk])
        eng = ("vector", "scalar")[self.ci % 2]; self.ci += 1
        self.cp(eng, bf[:, 0:nfree], st[:, 0:nfree], [sk], [bk])
        return bf, bk

    def norm_to_hT(self, ncol0, want32=None):
        P = self.P
        TBt = self.tb // 128
        for t in range(TBt):
            xn = self.xn[t % 2]; xk = "xn%d" % (t % 2)
            self.act(self.sq[:], self.x[:, t, :], AF.Square, ["x"], ["sqj", "ssv"], accum_out=self.ssv[:, 0:1])
            self.act(self.ssv[:, 0:1], self.ssv[:, 0:1], AF.Sqrt, ["ssv"], ["ssv"], scale=1.0 / D, bias=1e-6)
            P.op("vector", lambda e: e.reciprocal(out=self.ssv[:, 1:2], in_=self.ssv[:, 0:1]), reads=["ssv"], writes=["ssv"])
            self.ts("vector", xn[:], self.x[:, t, :], self.ssv[:, 1:2], ALU.mult, ["x", "ssv"], [xk])
            for half in range(2):
                pt = self.pb[half]; pk = "pb%d" % half
                for c4 in range(4):
                    c = half * 4 + c4
                    self.tr(pt[:, c4 * 128:(c4 + 1) * 128], xn[:, c * 128:(c + 1) * 128], [xk], [pk])
                gsl = self.norms[:, ncol0 + half * 4:ncol0 + half * 4 + 4]
                self.tt("vector", self.hT[:, half * 4:(half + 1) * 4, t * 128:(t + 1) * 128],
                        pt[:].rearrange("p (c t) -> p c t", c=4), gsl.unsqueeze(2).to_broadcast([128, 4, 128]), ALU.mult,
                        [pk, "norms"], ["hT"])
                if want32 is not None and want32 == t:
                    self.tt("vector", self.h32[:, half * 4:(half + 1) * 4, :],
                            pt[:].rearrange("p (c t) -> p c t", c=4), gsl.unsqueeze(2).to_broadcast([128, 4, 128]), ALU.mult,
                            [pk, "norms"], ["h32"])
            if want32 is not None and want32 == "all":
                pass

    def block(self, blk):
        P, I = self.P, self.I
        tb = self.tb; TBt = tb // 128; t0 = blk * tb; sbw = self.sbw; NSB = tb // sbw
        pb = self.pb
        x = self.x
        if blk > 0:
            P.barrier()
        for t in range(TBt):
            P.dma(x[:, t, :], (lambda e, r0=t0 + t * 128: self.xsrc_fn(e, r0, 128)), writes=["x"])
        self.norm_to_hT(0)
        if not hasattr(self, "yT"):
            self.yT = P.sb("yTact", [128, 8, tb], BF16); self.mT = P.sb("mT", [128, 8, tb], BF16)
            self.ystg = P.sb("ystg", [128, tb]); self.sg = [P.sb("sg%d" % i, [128, 512]) for i in range(2)]
            self.macc = P.sb("macc", [128, 512]); self.mtmp = P.sb("mtmp", [128, 512])
        yT, mT = self.yT, self.mT
        if self.ysrc_fn is None:
            for q in range(8):
                P.dma(self.ystg[:], I["ysT"][q, :, t0:t0 + tb], writes=["ystg"])
                self.cp("gpsimd", yT[:, q, :], self.ystg[:], ["ystg"], ["yT"])
        else:
            for t in range(TBt):
                yt = self.xn[t % 2]; ytk = "xn%d" % (t % 2)
                ytv = yt[:].rearrange("p (b h c) -> p b h c", b=4, h=4)
                for r in range(4):
                    if getattr(self, "ysplit", False):
                        P.dma(ytv[:, 1:3, r, :], (lambda e, r=r, r0=t0 + t * 128: self.ysrc_fn(e, "A", r, r0, 128).rearrange("p (b c) -> p b c", b=2)), writes=[ytk])
                        P.dma(ytv[:, 0:4:3, r, :], (lambda e, r=r, r0=t0 + t * 128: self.ysrc_fn(e, "S", r, r0, 128).rearrange("p (b c) -> p b c", b=2)), writes=[ytk])
                    else:
                        P.dma(ytv[:, :, r, :],
                              (lambda e, r=r, r0=t0 + t * 128: self.ysrc_fn(e, r, r0, 128).rearrange("p (b c) -> p b c", b=4)), writes=[ytk])
                for half in range(2):
                    pt = self.pb[half]; pk = "pb%d" % half
                    for c4 in range(4):
                        q = half * 4 + c4
                        self.tr(pt[:, c4 * 128:(c4 + 1) * 128], yt[:, q * 128:(q + 1) * 128], [ytk], [pk])
                    self.cp("vector" if half else "scalar", yT[:, half * 4:(half + 1) * 4, t * 128:(t + 1) * 128],
                            pt[:].rearrange("p (c t) -> p c t", c=4), [pk], ["yT"])
        si = 0
        for j in range(8):
            wg, wgk = self.load_w(None, [(I["wgate"][:, b * 1024 + j * 128:b * 1024 + (j + 1) * 128].rearrange("(c p) n -> p c n", p=128),
                                          (lambda st, b=b: st[:, 0:4096].rearrange("p (c b n) -> p c b n", c=8, b=4)[:, :, b, :])) for b in range(4)], 4096)
            wb, wbk = self.load_w(None, [(I["wbo"][:, :, j * 128:(j + 1) * 128].rearrange("b (c2 p) n -> p (b c2) n", p=128),
                                          (lambda st: st[:, 0:1024].rearrange("p (q n) -> p q n", q=8)))], 1024)
            for sb_ in range(NSB):
                ts_ = slice(sb_ * sbw, (sb_ + 1) * sbw)
                for b in range(4):
                    for c in range(8):
                        self.mm(pb[2][:, 0:sbw], wg[:, c * 512 + b * 128:c * 512 + (b + 1) * 128], self.hT[:, c, ts_], [wgk, "hT"], ["pb2"],
                                start=(c == 0), stop=(c == 7))
                    for c2 in range(2):
                        self.mm(pb[3][:, 0:sbw], wb[:, (b * 2 + c2) * 128:(b * 2 + c2 + 1) * 128], yT[:, b * 2 + c2, ts_], [wbk, "yT"], ["pb3"],
                                start=(c2 == 0), stop=(c2 == 1))
                    sg = self.sg[si % 2]; sgk = "sg%d" % (si % 2); si += 1
                    self.act(sg[:, 0:sbw], pb[2][:, 0:sbw], AF.Sigmoid, ["pb2"], [sgk])
                    if b == 0:
                        self.tt("vector", self.macc[:, 0:sbw], pb[3][:, 0:sbw], sg[:, 0:sbw], ALU.mult, ["pb3", sgk], ["macc"])
                    else:
                        self.tt("vector", self.mtmp[:, 0:sbw], pb[3][:, 0:sbw], sg[:, 0:sbw], ALU.mult, ["pb3", sgk], ["mtmp"])
                        if b < 3:
                            self.tt("gpsimd", self.macc[:, 0:sbw], self.macc[:, 0:sbw], self.mtmp[:, 0:sbw], ALU.add, ["macc", "mtmp"], ["macc"])
                        else:
                            self.tt("gpsimd", mT[:, j, ts_], self.macc[:, 0:sbw], self.mtmp[:, 0:sbw], ALU.add, ["macc", "mtmp"], ["mT"])
        for hc in range(2):
            wo, wok = self.load_w(None, [(I["wout"][:, hc * 512:(hc + 1) * 512].rearrange("(c p) n -> p c n", p=128),
                                          (lambda st: st[:, 0:4096].rearrange("p (c n) -> p c n", c=8)))], 4096)
            for t in range(TBt):
                pz = pb[4 + t % 2]; pzk = "pb%d" % (4 + t % 2)
                for c in range(8):
                    self.mm(pz[:, :], mT[:, c, t * 128:(t + 1) * 128], wo[:, c * 512:(c + 1) * 512], ["mT", wok], [pzk], start=(c == 0), stop=(c == 7))
                self.tt("vector", x[:, t, hc * 512:(hc + 1) * 512], pz[:, :], x[:, t, hc * 512:(hc + 1) * 512], ALU.add, [pzk, "x"], ["x"])
        P.barrier()
        F = self.F; NF = F // 128
        if not hasattr(self, "actT"):
            self.actT = self.yT
            self.gs = [P.sb("gs%d" % i, [128, 512]) for i in range(2)]
            if self.moe:
                self.rt = P.sb("router", [128, 8, 8]); self.gw = P.sb("gatew", [128, TBt, 8])
                self.r1 = P.sb("r1", [128, 8]); self.r2 = P.sb("r2", [128, 8]); self.rm = P.sb("rm", [128, 4])
                self.m1 = P.sb("rmask1", [128, 8]); self.m2 = P.sb("rmask2", [128, 8])
                P.dma(self.rt[:], I["router"].rearrange("(c p) e -> p c e", p=128), writes=["router"])
        actT = self.actT
        if self.moe:
            for t in range(TBt):
                self.norm_to_hT_tile32(t)
                for c in range(8):
                    self.mm(pb[6][:, 0:8], self.h32[:, c, :], self.rt[:, c, :], ["h32", "router"], ["pb6"], start=(c == 0), stop=(c == 7))
                r1, r2, rm, m1, m2, gw = self.r1, self.r2, self.rm, self.m1, self.m2, self.gw
                self.cp("vector", r1[:], pb[6][:, 0:8], ["pb6"], ["r1"])
                P.op("vector", lambda e: e.reduce_max(out=rm[:, 0:1], in_=r1[:], axis=AX.X), reads=["r1"], writes=["rm"])
                self.ts("vector", m1[:], r1[:], rm[:, 0:1], ALU.is_equal, ["r1", "rm"], ["rmask1"])
                self.stt(r2[:], m1[:], -1e30, r1[:], ALU.mult, ALU.add, ["rmask1", "r1"], ["r2"])
                P.op("vector", lambda e: e.reduce_max(out=rm[:, 1:2], in_=r2[:], axis=AX.X), reads=["r2"], writes=["rm"])
                self.ts("vector", m2[:], r2[:], rm[:, 1:2], ALU.is_equal, ["r2", "rm"], ["rmask2"])
                self.tt("vector", rm[:, 2:3], rm[:, 1:2], rm[:, 0:1], ALU.subtract, ["rm"], ["rm"])
                self.act(rm[:, 2:3], rm[:, 2:3], AF.Exp, ["rm"], ["rm"])
                self.ts("vector", rm[:, 2:3], rm[:, 2:3], 1.0, ALU.add, ["rm"], ["rm"])
                P.op("vector", lambda e: e.reciprocal(out=rm[:, 2:3], in_=rm[:, 2:3]), reads=["rm"], writes=["rm"])
                self.ts("vector", rm[:, 3:4], rm[:, 2:3], -1.0, ALU.mult, ["rm"], ["rm"], s2=1.0, op1=ALU.add)
                self.ts("vector", m1[:], m1[:], rm[:, 2:3], ALU.mult, ["rmask1", "rm"], ["rmask1"])
                self.stt(gw[:, t, :], m2[:], rm[:, 3:4], m1[:], ALU.mult, ALU.add, ["rmask2", "rm", "rmask1"], ["gatew"])
        self.norm_to_hT(8)
        gi = 0
        for e in range(self.NE):
            for f0 in range(0, NF, 8):
                nf = min(8, NF - f0)
                for g0 in range(0, nf, 4):
                    ng = min(4, nf - g0)
                    fa = f0 + g0
                    wg_, wgk_ = self.load_w(None, [(I["fwg"][e, :, fa * 128:(fa + ng) * 128].rearrange("(c p) n -> p c n", p=128),
                                                    (lambda st, ng=ng: st[:, 0:4096].rearrange("p (c n) -> p c n", c=8)[:, :, 0:ng * 128]))], 4096)
                    wu_, wuk_ = self.load_w(None, [(I["fwu"][e, :, fa * 128:(fa + ng) * 128].rearrange("(c p) n -> p c n", p=128),
                                                    (lambda st, ng=ng: st[:, 0:4096].rearrange("p (c n) -> p c n", c=8)[:, :, 0:ng * 128]))], 4096)
                    for ii in range(ng):
                        i = g0 + ii
                        for sb_ in range(NSB):
                            ts_ = slice(sb_ * sbw, (sb_ + 1) * sbw)
                            for c in range(8):
                                self.mm(pb[2][:, 0:sbw], wg_[:, c * 512 + ii * 128:c * 512 + (ii + 1) * 128], self.hT[:, c, ts_], [wgk_, "hT"], ["pb2"], start=(c == 0), stop=(c == 7))
                            for c in range(8):
                                self.mm(pb[3][:, 0:sbw], wu_[:, c * 512 + ii * 128:c * 512 + (ii + 1) * 128], self.hT[:, c, ts_], [wuk_, "hT"], ["pb3"], start=(c == 0), stop=(c == 7))
                            gs = self.gs[gi % 2]; gsk = "gs%d" % (gi % 2); gi += 1
                            self.act(gs[:, 0:sbw], pb[2][:, 0:sbw], AF.Silu, ["pb2"], [gsk])
                            self.tt("vector", actT[:, i, ts_], pb[3][:, 0:sbw], gs[:, 0:sbw], ALU.mult, ["pb3", gsk], ["actT"])
                for hc in range(2):
                    wd, wdk = self.load_w(None, [(I["fwd"][e, f0 * 128:(f0 + nf) * 128, hc * 512:(hc + 1) * 512].rearrange("(i p) n -> p i n", p=128),
                                                  (lambda st, nf=nf: st[:, 0:nf * 512].rearrange("p (i n) -> p i n", n=512)))], nf * 512)
                    for t in range(TBt):
                        pz = pb[4 + t % 2]; pzk = "pb%d" % (4 + t % 2)
                        for i in range(nf):
                            self.mm(pz[:, :], actT[:, i, t * 128:(t + 1) * 128], wd[:, i * 512:(i + 1) * 512], ["actT", wdk], [pzk],
                                    start=(i == 0), stop=(i == nf - 1))
                        xs = x[:, t, hc * 512:(hc + 1) * 512]
                        if self.moe:
                            self.stt(xs, pz[:, :], self.gw[:, t, e:e + 1], xs, ALU.mult, ALU.add, [pzk, "gatew", "x"], ["x"])
                        else:
                            self.tt("vector", xs, pz[:, :], xs, ALU.add, [pzk, "x"], ["x"])
        self.norm_to_hT(16)
        if not hasattr(self, "pTt"):
            self.pTt = P.sb("pTt", [128, 2, tb], BF16)
        for c2 in range(2):
            P.dma(self.ystg[:], I["pT"][c2, :, t0:t0 + tb], writes=["ystg"])
            self.cp("gpsimd", self.pTt[:, c2, :], self.ystg[:], ["ystg"], ["pTt"])
        for hc in range(2):
            wpg, wpgk = self.load_w(None, [(I["plegate"][:, hc * 512:(hc + 1) * 512].rearrange("(c p) n -> p c n", p=128),
                                            (lambda st: st[:, 0:4096].rearrange("p (c n) -> p c n", c=8)))], 4096)
            wpp, wppk = self.load_w(None, [(I["pleproj"][:, hc * 512:(hc + 1) * 512].rearrange("(c2 p) n -> p c2 n", p=128),
                                            (lambda st: st[:, 0:1024].rearrange("p (c2 n) -> p c2 n", c2=2)))], 1024)
            for t in range(TBt):
                for c in range(8):
                    self.mm(pb[2][:, :], self.hT[:, c, t * 128:(t + 1) * 128], wpg[:, c * 512:(c + 1) * 512], ["hT", wpgk], ["pb2"], start=(c == 0), stop=(c == 7))
                for c2 in range(2):
                    self.mm(pb[3][:, :], self.pTt[:, c2, t * 128:(t + 1) * 128], wpp[:, c2 * 512:(c2 + 1) * 512], ["pTt", wppk], ["pb3"], start=(c2 == 0), stop=(c2 == 1))
                gs = self.gs[gi % 2]; gsk = "gs%d" % (gi % 2); gi += 1
                self.act(gs[:], pb[2][:, :], AF.Sigmoid, ["pb2"], [gsk])
                self.tt("vector", gs[:], pb[3][:, :], gs[:], ALU.mult, ["pb3", gsk], [gsk])
                xs = x[:, t, hc * 512:(hc + 1) * 512]
                self.tt("gpsimd", xs, xs, gs[:], ALU.add, ["x", gsk], ["x"])
        for t in range(TBt):
            if self.final:
                xn = self.xn[t % 2]; xk = "xn%d" % (t % 2)
                self.act(self.sq[:], x[:, t, :], AF.Square, ["x"], ["sqj", "ssv"], accum_out=self.ssv[:, 0:1])
                self.act(self.ssv[:, 0:1], self.ssv[:, 0:1], AF.Sqrt, ["ssv"], ["ssv"], scale=1.0 / D, bias=1e-6)
                P.op("vector", lambda e: e.reciprocal(out=self.ssv[:, 1:2], in_=self.ssv[:, 0:1]), reads=["ssv"], writes=["ssv"])
                self.stt(xn[:], x[:, t, :], self.ssv[:, 1:2], self.fn[:], ALU.mult, ALU.mult, ["x", "ssv", "fnorm"], [xk])
                P.dma(self.O[t0 + t * 128:t0 + (t + 1) * 128, :], xn[:], reads=[xk], writes=["O"])
            else:
                P.dma(self.O[t0 + t * 128:t0 + (t + 1) * 128, :], x[:, t, :], reads=["x"], writes=["O"])

    def norm_to_hT_tile32(self, t):
        P = self.P
        xn = self.xn[t % 2]; xk = "xn%d" % (t % 2)
        self.act(self.sq[:], self.x[:, t, :], AF.Square, ["x"], ["sqj", "ssv"], accum_out=self.ssv[:, 0:1])
        self.act(self.ssv[:, 0:1], self.ssv[:, 0:1], AF.Sqrt, ["ssv"], ["ssv"], scale=1.0 / D, bias=1e-6)
        P.op("vector", lambda e: e.reciprocal(out=self.ssv[:, 1:2], in_=self.ssv[:, 0:1]), reads=["ssv"], writes=["ssv"])
        self.ts("vector", xn[:], self.x[:, t, :], self.ssv[:, 1:2], ALU.mult, ["x", "ssv"], [xk])
        for half in range(2):
            pt = self.pb[half]; pk = "pb%d" % half
            for c4 in range(4):
                c = half * 4 + c4
                self.tr(pt[:, c4 * 128:(c4 + 1) * 128], xn[:, c * 128:(c + 1) * 128], [xk], [pk])
            gsl = self.norms[:, 8 + half * 4:8 + half * 4 + 4]
            self.tt("vector", self.h32[:, half * 4:(half + 1) * 4, :],
                    pt[:].rearrange("p (c t) -> p c t", c=4), gsl.unsqueeze(2).to_broadcast([128, 4, 128]), ALU.mult,
                    [pk, "norms"], ["h32"])


def make_inputs_b(inp, layer, core, ys_full, x_full, ntok=2048, final=None, fused=False, sfx=""):
    sl = slice(core * ntok, (core + 1) * ntok)
    if not fused:
        xs = x_full.reshape(-1, D)[sl]
        ys = ys_full.reshape(-1, 4, 2, 128)[sl]
        ysT = np.ascontiguousarray(ys.transpose(1, 2, 3, 0).reshape(8, 128, ntok))
    p = inp["p"][layer].reshape(-1, 2, 128)[sl]
    pT = np.ascontiguousarray(p.transpose(1, 2, 0))
    moe = (layer % 2 == 1); j = layer // 2
    norms = np.concatenate([inp["norm_mix"][layer].reshape(8, 128).T, inp["norm_ffn"][layer].reshape(8, 128).T,
                            inp["norm_ple"][layer].reshape(8, 128).T], axis=1)
    if final is None:
        final = (layer == 1)
    d = {
        "pT": pT,
        "wgate": np.ascontiguousarray(inp["w_in"][layer][:, 3596:7692]), "wbo": inp["w_bo"][layer], "wout": inp["w_out"][layer],
        "norms": np.ascontiguousarray(norms.astype(np.float32)),
        "plegate": inp["ple_gate"][layer], "pleproj": inp["ple_proj"][layer],
    }
    if not fused:
        d["x"] = np.ascontiguousarray(xs); d["ysT"] = ysT
    if final:
        d["fnorm"] = inp["final_norm"].reshape(1, D)
    if moe:
        d["fwg"] = inp["moe_w_gate"][j]; d["fwu"] = inp["moe_w_up"][j]; d["fwd"] = inp["moe_w_down"][j]; d["router"] = inp["moe_router"][j]
    else:
        d["fwg"] = inp["ffn_w_gate"][j][None]; d["fwu"] = inp["ffn_w_up"][j][None]; d["fwd"] = inp["ffn_w_down"][j][None]
    return {k + sfx: v for k, v in d.items()}


def build_fused(do=("diff", "fox", "rwkv", "gdn"), nlayers=2):
    S_ = S
    nc = bass.Bass("TRN2", target_bir_lowering=False)
    ntok = S_ // 4; tb = min(1024, ntok)
    CRY = S_ // 4
    CRX = max(128, ntok // 8)
    NCHX = ntok // CRX
    G16 = CRY // 16
    kas = [KA(l, do=do) for l in range(nlayers)]
    kbs = [KB(l, ntok=ntok, tb=tb, moe=(l % 2 == 1), final=(l == 1)) for l in range(nlayers)]
    x_in = nc.dram_tensor("x", [S_, D], F32, kind="ExternalInput").ap()
    rope = nc.dram_tensor("rope", [2, 64, S_], F32, kind="ExternalInput").ap()
    for l in range(nlayers):
        kas[l].declare(nc, sfx="_a%d" % l)
        kbs[l].declare(nc, sfx="_b%d" % l, fused=True)
    out = nc.dram_tensor("out", [ntok, D], F32, kind="ExternalOutput").ap()
    ybufA = [nc.dram_tensor("ybufA%d" % l, [S_, 128], F32).ap() for l in range(nlayers)]
    ybufS = [nc.dram_tensor("ybufS%d" % l, [S_, 128], F32).ap() for l in range(nlayers)]
    ygA = [nc.dram_tensor("ygA%d" % l, [4 * S_, 128], F32).ap() for l in range(nlayers)]
    ygS = [nc.dram_tensor("ygS%d" % l, [4 * S_, 128], F32).ap() for l in range(nlayers)]
    xq = nc.dram_tensor("xq", [ntok, D], F32).ap()
    xgc = nc.dram_tensor("xgc", [S_, D], F32).ap()
    xq0 = nc.dram_tensor("xq0", [ntok, D], F32).ap()
    yselA = nc.dram_tensor("yselA", [4 * ntok, 128], F32).ap()
    yselS = nc.dram_tensor("yselS", [4 * ntok, 128], F32).ap()
    P = Prog(nc, n_chan=16)
    sh = KA.make_shared(nc, P)
    groups = [[0, 1, 2, 3], [4, 5, 6, 7]]

    def xg_rows(t0):
        r = t0 // ntok; w = t0 % ntok; k = w // CRX; i = w % CRX
        row = (k * 4 + r) * CRX + i
        return xgc[row:row + 128, :]

    for l in range(nlayers):
        xsrc = x_in if l == 0 else xg_rows
        YA = ybufA[l].rearrange("s (b c) -> s b c", b=2); YS = ybufS[l].rearrange("s (b c) -> s b c", b=2)
        Ymap = {0: YS[:, 0, :], 1: YA[:, 0, :], 2: YA[:, 1, :], 3: YS[:, 1, :]}

        def after_attn(l=l):
            for k in range(4):
                P.collective("AllGather", groups, ybufA[l][k * CRY:(k + 1) * CRY, :], ygA[l][k * 4 * CRY:(k + 1) * 4 * CRY, :], block=False)
        kas[l].emit(nc, P, sh, xsrc, rope, Ymap, after_attn=after_attn)
        for k in range(4):
            P.collective("AllGather", groups, ybufS[l][k * CRY:(k + 1) * CRY, :], ygS[l][k * 4 * CRY:(k + 1) * 4 * CRY, :], block=False)
        P.collective_wait()
        P.phase_begin()
        for (yg_, ysel_, key) in ((ygA[l], yselA, "yselA"), (ygS[l], yselS, "yselS")):
            ygv = yg_.rearrange("(a b) c -> a (b c)", b=32)
            P.dma(ysel_.rearrange("(a b) c -> a (b c)", b=32),
                  (lambda e, ygv=ygv: ygv[bass.ds(P.qid(e) * (4 * CRY // 32), 4 * CRY // 32), :]), writes=[key])
        if l == 0:
            P.dma(xq0, (lambda e: x_in[bass.ds(P.qid(e) * ntok, ntok), :]), writes=["xq0"])
        P.phase_end()
        P.phase_begin()
        if l == 0:
            xfn = lambda e, r0, n: xq0[r0:r0 + n, :]
        else:
            xfn = lambda e, r0, n: xq[r0:r0 + n, :]
        yfn = lambda e, grp, r, r0, n: (yselA if grp == "A" else yselS)[r * ntok + r0:r * ntok + r0 + n, :]
        kbs[l].ysplit = True
        def after_block(blk, l=l):
            if l < nlayers - 1:
                for k in range(blk * tb // CRX, (blk + 1) * tb // CRX):
                    P.collective("AllGather", groups, xq[k * CRX:(k + 1) * CRX, :], xgc[k * 4 * CRX:(k + 1) * 4 * CRX, :],
                                 reads=["O"], block=False)
        kbs[l].emit(nc, P, sh["pb"], sh["ident"], xfn, (xq if l < nlayers - 1 else out), yfn, after_block=after_block)
        P.phase_end()
        if l < nlayers - 1:
            P.collective_wait()
    P.wait_all_dma("sync")
    P.emit()
    return nc


def make_inputs_fused(inp, core, nlayers=2):
    b, h = core // 4, core % 4
    ntok = S // 4
    m = {"x": np.ascontiguousarray(inp["x"][b]), "rope": rope_tables()}
    for l in range(nlayers):
        a = make_inputs(inp, l, core)
        for k_, v in a.items():
            if k_ != "rope":
                m[k_ + "_a%d" % l] = v
        d = make_inputs_b(inp, l, 0, None, None, ntok=ntok, fused=True, sfx="_b%d" % l)
        p = inp["p"][l][b].reshape(S, 2, 128)[h * ntok:(h + 1) * ntok]
        d["pT_b%d" % l] = np.ascontiguousarray(p.transpose(1, 2, 0))
        m.update(d)
    return m


_CACHE = {}


def kernel(**inputs):
    inp = {k: np.asarray(v) for k, v in inputs.items()}
    if "nc" not in _CACHE:
        _CACHE["nc"] = build_fused()
    cores = list(range(8))
    maps = [make_inputs_fused(inp, core) for core in cores]
    res = run_bass_kernel_spmd(_CACHE["nc"], maps, core_ids=cores)
    out = np.concatenate([res.results[c]["out"] for c in cores], axis=0)
    return out.reshape(2, S, 1024).astype(np.float32)
```

```python
import math
import contextlib
import numpy as np
import concourse.bass as bass
import concourse.mybir as mybir
from concourse.bass_utils import run_bass_kernel_spmd


F32 = mybir.dt.float32
BF16 = mybir.dt.bfloat16
AF = mybir.ActivationFunctionType
ALU = mybir.AluOpType
AX = mybir.AxisListType

COMPUTE = ("tensor", "vector", "scalar", "gpsimd")
QUEUES = ("sync", "gpsimd", "scalar")


class Chan:
    def __init__(self, sem):
        self.sem = sem
        self.n = 0
        self.T = 0
        self.issue_waited = 0


class Prog:
    def __init__(self, nc, n_chan=12):
        self.nc = nc
        self.es = contextlib.ExitStack()
        self.ops = {e: [] for e in ("tensor", "vector", "scalar", "gpsimd", "sync")}
        self.cnt = {e: 0 for e in COMPUTE}
        self.sem = {e: self.es.enter_context(nc.semaphore("s_" + e)) for e in COMPUTE}
        self.chans = [Chan(self.es.enter_context(nc.semaphore("c%d" % i))) for i in range(n_chan)]
        self.rr = 0
        self.last_w = {}
        self.readers = {}
        self.known = {e: {} for e in self.ops}
        self.tensors = {}
        self.pes = None
        self.phase_id = 0
        self.flush_id = 0
        self.qcache = {}

    def sb(self, name, shape, dt=F32):
        es = self.pes if self.pes is not None else self.es
        t = es.enter_context(self.nc.sbuf_tensor("sb%d_" % self.phase_id + name, list(shape), dt))
        return t

    def phase_begin(self):
        self.phase_id += 1
        self.pes = contextlib.ExitStack()

    def phase_end(self):
        self.barrier()
        self.flush()
        self.pes.close()
        self.pes = None

    def barrier(self):
        for e in self.ops:
            waits = []
            kn = self.known[e]
            for e2 in COMPUTE:
                v = self.cnt[e2]
                if v and kn.get(("eng", e2), 0) < v:
                    kn[("eng", e2)] = v
                    waits.append((self.sem[e2], v))
            for ci, ch in enumerate(self.chans):
                if ch.n and kn.get(("chan", ci), 0) < ch.n:
                    kn[("chan", ci)] = ch.n
                    waits.append((ch.sem, 16 * ch.n))
                ch.T = ch.n
                ch.issue_waited = ch.n
            if waits:
                self.ops[e].append((None, waits, None))
        self.last_w = {}
        self.readers = {}

    def flush(self):
        nc = self.nc
        self.flush_id += 1
        with nc.Block() as block:
            def mk(engname):
                def body(e):
                    for fn, waits, inc in self.ops[engname]:
                        for s, v in waits:
                            e.wait_ge(s, v)
                        if fn is not None:
                            ins = fn(e)
                            if inc is not None:
                                ins.then_inc(inc[0], inc[1])
                return body
            block.sync(mk("sync"))
            block.tensor(mk("tensor"))
            block.vector(mk("vector"))
            block.scalar(mk("scalar"))
            block.gpsimd(mk("gpsimd"))
        self.ops = {e: [] for e in self.ops}

    def ps(self, name, shape, dt=F32):
        t = self.es.enter_context(self.nc.psum_tensor("ps_" + name, list(shape), dt))
        return t

    def _dep_waits(self, eng, reads, writes):
        deps = []
        for k in reads:
            w = self.last_w.get(k)
            if w is not None:
                deps.append(w)
        relax = getattr(self, "relax_same_engine", False)
        for k in writes:
            w = self.last_w.get(k)
            if w is not None and not (relax and w[0] == "eng" and w[1] == eng):
                deps.append(w)
            for rd in self.readers.get(k, ()):
                if relax and rd[0] == "eng" and rd[1] == eng:
                    continue
                deps.append(rd)
        waits = {}
        for d in deps:
            if d[0] == "eng":
                _, e2, n = d
                if e2 == eng and eng == "tensor":
                    continue
                key = ("eng", e2)
                waits[key] = max(waits.get(key, 0), n)
            else:
                _, ci = d
                ch = self.chans[ci]
                key = ("chan", ci)
                waits[key] = max(waits.get(key, 0), ch.n)
                ch.T = max(ch.T, ch.n)
        out = []
        kn = self.known[eng]
        for key, v in waits.items():
            if kn.get(key, 0) >= v:
                continue
            kn[key] = v
            if key[0] == "eng":
                out.append((self.sem[key[1]], v))
            else:
                out.append((self.chans[key[1]].sem, 16 * v))
        return out

    def _record(self, tag, reads, writes):
        for k in reads:
            self.readers.setdefault(k, []).append(tag)
        for k in writes:
            self.last_w[k] = tag
            self.readers[k] = []

    def op(self, eng, fn, reads=(), writes=()):
        waits = self._dep_waits(eng, reads, writes)
        self.cnt[eng] += 1
        n = self.cnt[eng]
        self.ops[eng].append((fn, waits, (self.sem[eng], 1)))
        self._record(("eng", eng, n), reads, writes)

    def dma(self, out, in_, reads=(), writes=(), q="sync", chan=None, **kw):
        if chan is None:
            chan = self.rr
            self.rr = (self.rr + 1) % len(self.chans)
        ch = self.chans[chan]
        waits = self._dep_waits(q, reads, writes)
        if ch.T > ch.issue_waited:
            kn = self.known[q]
            key = ("chan", chan)
            if kn.get(key, 0) < ch.T:
                kn[key] = ch.T
                waits.append((ch.sem, 16 * ch.T))
            ch.issue_waited = ch.T
        ch.n += 1
        def fn(e, out=out, in_=in_, kw=kw):
            o = out(e) if callable(out) else out
            i = in_(e) if callable(in_) else in_
            try:
                return e.dma_start(out=o, in_=i, **kw)
            except Exception:
                print("DMA FAIL out=", o, " in=", i)
                raise
        self.ops[q].append((fn, waits, (ch.sem, 16)))
        self._record(("chan", chan), reads, writes)

    def qid(self, e):
        key = (self.flush_id, id(e))
        if key not in self.qcache:
            self.qcache[key] = e.snap(e.partition_id() % 4, min_val=0, max_val=3)
        return self.qcache[key]

    def collective(self, kind, groups, src, dst, reads=(), writes=(), block=True):
        if not hasattr(self, "cc_sem"):
            self.cc_sem = self.es.enter_context(self.nc.semaphore("cc_sem")); self.cc_n = 0
        waits = self._dep_waits("gpsimd", reads, writes)
        self.cc_n += 1
        n = self.cc_n
        self.ops["gpsimd"].append((lambda e: e.collective_compute(kind, ALU.bypass, replica_groups=groups, ins=[src], outs=[dst]), waits, (self.cc_sem, 1)))
        if block:
            self.collective_wait(n)
        return n

    def collective_wait(self, n=None):
        n = self.cc_n if n is None else n
        for e in self.ops:
            self.ops[e].append((None, [(self.cc_sem, n)], None))

    def wait_all_dma(self, eng="sync"):
        waits = []
        for ch in self.chans:
            if ch.n:
                waits.append((ch.sem, 16 * ch.n))
        self.ops[eng].append((None, waits, None))

    def emit(self):
        self.flush()
        self.es.close()


S = 8192
D = 1024
NTB = S // 512
NT = S // 128

def colsel(h):
    cm = []
    r0 = 0; dq = 1024; dk = 1280; dv = 1536
    fq = 1792; fk = 2048; fv = 2304; ffl = 2560
    gq = 2564; gk = 2820; gv = 3076; gb = 3332; ga = 3336; gg = 3340
    hs = np.arange(64) + h * 64

    def swap(base):
        idx = base + hs
        sw = idx.copy()
        for c in range(2):
            for d in range(4):
                sw[c * 32 + d] = idx[c * 32 + d + 4]
                sw[c * 32 + d + 4] = idx[c * 32 + d]
        return sw
    cm.append(("qd", dq + hs)); cm.append(("qds", swap(dq)))
    cm.append(("kd", dk + hs)); cm.append(("kds", swap(dk)))
    cm.append(("qf", fq + hs)); cm.append(("kf", fk + hs))
    cm.append(("fl", np.array([ffl + h])))
    cm.append(("rr", 0 + hs)); cm.append(("rk", 256 + hs)); cm.append(("rv", 512 + hs))
    cm.append(("xw", 768 + np.arange(64))); cm.append(("xa", 832 + np.arange(64)))
    cm.append(("xg", 896 + np.arange(128)))
    cm.append(("gq", gq + hs)); cm.append(("gk", gk + hs)); cm.append(("gv", gv + hs))
    cm.append(("gb", np.full(128, gb + h))); cm.append(("ga", np.full(128, ga + h)))
    names = {}
    idx = []
    o = 0
    for n, ix in cm:
        names[n] = (o, len(ix)); o += len(ix); idx.append(ix)
    tm = [("vd", dv + hs), ("vf", fv + hs), ("ggt", gg + hs)]
    tnames = {}; tidx = []; o = 0
    for n, ix in tm:
        tnames[n] = (o, len(ix)); o += len(ix); tidx.append(ix)
    return names, np.concatenate(idx), tnames, np.concatenate(tidx)


def rope_tables():
    pos = np.arange(S, dtype=np.float32)
    inv = (500000.0 ** (-np.arange(4, dtype=np.float32) * 2.0 / 8)).astype(np.float32)
    ang = pos[None, :] * inv[:, None]
    cos = np.cos(ang).astype(np.float32); sin = np.sin(ang).astype(np.float32)
    ct = np.ones((64, S), np.float32); st = np.zeros((64, S), np.float32)
    for c in range(2):
        for d in range(4):
            ct[c * 32 + d] = cos[d]; ct[c * 32 + d + 4] = cos[d]
            st[c * 32 + d] = -sin[d]; st[c * 32 + d + 4] = sin[d]
    return np.stack([ct, st])


class KA:
    def __init__(self, layer_idx, do=("diff", "fox", "rwkv", "gdn")):
        self.layer_idx = layer_idx
        self.do = do
        self.names, _, self.tnames, _ = colsel(0)
        self.NCc = sum(n for _, n in self.names.values())
        self.NCv = sum(n for _, n in self.tnames.values())

    def declare(self, nc, sfx=""):
        NCc, NCv = self.NCc, self.NCv
        I = {}
        def inp(name, shape):
            I[name] = nc.dram_tensor(name + sfx, list(shape), F32, kind="ExternalInput").ap()
        inp("wc", [D, NCc]); inp("wv", [D, NCv]); inp("gm", [128, 8])
        inp("dlam", [1, 128]); inp("dsub", [1, 64]); inp("fbias", [1, 1])
        inp("rp", [128, 16]); inp("w2h", [64, 64]); inp("a2h", [64, 64]); inp("g2h", [128, 64]); inp("rln", [2, 64])
        inp("gp", [128, 16]); inp("gnorm", [1, 64])
        self.I = I
        self.UT = nc.dram_tensor("ut" + sfx, [NCc, S], F32).ap()
        self.UV = nc.dram_tensor("uv" + sfx, [S, NCv], F32).ap()
        self.sfx = sfx

    def Ybr(self, br):
        if isinstance(self.Y, dict):
            return self.Y[br]
        return self.Y[:, br, :]

    def emit(self, nc, P, shared, xsrc, rope, Ydst, after_attn=None):
        self.nc = nc; self.P = P
        self.pb = shared["pb"]; self.ident = shared["ident"]
        self.mask = shared["mask"]; self.BTi = shared["BTi"]; self.BTe = shared["BTe"]; self.BTeT = shared["BTeT"]; self.ones64 = shared["ones64"]
        self.I["x"] = xsrc; self.I["rope"] = rope
        self.Y = Ydst
        P.phase_begin(); self.phase0(); P.phase_end()
        if "diff" in self.do or "fox" in self.do:
            P.phase_begin()
            if "diff" in self.do:
                self.diff()
            if "fox" in self.do:
                self.fox()
            P.phase_end()
            for nm in ("qT", "stg", "on"):
                if hasattr(self, nm):
                    delattr(self, nm)
        if after_attn is not None:
            after_attn()
        gens = []
        if "rwkv" in self.do or "gdn" in self.do:
            P.phase_begin()
            if "rwkv" in self.do:
                gens.append(self.rwkv())
            if "gdn" in self.do:
                gens.append(self.gdn())
            while gens:
                for g in list(gens):
                    try:
                        next(g)
                    except StopIteration:
                        gens.remove(g)
            P.phase_end()

    @staticmethod
    def make_shared(nc, P):
        sh = {}
        sh["pb"] = [P.ps("pb%d" % i, [128, 512]) for i in range(8)]
        ident = P.sb("ident", [128, 128]); sh["ident"] = ident
        P.op("gpsimd", lambda e: e.memset(ident[:], 1.0), writes=["ident"])
        P.op("gpsimd", lambda e: e.affine_select(out=ident[:], in_=ident[:], compare_op=ALU.is_equal,
                                                   fill=0.0, base=0, pattern=[[-1, 128]], channel_multiplier=1),
             reads=["ident"], writes=["ident"])
        tmp = KA(0); tmp.P = P; tmp.nc = nc
        tmp.attn_common_masks()
        sh["mask"] = tmp.mask; sh["BTi"] = tmp.BTi; sh["BTe"] = tmp.BTe; sh["BTeT"] = tmp.BTeT; sh["ones64"] = tmp.ones64
        return sh

    def build(self):
        nc = bass.Bass("TRN2", target_bir_lowering=False)
        self.declare(nc)
        xsrc = nc.dram_tensor("x", [S, D], F32, kind="ExternalInput").ap()
        rope = nc.dram_tensor("rope", [2, 64, S], F32, kind="ExternalInput").ap()
        dbg = getattr(self, "dbg", False)
        Y = nc.dram_tensor("y", [S, 4, 64], F32, kind="ExternalOutput").ap()
        P = Prog(nc, n_chan=16)
        P.relax_same_engine = getattr(self, 'relax', False)
        sh = KA.make_shared(nc, P)
        self.emit(nc, P, sh, xsrc, rope, Y)
        P.wait_all_dma("sync")
        P.emit()
        return nc

    def tt(self, eng, out, in0, in1, op, r, w):
        self.P.op(eng, lambda e: e.tensor_tensor(out=out, in0=in0, in1=in1, op=op), reads=r, writes=w)

    def ts(self, eng, out, in0, s1, op0, r, w, s2=None, op1=None):
        if op1 is None:
            self.P.op(eng, lambda e: e.tensor_scalar(out=out, in0=in0, scalar1=s1, scalar2=None, op0=op0), reads=r, writes=w)
        else:
            self.P.op(eng, lambda e: e.tensor_scalar(out=out, in0=in0, scalar1=s1, scalar2=s2, op0=op0, op1=op1), reads=r, writes=w)

    def stt(self, eng, out, in0, sc, in1, op0, op1, r, w):
        eng = "vector"
        self.P.op(eng, lambda e: e.scalar_tensor_tensor(out=out, in0=in0, scalar=sc, in1=in1, op0=op0, op1=op1), reads=r, writes=w)

    def act(self, out, in_, func, r, w, **kw):
        self.P.op("scalar", lambda e: e.activation(out=out, in_=in_, func=func, **kw), reads=r, writes=w)

    def cp(self, eng, out, in_, r, w):
        if eng == "scalar":
            self.P.op(eng, lambda e: e.copy(out=out, in_=in_), reads=r, writes=w)
        else:
            self.P.op(eng, lambda e: e.tensor_copy(out=out, in_=in_), reads=r, writes=w)

    def mm(self, out, lhsT, rhs, r, w, start=True, stop=True):
        self.P.op("tensor", lambda e: e.matmul(out, lhsT=lhsT, rhs=rhs, start=start, stop=stop), reads=r, writes=w)

    def tr(self, out, in_, r, w, n=128):
        self.P.op("tensor", lambda e: e.transpose(out=out, in_=in_, identity=self.ident[0:n, 0:n]), reads=list(r) + ["ident"], writes=w)

    def ms(self, eng, out, val, w):
        self.P.op(eng, lambda e: e.memset(out, val), writes=w)

    def phase0(self):
        P, nc, I = self.P, self.nc, self.I
        NCc, NCv = self.NCc, self.NCv
        gm = P.sb("gm", [128, 8])
        P.dma(gm[:], I["gm"], writes=["gm"])
        wcb = P.sb("wcb", [128, 8, NCc], BF16)
        wvb = P.sb("wvb", [128, 8, NCv], BF16)
        wst = [P.sb("wst%d" % i, [128, 1408]) for i in range(2)]
        k = 0
        for (src, dst, n, dkey) in ((I["wc"], wcb, NCc, "wcb"), (I["wv"], wvb, NCv, "wvb")):
            for c in range(8):
                st = wst[k % 2]; key = "wst%d" % (k % 2); k += 1
                P.dma(st[:, 0:n], src[c * 128:(c + 1) * 128, :], writes=[key])
                P.op("vector", lambda e, st=st, dst=dst, c=c, n=n: e.tensor_scalar(
                    out=dst[:, c, :], in0=st[:, 0:n], scalar1=gm[:, c:c + 1], scalar2=None, op0=ALU.mult),
                    reads=[key, "gm"], writes=[dkey])
        groups = []
        o = 0
        while o < NCc:
            n = min(128, NCc - o); groups.append((o, n)); o += n
        xt = [P.sb("xt%d" % i, [128, D]) for i in range(2)]
        sq = P.sb("sqj", [128, D])
        ss = [P.sb("ss%d" % i, [128, 1]) for i in range(2)]
        hT = [P.sb("hT%d" % i, [128, 8, 512], BF16) for i in range(2)]
        og = [P.sb("og%d" % i, [128, 512]) for i in range(3)]
        ov = [P.sb("ov%d" % i, [128, NCv]) for i in range(2)]
        pT = [self.pb[0], self.pb[1]]
        gi = 0; xi = 0; vi = 0
        wckey = "wcb"; wvkey = "wvb"
        for tb in range(NTB):
            h = hT[tb % 2]; hk = "hT%d" % (tb % 2)
            for st in range(4):
                t0 = tb * 512 + st * 128
                x = xt[xi % 2]; xk = "xt%d" % (xi % 2); s_ = ss[xi % 2]; sk = "ss%d" % (xi % 2); xi += 1
                P.dma(x[:], (I["x"](t0) if callable(I["x"]) else I["x"][t0:t0 + 128, :]), writes=[xk], q="sync")
                P.op("scalar", lambda e, x=x, s_=s_: e.activation(out=sq[:], in_=x[:], func=AF.Square, accum_out=s_[:]),
                     reads=[xk], writes=["sqj", sk])
                P.op("scalar", lambda e, s_=s_: e.activation(out=s_[:], in_=s_[:], func=AF.Sqrt, scale=1.0 / D, bias=1e-6),
                     reads=[sk], writes=[sk])
                P.op("vector", lambda e, s_=s_: e.reciprocal(out=s_[:], in_=s_[:]), reads=[sk], writes=[sk])
                P.op("vector", lambda e, x=x, s_=s_: e.tensor_scalar(out=x[:], in0=x[:], scalar1=s_[:, 0:1], scalar2=None, op0=ALU.mult),
                     reads=[xk, sk], writes=[xk])
                for half in range(2):
                    pt = pT[half]; pk = "pb%d" % half
                    for c4 in range(4):
                        c = half * 4 + c4
                        P.op("tensor", lambda e, pt=pt, c4=c4, c=c, x=x: e.transpose(
                            out=pt[:, c4 * 128:(c4 + 1) * 128], in_=x[:, c * 128:(c + 1) * 128], identity=self.ident[:]),
                            reads=[xk, "ident"], writes=[pk])
                    eng = "scalar" if half == 0 else "vector"
                    if eng == "scalar":
                        P.op("scalar", lambda e, pt=pt, h=h, half=half, st=st: e.copy(
                            out=h[:, half * 4:(half + 1) * 4, st * 128:(st + 1) * 128],
                            in_=pt[:].rearrange("p (c t) -> p c t", c=4)), reads=[pk], writes=[hk])
                    else:
                        P.op("vector", lambda e, pt=pt, h=h, half=half, st=st: e.tensor_copy(
                            out=h[:, half * 4:(half + 1) * 4, st * 128:(st + 1) * 128],
                            in_=pt[:].rearrange("p (c t) -> p c t", c=4)), reads=[pk], writes=[hk])
            for (o, n) in groups:
                pg = self.pb[2 + gi % 2]; pk = "pb%d" % (2 + gi % 2)
                ob = og[gi % 3]; ok = "og%d" % (gi % 3); gi += 1
                for c in range(8):
                    P.op("tensor", lambda e, pg=pg, c=c, o=o, n=n, h=h: e.matmul(
                        pg[0:n, :], lhsT=wcb[:, c, o:o + n], rhs=h[:, c, :], start=(c == 0), stop=(c == 7)),
                        reads=[wckey, hk], writes=[pk])
                eng = "scalar" if gi % 2 == 0 else "vector"
                if eng == "scalar":
                    P.op("scalar", lambda e, pg=pg, ob=ob, n=n: e.copy(out=ob[0:n, :], in_=pg[0:n, :]), reads=[pk], writes=[ok])
                else:
                    P.op("vector", lambda e, pg=pg, ob=ob, n=n: e.tensor_copy(out=ob[0:n, :], in_=pg[0:n, :]), reads=[pk], writes=[ok])
                P.dma(self.UT[o:o + n, tb * 512:(tb + 1) * 512], ob[0:n, :], reads=[ok], writes=["UT"], q="sync")
            for st in range(4):
                t0 = tb * 512 + st * 128
                pv = self.pb[4 + vi % 2]; pk = "pb%d" % (4 + vi % 2)
                ob = ov[vi % 2]; ok = "ov%d" % (vi % 2); vi += 1
                for c in range(8):
                    P.op("tensor", lambda e, pv=pv, c=c, h=h, st=st: e.matmul(
                        pv[:, 0:NCv], lhsT=h[:, c, st * 128:(st + 1) * 128], rhs=wvb[:, c, :], start=(c == 0), stop=(c == 7)),
                        reads=[wvkey, hk], writes=[pk])
                P.op("vector", lambda e, pv=pv, ob=ob: e.tensor_copy(out=ob[:], in_=pv[:, 0:NCv]), reads=[pk], writes=[ok])
                P.dma(self.UV[t0:t0 + 128, :], ob[:], reads=[ok], writes=["UV"], q="sync")

    def attn_common_masks(self):
        if hasattr(self, "mask"):
            return
        P = self.P
        self.chunk_masks()
        self.mask = P.sb("amask", [128, 4, 512], BF16)
        P.op("gpsimd", lambda e: e.memset(self.mask[:], 1.0), writes=["amask"])
        for r in range(4):
            P.op("gpsimd", lambda e, r=r: e.affine_select(out=self.mask[:, r, :], in_=self.mask[:, r, :], compare_op=ALU.is_ge,
                                                        fill=0.0, base=-r * 128, pattern=[[1, 512]], channel_multiplier=-1),
                 reads=["amask"], writes=["amask"])

    def attn_loop(self, name, ncomp, kq_fn, bias_fn, scale, Vaug, vkey, rd_keys, epilogue):
        P = self.P
        self.attn_common_masks()
        PT = [P.sb("%s_pt%d" % (name, i), [128, 512], BF16) for i in range(3)]
        stb = [self.pb[0], self.pb[1], self.pb[2]]
        ob = [self.pb[4], self.pb[5]]
        n = 0
        for i in range(NTB):
            pairs = [(c, j) for c in range(ncomp) for j in range(4 * i + 4)]
            def issue_S(idx, n):
                c, j = pairs[idx]
                lhsT, rhs = kq_fn(c, j, i)
                sp = stb[n % 3]; sk = "pb%d" % (n % 3)
                P.op("tensor", lambda e, sp=sp, lhsT=lhsT, rhs=rhs: e.matmul(sp[:], lhsT=lhsT, rhs=rhs, start=True, stop=True),
                     reads=rd_keys, writes=[sk])
            issue_S(0, n)
            for idx, (c, j) in enumerate(pairs):
                if idx + 1 < len(pairs):
                    issue_S(idx + 1, n + 1)
                sp = stb[n % 3]; sk = "pb%d" % (n % 3)
                pt = PT[n % 3]; ptk = "%s_pt%d" % (name, n % 3)
                b = bias_fn(j)
                if b is None:
                    P.op("scalar", lambda e, sp=sp, pt=pt: e.activation(out=pt[:], in_=sp[:], func=AF.Exp, scale=scale),
                         reads=[sk], writes=[ptk])
                else:
                    P.op("scalar", lambda e, sp=sp, pt=pt, b=b: e.activation(out=pt[:], in_=sp[:], func=AF.Exp, scale=scale, bias=b),
                         reads=[sk, name + "_bias"], writes=[ptk])
                r = j - 4 * i
                if r >= 0:
                    P.op("vector", lambda e, pt=pt, r=r: e.tensor_tensor(out=pt[:], in0=pt[:], in1=self.mask[:, r, :], op=ALU.mult),
                         reads=[ptk, "amask"], writes=[ptk])
                o = ob[c]; okey = "pb%d" % (4 + c)
                for s in range(4):
                    P.op("tensor", lambda e, o=o, pt=pt, s=s, j=j, last=(j == 4 * i + 3): e.matmul(
                        o[:, s * 65:(s + 1) * 65], lhsT=pt[:, s * 128:(s + 1) * 128], rhs=Vaug[:, j, :],
                        start=(j == 0 and s == 0), stop=(last and s == 3)), reads=[ptk, vkey], writes=[okey])
                n += 1
            epilogue(i, ob)

    def load_qk(self, dst, dkey, src_name, swap_name=None, rope=None, extra_scale=None):
        P = self.P
        o, n = self.names[src_name]
        W = 2048
        for b in range(S // W):
            a = self.stg[0]; P.dma(a[0:64, :], self.UT[o:o + 64, b * W:(b + 1) * W], reads=["UT"], writes=["stg0"])
            if swap_name is not None:
                o2, _ = self.names[swap_name]
                a2 = self.stg[1]; P.dma(a2[0:64, :], self.UT[o2:o2 + 64, b * W:(b + 1) * W], reads=["UT"], writes=["stg1"])
                cs = self.stg[2]; P.dma(cs[0:64, :], self.I["rope"][0, :, b * W:(b + 1) * W], writes=["stg2"])
                sn = self.stg[3]; P.dma(sn[0:64, :], self.I["rope"][1, :, b * W:(b + 1) * W], writes=["stg3"])
                P.op("vector", lambda e, a=a, cs=cs: e.tensor_tensor(out=a[0:64, :], in0=a[0:64, :], in1=cs[0:64, :], op=ALU.mult),
                     reads=["stg0", "stg2"], writes=["stg0"])
                P.op("vector", lambda e, a2=a2, sn=sn: e.tensor_tensor(out=a2[0:64, :], in0=a2[0:64, :], in1=sn[0:64, :], op=ALU.mult),
                     reads=["stg1", "stg3"], writes=["stg1"])
                P.op("vector", lambda e, a=a, a2=a2, b=b: e.tensor_tensor(out=dst[0:64, b * W:(b + 1) * W], in0=a[0:64, :], in1=a2[0:64, :], op=ALU.add),
                     reads=["stg0", "stg1"], writes=[dkey])
            else:
                if extra_scale is None:
                    P.op("vector", lambda e, a=a, b=b: e.tensor_copy(out=dst[0:64, b * W:(b + 1) * W], in_=a[0:64, :]),
                         reads=["stg0"], writes=[dkey])
                else:
                    P.op("vector", lambda e, a=a, b=b: e.tensor_scalar(out=dst[0:64, b * W:(b + 1) * W], in0=a[0:64, :],
                                                                         scalar1=extra_scale, scalar2=None, op0=ALU.mult),
                         reads=["stg0"], writes=[dkey])

    def load_v(self, Vaug, vkey, tname):
        P = self.P
        o, n = self.tnames[tname]
        P.op("gpsimd", lambda e: e.memset(Vaug[:, :, 64:65], 1.0), writes=[vkey])
        for b in range(S // 2048):
            a = self.stg[0]
            P.dma(a[:, 0:16 * 64].rearrange("p (t d) -> p t d", d=64),
                  self.UV[b * 2048:(b + 1) * 2048, o:o + 64].rearrange("(t p) d -> p t d", p=128), reads=["UV"], writes=["stg0"])
            P.op("vector", lambda e, a=a, b=b: e.tensor_copy(out=Vaug[:, b * 16:(b + 1) * 16, 0:64],
                                                             in_=a[:, 0:16 * 64].rearrange("p (t d) -> p t d", d=64)),
                 reads=["stg0"], writes=[vkey])

    def attn_alloc(self):
        if hasattr(self, "qT"):
            return
        P = self.P
        self.stg = [P.sb("stg%d" % i, [128, 2048]) for i in range(4)]
        self.qT = P.sb("qT", [128, S], BF16)
        self.kT = P.sb("kT", [128, S], BF16)
        self.Vaug = P.sb("Vaug", [128, NT, 65], BF16)
        self.ostage = [P.sb("ostage%d" % i, [128, 4, 64]) for i in range(2)]
        self.osc = P.sb("osc", [128, 16])
        self.otmp = [P.sb("otmp%d" % i, [128, 512]) for i in range(2)]
        self.on = 0

    def diff(self):
        P, I = self.P, self.I
        self.attn_alloc()
        qT, kT, Vaug = self.qT, self.kT, self.Vaug
        self.load_qk(qT, "qT", "qd", "qds", rope=True)
        self.load_qk(kT, "kT", "kd", "kds", rope=True)
        self.load_v(Vaug, "Vaug", "vd")
        lam_init = 0.8 - 0.6 * math.exp(-0.3 * self.layer_idx)
        lt = P.sb("lamt", [128, 128]); lp = P.sb("lamp", [128, 64]); ls = P.sb("lams", [128, 2]); nl = P.sb("neglam", [128, 1])
        P.dma(lt[:], I["dlam"].partition_broadcast(128), writes=["lamt"])
        P.op("vector", lambda e: e.tensor_tensor(out=lp[:].rearrange("p (a d) -> p a d", a=2),
                                                 in0=lt[:].rearrange("p (a b d) -> p a b d", a=2, b=2)[:, :, 0, :],
                                                 in1=lt[:].rearrange("p (a b d) -> p a b d", a=2, b=2)[:, :, 1, :], op=ALU.mult),
             reads=["lamt"], writes=["lamp"])
        P.op("vector", lambda e: e.reduce_sum(out=ls[:], in_=lp[:].rearrange("p (a d) -> p a d", a=2), axis=AX.X), reads=["lamp"], writes=["lams"])
        P.op("scalar", lambda e: e.activation(out=ls[:], in_=ls[:], func=AF.Exp), reads=["lams"], writes=["lams"])
        P.op("vector", lambda e: e.tensor_tensor(out=nl[:], in0=ls[:, 1:2], in1=ls[:, 0:1], op=ALU.subtract), reads=["lams"], writes=["neglam"])
        P.op("vector", lambda e: e.tensor_scalar(out=nl[:], in0=nl[:], scalar1=-lam_init, scalar2=None, op0=ALU.add), reads=["neglam"], writes=["neglam"])
        sub = P.sb("dsub", [128, 64])
        P.dma(sub[:], I["dsub"].partition_broadcast(128), writes=["dsub"])
        P.op("vector", lambda e: e.tensor_scalar(out=sub[:], in0=sub[:], scalar1=(1.0 - lam_init), scalar2=None, op0=ALU.mult), reads=["dsub"], writes=["dsub"])
        scale = 32 ** -0.5

        def kq(c, j, i):
            return kT[c * 32:(c + 1) * 32, j * 128:(j + 1) * 128], qT[c * 32:(c + 1) * 32, i * 512:(i + 1) * 512]

        def epi(i, ob):
            osc = self.osc
            o0 = ob[0][:, 0:260].rearrange("p (s d) -> p s d", d=65)
            o1 = ob[1][:, 0:260].rearrange("p (s d) -> p s d", d=65)
            og = self.ostage[self.on % 2]; ogk = "ostage%d" % (self.on % 2); self.on += 1
            P.op("vector", lambda e: e.reciprocal(out=osc[:, 0:4], in_=o0[:, :, 64]), reads=["pb4"], writes=["osc"])
            P.op("vector", lambda e: e.reciprocal(out=osc[:, 4:8], in_=o1[:, :, 64]), reads=["pb5"], writes=["osc"])
            P.op("vector", lambda e: e.tensor_scalar(out=osc[:, 4:8], in0=osc[:, 4:8], scalar1=nl[:, 0:1], scalar2=None, op0=ALU.mult),
                 reads=["osc", "neglam"], writes=["osc"])
            for s in range(4):
                P.op("vector", lambda e, s=s: e.tensor_scalar(out=og[:, s, :], in0=o0[:, s, 0:64], scalar1=osc[:, s:s + 1], scalar2=None, op0=ALU.mult),
                     reads=["pb4", "osc"], writes=[ogk])
                P.op("vector", lambda e, s=s: e.scalar_tensor_tensor(out=og[:, s, :], in0=o1[:, s, 0:64], scalar=osc[:, 4 + s:5 + s], in1=og[:, s, :],
                                                                      op0=ALU.mult, op1=ALU.add), reads=["pb5", "osc", ogk], writes=[ogk])
                P.op("scalar", lambda e, s=s: e.activation(out=self.stg[3][:, 0:64], in_=og[:, s, :], func=AF.Square, accum_out=osc[:, 8 + s:9 + s]),
                     reads=[ogk], writes=["stg3", "osc"])
            P.op("scalar", lambda e: e.activation(out=osc[:, 8:12], in_=osc[:, 8:12], func=AF.Sqrt, scale=1.0 / 64, bias=1e-5), reads=["osc"], writes=["osc"])
            P.op("vector", lambda e: e.reciprocal(out=osc[:, 8:12], in_=osc[:, 8:12]), reads=["osc"], writes=["osc"])
            for s in range(4):
                P.op("vector", lambda e, s=s: e.scalar_tensor_tensor(out=og[:, s, :], in0=og[:, s, :], scalar=osc[:, 8 + s:9 + s], in1=sub[:],
                                                                      op0=ALU.mult, op1=ALU.mult), reads=[ogk, "osc", "dsub"], writes=[ogk])
            P.dma(self.Ybr(1)[i * 512:(i + 1) * 512, :].rearrange("(s p) d -> p s d", p=128), og[:], reads=[ogk], writes=["Y1"], q="sync")

        self.attn_loop("diff", 2, kq, lambda j: None, scale, Vaug, "Vaug", ["qT", "kT"], epi)

    def fox(self):
        P, I = self.P, self.I
        self.attn_alloc()
        qT, kT, Vaug = self.qT, self.kT, self.Vaug
        scale = 64 ** -0.5
        self.load_qk(qT, "qT", "qf", extra_scale=scale)
        self.load_qk(kT, "kT", "kf")
        self.load_v(Vaug, "Vaug", "vf")
        o, _ = self.names["fl"]
        W = 2048
        z = self.stg[0]; t1 = self.stg[1]; crow = self.stg[2]; tmp = self.stg[3]
        one = P.sb("fone", [1, W]); fb = P.sb("ffb", [1, 1]); cp = P.sb("fcp", [1, 3, W], BF16)
        carry = P.sb("fcarry", [1, 1]); onesb = P.sb("fonesb", [1, W], BF16)
        negc = P.sb("fnegc", [128, NT]); one1 = P.sb("fone1", [1, 1])
        P.dma(fb[:], I["fbias"], writes=["ffb"])
        P.op("gpsimd", lambda e: e.memset(one[:], 1.0), writes=["fone"])
        P.op("gpsimd", lambda e: e.memset(one1[:], -1.0), writes=["fone1"])
        P.op("gpsimd", lambda e: e.memset(carry[:], 0.0), writes=["fcarry"])
        P.op("vector", lambda e: e.tensor_copy(out=onesb[:], in_=one[:]), reads=["fone"], writes=["fonesb"])
        pc = self.pb[6]
        for ch in range(S // W):
            zz = z[0:1, :]; tt = t1[0:1, :]; cc = crow[0:1, :]; mm = tmp[0:1, :]
            P.dma(zz, self.UT[o:o + 1, ch * W:(ch + 1) * W], reads=["UT"], writes=["stg0"])
            P.op("vector", lambda e, zz=zz: e.tensor_scalar(out=zz, in0=zz, scalar1=fb[:, 0:1], scalar2=None, op0=ALU.add), reads=["stg0", "ffb"], writes=["stg0"])
            P.op("scalar", lambda e, zz=zz, tt=tt: e.activation(out=tt, in_=zz, func=AF.Abs), reads=["stg0"], writes=["stg1"])
            P.op("scalar", lambda e, tt=tt: e.activation(out=tt, in_=tt, func=AF.Exp, scale=-1.0), reads=["stg1"], writes=["stg1"])
            P.op("scalar", lambda e, tt=tt: e.activation(out=tt, in_=tt, func=AF.Ln, bias=1.0), reads=["stg1"], writes=["stg1"])
            P.op("vector", lambda e, zz=zz: e.tensor_scalar(out=zz, in0=zz, scalar1=0.0, scalar2=None, op0=ALU.min), reads=["stg0"], writes=["stg0"])
            P.op("vector", lambda e, zz=zz, tt=tt: e.tensor_tensor(out=zz, in0=zz, in1=tt, op=ALU.subtract), reads=["stg0", "stg1"], writes=["stg0"])
            P.op("vector", lambda e, zz=zz, cc=cc: e.tensor_tensor_scan(out=cc, data0=one[:], data1=zz, initial=carry[:, 0:1], op0=ALU.mult, op1=ALU.add),
                 reads=["fone", "stg0", "fcarry"], writes=["stg2"])
            P.op("vector", lambda e, cc=cc: e.tensor_copy(out=carry[:], in_=cc[:, W - 1:W]), reads=["stg2"], writes=["fcarry"])
            P.op("vector", lambda e, cc=cc: e.tensor_copy(out=cp[:, 0, :], in_=cc), reads=["stg2"], writes=["fcp"])
            P.op("vector", lambda e, cc=cc, mm=mm: e.tensor_tensor(out=mm, in0=cc, in1=cp[:, 0, :], op=ALU.subtract), reads=["stg2", "fcp"], writes=["stg3"])
            P.op("vector", lambda e, mm=mm: e.tensor_copy(out=cp[:, 1, :], in_=mm), reads=["stg3"], writes=["fcp"])
            P.op("vector", lambda e, mm=mm: e.tensor_tensor(out=mm, in0=mm, in1=cp[:, 1, :], op=ALU.subtract), reads=["stg3", "fcp"], writes=["stg3"])
            P.op("vector", lambda e, mm=mm: e.tensor_copy(out=cp[:, 2, :], in_=mm), reads=["stg3"], writes=["fcp"])
            for r in range(3):
                P.dma(qT[64 + r:65 + r, ch * W:(ch + 1) * W], cp[:, r, :], reads=["fcp"], writes=["qT"])
                P.dma(kT[64 + r:65 + r, ch * W:(ch + 1) * W], onesb[:], reads=["fonesb"], writes=["kT"])
            for t in range(W // 128):
                tg = ch * (W // 128) + t
                P.op("tensor", lambda e, t=t, tg=tg, cc=cc: e.matmul(pc[:, tg:tg + 1], lhsT=cc[0:1, t * 128:(t + 1) * 128], rhs=one1[0:1, 0:1], start=True, stop=True),
                     reads=["stg2", "fone1"], writes=["pb6"])
        P.op("vector", lambda e: e.tensor_copy(out=negc[:], in_=pc[:, 0:NT]), reads=["pb6"], writes=["fox_bias"])

        def kq(c, j, i):
            return kT[0:67, j * 128:(j + 1) * 128], qT[0:67, i * 512:(i + 1) * 512]

        def epi(i, ob):
            osc = self.osc
            o0 = ob[0][:, 0:260].rearrange("p (s d) -> p s d", d=65)
            og = self.ostage[self.on % 2]; ogk = "ostage%d" % (self.on % 2); self.on += 1
            P.op("vector", lambda e: e.reciprocal(out=osc[:, 0:4], in_=o0[:, :, 64]), reads=["pb4"], writes=["osc"])
            for s in range(4):
                P.op("vector", lambda e, s=s: e.tensor_scalar(out=og[:, s, :], in0=o0[:, s, 0:64], scalar1=osc[:, s:s + 1], scalar2=None, op0=ALU.mult),
                     reads=["pb4", "osc"], writes=[ogk])
            P.dma(self.Ybr(2)[i * 512:(i + 1) * 512, :].rearrange("(s p) d -> p s d", p=128), og[:], reads=[ogk], writes=["Y2"], q="sync")

        self.attn_loop("fox", 1, kq, lambda j: negc[:, j:j + 1], 1.0, Vaug, "Vaug", ["qT", "kT"], epi)


    def chunk_masks(self):
        P = self.P
        self.BTi = P.sb("BTi", [128, 128]); self.BTe = P.sb("BTe", [128, 128]); self.BTeT = P.sb("BTeT", [128, 128])
        self.ones64 = P.sb("ones64", [128, 64])
        self.ms("gpsimd", self.ones64[:], 1.0, ["ones64"])
        for (t, key, op, pat, cm, zb) in ((self.BTi, "BTi", ALU.is_ge, [[1, 128]], -1, (0, 64)),
                                          (self.BTe, "BTe", ALU.is_gt, [[1, 128]], -1, (0, 64)),
                                          (self.BTeT, "BTeT", ALU.is_gt, [[-1, 128]], 1, (64, 0))):
            self.ms("gpsimd", t[:], 1.0, [key])
            P.op("gpsimd", lambda e, t=t, op=op, pat=pat, cm=cm: e.affine_select(out=t[:], in_=t[:], compare_op=op, fill=0.0, base=0,
                                                                              pattern=pat, channel_multiplier=cm), reads=[key], writes=[key])
            self.ms("gpsimd", t[zb[0]:zb[0] + 64, zb[1]:zb[1] + 64], 0.0, [key])

    def nm_alloc(self, pfx):
        P = self.P
        if not hasattr(self, "nmN") or not isinstance(self.nmN, dict):
            self.nmN = {}; self.nmNT = {}; self.nmX = {}; self._nm_result = {}
        self.nmN[pfx] = [P.sb(pfx + "nmN%d" % i, [128, 128]) for i in range(2)]
        self.nmNT[pfx] = [P.sb(pfx + "nmNT%d" % i, [128, 128]) for i in range(2)]
        self.nmX[pfx] = P.sb(pfx + "nmXb", [128, 128])

    def rwkv(self):
        P, I = self.P, self.I
        N = 512
        names = self.names
        rp = P.sb("rp", [128, 32]); w2h = P.sb("w2h", [64, 64]); a2h = P.sb("a2h", [64, 64]); g2h = P.sb("g2h", [128, 64])
        lnw = P.sb("lnw", [128, 64]); lnb = P.sb("lnb", [128, 64])
        P.dma(rp[:, 0:16], I["rp"], writes=["rp"])
        P.dma(w2h[:], I["w2h"], writes=["w2h"]); P.dma(a2h[:], I["a2h"], writes=["a2h"]); P.dma(g2h[:], I["g2h"], writes=["g2h"])
        P.dma(lnw[:], I["rln"][0:1, :].partition_broadcast(128), writes=["lnw"])
        P.dma(lnb[:], I["rln"][1:2, :].partition_broadcast(128), writes=["lnb"])
        self.ts("vector", rp[:, 16:22], rp[:, 0:6], -1.0, ALU.mult, ["rp"], ["rp"], s2=1.0, op1=ALU.add)
        self.ts("vector", rp[:, 22:23], rp[:, 9:10], -1.0, ALU.mult, ["rp"], ["rp"], s2=1.0, op1=ALU.add)
        MU = {"rr": 0, "rk": 1, "rv": 2, "xw": 3, "xa": 4, "xg": 5}
        self.nm_alloc("rw_")
        inb = {nm: [P.sb("rin_%s%d" % (nm, i), [128 if nm == "xg" else 64, N + 1]) for i in range(2)] for nm in MU}
        def T64(nm):
            return P.sb("rw_" + nm, [64, N])
        r = T64("r"); k0 = T64("k0"); v = T64("v"); tw = T64("tw"); xa = T64("xa"); xg = P.sb("rw_xg", [128, N])
        ld = T64("ld"); a = T64("a"); kkr = T64("kkr"); kk = T64("kk"); k = T64("k"); al = T64("al")
        PI = T64("PI"); PE = T64("PE"); PV = T64("PV"); rt = T64("rt"); bt = T64("bt"); at = T64("at"); kt = T64("kt")
        tmp = T64("tmp"); prod = T64("prod")
        tok = P.sb("rw_tok", [128, 5, 64])
        AT = P.sb("rw_AT", [128, 4, 128])
        A0 = P.sb("rw_nmA", [128, 128])
        X0 = P.sb("rw_nmXa", [128, 128])
        RT = P.sb("rw_RT", [64, 128]); MT = P.sb("rw_MT", [64, 64]); H = P.sb("rw_H", [64, 64])
        Tst = [P.sb("rw_T%d" % i, [64, 64]) for i in range(2)]
        oo = P.sb("rw_oo", [128, 64]); o2 = P.sb("rw_o2", [128, 64]); sc = P.sb("rw_sc", [128, 8])
        yst = [P.sb("rw_y%d" % i, [128, 64]) for i in range(2)]
        pb = self.pb
        self.ms("vector", Tst[0][:], 0.0, ["rw_T0"])
        ti = 0; yi = 0
        for tb in range(NTB):
            t0 = tb * N
            cur = {}
            for nm in MU:
                o, n = names[nm]
                buf = inb[nm][tb % 2]; key = "rin_%s%d" % (nm, tb % 2)
                if tb == 0:
                    self.ms("gpsimd", buf[0:n, 0:1], 0.0, [key])
                    P.dma(buf[0:n, 1:N + 1], self.UT[o:o + n, 0:N], reads=["UT"], writes=[key])
                else:
                    P.dma(buf[0:n, :], self.UT[o:o + n, t0 - 1:t0 + N], reads=["UT"], writes=[key])
                cur[nm] = (buf, key, n)
            for nm, dst, dk, eng in (("rr", r, "rw_r", "vector"), ("rk", k0, "rw_k0", "scalar"), ("rv", v, "rw_v", "vector"),
                                      ("xw", tw, "rw_tw", "scalar"), ("xa", xa, "rw_xa", "vector"), ("xg", xg, "rw_xg", "scalar")):
                buf, key, n = cur[nm]; c = MU[nm]
                if eng == "scalar":
                    P.op("scalar", lambda e, dst=dst, buf=buf, n=n, c=c: e.mul(out=dst[0:n, :], in_=buf[0:n, 1:N + 1], mul=rp[0:n, 16 + c:17 + c]),
                         reads=[key, "rp"], writes=[dk])
                else:
                    self.ts(eng, dst[0:n, :], buf[0:n, 1:N + 1], rp[0:n, 16 + c:17 + c], ALU.mult, [key, "rp"], [dk])
                self.stt(eng, dst[0:n, :], buf[0:n, 0:N], rp[0:n, c:c + 1], dst[0:n, :], ALU.mult, ALU.add, [key, "rp", dk], [dk])
            yield
            self.act(tw[:], tw[:], AF.Tanh, ["rw_tw"], ["rw_tw"])
            self.mm(pb[0][0:64, :], w2h[:], tw[:], ["w2h", "rw_tw"], ["pb0"])
            self.act(ld[:], pb[0][0:64, :], AF.Sigmoid, ["pb0", "rp"], ["rw_ld"], bias=rp[0:64, 6:7])
            self.ts("vector", ld[:], ld[:], -math.exp(-0.5), ALU.mult, ["rw_ld"], ["rw_ld"])
            self.mm(pb[1][0:64, :], a2h[:], xa[:], ["a2h", "rw_xa"], ["pb1"])
            self.act(a[:], pb[1][0:64, :], AF.Sigmoid, ["pb1", "rp"], ["rw_a"], bias=rp[0:64, 7:8])
            self.act(xg[:], xg[:], AF.Sigmoid, ["rw_xg"], ["rw_xg"])
            yield
            self.ts("vector", kkr[:], k0[:], rp[0:64, 8:9], ALU.mult, ["rw_k0", "rp"], ["rw_kkr"])
            self.tt("vector", tmp[:], kkr[:], kkr[:], ALU.mult, ["rw_kkr"], ["rw_tmp"])
            self.mm(pb[0][0:64, :], self.ones64[0:64, :], tmp[:], ["ones64", "rw_tmp"], ["pb0"])
            self.act(tmp[:], pb[0][0:64, :], AF.Sqrt, ["pb0"], ["rw_tmp"], bias=1e-6)
            P.op("vector", lambda e: e.reciprocal(out=tmp[:], in_=tmp[:]), reads=["rw_tmp"], writes=["rw_tmp"])
            self.tt("vector", kk[:], kkr[:], tmp[:], ALU.mult, ["rw_kkr", "rw_tmp"], ["rw_kk"])
            self.ts("vector", tmp[:], a[:], rp[0:64, 9:10], ALU.mult, ["rw_a", "rp"], ["rw_tmp"], s2=rp[0:64, 22:23], op1=ALU.add)
            self.tt("vector", k[:], k0[:], tmp[:], ALU.mult, ["rw_k0", "rw_tmp"], ["rw_k"])
            self.tt("vector", al[:], kk[:], a[:], ALU.mult, ["rw_kk", "rw_a"], ["rw_al"])
            self.stt("gpsimd", prod[:], r[:], rp[0:64, 10:11], k[:], ALU.mult, ALU.mult, ["rw_r", "rp", "rw_k"], ["rw_prod"])
            yield
            for st in range(4):
                sl = slice(st * 128, (st + 1) * 128)
                self.tr(pb[4][:, 256:320], ld[:, sl], ["rw_ld"], ["pb4"], n=64)
                self.cp("vector", tok[:, 4, :], pb[4][:, 256:320], ["pb4"], ["rw_tok"])
                self.mm(pb[2][0:64, sl], tok[:, 4, :], self.BTi[:], ["rw_tok", "BTi"], ["pb2"])
                self.mm(pb[3][0:64, sl], tok[:, 4, :], self.BTe[:], ["rw_tok", "BTe"], ["pb3"])
            self.act(PI[:], pb[2][0:64, :], AF.Exp, ["pb2"], ["rw_PI"])
            self.act(PV[:], pb[2][0:64, :], AF.Exp, ["pb2"], ["rw_PV"], scale=-1.0)
            self.act(PE[:], pb[3][0:64, :], AF.Exp, ["pb3"], ["rw_PE"])
            self.tt("vector", rt[:], r[:], PI[:], ALU.mult, ["rw_r", "rw_PI"], ["rw_rt"])
            self.stt("gpsimd", bt[:], kk[:], -1.0, PE[:], ALU.mult, ALU.mult, ["rw_kk", "rw_PE"], ["rw_bt"])
            self.tt("vector", at[:], al[:], PV[:], ALU.mult, ["rw_al", "rw_PV"], ["rw_at"])
            self.tt("vector", kt[:], k[:], PV[:], ALU.mult, ["rw_k", "rw_PV"], ["rw_kt"])
            yield
            for st in range(4):
                sl = slice(st * 128, (st + 1) * 128)
                tk0 = t0 + st * 128
                for q, (src, sk) in enumerate(((v, "rw_v"), (at, "rw_at"), (kt, "rw_kt"), (bt, "rw_bt"))):
                    self.tr(pb[4][:, q * 64:(q + 1) * 64], src[:, sl], [sk], ["pb4"], n=64)
                self.cp("scalar", tok[:, 0:4, :], pb[4][:, 0:256].rearrange("p (q d) -> p q d", q=4), ["pb4"], ["rw_tok"])
                self.mm(pb[5][:, 0:128], at[:, sl], bt[:, sl], ["rw_at", "rw_bt"], ["pb5"])
                self.mm(pb[5][:, 128:256], at[:, sl], rt[:, sl], ["rw_at", "rw_rt"], ["pb5"])
                self.mm(pb[5][:, 256:384], kt[:, sl], bt[:, sl], ["rw_kt", "rw_bt"], ["pb5"])
                self.mm(pb[5][:, 384:512], kt[:, sl], rt[:, sl], ["rw_kt", "rw_rt"], ["pb5"])
                self.mm(pb[7][:, 256:384], bt[:, sl], at[:, sl], ["rw_at", "rw_bt"], ["pb7"])
                p5 = pb[5][:].rearrange("p (q t) -> p q t", q=4)
                self.tt("vector", AT[:, 0, :], p5[:, 0, :], self.BTe[:], ALU.mult, ["pb5", "BTe"], ["rw_AT0"])
                self.tt("vector", AT[:, 1, :], p5[:, 1, :], self.BTi[:], ALU.mult, ["pb5", "BTi"], ["rw_AT1"])
                self.tt("vector", AT[:, 2, :], p5[:, 2, :], self.BTe[:], ALU.mult, ["pb5", "BTe"], ["rw_AT2"])
                self.tt("vector", AT[:, 3, :], p5[:, 3, :], self.BTi[:], ALU.mult, ["pb5", "BTi"], ["rw_AT3"])
                self.tt("vector", A0[:], pb[7][:, 256:384], self.BTeT[:], ALU.mult, ["pb7", "BTeT"], ["rw_nmA"])
                yield
                self.mm(pb[7][:, 0:64], AT[:, 2, :], tok[:, 0, :], ["rw_AT2", "rw_tok"], ["pb7"])
                self.cp("vector", X0[:, 0:64], tok[:, 3, :], ["rw_tok"], ["rw_nmXa"])
                self.cp("scalar", X0[:, 64:128], pb[7][:, 0:64], ["pb7"], ["rw_nmXa"])
                yield
                yield from self.neumann_gen("rw_", X0, A0[:], "rw_nmA", AT[:, 0, :], "rw_AT0", pb[7][:, 384:512], "pb7")
                X, Xk = self._nm_result["rw_"]
                self.mm(pb[7][0:64, 64:192], X[:, 0:64], AT[:, 1, :], [Xk, "rw_AT1"], ["pb7"])
                self.tt("vector", RT[:], pb[7][0:64, 64:192], rt[:, sl], ALU.add, ["pb7", "rw_rt"], ["rw_RT"])
                self.mm(pb[0][:, 0:64], AT[:, 1, :], X[:, 64:128], ["rw_AT1", Xk], ["pb0"], start=True, stop=False)
                self.mm(pb[0][:, 0:64], AT[:, 3, :], tok[:, 0, :], ["rw_AT3", "rw_tok"], ["pb0"], start=False, stop=False)
                for c in range(2):
                    cs = slice(c * 64, (c + 1) * 64)
                    Tc = Tst[ti % 2]; Tk = "rw_T%d" % (ti % 2); Tn = Tst[(ti + 1) % 2]; Tnk = "rw_T%d" % ((ti + 1) % 2); ti += 1
                    self.mm(pb[0][cs, 0:64], RT[:, cs], Tc[:], ["rw_RT", Tk], ["pb0"], start=False, stop=True)
                    self.mm(pb[1][0:64, 0:64], X[cs, 0:64], tok[cs, 1, :], [Xk, "rw_tok"], ["pb1"])
                    self.tt("vector", MT[:], pb[1][0:64, 0:64], self.ident[0:64, 0:64], ALU.add, ["pb1", "ident"], ["rw_MT"])
                    self.mm(pb[3][0:64, 0:64], tok[cs, 1, :], X[cs, 64:128], ["rw_tok", Xk], ["pb3"], start=True, stop=False)
                    self.mm(pb[3][0:64, 0:64], tok[cs, 2, :], tok[cs, 0, :], ["rw_tok"], ["pb3"], start=False, stop=True)
                    pc_col = PI[:, st * 128 + c * 64 + 63:st * 128 + c * 64 + 64]
                    self.ts("vector", H[:], pb[3][0:64, 0:64], pc_col, ALU.mult, ["pb3", "rw_PI"], ["rw_H"])
                    self.mm(pb[2][0:64, 0:64], MT[:], Tc[:], ["rw_MT", Tk], ["pb2"])
                    self.stt("vector", Tn[:], pb[2][0:64, 0:64], pc_col, H[:], ALU.mult, ALU.add, ["pb2", "rw_PI", "rw_H"], [Tnk])
                yield
                self.cp("scalar", oo[:], pb[0][:, 0:64], ["pb0"], ["rw_oo"])
                P.op("vector", lambda e: e.reduce_sum(out=sc[:, 0:1], in_=oo[:], axis=AX.X), reads=["rw_oo"], writes=["rw_sc"])
                self.ts("vector", sc[:, 0:1], sc[:, 0:1], -1.0 / 64, ALU.mult, ["rw_sc"], ["rw_sc"])
                self.ts("vector", oo[:], oo[:], sc[:, 0:1], ALU.add, ["rw_oo", "rw_sc"], ["rw_oo"])
                self.act(o2[:], oo[:], AF.Square, ["rw_oo"], ["rw_o2", "rw_sc"], accum_out=sc[:, 1:2])
                self.act(sc[:, 1:2], sc[:, 1:2], AF.Sqrt, ["rw_sc"], ["rw_sc"], scale=1.0 / 64, bias=64e-5)
                P.op("vector", lambda e: e.reciprocal(out=sc[:, 1:2], in_=sc[:, 1:2]), reads=["rw_sc"], writes=["rw_sc"])
                self.stt("vector", oo[:], oo[:], sc[:, 1:2], lnw[:], ALU.mult, ALU.mult, ["rw_oo", "rw_sc", "lnw"], ["rw_oo"])
                self.tt("vector", oo[:], oo[:], lnb[:], ALU.add, ["rw_oo", "lnb"], ["rw_oo"])
                self.mm(pb[1][:, 64:65], prod[:, sl], self.ones64[0:64, 0:1], ["rw_prod", "ones64"], ["pb1"])
                self.mm(pb[1][:, 128:192], xg[:, sl], g2h[:], ["rw_xg", "g2h"], ["pb1"])
                self.cp("scalar", sc[:, 2:3], pb[1][:, 64:65], ["pb1"], ["rw_sc"])
                self.stt("vector", oo[:], tok[:, 0, :], sc[:, 2:3], oo[:], ALU.mult, ALU.add, ["rw_tok", "rw_sc", "rw_oo"], ["rw_oo"])
                y = yst[yi % 2]; yk = "rw_y%d" % (yi % 2); yi += 1
                self.tt("vector", y[:], pb[1][:, 128:192], oo[:], ALU.mult, ["rw_oo", "pb1"], [yk])
                P.dma(self.Ybr(0)[tk0:tk0 + 128, :], y[:], reads=[yk], writes=["Y0"], q="sync")
                yield

    def gdn(self):
        P, I = self.P, self.I
        N = 512
        names = self.names; pb = self.pb
        gp = P.sb("gp", [128, 16]); gnw = P.sb("gnw", [128, 64])
        P.dma(gp[:], I["gp"], writes=["gp"])
        P.dma(gnw[:], I["gnorm"].partition_broadcast(128), writes=["gnw"])
        self.act(gp[:, 10:11], gp[:, 8:9], AF.Exp, ["gp"], ["gp"])
        self.ts("vector", gp[:, 10:11], gp[:, 10:11], -1.0, ALU.mult, ["gp"], ["gp"])
        self.nm_alloc("gd_")
        qin = [P.sb("gd_qin%d" % i, [64, N + 3]) for i in range(2)]
        kvin = [P.sb("gd_kvin%d" % i, [128, N + 3]) for i in range(2)]
        bin_ = [P.sb("gd_bin%d" % i, [128, N]) for i in range(2)]
        ain = [P.sb("gd_ain%d" % i, [128, N]) for i in range(2)]
        q = P.sb("gd_q", [64, N]); kv = P.sb("gd_kv", [128, N]); beta = P.sb("gd_beta", [128, N]); gb_ = P.sb("gd_g", [128, N])
        t1 = P.sb("gd_t1", [128, N]); t2 = P.sb("gd_t2", [128, N])
        gtok = P.sb("gd_gtok", [128, 128]); gam = P.sb("gd_gam", [128, 128]); egam = P.sb("gd_egam", [128, 128]); gamt = P.sb("gd_gamt", [128, 1])
        BT = P.sb("gd_BT", [128, 128]); kb = P.sb("gd_kb", [64, 128]); qe = P.sb("gd_qe", [64, 128])
        DT = P.sb("gd_DT", [128, 128]); Dm = P.sb("gd_D", [128, 128])
        NT = P.sb("gd_NT", [128, 128]); Nm = P.sb("gd_N", [128, 128]); QK = P.sb("gd_QK", [128, 128])
        X0 = P.sb("gd_nmXa", [128, 128])
        ek = P.sb("gd_ek", [64, 128]); KhT = P.sb("gd_KhT", [64, 128]); Kh = P.sb("gd_Kh", [128, 64])
        RT = P.sb("gd_RT", [64, 128]); MT = P.sb("gd_MT", [64, 64]); H = P.sb("gd_H", [64, 64])
        Tst = [P.sb("gd_T%d" % i, [64, 64]) for i in range(2)]
        oo = P.sb("gd_oo", [128, 64]); o2 = P.sb("gd_o2", [128, 64]); sc = P.sb("gd_sc", [128, 4])
        gate = [P.sb("gd_gate%d" % i, [128, 64]) for i in range(2)]
        yst = [P.sb("gd_y%d" % i, [128, 64]) for i in range(2)]
        self.ms("vector", Tst[0][:], 0.0, ["gd_T0"])
        ti = 0; yi = 0
        oq, _ = names["gq"]; ok_, _ = names["gk"]; ov_, _ = names["gv"]; ob_, _ = names["gb"]; oa_, _ = names["ga"]
        ogg, _ = self.tnames["ggt"]
        for tb in range(NTB):
            t0 = tb * N
            qi = qin[tb % 2]; qk_ = "gd_qin%d" % (tb % 2); kvi = kvin[tb % 2]; kvk = "gd_kvin%d" % (tb % 2)
            bi = bin_[tb % 2]; bk = "gd_bin%d" % (tb % 2); ai = ain[tb % 2]; ak = "gd_ain%d" % (tb % 2)
            if tb == 0:
                self.ms("gpsimd", qi[:, 0:3], 0.0, [qk_]); self.ms("gpsimd", kvi[:, 0:3], 0.0, [kvk])
                P.dma(qi[:, 3:N + 3], self.UT[oq:oq + 64, 0:N], reads=["UT"], writes=[qk_])
                P.dma(kvi[0:64, 3:N + 3], self.UT[ok_:ok_ + 64, 0:N], reads=["UT"], writes=[kvk])
                P.dma(kvi[64:128, 3:N + 3], self.UT[ov_:ov_ + 64, 0:N], reads=["UT"], writes=[kvk])
            else:
                P.dma(qi[:, :], self.UT[oq:oq + 64, t0 - 3:t0 + N], reads=["UT"], writes=[qk_])
                P.dma(kvi[0:64, :], self.UT[ok_:ok_ + 64, t0 - 3:t0 + N], reads=["UT"], writes=[kvk])
                P.dma(kvi[64:128, :], self.UT[ov_:ov_ + 64, t0 - 3:t0 + N], reads=["UT"], writes=[kvk])
            P.dma(bi[:], self.UT[ob_:ob_ + 128, t0:t0 + N], reads=["UT"], writes=[bk])
            P.dma(ai[:], self.UT[oa_:oa_ + 128, t0:t0 + N], reads=["UT"], writes=[ak])
            for (src, sk, dst, dk, n, wc0) in ((qi, qk_, q, "gd_q", 64, 0), (kvi, kvk, kv, "gd_kv", 128, 4)):
                self.ts("vector", dst[0:n, :], src[0:n, 3:N + 3], gp[0:n, wc0 + 3:wc0 + 4], ALU.mult, [sk, "gp"], [dk])
                for j in range(3):
                    self.stt("vector", dst[0:n, :], src[0:n, j:N + j], gp[0:n, wc0 + j:wc0 + j + 1], dst[0:n, :], ALU.mult, ALU.add, [sk, "gp", dk], [dk])
                self.act(dst[0:n, :], dst[0:n, :], AF.Silu, [dk], [dk])
            yield
            for (dst, dk, mul) in ((q, "gd_q", 64 ** -0.5), (kv, "gd_kv", 1.0)):
                self.tt("vector", t1[0:64, :], dst[0:64, :], dst[0:64, :], ALU.mult, [dk], ["gd_t1"])
                self.mm(pb[1][0:64, :], self.ones64[0:64, :], t1[0:64, :], ["ones64", "gd_t1"], ["pb1"])
                self.act(t1[0:64, :], pb[1][0:64, :], AF.Sqrt, ["pb1"], ["gd_t1"], bias=1e-6)
                P.op("vector", lambda e: e.reciprocal(out=t1[0:64, :], in_=t1[0:64, :]), reads=["gd_t1"], writes=["gd_t1"])
                self.stt("vector", dst[0:64, :], dst[0:64, :], mul, t1[0:64, :], ALU.mult, ALU.mult, [dk, "gd_t1"], [dk])
            yield
            self.act(beta[:], bi[:], AF.Sigmoid, [bk], ["gd_beta"])
            self.ts("vector", t1[:], ai[:], gp[:, 9:10], ALU.add, [ak, "gp"], ["gd_t1"])
            self.act(t2[:], t1[:], AF.Abs, ["gd_t1"], ["gd_t2"])
            self.act(t2[:], t2[:], AF.Exp, ["gd_t2"], ["gd_t2"], scale=-1.0)
            self.act(t2[:], t2[:], AF.Ln, ["gd_t2"], ["gd_t2"], bias=1.0)
            self.ts("vector", t1[:], t1[:], 0.0, ALU.max, ["gd_t1"], ["gd_t1"])
            self.tt("vector", t1[:], t1[:], t2[:], ALU.add, ["gd_t1", "gd_t2"], ["gd_t1"])
            self.ts("vector", gb_[:], t1[:], gp[:, 10:11], ALU.mult, ["gd_t1", "gp"], ["gd_g"])
            for st in range(4):
                sl = slice(st * 128, (st + 1) * 128)
                tk0 = t0 + st * 128
                gt = gate[yi % 2]; gtk = "gd_gate%d" % (yi % 2)
                P.dma(gt[:], self.UV[tk0:tk0 + 128, ogg:ogg + 64], reads=["UV"], writes=[gtk])
                self.act(gt[:], gt[:], AF.Silu, [gtk], [gtk])
                self.tr(pb[4][:, 0:128], gb_[:, sl], ["gd_g"], ["pb4"], n=128)
                self.cp("vector", gtok[:], pb[4][:, 0:128], ["pb4"], ["gd_gtok"])
                self.mm(pb[4][:, 128:256], gtok[:], self.BTi[:], ["gd_gtok", "BTi"], ["pb4"])
                self.mm(pb[4][:, 256:257], self.BTi[:], gtok[:, 0:1], ["gd_gtok", "BTi"], ["pb4"])
                self.cp("vector", gam[:], pb[4][:, 128:256], ["pb4"], ["gd_gam"])
                self.cp("vector", gamt[:], pb[4][:, 256:257], ["pb4"], ["gd_gamt"])
                self.act(egam[:], pb[4][:, 128:256], AF.Exp, ["pb4"], ["gd_egam"])
                yield
                self.tt("vector", kb[:], kv[0:64, sl], beta[0:64, sl], ALU.mult, ["gd_kv", "gd_beta"], ["gd_kb"])
                self.tt("vector", BT[0:64, :], kb[:], egam[0:64, :], ALU.mult, ["gd_kb", "gd_egam"], ["gd_BT"])
                self.tt("vector", BT[64:128, :], kv[64:128, sl], beta[64:128, sl], ALU.mult, ["gd_kv", "gd_beta"], ["gd_BT"])
                self.tt("vector", qe[:], q[:, sl], egam[0:64, :], ALU.mult, ["gd_q", "gd_egam"], ["gd_qe"])
                self.ts("vector", DT[:], gam[:], gamt[:, 0:1], ALU.subtract, ["gd_gam", "gd_gamt"], ["gd_DT"], s2=0.0, op1=ALU.min)
                self.act(DT[:], DT[:], AF.Exp, ["gd_DT"], ["gd_DT"])
                self.ts("vector", Dm[:], gam[:], gamt[:, 0:1], ALU.subtract, ["gd_gam", "gd_gamt"], ["gd_D"], s2=0.0, op1=ALU.max)
                self.act(Dm[:], Dm[:], AF.Exp, ["gd_D"], ["gd_D"], scale=-1.0)
                yield
                self.mm(pb[5][:, 0:128], kv[0:64, sl], kb[:], ["gd_kv", "gd_kb"], ["pb5"])
                self.mm(pb[5][:, 128:256], kv[0:64, sl], q[:, sl], ["gd_kv", "gd_q"], ["pb5"])
                self.mm(pb[5][:, 256:384], kb[:], kv[0:64, sl], ["gd_kv", "gd_kb"], ["pb5"])
                self.stt("vector", NT[:], pb[5][:, 0:128], -1.0, DT[:], ALU.mult, ALU.mult, ["pb5", "gd_DT"], ["gd_NT"])
                self.tt("vector", NT[:], NT[:], self.BTe[:], ALU.mult, ["gd_NT", "BTe"], ["gd_NT"])
                self.tt("vector", QK[:], pb[5][:, 128:256], DT[:], ALU.mult, ["pb5", "gd_DT"], ["gd_QK"])
                self.tt("vector", QK[:], QK[:], self.BTi[:], ALU.mult, ["gd_QK", "BTi"], ["gd_QK"])
                self.stt("vector", Nm[:], pb[5][:, 256:384], -1.0, Dm[:], ALU.mult, ALU.mult, ["pb5", "gd_D"], ["gd_N"])
                self.tt("vector", Nm[:], Nm[:], self.BTeT[:], ALU.mult, ["gd_N", "BTeT"], ["gd_N"])
                yield
                self.tr(pb[7][:, 0:128], BT[:], ["gd_BT"], ["pb7"], n=128)
                self.cp("scalar", X0[:], pb[7][:, 0:128], ["pb7"], ["gd_nmXa"])
                yield
                yield from self.neumann_gen("gd_", X0, Nm[:], "gd_N", NT[:], "gd_NT", pb[6][:, 128:256], "pb6")
                X, Xk = self._nm_result["gd_"]
                self.mm(pb[7][0:64, 128:256], X[:, 0:64], QK[:], [Xk, "gd_QK"], ["pb7"])
                self.stt("vector", RT[:], pb[7][0:64, 128:256], -1.0, qe[:], ALU.mult, ALU.add, ["pb7", "gd_qe"], ["gd_RT"])
                yield
                for c in range(2):
                    cs = slice(c * 64, (c + 1) * 64)
                    self.act(ek[:, cs], gam[0:64, cs], AF.Exp, ["gd_gam"], ["gd_ek"], scale=-1.0, bias=gam[0:64, c * 64 + 63:c * 64 + 64])
                self.tt("vector", KhT[:], kv[0:64, sl], ek[:], ALU.mult, ["gd_kv", "gd_ek"], ["gd_KhT"])
                self.tr(pb[7][:, 256:320], KhT[:], ["gd_KhT"], ["pb7"], n=64)
                self.cp("scalar", Kh[:], pb[7][:, 256:320], ["pb7"], ["gd_Kh"])
                self.mm(pb[6][:, 0:64], QK[:], X[:, 64:128], ["gd_QK", Xk], ["pb6"], start=True, stop=False)
                for c in range(2):
                    cs = slice(c * 64, (c + 1) * 64)
                    Tc = Tst[ti % 2]; Tk = "gd_T%d" % (ti % 2); Tn = Tst[(ti + 1) % 2]; Tnk = "gd_T%d" % ((ti + 1) % 2); ti += 1
                    self.mm(pb[6][cs, 0:64], RT[:, cs], Tc[:], ["gd_RT", Tk], ["pb6"], start=False, stop=True)
                    self.mm(pb[1][0:64, 0:64], X[cs, 0:64], Kh[cs, :], [Xk, "gd_Kh"], ["pb1"])
                    egl = egam[0:64, c * 64 + 63:c * 64 + 64]
                    self.stt("vector", MT[:], self.ident[0:64, 0:64], egl, pb[1][0:64, 0:64], ALU.mult, ALU.subtract, ["ident", "gd_egam", "pb1"], ["gd_MT"])
                    self.mm(pb[3][0:64, 0:64], Kh[cs, :], X[cs, 64:128], ["gd_Kh", Xk], ["pb3"])
                    self.cp("scalar", H[:], pb[3][0:64, 0:64], ["pb3"], ["gd_H"])
                    self.mm(pb[2][0:64, 0:64], MT[:], Tc[:], ["gd_MT", Tk], ["pb2"])
                    self.tt("vector", Tn[:], pb[2][0:64, 0:64], H[:], ALU.add, ["pb2", "gd_H"], [Tnk])
                yield
                self.cp("scalar", oo[:], pb[6][:, 0:64], ["pb6"], ["gd_oo"])
                self.act(o2[:], oo[:], AF.Square, ["gd_oo"], ["gd_o2", "gd_sc"], accum_out=sc[:, 0:1])
                self.act(sc[:, 0:1], sc[:, 0:1], AF.Sqrt, ["gd_sc"], ["gd_sc"], scale=1.0 / 64, bias=1e-6)
                P.op("vector", lambda e: e.reciprocal(out=sc[:, 0:1], in_=sc[:, 0:1]), reads=["gd_sc"], writes=["gd_sc"])
                self.stt("vector", oo[:], oo[:], sc[:, 0:1], gnw[:], ALU.mult, ALU.mult, ["gd_oo", "gd_sc", "gnw"], ["gd_oo"])
                y = yst[yi % 2]; yk = "gd_y%d" % (yi % 2); yi += 1
                self.tt("vector", y[:], oo[:], gt[:], ALU.mult, ["gd_oo", gtk], [yk])
                P.dma(self.Ybr(3)[tk0:tk0 + 128, :], y[:], reads=[yk], writes=["Y3"], q="sync")
                yield

    def neumann_gen(self, pfx, X0, n0, n0k, n0t, n0tk, pX, pXk):
        Ns = self.nmN[pfx]; NTs = self.nmNT[pfx]; Xs = [X0, self.nmX[pfx]]
        Nk = [pfx + "nmN0", pfx + "nmN1"]; NTk = [pfx + "nmNT0", pfx + "nmNT1"]; Xk = [pfx + "nmXa", pfx + "nmXb"]
        pT = self.pb[2]; pN = self.pb[3]
        curN, curNT, curNk, curNTk = n0, n0t, n0k, n0tk
        xi = 0
        nr = 6
        for i in range(nr):
            self.mm(pX, curNT, Xs[xi][:], [curNTk, Xk[xi]], [pXk])
            self.tt("vector", Xs[1 - xi][:], pX, Xs[xi][:], ALU.add, [Xk[xi], pXk], [Xk[1 - xi]])
            xi = 1 - xi
            if i < nr - 1:
                self.mm(pT[:, 0:128], curN, curNT, [curNk, curNTk], ["pb2"])
                self.mm(pN[:, 0:128], curNT, curN, [curNk, curNTk], ["pb3"])
                self.cp("scalar", NTs[i % 2][:], pT[:, 0:128], ["pb2"], [NTk[i % 2]])
                self.cp("vector", Ns[i % 2][:], pN[:, 0:128], ["pb3"], [Nk[i % 2]])
                curN, curNT, curNk, curNTk = Ns[i % 2][:], NTs[i % 2][:], Nk[i % 2], NTk[i % 2]
            yield
        self._nm_result[pfx] = (Xs[xi], Xk[xi])


def make_inputs(inp, layer, core):
    b, h = core // 4, core % 4
    names, cidx, tnames, tidx = colsel(h)
    w = inp["w_in"][layer]
    d = {
        "wc": np.ascontiguousarray(w[:, cidx]),
        "wv": np.ascontiguousarray(w[:, tidx]),
        "gm": np.ascontiguousarray(inp["norm_mix"][layer].reshape(8, 128).T),
        "rope": rope_tables(),
        "dlam": np.ascontiguousarray(inp["diff_lam"][layer].reshape(1, 128)),
        "dsub": np.ascontiguousarray(inp["diff_subln"][layer].reshape(1, 64)),
        "fbias": np.ascontiguousarray(inp["fox_fbias"][layer][h].reshape(1, 1)),
    }
    hs = slice(h * 64, (h + 1) * 64)
    rp = np.zeros((128, 16), np.float32)
    mu = inp["rwkv_mu"][layer]
    rp[0:64, 0] = mu[0 + h * 64:0 + h * 64 + 64]; rp[0:64, 1] = mu[256 + h * 64:256 + h * 64 + 64]; rp[0:64, 2] = mu[512 + h * 64:512 + h * 64 + 64]
    rp[0:64, 3] = mu[768:832]; rp[0:64, 4] = mu[832:896]; rp[0:128, 5] = mu[896:1024]
    rp[0:64, 6] = inp["rwkv_w0"][layer][hs]; rp[0:64, 7] = inp["rwkv_a0"][layer][hs]
    rp[0:64, 8] = inp["rwkv_kk"][layer][hs]; rp[0:64, 9] = inp["rwkv_ka"][layer][hs]; rp[0:64, 10] = inp["rwkv_rk"][layer][h]
    d["rp"] = rp
    d["w2h"] = np.ascontiguousarray(inp["rwkv_w2"][layer][:, hs]); d["a2h"] = np.ascontiguousarray(inp["rwkv_a2"][layer][:, hs])
    d["g2h"] = np.ascontiguousarray(inp["rwkv_g2"][layer][:, hs])
    d["rln"] = np.ascontiguousarray(np.stack([inp["rwkv_ln_w"][layer][hs], inp["rwkv_ln_b"][layer][hs]]))
    gp = np.zeros((128, 16), np.float32)
    cw = inp["gdn_conv"][layer]
    gp[0:64, 0:4] = cw[h * 64:(h + 1) * 64]; gp[0:64, 4:8] = cw[256 + h * 64:256 + (h + 1) * 64]; gp[64:128, 4:8] = cw[512 + h * 64:512 + (h + 1) * 64]
    gp[:, 8] = inp["gdn_a_log"][layer][h]; gp[:, 9] = inp["gdn_dt_bias"][layer][h]
    d["gp"] = gp; d["gnorm"] = np.ascontiguousarray(inp["gdn_norm"][layer].reshape(1, 64))
    return d


D = 1024


class KB:
    def __init__(self, layer_idx, ntok=2048, tb=1024, moe=False, final=False, F=None, NE=8):
        self.layer_idx = layer_idx; self.ntok = ntok; self.tb = tb; self.moe = moe; self.final = final
        self.F = F if F is not None else (3584 if moe else 2816)
        self.NE = NE if moe else 1
        self.sbw = min(512, tb)

    def tt(self, eng, out, in0, in1, op, r, w):
        self.P.op(eng, lambda e: e.tensor_tensor(out=out, in0=in0, in1=in1, op=op), reads=r, writes=w)

    def ts(self, eng, out, in0, s1, op0, r, w, s2=None, op1=None):
        if op1 is None:
            self.P.op(eng, lambda e: e.tensor_scalar(out=out, in0=in0, scalar1=s1, scalar2=None, op0=op0), reads=r, writes=w)
        else:
            self.P.op(eng, lambda e: e.tensor_scalar(out=out, in0=in0, scalar1=s1, scalar2=s2, op0=op0, op1=op1), reads=r, writes=w)

    def stt(self, out, in0, sc, in1, op0, op1, r, w):
        self.P.op("vector", lambda e: e.scalar_tensor_tensor(out=out, in0=in0, scalar=sc, in1=in1, op0=op0, op1=op1), reads=r, writes=w)

    def act(self, out, in_, func, r, w, **kw):
        self.P.op("scalar", lambda e: e.activation(out=out, in_=in_, func=func, **kw), reads=r, writes=w)

    def cp(self, eng, out, in_, r, w):
        if eng == "scalar":
            self.P.op(eng, lambda e: e.copy(out=out, in_=in_), reads=r, writes=w)
        else:
            self.P.op(eng, lambda e: e.tensor_copy(out=out, in_=in_), reads=r, writes=w)

    def mm(self, out, lhsT, rhs, r, w, start=True, stop=True):
        self.P.op("tensor", lambda e: e.matmul(out, lhsT=lhsT, rhs=rhs, start=start, stop=stop), reads=r, writes=w)

    def tr(self, out, in_, r, w):
        self.P.op("tensor", lambda e: e.transpose(out=out, in_=in_, identity=self.ident[:]), reads=list(r) + ["ident"], writes=w)

    def declare(self, nc, sfx="", fused=False):
        NT_, F, NE = self.ntok, self.F, self.NE
        I = {}
        def inp(name, shape):
            I[name] = nc.dram_tensor(name + sfx, list(shape), F32, kind="ExternalInput").ap()
        if not fused:
            inp("x", [NT_, D]); inp("ysT", [8, 128, NT_])
        inp("pT", [2, 128, NT_])
        inp("wgate", [D, 4 * D]); inp("wbo", [4, 256, D]); inp("wout", [D, D])
        inp("norms", [128, 24])
        if self.final:
            inp("fnorm", [1, D])
        inp("fwg", [NE, D, F]); inp("fwu", [NE, D, F]); inp("fwd", [NE, F, D])
        if self.moe:
            inp("router", [D, 8])
        inp("plegate", [D, D]); inp("pleproj", [256, D])
        self.I = I
        self.fused = fused

    def emit(self, nc, P, pb, ident, xsrc_fn, out_ap, ysrc_fn=None, after_block=None):
        self.nc = nc; self.P = P; self.pb = pb; self.ident = ident
        self.xsrc_fn = xsrc_fn; self.ysrc_fn = ysrc_fn
        self.O = out_ap
        I = self.I
        self.norms = P.sb("norms", [128, 24])
        P.dma(self.norms[:], I["norms"], writes=["norms"])
        if self.final:
            self.fn = P.sb("fnorm", [128, D])
            P.dma(self.fn[:], I["fnorm"].partition_broadcast(128), writes=["fnorm"])
        TBt = self.tb // 128
        self.x = P.sb("x", [128, TBt, D])
        self.hT = P.sb("hT", [128, 8, self.tb], BF16)
        self.h32 = P.sb("h32", [128, 8, 128])
        self.sq = P.sb("sqj", [128, D]); self.ssv = P.sb("ssv", [128, 2])
        self.xn = [P.sb("xn%d" % i, [128, D]) for i in range(2)]
        self.wst = [P.sb("wst%d" % i, [128, 4096]) for i in range(3)]
        self.wbf = [P.sb("wbf%d" % i, [128, 4096], BF16) for i in range(4)]
        self.wi = 0; self.bi = 0; self.ci = 0
        for blk in range(self.ntok // self.tb):
            self.block(blk)
            if after_block is not None:
                after_block(blk)

    def build(self):
        nc = bass.Bass("TRN2", target_bir_lowering=False)
        self.declare(nc)
        I = self.I
        O = nc.dram_tensor("out", [self.ntok, D], F32, kind="ExternalOutput").ap()
        P = Prog(nc, n_chan=16)
        pb = [P.ps("pb%d" % i, [128, 512]) for i in range(8)]
        ident = P.sb("ident", [128, 128])
        P.op("gpsimd", lambda e: e.memset(ident[:], 1.0), writes=["ident"])
        P.op("gpsimd", lambda e: e.affine_select(out=ident[:], in_=ident[:], compare_op=ALU.is_equal,
                                                   fill=0.0, base=0, pattern=[[-1, 128]], channel_multiplier=1),
             reads=["ident"], writes=["ident"])
        P.phase_begin()
        self.emit(nc, P, pb, ident, lambda e, r0, n: I["x"][r0:r0 + n, :], O)
        P.phase_end()
        P.wait_all_dma("sync")
        P.emit()
        return nc

    def load_w(self, dst_view_fn, src_ap_list, nfree):
        P = self.P
        st = self.wst[self.wi % 3]; sk = "wst%d" % (self.wi % 3); self.wi += 1
        bf = self.wbf[self.bi % 4]; bk = "wbf%d" % (self.bi % 4); self.bi += 1
        for (src, view) in src_ap_list:
            P.dma(view(st), src, writes=[sk])
        eng = ("vector", "scalar")[self.ci % 2]; self.ci += 1
        self.cp(eng, bf[:, 0:nfree], st[:, 0:nfree], [sk], [bk])
        return bf, bk

    def norm_to_hT(self, ncol0, want32=None):
        P = self.P
        TBt = self.tb // 128
        for t in range(TBt):
            xn = self.xn[t % 2]; xk = "xn%d" % (t % 2)
            self.act(self.sq[:], self.x[:, t, :], AF.Square, ["x"], ["sqj", "ssv"], accum_out=self.ssv[:, 0:1])
            self.act(self.ssv[:, 0:1], self.ssv[:, 0:1], AF.Sqrt, ["ssv"], ["ssv"], scale=1.0 / D, bias=1e-6)
            P.op("vector", lambda e: e.reciprocal(out=self.ssv[:, 1:2], in_=self.ssv[:, 0:1]), reads=["ssv"], writes=["ssv"])
            self.ts("vector", xn[:], self.x[:, t, :], self.ssv[:, 1:2], ALU.mult, ["x", "ssv"], [xk])
            for half in range(2):
                pt = self.pb[half]; pk = "pb%d" % half
                for c4 in range(4):
                    c = half * 4 + c4
                    self.tr(pt[:, c4 * 128:(c4 + 1) * 128], xn[:, c * 128:(c + 1) * 128], [xk], [pk])
                gsl = self.norms[:, ncol0 + half * 4:ncol0 + half * 4 + 4]
                self.tt("vector", self.hT[:, half * 4:(half + 1) * 4, t * 128:(t + 1) * 128],
                        pt[:].rearrange("p (c t) -> p c t", c=4), gsl.unsqueeze(2).to_broadcast([128, 4, 128]), ALU.mult,
                        [pk, "norms"], ["hT"])
                if want32 is not None and want32 == t:
                    self.tt("vector", self.h32[:, half * 4:(half + 1) * 4, :],
                            pt[:].rearrange("p (c t) -> p c t", c=4), gsl.unsqueeze(2).to_broadcast([128, 4, 128]), ALU.mult,
                            [pk, "norms"], ["h32"])
            if want32 is not None and want32 == "all":
                pass

    def block(self, blk):
        P, I = self.P, self.I
        tb = self.tb; TBt = tb // 128; t0 = blk * tb; sbw = self.sbw; NSB = tb // sbw
        pb = self.pb
        x = self.x
        if blk > 0:
            P.barrier()
        for t in range(TBt):
            P.dma(x[:, t, :], (lambda e, r0=t0 + t * 128: self.xsrc_fn(e, r0, 128)), writes=["x"])
        self.norm_to_hT(0)
        if not hasattr(self, "yT"):
            self.yT = P.sb("yTact", [128, 8, tb], BF16); self.mT = P.sb("mT", [128, 8, tb], BF16)
            self.ystg = P.sb("ystg", [128, tb]); self.sg = [P.sb("sg%d" % i, [128, 512]) for i in range(2)]
            self.macc = P.sb("macc", [128, 512]); self.mtmp = P.sb("mtmp", [128, 512])
        yT, mT = self.yT, self.mT
        if self.ysrc_fn is None:
            for q in range(8):
                P.dma(self.ystg[:], I["ysT"][q, :, t0:t0 + tb], writes=["ystg"])
                self.cp("vector", yT[:, q, :], self.ystg[:], ["ystg"], ["yT"])
        else:
            for t in range(TBt):
                yt = self.xn[t % 2]; ytk = "xn%d" % (t % 2)
                ytv = yt[:].rearrange("p (b h c) -> p b h c", b=4, h=4)
                for r in range(4):
                    if getattr(self, "ysplit", False):
                        P.dma(ytv[:, 1:3, r, :], (lambda e, r=r, r0=t0 + t * 128: self.ysrc_fn(e, "A", r, r0, 128).rearrange("p (b c) -> p b c", b=2)), writes=[ytk])
                        P.dma(ytv[:, 0:4:3, r, :], (lambda e, r=r, r0=t0 + t * 128: self.ysrc_fn(e, "S", r, r0, 128).rearrange("p (b c) -> p b c", b=2)), writes=[ytk])
                    else:
                        P.dma(ytv[:, :, r, :],
                              (lambda e, r=r, r0=t0 + t * 128: self.ysrc_fn(e, r, r0, 128).rearrange("p (b c) -> p b c", b=4)), writes=[ytk])
                for half in range(2):
                    pt = self.pb[half]; pk = "pb%d" % half
                    for c4 in range(4):
                        q = half * 4 + c4
                        self.tr(pt[:, c4 * 128:(c4 + 1) * 128], yt[:, q * 128:(q + 1) * 128], [ytk], [pk])
                    self.cp("vector" if half else "scalar", yT[:, half * 4:(half + 1) * 4, t * 128:(t + 1) * 128],
                            pt[:].rearrange("p (c t) -> p c t", c=4), [pk], ["yT"])
        si = 0
        for j in range(8):
            wg, wgk = self.load_w(None, [(I["wgate"][:, b * 1024 + j * 128:b * 1024 + (j + 1) * 128].rearrange("(c p) n -> p c n", p=128),
                                          (lambda st, b=b: st[:, 0:4096].rearrange("p (c b n) -> p c b n", c=8, b=4)[:, :, b, :])) for b in range(4)], 4096)
            wb, wbk = self.load_w(None, [(I["wbo"][:, :, j * 128:(j + 1) * 128].rearrange("b (c2 p) n -> p (b c2) n", p=128),
                                          (lambda st: st[:, 0:1024].rearrange("p (q n) -> p q n", q=8)))], 1024)
            for sb_ in range(NSB):
                ts_ = slice(sb_ * sbw, (sb_ + 1) * sbw)
                for b in range(4):
                    for c in range(8):
                        self.mm(pb[2][:, 0:sbw], wg[:, c * 512 + b * 128:c * 512 + (b + 1) * 128], self.hT[:, c, ts_], [wgk, "hT"], ["pb2"],
                                start=(c == 0), stop=(c == 7))
                    for c2 in range(2):
                        self.mm(pb[3][:, 0:sbw], wb[:, (b * 2 + c2) * 128:(b * 2 + c2 + 1) * 128], yT[:, b * 2 + c2, ts_], [wbk, "yT"], ["pb3"],
                                start=(c2 == 0), stop=(c2 == 1))
                    sg = self.sg[si % 2]; sgk = "sg%d" % (si % 2); si += 1
                    self.act(sg[:, 0:sbw], pb[2][:, 0:sbw], AF.Sigmoid, ["pb2"], [sgk])
                    if b == 0:
                        self.tt("vector", self.macc[:, 0:sbw], pb[3][:, 0:sbw], sg[:, 0:sbw], ALU.mult, ["pb3", sgk], ["macc"])
                    else:
                        self.tt("vector", self.mtmp[:, 0:sbw], pb[3][:, 0:sbw], sg[:, 0:sbw], ALU.mult, ["pb3", sgk], ["mtmp"])
                        if b < 3:
                            self.tt("vector", self.macc[:, 0:sbw], self.macc[:, 0:sbw], self.mtmp[:, 0:sbw], ALU.add, ["macc", "mtmp"], ["macc"])
                        else:
                            self.tt("vector", mT[:, j, ts_], self.macc[:, 0:sbw], self.mtmp[:, 0:sbw], ALU.add, ["macc", "mtmp"], ["mT"])
        for hc in range(2):
            wo, wok = self.load_w(None, [(I["wout"][:, hc * 512:(hc + 1) * 512].rearrange("(c p) n -> p c n", p=128),
                                          (lambda st: st[:, 0:4096].rearrange("p (c n) -> p c n", c=8)))], 4096)
            for t in range(TBt):
                pz = pb[4 + t % 2]; pzk = "pb%d" % (4 + t % 2)
                for c in range(8):
                    self.mm(pz[:, :], mT[:, c, t * 128:(t + 1) * 128], wo[:, c * 512:(c + 1) * 512], ["mT", wok], [pzk], start=(c == 0), stop=(c == 7))
                self.tt("vector", x[:, t, hc * 512:(hc + 1) * 512], pz[:, :], x[:, t, hc * 512:(hc + 1) * 512], ALU.add, [pzk, "x"], ["x"])
        P.barrier()
        F = self.F; NF = F // 128
        if not hasattr(self, "actT"):
            self.actT = self.yT
            self.gs = [P.sb("gs%d" % i, [128, 512]) for i in range(2)]
            if self.moe:
                self.rt = P.sb("router", [128, 8, 8]); self.gw = P.sb("gatew", [128, TBt, 8])
                self.r1 = P.sb("r1", [128, 8]); self.r2 = P.sb("r2", [128, 8]); self.rm = P.sb("rm", [128, 4])
                self.m1 = P.sb("rmask1", [128, 8]); self.m2 = P.sb("rmask2", [128, 8])
                P.dma(self.rt[:], I["router"].rearrange("(c p) e -> p c e", p=128), writes=["router"])
        actT = self.actT
        if self.moe:
            for t in range(TBt):
                self.norm_to_hT_tile32(t)
                for c in range(8):
                    self.mm(pb[6][:, 0:8], self.h32[:, c, :], self.rt[:, c, :], ["h32", "router"], ["pb6"], start=(c == 0), stop=(c == 7))
                r1, r2, rm, m1, m2, gw = self.r1, self.r2, self.rm, self.m1, self.m2, self.gw
                self.cp("vector", r1[:], pb[6][:, 0:8], ["pb6"], ["r1"])
                P.op("vector", lambda e: e.reduce_max(out=rm[:, 0:1], in_=r1[:], axis=AX.X), reads=["r1"], writes=["rm"])
                self.ts("vector", m1[:], r1[:], rm[:, 0:1], ALU.is_equal, ["r1", "rm"], ["rmask1"])
                self.stt(r2[:], m1[:], -1e30, r1[:], ALU.mult, ALU.add, ["rmask1", "r1"], ["r2"])
                P.op("vector", lambda e: e.reduce_max(out=rm[:, 1:2], in_=r2[:], axis=AX.X), reads=["r2"], writes=["rm"])
                self.ts("vector", m2[:], r2[:], rm[:, 1:2], ALU.is_equal, ["r2", "rm"], ["rmask2"])
                self.tt("vector", rm[:, 2:3], rm[:, 1:2], rm[:, 0:1], ALU.subtract, ["rm"], ["rm"])
                self.act(rm[:, 2:3], rm[:, 2:3], AF.Exp, ["rm"], ["rm"])
                self.ts("vector", rm[:, 2:3], rm[:, 2:3], 1.0, ALU.add, ["rm"], ["rm"])
                P.op("vector", lambda e: e.reciprocal(out=rm[:, 2:3], in_=rm[:, 2:3]), reads=["rm"], writes=["rm"])
                self.ts("vector", rm[:, 3:4], rm[:, 2:3], -1.0, ALU.mult, ["rm"], ["rm"], s2=1.0, op1=ALU.add)
                self.ts("vector", m1[:], m1[:], rm[:, 2:3], ALU.mult, ["rmask1", "rm"], ["rmask1"])
                self.stt(gw[:, t, :], m2[:], rm[:, 3:4], m1[:], ALU.mult, ALU.add, ["rmask2", "rm", "rmask1"], ["gatew"])
        self.norm_to_hT(8)
        gi = 0
        for e in range(self.NE):
            for f0 in range(0, NF, 8):
                nf = min(8, NF - f0)
                for g0 in range(0, nf, 4):
                    ng = min(4, nf - g0)
                    fa = f0 + g0
                    wg_, wgk_ = self.load_w(None, [(I["fwg"][e, :, fa * 128:(fa + ng) * 128].rearrange("(c p) n -> p c n", p=128),
                                                    (lambda st, ng=ng: st[:, 0:4096].rearrange("p (c n) -> p c n", c=8)[:, :, 0:ng * 128]))], 4096)
                    wu_, wuk_ = self.load_w(None, [(I["fwu"][e, :, fa * 128:(fa + ng) * 128].rearrange("(c p) n -> p c n", p=128),
                                                    (lambda st, ng=ng: st[:, 0:4096].rearrange("p (c n) -> p c n", c=8)[:, :, 0:ng * 128]))], 4096)
                    for ii in range(ng):
                        i = g0 + ii
                        for sb_ in range(NSB):
                            ts_ = slice(sb_ * sbw, (sb_ + 1) * sbw)
                            for c in range(8):
                                self.mm(pb[2][:, 0:sbw], wg_[:, c * 512 + ii * 128:c * 512 + (ii + 1) * 128], self.hT[:, c, ts_], [wgk_, "hT"], ["pb2"], start=(c == 0), stop=(c == 7))
                            for c in range(8):
                                self.mm(pb[3][:, 0:sbw], wu_[:, c * 512 + ii * 128:c * 512 + (ii + 1) * 128], self.hT[:, c, ts_], [wuk_, "hT"], ["pb3"], start=(c == 0), stop=(c == 7))
                            gs = self.gs[gi % 2]; gsk = "gs%d" % (gi % 2); gi += 1
                            self.act(gs[:, 0:sbw], pb[2][:, 0:sbw], AF.Silu, ["pb2"], [gsk])
                            self.tt("vector", actT[:, i, ts_], pb[3][:, 0:sbw], gs[:, 0:sbw], ALU.mult, ["pb3", gsk], ["actT"])
                for hc in range(2):
                    wd, wdk = self.load_w(None, [(I["fwd"][e, f0 * 128:(f0 + nf) * 128, hc * 512:(hc + 1) * 512].rearrange("(i p) n -> p i n", p=128),
                                                  (lambda st, nf=nf: st[:, 0:nf * 512].rearrange("p (i n) -> p i n", n=512)))], nf * 512)
                    for t in range(TBt):
                        pz = pb[4 + t % 2]; pzk = "pb%d" % (4 + t % 2)
                        for i in range(nf):
                            self.mm(pz[:, :], actT[:, i, t * 128:(t + 1) * 128], wd[:, i * 512:(i + 1) * 512], ["actT", wdk], [pzk],
                                    start=(i == 0), stop=(i == nf - 1))
                        xs = x[:, t, hc * 512:(hc + 1) * 512]
                        if self.moe:
                            self.stt(xs, pz[:, :], self.gw[:, t, e:e + 1], xs, ALU.mult, ALU.add, [pzk, "gatew", "x"], ["x"])
                        else:
                            self.tt("vector", xs, pz[:, :], xs, ALU.add, [pzk, "x"], ["x"])
        self.norm_to_hT(16)
        if not hasattr(self, "pTt"):
            self.pTt = P.sb("pTt", [128, 2, tb], BF16)
        for c2 in range(2):
            P.dma(self.ystg[:], I["pT"][c2, :, t0:t0 + tb], writes=["ystg"])
            self.cp("vector", self.pTt[:, c2, :], self.ystg[:], ["ystg"], ["pTt"])
        for hc in range(2):
            wpg, wpgk = self.load_w(None, [(I["plegate"][:, hc * 512:(hc + 1) * 512].rearrange("(c p) n -> p c n", p=128),
                                            (lambda st: st[:, 0:4096].rearrange("p (c n) -> p c n", c=8)))], 4096)
            wpp, wppk = self.load_w(None, [(I["pleproj"][:, hc * 512:(hc + 1) * 512].rearrange("(c2 p) n -> p c2 n", p=128),
                                            (lambda st: st[:, 0:1024].rearrange("p (c2 n) -> p c2 n", c2=2)))], 1024)
            for t in range(TBt):
                for c in range(8):
                    self.mm(pb[2][:, :], self.hT[:, c, t * 128:(t + 1) * 128], wpg[:, c * 512:(c + 1) * 512], ["hT", wpgk], ["pb2"], start=(c == 0), stop=(c == 7))
                for c2 in range(2):
                    self.mm(pb[3][:, :], self.pTt[:, c2, t * 128:(t + 1) * 128], wpp[:, c2 * 512:(c2 + 1) * 512], ["pTt", wppk], ["pb3"], start=(c2 == 0), stop=(c2 == 1))
                gs = self.gs[gi % 2]; gsk = "gs%d" % (gi % 2); gi += 1
                self.act(gs[:], pb[2][:, :], AF.Sigmoid, ["pb2"], [gsk])
                self.tt("vector", gs[:], pb[3][:, :], gs[:], ALU.mult, ["pb3", gsk], [gsk])
                xs = x[:, t, hc * 512:(hc + 1) * 512]
                self.tt("vector", xs, xs, gs[:], ALU.add, ["x", gsk], ["x"])
        for t in range(TBt):
            if self.final:
                xn = self.xn[t % 2]; xk = "xn%d" % (t % 2)
                self.act(self.sq[:], x[:, t, :], AF.Square, ["x"], ["sqj", "ssv"], accum_out=self.ssv[:, 0:1])
                self.act(self.ssv[:, 0:1], self.ssv[:, 0:1], AF.Sqrt, ["ssv"], ["ssv"], scale=1.0 / D, bias=1e-6)
                P.op("vector", lambda e: e.reciprocal(out=self.ssv[:, 1:2], in_=self.ssv[:, 0:1]), reads=["ssv"], writes=["ssv"])
                self.stt(xn[:], x[:, t, :], self.ssv[:, 1:2], self.fn[:], ALU.mult, ALU.mult, ["x", "ssv", "fnorm"], [xk])
                P.dma(self.O[t0 + t * 128:t0 + (t + 1) * 128, :], xn[:], reads=[xk], writes=["O"])
            else:
                P.dma(self.O[t0 + t * 128:t0 + (t + 1) * 128, :], x[:, t, :], reads=["x"], writes=["O"])

    def norm_to_hT_tile32(self, t):
        P = self.P
        xn = self.xn[t % 2]; xk = "xn%d" % (t % 2)
        self.act(self.sq[:], self.x[:, t, :], AF.Square, ["x"], ["sqj", "ssv"], accum_out=self.ssv[:, 0:1])
        self.act(self.ssv[:, 0:1], self.ssv[:, 0:1], AF.Sqrt, ["ssv"], ["ssv"], scale=1.0 / D, bias=1e-6)
        P.op("vector", lambda e: e.reciprocal(out=self.ssv[:, 1:2], in_=self.ssv[:, 0:1]), reads=["ssv"], writes=["ssv"])
        self.ts("vector", xn[:], self.x[:, t, :], self.ssv[:, 1:2], ALU.mult, ["x", "ssv"], [xk])
        for half in range(2):
            pt = self.pb[half]; pk = "pb%d" % half
            for c4 in range(4):
                c = half * 4 + c4
                self.tr(pt[:, c4 * 128:(c4 + 1) * 128], xn[:, c * 128:(c + 1) * 128], [xk], [pk])
            gsl = self.norms[:, 8 + half * 4:8 + half * 4 + 4]
            self.tt("vector", self.h32[:, half * 4:(half + 1) * 4, :],
                    pt[:].rearrange("p (c t) -> p c t", c=4), gsl.unsqueeze(2).to_broadcast([128, 4, 128]), ALU.mult,
                    [pk, "norms"], ["h32"])


def make_inputs_b(inp, layer, core, ys_full, x_full, ntok=2048, final=None, fused=False, sfx=""):
    sl = slice(core * ntok, (core + 1) * ntok)
    if not fused:
        xs = x_full.reshape(-1, D)[sl]
        ys = ys_full.reshape(-1, 4, 2, 128)[sl]
        ysT = np.ascontiguousarray(ys.transpose(1, 2, 3, 0).reshape(8, 128, ntok))
    p = inp["p"][layer].reshape(-1, 2, 128)[sl]
    pT = np.ascontiguousarray(p.transpose(1, 2, 0))
    moe = (layer % 2 == 1); j = layer // 2
    norms = np.concatenate([inp["norm_mix"][layer].reshape(8, 128).T, inp["norm_ffn"][layer].reshape(8, 128).T,
                            inp["norm_ple"][layer].reshape(8, 128).T], axis=1)
    if final is None:
        final = (layer == 1)
    d = {
        "pT": pT,
        "wgate": np.ascontiguousarray(inp["w_in"][layer][:, 3596:7692]), "wbo": inp["w_bo"][layer], "wout": inp["w_out"][layer],
        "norms": np.ascontiguousarray(norms.astype(np.float32)),
        "plegate": inp["ple_gate"][layer], "pleproj": inp["ple_proj"][layer],
    }
    if not fused:
        d["x"] = np.ascontiguousarray(xs); d["ysT"] = ysT
    if final:
        d["fnorm"] = inp["final_norm"].reshape(1, D)
    if moe:
        d["fwg"] = inp["moe_w_gate"][j]; d["fwu"] = inp["moe_w_up"][j]; d["fwd"] = inp["moe_w_down"][j]; d["router"] = inp["moe_router"][j]
    else:
        d["fwg"] = inp["ffn_w_gate"][j][None]; d["fwu"] = inp["ffn_w_up"][j][None]; d["fwd"] = inp["ffn_w_down"][j][None]
    return {k + sfx: v for k, v in d.items()}


def build_fused(do=("diff", "fox", "rwkv", "gdn"), nlayers=2):
    S_ = S
    nc = bass.Bass("TRN2", target_bir_lowering=False)
    ntok = S_ // 4; tb = min(1024, ntok)
    CRY = S_ // 4
    CRX = max(128, ntok // 8)
    NCHX = ntok // CRX
    G16 = CRY // 16
    kas = [KA(l, do=do) for l in range(nlayers)]
    kbs = [KB(l, ntok=ntok, tb=tb, moe=(l % 2 == 1), final=(l == 1)) for l in range(nlayers)]
    x_in = nc.dram_tensor("x", [S_, D], F32, kind="ExternalInput").ap()
    rope = nc.dram_tensor("rope", [2, 64, S_], F32, kind="ExternalInput").ap()
    for l in range(nlayers):
        kas[l].declare(nc, sfx="_a%d" % l)
        kbs[l].declare(nc, sfx="_b%d" % l, fused=True)
    out = nc.dram_tensor("out", [ntok, D], F32, kind="ExternalOutput").ap()
    ybufA = [nc.dram_tensor("ybufA%d" % l, [S_, 128], F32).ap() for l in range(nlayers)]
    ybufS = [nc.dram_tensor("ybufS%d" % l, [S_, 128], F32).ap() for l in range(nlayers)]
    ygA = [nc.dram_tensor("ygA%d" % l, [4 * S_, 128], F32).ap() for l in range(nlayers)]
    ygS = [nc.dram_tensor("ygS%d" % l, [4 * S_, 128], F32).ap() for l in range(nlayers)]
    xq = nc.dram_tensor("xq", [ntok, D], F32).ap()
    xgc = nc.dram_tensor("xgc", [S_, D], F32).ap()
    xq0 = nc.dram_tensor("xq0", [ntok, D], F32).ap()
    yselA = nc.dram_tensor("yselA", [4 * ntok, 128], F32).ap()
    yselS = nc.dram_tensor("yselS", [4 * ntok, 128], F32).ap()
    P = Prog(nc, n_chan=16)
    sh = KA.make_shared(nc, P)
    groups = [[0, 1, 2, 3], [4, 5, 6, 7]]

    def xg_rows(t0):
        r = t0 // ntok; w = t0 % ntok; k = w // CRX; i = w % CRX
        row = (k * 4 + r) * CRX + i
        return xgc[row:row + 128, :]

    for l in range(nlayers):
        xsrc = x_in if l == 0 else xg_rows
        YA = ybufA[l].rearrange("s (b c) -> s b c", b=2); YS = ybufS[l].rearrange("s (b c) -> s b c", b=2)
        Ymap = {0: YS[:, 0, :], 1: YA[:, 0, :], 2: YA[:, 1, :], 3: YS[:, 1, :]}

        def after_attn(l=l):
            for k in range(4):
                P.collective("AllGather", groups, ybufA[l][k * CRY:(k + 1) * CRY, :], ygA[l][k * 4 * CRY:(k + 1) * 4 * CRY, :], block=False)
        kas[l].emit(nc, P, sh, xsrc, rope, Ymap, after_attn=after_attn)
        for k in range(4):
            P.collective("AllGather", groups, ybufS[l][k * CRY:(k + 1) * CRY, :], ygS[l][k * 4 * CRY:(k + 1) * 4 * CRY, :], block=False)
        P.collective_wait()
        P.phase_begin()
        for (yg_, ysel_, key) in ((ygA[l], yselA, "yselA"), (ygS[l], yselS, "yselS")):
            ygv = yg_.rearrange("(a b) c -> a (b c)", b=32)
            P.dma(ysel_.rearrange("(a b) c -> a (b c)", b=32),
                  (lambda e, ygv=ygv: ygv[bass.ds(P.qid(e) * (4 * CRY // 32), 4 * CRY // 32), :]), writes=[key])
        if l == 0:
            P.dma(xq0, (lambda e: x_in[bass.ds(P.qid(e) * ntok, ntok), :]), writes=["xq0"])
        P.phase_end()
        P.phase_begin()
        if l == 0:
            xfn = lambda e, r0, n: xq0[r0:r0 + n, :]
        else:
            xfn = lambda e, r0, n: xq[r0:r0 + n, :]
        yfn = lambda e, grp, r, r0, n: (yselA if grp == "A" else yselS)[r * ntok + r0:r * ntok + r0 + n, :]
        kbs[l].ysplit = True
        def after_block(blk, l=l):
            if l < nlayers - 1:
                for k in range(blk * tb // CRX, (blk + 1) * tb // CRX):
                    P.collective("AllGather", groups, xq[k * CRX:(k + 1) * CRX, :], xgc[k * 4 * CRX:(k + 1) * 4 * CRX, :],
                                 reads=["O"], block=False)
        kbs[l].emit(nc, P, sh["pb"], sh["ident"], xfn, (xq if l < nlayers - 1 else out), yfn, after_block=after_block)
        P.phase_end()
        if l < nlayers - 1:
            P.collective_wait()
    P.wait_all_dma("sync")
    P.emit()
    return nc


def make_inputs_fused(inp, core, nlayers=2):
    b, h = core // 4, core % 4
    ntok = S // 4
    m = {"x": np.ascontiguousarray(inp["x"][b]), "rope": rope_tables()}
    for l in range(nlayers):
        a = make_inputs(inp, l, core)
        for k_, v in a.items():
            if k_ != "rope":
                m[k_ + "_a%d" % l] = v
        d = make_inputs_b(inp, l, 0, None, None, ntok=ntok, fused=True, sfx="_b%d" % l)
        p = inp["p"][l][b].reshape(S, 2, 128)[h * ntok:(h + 1) * ntok]
        d["pT_b%d" % l] = np.ascontiguousarray(p.transpose(1, 2, 0))
        m.update(d)
    return m


_CACHE = {}


def kernel(**inputs):
    inp = {k: np.asarray(v) for k, v in inputs.items()}
    if "nc" not in _CACHE:
        _CACHE["nc"] = build_fused()
    cores = list(range(8))
    maps = [make_inputs_fused(inp, core) for core in cores]
    res = run_bass_kernel_spmd(_CACHE["nc"], maps, core_ids=cores)
    out = np.concatenate([res.results[c]["out"] for c in cores], axis=0)
    return out.reshape(2, S, 1024).astype(np.float32)
```

```python
import math
import contextlib
import numpy as np
import concourse.bass as bass
import concourse.mybir as mybir
from concourse.bass_utils import run_bass_kernel_spmd


F32 = mybir.dt.float32
BF16 = mybir.dt.bfloat16
AF = mybir.ActivationFunctionType
ALU = mybir.AluOpType
AX = mybir.AxisListType

COMPUTE = ("tensor", "vector", "scalar", "gpsimd")
QUEUES = ("sync", "gpsimd", "scalar")


class Chan:
    def __init__(self, sem):
        self.sem = sem
        self.n = 0
        self.T = 0
        self.issue_waited = 0


class Prog:
    def __init__(self, nc, n_chan=12):
        self.nc = nc
        self.es = contextlib.ExitStack()
        self.ops = {e: [] for e in ("tensor", "vector", "scalar", "gpsimd", "sync")}
        self.cnt = {e: 0 for e in COMPUTE}
        self.sem = {e: self.es.enter_context(nc.semaphore("s_" + e)) for e in COMPUTE}
        self.chans = [Chan(self.es.enter_context(nc.semaphore("c%d" % i))) for i in range(n_chan)]
        self.rr = 0
        self.last_w = {}
        self.readers = {}
        self.known = {e: {} for e in self.ops}
        self.tensors = {}
        self.pes = None
        self.phase_id = 0
        self.flush_id = 0
        self.qcache = {}

    def sb(self, name, shape, dt=F32):
        es = self.pes if self.pes is not None else self.es
        t = es.enter_context(self.nc.sbuf_tensor("sb%d_" % self.phase_id + name, list(shape), dt))
        return t

    def phase_begin(self):
        self.phase_id += 1
        self.pes = contextlib.ExitStack()

    def phase_end(self):
        self.barrier()
        self.flush()
        self.pes.close()
        self.pes = None

    def barrier(self):
        for e in self.ops:
            waits = []
            kn = self.known[e]
            for e2 in COMPUTE:
                v = self.cnt[e2]
                if v and kn.get(("eng", e2), 0) < v:
                    kn[("eng", e2)] = v
                    waits.append((self.sem[e2], v))
            for ci, ch in enumerate(self.chans):
                if ch.n and kn.get(("chan", ci), 0) < ch.n:
                    kn[("chan", ci)] = ch.n
                    waits.append((ch.sem, 16 * ch.n))
                ch.T = ch.n
                ch.issue_waited = ch.n
            if waits:
                self.ops[e].append((None, waits, None))
        self.last_w = {}
        self.readers = {}

    def flush(self):
        nc = self.nc
        self.flush_id += 1
        with nc.Block() as block:
            def mk(engname):
                def body(e):
                    for fn, waits, inc in self.ops[engname]:
                        for s, v in waits:
                            e.wait_ge(s, v)
                        if fn is not None:
                            ins = fn(e)
                            if inc is not None:
                                ins.then_inc(inc[0], inc[1])
                return body
            block.sync(mk("sync"))
            block.tensor(mk("tensor"))
            block.vector(mk("vector"))
            block.scalar(mk("scalar"))
            block.gpsimd(mk("gpsimd"))
        self.ops = {e: [] for e in self.ops}

    def ps(self, name, shape, dt=F32):
        t = self.es.enter_context(self.nc.psum_tensor("ps_" + name, list(shape), dt))
        return t

    def _dep_waits(self, eng, reads, writes):
        deps = []
        for k in reads:
            w = self.last_w.get(k)
            if w is not None:
                deps.append(w)
        relax = getattr(self, "relax_same_engine", False)
        for k in writes:
            w = self.last_w.get(k)
            if w is not None and not (relax and w[0] == "eng" and w[1] == eng):
                deps.append(w)
            for rd in self.readers.get(k, ()):
                if relax and rd[0] == "eng" and rd[1] == eng:
                    continue
                deps.append(rd)
        waits = {}
        for d in deps:
            if d[0] == "eng":
                _, e2, n = d
                if e2 == eng and eng == "tensor":
                    continue
                key = ("eng", e2)
                waits[key] = max(waits.get(key, 0), n)
            else:
                _, ci = d
                ch = self.chans[ci]
                key = ("chan", ci)
                waits[key] = max(waits.get(key, 0), ch.n)
                ch.T = max(ch.T, ch.n)
        out = []
        kn = self.known[eng]
        for key, v in waits.items():
            if kn.get(key, 0) >= v:
                continue
            kn[key] = v
            if key[0] == "eng":
                out.append((self.sem[key[1]], v))
            else:
                out.append((self.chans[key[1]].sem, 16 * v))
        return out

    def _record(self, tag, reads, writes):
        for k in reads:
            self.readers.setdefault(k, []).append(tag)
        for k in writes:
            self.last_w[k] = tag
            self.readers[k] = []

    def op(self, eng, fn, reads=(), writes=()):
        waits = self._dep_waits(eng, reads, writes)
        self.cnt[eng] += 1
        n = self.cnt[eng]
        self.ops[eng].append((fn, waits, (self.sem[eng], 1)))
        self._record(("eng", eng, n), reads, writes)

    def dma(self, out, in_, reads=(), writes=(), q="sync", chan=None, **kw):
        if chan is None:
            chan = self.rr
            self.rr = (self.rr + 1) % len(self.chans)
        ch = self.chans[chan]
        waits = self._dep_waits(q, reads, writes)
        if ch.T > ch.issue_waited:
            kn = self.known[q]
            key = ("chan", chan)
            if kn.get(key, 0) < ch.T:
                kn[key] = ch.T
                waits.append((ch.sem, 16 * ch.T))
            ch.issue_waited = ch.T
        ch.n += 1
        def fn(e, out=out, in_=in_, kw=kw):
            o = out(e) if callable(out) else out
            i = in_(e) if callable(in_) else in_
            try:
                return e.dma_start(out=o, in_=i, **kw)
            except Exception:
                print("DMA FAIL out=", o, " in=", i)
                raise
        self.ops[q].append((fn, waits, (ch.sem, 16)))
        self._record(("chan", chan), reads, writes)

    def qid(self, e):
        key = (self.flush_id, id(e))
        if key not in self.qcache:
            self.qcache[key] = e.snap(e.partition_id() % 4, min_val=0, max_val=3)
        return self.qcache[key]

    def collective(self, kind, groups, src, dst, reads=(), writes=(), block=True):
        if not hasattr(self, "cc_sem"):
            self.cc_sem = self.es.enter_context(self.nc.semaphore("cc_sem")); self.cc_n = 0
        waits = self._dep_waits("gpsimd", reads, writes)
        self.cc_n += 1
        n = self.cc_n
        self.ops["gpsimd"].append((lambda e: e.collective_compute(kind, ALU.bypass, replica_groups=groups, ins=[src], outs=[dst]), waits, (self.cc_sem, 1)))
        if block:
            self.collective_wait(n)
        return n

    def collective_wait(self, n=None):
        n = self.cc_n if n is None else n
        for e in self.ops:
            self.ops[e].append((None, [(self.cc_sem, n)], None))

    def wait_all_dma(self, eng="sync"):
        waits = []
        for ch in self.chans:
            if ch.n:
                waits.append((ch.sem, 16 * ch.n))
        self.ops[eng].append((None, waits, None))

    def emit(self):
        self.flush()
        self.es.close()


S = 8192
D = 1024
NTB = S // 512
NT = S // 128

def colsel(h):
    cm = []
    r0 = 0; dq = 1024; dk = 1280; dv = 1536
    fq = 1792; fk = 2048; fv = 2304; ffl = 2560
    gq = 2564; gk = 2820; gv = 3076; gb = 3332; ga = 3336; gg = 3340
    hs = np.arange(64) + h * 64

    def swap(base):
        idx = base + hs
        sw = idx.copy()
        for c in range(2):
            for d in range(4):
                sw[c * 32 + d] = idx[c * 32 + d + 4]
                sw[c * 32 + d + 4] = idx[c * 32 + d]
        return sw
    cm.append(("qd", dq + hs)); cm.append(("qds", swap(dq)))
    cm.append(("kd", dk + hs)); cm.append(("kds", swap(dk)))
    cm.append(("qf", fq + hs)); cm.append(("kf", fk + hs))
    cm.append(("fl", np.array([ffl + h])))
    cm.append(("rr", 0 + hs)); cm.append(("rk", 256 + hs)); cm.append(("rv", 512 + hs))
    cm.append(("xw", 768 + np.arange(64))); cm.append(("xa", 832 + np.arange(64)))
    cm.append(("xg", 896 + np.arange(128)))
    cm.append(("gq", gq + hs)); cm.append(("gk", gk + hs)); cm.append(("gv", gv + hs))
    cm.append(("gb", np.full(128, gb + h))); cm.append(("ga", np.full(128, ga + h)))
    names = {}
    idx = []
    o = 0
    for n, ix in cm:
        names[n] = (o, len(ix)); o += len(ix); idx.append(ix)
    tm = [("vd", dv + hs), ("vf", fv + hs), ("ggt", gg + hs)]
    tnames = {}; tidx = []; o = 0
    for n, ix in tm:
        tnames[n] = (o, len(ix)); o += len(ix); tidx.append(ix)
    return names, np.concatenate(idx), tnames, np.concatenate(tidx)


def rope_tables():
    pos = np.arange(S, dtype=np.float32)
    inv = (500000.0 ** (-np.arange(4, dtype=np.float32) * 2.0 / 8)).astype(np.float32)
    ang = pos[None, :] * inv[:, None]
    cos = np.cos(ang).astype(np.float32); sin = np.sin(ang).astype(np.float32)
    ct = np.ones((64, S), np.float32); st = np.zeros((64, S), np.float32)
    for c in range(2):
        for d in range(4):
            ct[c * 32 + d] = cos[d]; ct[c * 32 + d + 4] = cos[d]
            st[c * 32 + d] = -sin[d]; st[c * 32 + d + 4] = sin[d]
    return np.stack([ct, st])


class KA:
    def __init__(self, layer_idx, do=("diff", "fox", "rwkv", "gdn")):
        self.layer_idx = layer_idx
        self.do = do
        self.names, _, self.tnames, _ = colsel(0)
        self.NCc = sum(n for _, n in self.names.values())
        self.NCv = sum(n for _, n in self.tnames.values())

    def declare(self, nc, sfx=""):
        NCc, NCv = self.NCc, self.NCv
        I = {}
        def inp(name, shape):
            I[name] = nc.dram_tensor(name + sfx, list(shape), F32, kind="ExternalInput").ap()
        inp("wc", [D, NCc]); inp("wv", [D, NCv]); inp("gm", [128, 8])
        inp("dlam", [1, 128]); inp("dsub", [1, 64]); inp("fbias", [1, 1])
        inp("rp", [128, 16]); inp("w2h", [64, 64]); inp("a2h", [64, 64]); inp("g2h", [128, 64]); inp("rln", [2, 64])
        inp("gp", [128, 16]); inp("gnorm", [1, 64])
        self.I = I
        self.UT = nc.dram_tensor("ut" + sfx, [NCc, S], F32).ap()
        self.UV = nc.dram_tensor("uv" + sfx, [S, NCv], F32).ap()
        self.sfx = sfx

    def Ybr(self, br):
        if isinstance(self.Y, dict):
            return self.Y[br]
        return self.Y[:, br, :]

    def emit(self, nc, P, shared, xsrc, rope, Ydst, after_attn=None):
        self.nc = nc; self.P = P
        self.pb = shared["pb"]; self.ident = shared["ident"]
        self.mask = shared["mask"]; self.BTi = shared["BTi"]; self.BTe = shared["BTe"]; self.BTeT = shared["BTeT"]; self.ones64 = shared["ones64"]
        self.I["x"] = xsrc; self.I["rope"] = rope
        self.Y = Ydst
        P.phase_begin(); self.phase0(); P.phase_end()
        if "diff" in self.do or "fox" in self.do:
            P.phase_begin()
            if "diff" in self.do:
                self.diff()
            if "fox" in self.do:
                self.fox()
            P.phase_end()
            for nm in ("qT", "stg", "on"):
                if hasattr(self, nm):
                    delattr(self, nm)
        if after_attn is not None:
            after_attn()
        gens = []
        if "rwkv" in self.do or "gdn" in self.do:
            P.phase_begin()
            if "rwkv" in self.do:
                gens.append(self.rwkv())
            if "gdn" in self.do:
                gens.append(self.gdn())
            while gens:
                for g in list(gens):
                    try:
                        next(g)
                    except StopIteration:
                        gens.remove(g)
            P.phase_end()

    @staticmethod
    def make_shared(nc, P):
        sh = {}
        sh["pb"] = [P.ps("pb%d" % i, [128, 512]) for i in range(8)]
        ident = P.sb("ident", [128, 128]); sh["ident"] = ident
        P.op("gpsimd", lambda e: e.memset(ident[:], 1.0), writes=["ident"])
        P.op("gpsimd", lambda e: e.affine_select(out=ident[:], in_=ident[:], compare_op=ALU.is_equal,
                                                   fill=0.0, base=0, pattern=[[-1, 128]], channel_multiplier=1),
             reads=["ident"], writes=["ident"])
        tmp = KA(0); tmp.P = P; tmp.nc = nc
        tmp.attn_common_masks()
        sh["mask"] = tmp.mask; sh["BTi"] = tmp.BTi; sh["BTe"] = tmp.BTe; sh["BTeT"] = tmp.BTeT; sh["ones64"] = tmp.ones64
        return sh

    def build(self):
        nc = bass.Bass("TRN2", target_bir_lowering=False)
        self.declare(nc)
        xsrc = nc.dram_tensor("x", [S, D], F32, kind="ExternalInput").ap()
        rope = nc.dram_tensor("rope", [2, 64, S], F32, kind="ExternalInput").ap()
        dbg = getattr(self, "dbg", False)
        Y = nc.dram_tensor("y", [S, 4, 64], F32, kind="ExternalOutput").ap()
        P = Prog(nc, n_chan=16)
        P.relax_same_engine = getattr(self, 'relax', False)
        sh = KA.make_shared(nc, P)
        self.emit(nc, P, sh, xsrc, rope, Y)
        P.wait_all_dma("sync")
        P.emit()
        return nc

    def tt(self, eng, out, in0, in1, op, r, w):
        self.P.op(eng, lambda e: e.tensor_tensor(out=out, in0=in0, in1=in1, op=op), reads=r, writes=w)

    def ts(self, eng, out, in0, s1, op0, r, w, s2=None, op1=None):
        if op1 is None:
            self.P.op(eng, lambda e: e.tensor_scalar(out=out, in0=in0, scalar1=s1, scalar2=None, op0=op0), reads=r, writes=w)
        else:
            self.P.op(eng, lambda e: e.tensor_scalar(out=out, in0=in0, scalar1=s1, scalar2=s2, op0=op0, op1=op1), reads=r, writes=w)

    def stt(self, eng, out, in0, sc, in1, op0, op1, r, w):
        eng = "vector"
        self.P.op(eng, lambda e: e.scalar_tensor_tensor(out=out, in0=in0, scalar=sc, in1=in1, op0=op0, op1=op1), reads=r, writes=w)

    def act(self, out, in_, func, r, w, **kw):
        self.P.op("scalar", lambda e: e.activation(out=out, in_=in_, func=func, **kw), reads=r, writes=w)

    def cp(self, eng, out, in_, r, w):
        if eng == "scalar":
            self.P.op(eng, lambda e: e.copy(out=out, in_=in_), reads=r, writes=w)
        else:
            self.P.op(eng, lambda e: e.tensor_copy(out=out, in_=in_), reads=r, writes=w)

    def mm(self, out, lhsT, rhs, r, w, start=True, stop=True):
        self.P.op("tensor", lambda e: e.matmul(out, lhsT=lhsT, rhs=rhs, start=start, stop=stop), reads=r, writes=w)

    def tr(self, out, in_, r, w, n=128):
        self.P.op("tensor", lambda e: e.transpose(out=out, in_=in_, identity=self.ident[0:n, 0:n]), reads=list(r) + ["ident"], writes=w)

    def ms(self, eng, out, val, w):
        self.P.op(eng, lambda e: e.memset(out, val), writes=w)

    def phase0(self):
        P, nc, I = self.P, self.nc, self.I
        NCc, NCv = self.NCc, self.NCv
        gm = P.sb("gm", [128, 8])
        P.dma(gm[:], I["gm"], writes=["gm"])
        wcb = P.sb("wcb", [128, 8, NCc], BF16)
        wvb = P.sb("wvb", [128, 8, NCv], BF16)
        wst = [P.sb("wst%d" % i, [128, 1408]) for i in range(2)]
        k = 0
        for (src, dst, n, dkey) in ((I["wc"], wcb, NCc, "wcb"), (I["wv"], wvb, NCv, "wvb")):
            for c in range(8):
                st = wst[k % 2]; key = "wst%d" % (k % 2); k += 1
                P.dma(st[:, 0:n], src[c * 128:(c + 1) * 128, :], writes=[key])
                P.op("vector", lambda e, st=st, dst=dst, c=c, n=n: e.tensor_scalar(
                    out=dst[:, c, :], in0=st[:, 0:n], scalar1=gm[:, c:c + 1], scalar2=None, op0=ALU.mult),
                    reads=[key, "gm"], writes=[dkey])
        groups = []
        o = 0
        while o < NCc:
            n = min(128, NCc - o); groups.append((o, n)); o += n
        xt = [P.sb("xt%d" % i, [128, D]) for i in range(2)]
        sq = P.sb("sqj", [128, D])
        ss = [P.sb("ss%d" % i, [128, 1]) for i in range(2)]
        hT = [P.sb("hT%d" % i, [128, 8, 512], BF16) for i in range(2)]
        og = [P.sb("og%d" % i, [128, 512]) for i in range(3)]
        ov = [P.sb("ov%d" % i, [128, NCv]) for i in range(2)]
        pT = [self.pb[0], self.pb[1]]
        gi = 0; xi = 0; vi = 0
        wckey = "wcb"; wvkey = "wvb"
        for tb in range(NTB):
            h = hT[tb % 2]; hk = "hT%d" % (tb % 2)
            for st in range(4):
                t0 = tb * 512 + st * 128
                x = xt[xi % 2]; xk = "xt%d" % (xi % 2); s_ = ss[xi % 2]; sk = "ss%d" % (xi % 2); xi += 1
                P.dma(x[:], (I["x"](t0) if callable(I["x"]) else I["x"][t0:t0 + 128, :]), writes=[xk], q="sync")
                P.op("scalar", lambda e, x=x, s_=s_: e.activation(out=sq[:], in_=x[:], func=AF.Square, accum_out=s_[:]),
                     reads=[xk], writes=["sqj", sk])
                P.op("scalar", lambda e, s_=s_: e.activation(out=s_[:], in_=s_[:], func=AF.Sqrt, scale=1.0 / D, bias=1e-6),
                     reads=[sk], writes=[sk])
                P.op("vector", lambda e, s_=s_: e.reciprocal(out=s_[:], in_=s_[:]), reads=[sk], writes=[sk])
                P.op("vector", lambda e, x=x, s_=s_: e.tensor_scalar(out=x[:], in0=x[:], scalar1=s_[:, 0:1], scalar2=None, op0=ALU.mult),
                     reads=[xk, sk], writes=[xk])
                for half in range(2):
                    pt = pT[half]; pk = "pb%d" % half
                    for c4 in range(4):
                        c = half * 4 + c4
                        P.op("tensor", lambda e, pt=pt, c4=c4, c=c, x=x: e.transpose(
                            out=pt[:, c4 * 128:(c4 + 1) * 128], in_=x[:, c * 128:(c + 1) * 128], identity=self.ident[:]),
                            reads=[xk, "ident"], writes=[pk])
                    eng = "scalar" if half == 0 else "vector"
                    if eng == "scalar":
                        P.op("scalar", lambda e, pt=pt, h=h, half=half, st=st: e.copy(
                            out=h[:, half * 4:(half + 1) * 4, st * 128:(st + 1) * 128],
                            in_=pt[:].rearrange("p (c t) -> p c t", c=4)), reads=[pk], writes=[hk])
                    else:
                        P.op("vector", lambda e, pt=pt, h=h, half=half, st=st: e.tensor_copy(
                            out=h[:, half * 4:(half + 1) * 4, st * 128:(st + 1) * 128],
                            in_=pt[:].rearrange("p (c t) -> p c t", c=4)), reads=[pk], writes=[hk])
            for (o, n) in groups:
                pg = self.pb[2 + gi % 2]; pk = "pb%d" % (2 + gi % 2)
                ob = og[gi % 3]; ok = "og%d" % (gi % 3); gi += 1
                for c in range(8):
                    P.op("tensor", lambda e, pg=pg, c=c, o=o, n=n, h=h: e.matmul(
                        pg[0:n, :], lhsT=wcb[:, c, o:o + n], rhs=h[:, c, :], start=(c == 0), stop=(c == 7)),
                        reads=[wckey, hk], writes=[pk])
                eng = "scalar" if gi % 2 == 0 else "vector"
                if eng == "scalar":
                    P.op("scalar", lambda e, pg=pg, ob=ob, n=n: e.copy(out=ob[0:n, :], in_=pg[0:n, :]), reads=[pk], writes=[ok])
                else:
                    P.op("vector", lambda e, pg=pg, ob=ob, n=n: e.tensor_copy(out=ob[0:n, :], in_=pg[0:n, :]), reads=[pk], writes=[ok])
                P.dma(self.UT[o:o + n, tb * 512:(tb + 1) * 512], ob[0:n, :], reads=[ok], writes=[("UT", tb, o)], q="sync")
            for st in range(4):
                t0 = tb * 512 + st * 128
                pv = self.pb[4 + vi % 2]; pk = "pb%d" % (4 + vi % 2)
                ob = ov[vi % 2]; ok = "ov%d" % (vi % 2); vi += 1
                for c in range(8):
                    P.op("tensor", lambda e, pv=pv, c=c, h=h, st=st: e.matmul(
                        pv[:, 0:NCv], lhsT=h[:, c, st * 128:(st + 1) * 128], rhs=wvb[:, c, :], start=(c == 0), stop=(c == 7)),
                        reads=[wvkey, hk], writes=[pk])
                P.op("vector", lambda e, pv=pv, ob=ob: e.tensor_copy(out=ob[:], in_=pv[:, 0:NCv]), reads=[pk], writes=[ok])
                P.dma(self.UV[t0:t0 + 128, :], ob[:], reads=[ok], writes=[("UV", t0)], q="sync")

    def attn_common_masks(self):
        if hasattr(self, "mask"):
            return
        P = self.P
        self.chunk_masks()
        self.mask = P.sb("amask", [128, 4, 512], BF16)
        P.op("gpsimd", lambda e: e.memset(self.mask[:], 1.0), writes=["amask"])
        for r in range(4):
            P.op("gpsimd", lambda e, r=r: e.affine_select(out=self.mask[:, r, :], in_=self.mask[:, r, :], compare_op=ALU.is_ge,
                                                        fill=0.0, base=-r * 128, pattern=[[1, 512]], channel_multiplier=-1),
                 reads=["amask"], writes=["amask"])

    def attn_loop(self, name, ncomp, kq_fn, bias_fn, scale, Vaug, vkey, rd_keys, epilogue):
        P = self.P
        self.attn_common_masks()
        PT = [P.sb("%s_pt%d" % (name, i), [128, 512], BF16) for i in range(3)]
        stb = [self.pb[0], self.pb[1], self.pb[2]]
        ob = [self.pb[4], self.pb[5]]
        n = 0
        for i in range(NTB):
            pairs = [(c, j) for c in range(ncomp) for j in range(4 * i + 4)]
            def issue_S(idx, n):
                c, j = pairs[idx]
                lhsT, rhs = kq_fn(c, j, i)
                sp = stb[n % 3]; sk = "pb%d" % (n % 3)
                P.op("tensor", lambda e, sp=sp, lhsT=lhsT, rhs=rhs: e.matmul(sp[:], lhsT=lhsT, rhs=rhs, start=True, stop=True),
                     reads=rd_keys, writes=[sk])
            issue_S(0, n)
            for idx, (c, j) in enumerate(pairs):
                if idx + 1 < len(pairs):
                    issue_S(idx + 1, n + 1)
                sp = stb[n % 3]; sk = "pb%d" % (n % 3)
                pt = PT[n % 3]; ptk = "%s_pt%d" % (name, n % 3)
                b = bias_fn(j)
                if b is None:
                    P.op("scalar", lambda e, sp=sp, pt=pt: e.activation(out=pt[:], in_=sp[:], func=AF.Exp, scale=scale),
                         reads=[sk], writes=[ptk])
                else:
                    P.op("scalar", lambda e, sp=sp, pt=pt, b=b: e.activation(out=pt[:], in_=sp[:], func=AF.Exp, scale=scale, bias=b),
                         reads=[sk, name + "_bias"], writes=[ptk])
                r = j - 4 * i
                if r >= 0:
                    P.op("vector", lambda e, pt=pt, r=r: e.tensor_tensor(out=pt[:], in0=pt[:], in1=self.mask[:, r, :], op=ALU.mult),
                         reads=[ptk, "amask"], writes=[ptk])
                o = ob[c]; okey = "pb%d" % (4 + c)
                for s in range(4):
                    P.op("tensor", lambda e, o=o, pt=pt, s=s, j=j, last=(j == 4 * i + 3): e.matmul(
                        o[:, s * 65:(s + 1) * 65], lhsT=pt[:, s * 128:(s + 1) * 128], rhs=Vaug[:, j, :],
                        start=(j == 0 and s == 0), stop=(last and s == 3)), reads=[ptk, vkey], writes=[okey])
                n += 1
            epilogue(i, ob)

    def load_qk(self, dst, dkey, src_name, swap_name=None, rope=None, extra_scale=None):
        P = self.P
        o, n = self.names[src_name]
        W = 2048
        for b in range(S // W):
            a = self.stg[0]; P.dma(a[0:64, :], self.UT[o:o + 64, b * W:(b + 1) * W], reads=["UT"], writes=["stg0"])
            if swap_name is not None:
                o2, _ = self.names[swap_name]
                a2 = self.stg[1]; P.dma(a2[0:64, :], self.UT[o2:o2 + 64, b * W:(b + 1) * W], reads=["UT"], writes=["stg1"])
                cs = self.stg[2]; P.dma(cs[0:64, :], self.I["rope"][0, :, b * W:(b + 1) * W], writes=["stg2"])
                sn = self.stg[3]; P.dma(sn[0:64, :], self.I["rope"][1, :, b * W:(b + 1) * W], writes=["stg3"])
                P.op("vector", lambda e, a=a, cs=cs: e.tensor_tensor(out=a[0:64, :], in0=a[0:64, :], in1=cs[0:64, :], op=ALU.mult),
                     reads=["stg0", "stg2"], writes=["stg0"])
                P.op("vector", lambda e, a2=a2, sn=sn: e.tensor_tensor(out=a2[0:64, :], in0=a2[0:64, :], in1=sn[0:64, :], op=ALU.mult),
                     reads=["stg1", "stg3"], writes=["stg1"])
                P.op("vector", lambda e, a=a, a2=a2, b=b: e.tensor_tensor(out=dst[0:64, b * W:(b + 1) * W], in0=a[0:64, :], in1=a2[0:64, :], op=ALU.add),
                     reads=["stg0", "stg1"], writes=[dkey])
            else:
                if extra_scale is None:
                    P.op("vector", lambda e, a=a, b=b: e.tensor_copy(out=dst[0:64, b * W:(b + 1) * W], in_=a[0:64, :]),
                         reads=["stg0"], writes=[dkey])
                else:
                    P.op("vector", lambda e, a=a, b=b: e.tensor_scalar(out=dst[0:64, b * W:(b + 1) * W], in0=a[0:64, :],
                                                                         scalar1=extra_scale, scalar2=None, op0=ALU.mult),
                         reads=["stg0"], writes=[dkey])

    def load_v(self, Vaug, vkey, tname):
        P = self.P
        o, n = self.tnames[tname]
        P.op("gpsimd", lambda e: e.memset(Vaug[:, :, 64:65], 1.0), writes=[vkey])
        for b in range(S // 2048):
            a = self.stg[0]
            P.dma(a[:, 0:16 * 64].rearrange("p (t d) -> p t d", d=64),
                  self.UV[b * 2048:(b + 1) * 2048, o:o + 64].rearrange("(t p) d -> p t d", p=128), reads=["UV"], writes=["stg0"])
            P.op("vector", lambda e, a=a, b=b: e.tensor_copy(out=Vaug[:, b * 16:(b + 1) * 16, 0:64],
                                                             in_=a[:, 0:16 * 64].rearrange("p (t d) -> p t d", d=64)),
                 reads=["stg0"], writes=[vkey])

    def attn_alloc(self):
        if hasattr(self, "qT"):
            return
        P = self.P
        self.stg = [P.sb("stg%d" % i, [128, 2048]) for i in range(4)]
        self.qT = P.sb("qT", [128, S], BF16)
        self.kT = P.sb("kT", [128, S], BF16)
        self.Vaug = P.sb("Vaug", [128, NT, 65], BF16)
        self.ostage = [P.sb("ostage%d" % i, [128, 4, 64]) for i in range(2)]
        self.osc = P.sb("osc", [128, 16])
        self.otmp = [P.sb("otmp%d" % i, [128, 512]) for i in range(2)]
        self.on = 0

    def diff(self):
        P, I = self.P, self.I
        self.attn_alloc()
        qT, kT, Vaug = self.qT, self.kT, self.Vaug
        self.load_qk(qT, "qT", "qd", "qds", rope=True)
        self.load_qk(kT, "kT", "kd", "kds", rope=True)
        self.load_v(Vaug, "Vaug", "vd")
        lam_init = 0.8 - 0.6 * math.exp(-0.3 * self.layer_idx)
        lt = P.sb("lamt", [128, 128]); lp = P.sb("lamp", [128, 64]); ls = P.sb("lams", [128, 2]); nl = P.sb("neglam", [128, 1])
        P.dma(lt[:], I["dlam"].partition_broadcast(128), writes=["lamt"])
        P.op("vector", lambda e: e.tensor_tensor(out=lp[:].rearrange("p (a d) -> p a d", a=2),
                                                 in0=lt[:].rearrange("p (a b d) -> p a b d", a=2, b=2)[:, :, 0, :],
                                                 in1=lt[:].rearrange("p (a b d) -> p a b d", a=2, b=2)[:, :, 1, :], op=ALU.mult),
             reads=["lamt"], writes=["lamp"])
        P.op("vector", lambda e: e.reduce_sum(out=ls[:], in_=lp[:].rearrange("p (a d) -> p a d", a=2), axis=AX.X), reads=["lamp"], writes=["lams"])
        P.op("scalar", lambda e: e.activation(out=ls[:], in_=ls[:], func=AF.Exp), reads=["lams"], writes=["lams"])
        P.op("vector", lambda e: e.tensor_tensor(out=nl[:], in0=ls[:, 1:2], in1=ls[:, 0:1], op=ALU.subtract), reads=["lams"], writes=["neglam"])
        P.op("vector", lambda e: e.tensor_scalar(out=nl[:], in0=nl[:], scalar1=-lam_init, scalar2=None, op0=ALU.add), reads=["neglam"], writes=["neglam"])
        sub = P.sb("dsub", [128, 64])
        P.dma(sub[:], I["dsub"].partition_broadcast(128), writes=["dsub"])
        P.op("vector", lambda e: e.tensor_scalar(out=sub[:], in0=sub[:], scalar1=(1.0 - lam_init), scalar2=None, op0=ALU.mult), reads=["dsub"], writes=["dsub"])
        scale = 32 ** -0.5

        def kq(c, j, i):
            return kT[c * 32:(c + 1) * 32, j * 128:(j + 1) * 128], qT[c * 32:(c + 1) * 32, i * 512:(i + 1) * 512]

        def epi(i, ob):
            osc = self.osc
            o0 = ob[0][:, 0:260].rearrange("p (s d) -> p s d", d=65)
            o1 = ob[1][:, 0:260].rearrange("p (s d) -> p s d", d=65)
            og = self.ostage[self.on % 2]; ogk = "ostage%d" % (self.on % 2); self.on += 1
            P.op("vector", lambda e: e.reciprocal(out=osc[:, 0:4], in_=o0[:, :, 64]), reads=["pb4"], writes=["osc"])
            P.op("vector", lambda e: e.reciprocal(out=osc[:, 4:8], in_=o1[:, :, 64]), reads=["pb5"], writes=["osc"])
            P.op("vector", lambda e: e.tensor_scalar(out=osc[:, 4:8], in0=osc[:, 4:8], scalar1=nl[:, 0:1], scalar2=None, op0=ALU.mult),
                 reads=["osc", "neglam"], writes=["osc"])
            for s in range(4):
                P.op("vector", lambda e, s=s: e.tensor_scalar(out=og[:, s, :], in0=o0[:, s, 0:64], scalar1=osc[:, s:s + 1], scalar2=None, op0=ALU.mult),
                     reads=["pb4", "osc"], writes=[ogk])
                P.op("vector", lambda e, s=s: e.scalar_tensor_tensor(out=og[:, s, :], in0=o1[:, s, 0:64], scalar=osc[:, 4 + s:5 + s], in1=og[:, s, :],
                                                                      op0=ALU.mult, op1=ALU.add), reads=["pb5", "osc", ogk], writes=[ogk])
                P.op("scalar", lambda e, s=s: e.activation(out=self.stg[3][:, 0:64], in_=og[:, s, :], func=AF.Square, accum_out=osc[:, 8 + s:9 + s]),
                     reads=[ogk], writes=["stg3", "osc"])
            P.op("scalar", lambda e: e.activation(out=osc[:, 8:12], in_=osc[:, 8:12], func=AF.Sqrt, scale=1.0 / 64, bias=1e-5), reads=["osc"], writes=["osc"])
            P.op("vector", lambda e: e.reciprocal(out=osc[:, 8:12], in_=osc[:, 8:12]), reads=["osc"], writes=["osc"])
            for s in range(4):
                P.op("vector", lambda e, s=s: e.scalar_tensor_tensor(out=og[:, s, :], in0=og[:, s, :], scalar=osc[:, 8 + s:9 + s], in1=sub[:],
                                                                      op0=ALU.mult, op1=ALU.mult), reads=[ogk, "osc", "dsub"], writes=[ogk])
            P.dma(self.Ybr(1)[i * 512:(i + 1) * 512, :].rearrange("(s p) d -> p s d", p=128), og[:], reads=[ogk], writes=["Y1"], q="sync")

        self.attn_loop("diff", 2, kq, lambda j: None, scale, Vaug, "Vaug", ["qT", "kT"], epi)

    def fox(self):
        P, I = self.P, self.I
        self.attn_alloc()
        qT, kT, Vaug = self.qT, self.kT, self.Vaug
        scale = 64 ** -0.5
        self.load_qk(qT, "qT", "qf", extra_scale=scale)
        self.load_qk(kT, "kT", "kf")
        self.load_v(Vaug, "Vaug", "vf")
        o, _ = self.names["fl"]
        W = 2048
        z = self.stg[0]; t1 = self.stg[1]; crow = self.stg[2]; tmp = self.stg[3]
        one = P.sb("fone", [1, W]); fb = P.sb("ffb", [1, 1]); cp = P.sb("fcp", [1, 3, W], BF16)
        carry = P.sb("fcarry", [1, 1]); onesb = P.sb("fonesb", [1, W], BF16)
        negc = P.sb("fnegc", [128, NT]); one1 = P.sb("fone1", [1, 1])
        P.dma(fb[:], I["fbias"], writes=["ffb"])
        P.op("gpsimd", lambda e: e.memset(one[:], 1.0), writes=["fone"])
        P.op("gpsimd", lambda e: e.memset(one1[:], -1.0), writes=["fone1"])
        P.op("gpsimd", lambda e: e.memset(carry[:], 0.0), writes=["fcarry"])
        P.op("vector", lambda e: e.tensor_copy(out=onesb[:], in_=one[:]), reads=["fone"], writes=["fonesb"])
        pc = self.pb[6]
        for ch in range(S // W):
            zz = z[0:1, :]; tt = t1[0:1, :]; cc = crow[0:1, :]; mm = tmp[0:1, :]
            P.dma(zz, self.UT[o:o + 1, ch * W:(ch + 1) * W], reads=["UT"], writes=["stg0"])
            P.op("vector", lambda e, zz=zz: e.tensor_scalar(out=zz, in0=zz, scalar1=fb[:, 0:1], scalar2=None, op0=ALU.add), reads=["stg0", "ffb"], writes=["stg0"])
            P.op("scalar", lambda e, zz=zz, tt=tt: e.activation(out=tt, in_=zz, func=AF.Abs), reads=["stg0"], writes=["stg1"])
            P.op("scalar", lambda e, tt=tt: e.activation(out=tt, in_=tt, func=AF.Exp, scale=-1.0), reads=["stg1"], writes=["stg1"])
            P.op("scalar", lambda e, tt=tt: e.activation(out=tt, in_=tt, func=AF.Ln, bias=1.0), reads=["stg1"], writes=["stg1"])
            P.op("vector", lambda e, zz=zz: e.tensor_scalar(out=zz, in0=zz, scalar1=0.0, scalar2=None, op0=ALU.min), reads=["stg0"], writes=["stg0"])
            P.op("vector", lambda e, zz=zz, tt=tt: e.tensor_tensor(out=zz, in0=zz, in1=tt, op=ALU.subtract), reads=["stg0", "stg1"], writes=["stg0"])
            P.op("vector", lambda e, zz=zz, cc=cc: e.tensor_tensor_scan(out=cc, data0=one[:], data1=zz, initial=carry[:, 0:1], op0=ALU.mult, op1=ALU.add),
                 reads=["fone", "stg0", "fcarry"], writes=["stg2"])
            P.op("vector", lambda e, cc=cc: e.tensor_copy(out=carry[:], in_=cc[:, W - 1:W]), reads=["stg2"], writes=["fcarry"])
            P.op("vector", lambda e, cc=cc: e.tensor_copy(out=cp[:, 0, :], in_=cc), reads=["stg2"], writes=["fcp"])
            P.op("vector", lambda e, cc=cc, mm=mm: e.tensor_tensor(out=mm, in0=cc, in1=cp[:, 0, :], op=ALU.subtract), reads=["stg2", "fcp"], writes=["stg3"])
            P.op("vector", lambda e, mm=mm: e.tensor_copy(out=cp[:, 1, :], in_=mm), reads=["stg3"], writes=["fcp"])
            P.op("vector", lambda e, mm=mm: e.tensor_tensor(out=mm, in0=mm, in1=cp[:, 1, :], op=ALU.subtract), reads=["stg3", "fcp"], writes=["stg3"])
            P.op("vector", lambda e, mm=mm: e.tensor_copy(out=cp[:, 2, :], in_=mm), reads=["stg3"], writes=["fcp"])
            for r in range(3):
                P.dma(qT[64 + r:65 + r, ch * W:(ch + 1) * W], cp[:, r, :], reads=["fcp"], writes=["qT"])
                P.dma(kT[64 + r:65 + r, ch * W:(ch + 1) * W], onesb[:], reads=["fonesb"], writes=["kT"])
            for t in range(W // 128):
                tg = ch * (W // 128) + t
                P.op("tensor", lambda e, t=t, tg=tg, cc=cc: e.matmul(pc[:, tg:tg + 1], lhsT=cc[0:1, t * 128:(t + 1) * 128], rhs=one1[0:1, 0:1], start=True, stop=True),
                     reads=["stg2", "fone1"], writes=["pb6"])
        P.op("vector", lambda e: e.tensor_copy(out=negc[:], in_=pc[:, 0:NT]), reads=["pb6"], writes=["fox_bias"])

        def kq(c, j, i):
            return kT[0:67, j * 128:(j + 1) * 128], qT[0:67, i * 512:(i + 1) * 512]

        def epi(i, ob):
            osc = self.osc
            o0 = ob[0][:, 0:260].rearrange("p (s d) -> p s d", d=65)
            og = self.ostage[self.on % 2]; ogk = "ostage%d" % (self.on % 2); self.on += 1
            P.op("vector", lambda e: e.reciprocal(out=osc[:, 0:4], in_=o0[:, :, 64]), reads=["pb4"], writes=["osc"])
            for s in range(4):
                P.op("vector", lambda e, s=s: e.tensor_scalar(out=og[:, s, :], in0=o0[:, s, 0:64], scalar1=osc[:, s:s + 1], scalar2=None, op0=ALU.mult),
                     reads=["pb4", "osc"], writes=[ogk])
            P.dma(self.Ybr(2)[i * 512:(i + 1) * 512, :].rearrange("(s p) d -> p s d", p=128), og[:], reads=[ogk], writes=["Y2"], q="sync")

        self.attn_loop("fox", 1, kq, lambda j: negc[:, j:j + 1], 1.0, Vaug, "Vaug", ["qT", "kT"], epi)


    def chunk_masks(self):
        P = self.P
        self.BTi = P.sb("BTi", [128, 128]); self.BTe = P.sb("BTe", [128, 128]); self.BTeT = P.sb("BTeT", [128, 128])
        self.ones64 = P.sb("ones64", [128, 64])
        self.ms("gpsimd", self.ones64[:], 1.0, ["ones64"])
        for (t, key, op, pat, cm, zb) in ((self.BTi, "BTi", ALU.is_ge, [[1, 128]], -1, (0, 64)),
                                          (self.BTe, "BTe", ALU.is_gt, [[1, 128]], -1, (0, 64)),
                                          (self.BTeT, "BTeT", ALU.is_gt, [[-1, 128]], 1, (64, 0))):
            self.ms("gpsimd", t[:], 1.0, [key])
            P.op("gpsimd", lambda e, t=t, op=op, pat=pat, cm=cm: e.affine_select(out=t[:], in_=t[:], compare_op=op, fill=0.0, base=0,
                                                                              pattern=pat, channel_multiplier=cm), reads=[key], writes=[key])
            self.ms("gpsimd", t[zb[0]:zb[0] + 64, zb[1]:zb[1] + 64], 0.0, [key])

    def nm_alloc(self, pfx):
        P = self.P
        if not hasattr(self, "nmN") or not isinstance(self.nmN, dict):
            self.nmN = {}; self.nmNT = {}; self.nmX = {}; self._nm_result = {}
        self.nmN[pfx] = [P.sb(pfx + "nmN%d" % i, [128, 128]) for i in range(2)]
        self.nmNT[pfx] = [P.sb(pfx + "nmNT%d" % i, [128, 128]) for i in range(2)]
        self.nmX[pfx] = P.sb(pfx + "nmXb", [128, 128])

    def rwkv(self):
        P, I = self.P, self.I
        N = 512
        names = self.names
        rp = P.sb("rp", [128, 32]); w2h = P.sb("w2h", [64, 64]); a2h = P.sb("a2h", [64, 64]); g2h = P.sb("g2h", [128, 64])
        lnw = P.sb("lnw", [128, 64]); lnb = P.sb("lnb", [128, 64])
        P.dma(rp[:, 0:16], I["rp"], writes=["rp"])
        P.dma(w2h[:], I["w2h"], writes=["w2h"]); P.dma(a2h[:], I["a2h"], writes=["a2h"]); P.dma(g2h[:], I["g2h"], writes=["g2h"])
        P.dma(lnw[:], I["rln"][0:1, :].partition_broadcast(128), writes=["lnw"])
        P.dma(lnb[:], I["rln"][1:2, :].partition_broadcast(128), writes=["lnb"])
        self.ts("vector", rp[:, 16:22], rp[:, 0:6], -1.0, ALU.mult, ["rp"], ["rp"], s2=1.0, op1=ALU.add)
        self.ts("vector", rp[:, 22:23], rp[:, 9:10], -1.0, ALU.mult, ["rp"], ["rp"], s2=1.0, op1=ALU.add)
        MU = {"rr": 0, "rk": 1, "rv": 2, "xw": 3, "xa": 4, "xg": 5}
        self.nm_alloc("rw_")
        inb = {nm: [P.sb("rin_%s%d" % (nm, i), [128 if nm == "xg" else 64, N + 1]) for i in range(2)] for nm in MU}
        def T64(nm):
            return P.sb("rw_" + nm, [64, N])
        r = T64("r"); k0 = T64("k0"); v = T64("v"); tw = T64("tw"); xa = T64("xa"); xg = P.sb("rw_xg", [128, N])
        ld = T64("ld"); a = T64("a"); kkr = T64("kkr"); kk = T64("kk"); k = T64("k"); al = T64("al")
        PI = T64("PI"); PE = T64("PE"); PV = T64("PV"); rt = T64("rt"); bt = T64("bt"); at = T64("at"); kt = T64("kt")
        tmp = T64("tmp"); prod = T64("prod")
        tok = P.sb("rw_tok", [128, 5, 64])
        AT = P.sb("rw_AT", [128, 4, 128])
        A0 = P.sb("rw_nmA", [128, 128])
        X0 = P.sb("rw_nmXa", [128, 128])
        RT = P.sb("rw_RT", [64, 128]); MT = P.sb("rw_MT", [64, 64]); H = P.sb("rw_H", [64, 64])
        Tst = [P.sb("rw_T%d" % i, [64, 64]) for i in range(2)]
        oo = P.sb("rw_oo", [128, 64]); o2 = P.sb("rw_o2", [128, 64]); sc = P.sb("rw_sc", [128, 8])
        yst = [P.sb("rw_y%d" % i, [128, 64]) for i in range(2)]
        pb = self.pb
        self.ms("vector", Tst[0][:], 0.0, ["rw_T0"])
        ti = 0; yi = 0
        for tb in range(NTB):
            t0 = tb * N
            cur = {}
            for nm in MU:
                o, n = names[nm]
                buf = inb[nm][tb % 2]; key = "rin_%s%d" % (nm, tb % 2)
                if tb == 0:
                    self.ms("gpsimd", buf[0:n, 0:1], 0.0, [key])
                    P.dma(buf[0:n, 1:N + 1], self.UT[o:o + n, 0:N], reads=["UT"], writes=[key])
                else:
                    P.dma(buf[0:n, :], self.UT[o:o + n, t0 - 1:t0 + N], reads=["UT"], writes=[key])
                cur[nm] = (buf, key, n)
            for nm, dst, dk, eng in (("rr", r, "rw_r", "vector"), ("rk", k0, "rw_k0", "scalar"), ("rv", v, "rw_v", "vector"),
                                      ("xw", tw, "rw_tw", "scalar"), ("xa", xa, "rw_xa", "vector"), ("xg", xg, "rw_xg", "scalar")):
                buf, key, n = cur[nm]; c = MU[nm]
                if eng == "scalar":
                    P.op("scalar", lambda e, dst=dst, buf=buf, n=n, c=c: e.mul(out=dst[0:n, :], in_=buf[0:n, 1:N + 1], mul=rp[0:n, 16 + c:17 + c]),
                         reads=[key, "rp"], writes=[dk])
                else:
                    self.ts(eng, dst[0:n, :], buf[0:n, 1:N + 1], rp[0:n, 16 + c:17 + c], ALU.mult, [key, "rp"], [dk])
                self.stt(eng, dst[0:n, :], buf[0:n, 0:N], rp[0:n, c:c + 1], dst[0:n, :], ALU.mult, ALU.add, [key, "rp", dk], [dk])
            yield
            self.act(tw[:], tw[:], AF.Tanh, ["rw_tw"], ["rw_tw"])
            self.mm(pb[0][0:64, :], w2h[:], tw[:], ["w2h", "rw_tw"], ["pb0"])
            self.act(ld[:], pb[0][0:64, :], AF.Sigmoid, ["pb0", "rp"], ["rw_ld"], bias=rp[0:64, 6:7])
            self.ts("vector", ld[:], ld[:], -math.exp(-0.5), ALU.mult, ["rw_ld"], ["rw_ld"])
            self.mm(pb[1][0:64, :], a2h[:], xa[:], ["a2h", "rw_xa"], ["pb1"])
            self.act(a[:], pb[1][0:64, :], AF.Sigmoid, ["pb1", "rp"], ["rw_a"], bias=rp[0:64, 7:8])
            self.act(xg[:], xg[:], AF.Sigmoid, ["rw_xg"], ["rw_xg"])
            yield
            self.ts("vector", kkr[:], k0[:], rp[0:64, 8:9], ALU.mult, ["rw_k0", "rp"], ["rw_kkr"])
            self.tt("vector", tmp[:], kkr[:], kkr[:], ALU.mult, ["rw_kkr"], ["rw_tmp"])
            self.mm(pb[0][0:64, :], self.ones64[0:64, :], tmp[:], ["ones64", "rw_tmp"], ["pb0"])
            self.act(tmp[:], pb[0][0:64, :], AF.Sqrt, ["pb0"], ["rw_tmp"], bias=1e-6)
            P.op("vector", lambda e: e.reciprocal(out=tmp[:], in_=tmp[:]), reads=["rw_tmp"], writes=["rw_tmp"])
            self.tt("vector", kk[:], kkr[:], tmp[:], ALU.mult, ["rw_kkr", "rw_tmp"], ["rw_kk"])
            self.ts("vector", tmp[:], a[:], rp[0:64, 9:10], ALU.mult, ["rw_a", "rp"], ["rw_tmp"], s2=rp[0:64, 22:23], op1=ALU.add)
            self.tt("vector", k[:], k0[:], tmp[:], ALU.mult, ["rw_k0", "rw_tmp"], ["rw_k"])
            self.tt("vector", al[:], kk[:], a[:], ALU.mult, ["rw_kk", "rw_a"], ["rw_al"])
            self.stt("gpsimd", prod[:], r[:], rp[0:64, 10:11], k[:], ALU.mult, ALU.mult, ["rw_r", "rp", "rw_k"], ["rw_prod"])
            yield
            for st in range(4):
                sl = slice(st * 128, (st + 1) * 128)
                self.tr(pb[4][:, 256:320], ld[:, sl], ["rw_ld"], ["pb4"], n=64)
                self.cp("vector", tok[:, 4, :], pb[4][:, 256:320], ["pb4"], ["rw_tok"])
                self.mm(pb[2][0:64, sl], tok[:, 4, :], self.BTi[:], ["rw_tok", "BTi"], ["pb2"])
                self.mm(pb[3][0:64, sl], tok[:, 4, :], self.BTe[:], ["rw_tok", "BTe"], ["pb3"])
            self.act(PI[:], pb[2][0:64, :], AF.Exp, ["pb2"], ["rw_PI"])
            self.act(PV[:], pb[2][0:64, :], AF.Exp, ["pb2"], ["rw_PV"], scale=-1.0)
            self.act(PE[:], pb[3][0:64, :], AF.Exp, ["pb3"], ["rw_PE"])
            self.tt("vector", rt[:], r[:], PI[:], ALU.mult, ["rw_r", "rw_PI"], ["rw_rt"])
            self.stt("gpsimd", bt[:], kk[:], -1.0, PE[:], ALU.mult, ALU.mult, ["rw_kk", "rw_PE"], ["rw_bt"])
            self.tt("vector", at[:], al[:], PV[:], ALU.mult, ["rw_al", "rw_PV"], ["rw_at"])
            self.tt("vector", kt[:], k[:], PV[:], ALU.mult, ["rw_k", "rw_PV"], ["rw_kt"])
            yield
            for st in range(4):
                sl = slice(st * 128, (st + 1) * 128)
                tk0 = t0 + st * 128
                for q, (src, sk) in enumerate(((v, "rw_v"), (at, "rw_at"), (kt, "rw_kt"), (bt, "rw_bt"))):
                    self.tr(pb[4][:, q * 64:(q + 1) * 64], src[:, sl], [sk], ["pb4"], n=64)
                self.cp("scalar", tok[:, 0:4, :], pb[4][:, 0:256].rearrange("p (q d) -> p q d", q=4), ["pb4"], ["rw_tok"])
                self.mm(pb[5][:, 0:128], at[:, sl], bt[:, sl], ["rw_at", "rw_bt"], ["pb5"])
                self.mm(pb[5][:, 128:256], at[:, sl], rt[:, sl], ["rw_at", "rw_rt"], ["pb5"])
                self.mm(pb[5][:, 256:384], kt[:, sl], bt[:, sl], ["rw_kt", "rw_bt"], ["pb5"])
                self.mm(pb[5][:, 384:512], kt[:, sl], rt[:, sl], ["rw_kt", "rw_rt"], ["pb5"])
                self.mm(pb[7][:, 256:384], bt[:, sl], at[:, sl], ["rw_at", "rw_bt"], ["pb7"])
                p5 = pb[5][:].rearrange("p (q t) -> p q t", q=4)
                self.tt("vector", AT[:, 0, :], p5[:, 0, :], self.BTe[:], ALU.mult, ["pb5", "BTe"], ["rw_AT0"])
                self.tt("vector", AT[:, 1, :], p5[:, 1, :], self.BTi[:], ALU.mult, ["pb5", "BTi"], ["rw_AT1"])
                self.tt("vector", AT[:, 2, :], p5[:, 2, :], self.BTe[:], ALU.mult, ["pb5", "BTe"], ["rw_AT2"])
                self.tt("vector", AT[:, 3, :], p5[:, 3, :], self.BTi[:], ALU.mult, ["pb5", "BTi"], ["rw_AT3"])
                self.tt("vector", A0[:], pb[7][:, 256:384], self.BTeT[:], ALU.mult, ["pb7", "BTeT"], ["rw_nmA"])
                yield
                self.mm(pb[7][:, 0:64], AT[:, 2, :], tok[:, 0, :], ["rw_AT2", "rw_tok"], ["pb7"])
                self.cp("vector", X0[:, 0:64], tok[:, 3, :], ["rw_tok"], ["rw_nmXa"])
                self.cp("scalar", X0[:, 64:128], pb[7][:, 0:64], ["pb7"], ["rw_nmXa"])
                yield
                yield from self.neumann_gen("rw_", X0, A0[:], "rw_nmA", AT[:, 0, :], "rw_AT0", pb[7][:, 384:512], "pb7")
                X, Xk = self._nm_result["rw_"]
                self.mm(pb[7][0:64, 64:192], X[:, 0:64], AT[:, 1, :], [Xk, "rw_AT1"], ["pb7"])
                self.tt("vector", RT[:], pb[7][0:64, 64:192], rt[:, sl], ALU.add, ["pb7", "rw_rt"], ["rw_RT"])
                self.mm(pb[0][:, 0:64], AT[:, 1, :], X[:, 64:128], ["rw_AT1", Xk], ["pb0"], start=True, stop=False)
                self.mm(pb[0][:, 0:64], AT[:, 3, :], tok[:, 0, :], ["rw_AT3", "rw_tok"], ["pb0"], start=False, stop=False)
                for c in range(2):
                    cs = slice(c * 64, (c + 1) * 64)
                    Tc = Tst[ti % 2]; Tk = "rw_T%d" % (ti % 2); Tn = Tst[(ti + 1) % 2]; Tnk = "rw_T%d" % ((ti + 1) % 2); ti += 1
                    self.mm(pb[0][cs, 0:64], RT[:, cs], Tc[:], ["rw_RT", Tk], ["pb0"], start=False, stop=True)
                    self.mm(pb[1][0:64, 0:64], X[cs, 0:64], tok[cs, 1, :], [Xk, "rw_tok"], ["pb1"])
                    self.tt("vector", MT[:], pb[1][0:64, 0:64], self.ident[0:64, 0:64], ALU.add, ["pb1", "ident"], ["rw_MT"])
                    self.mm(pb[3][0:64, 0:64], tok[cs, 1, :], X[cs, 64:128], ["rw_tok", Xk], ["pb3"], start=True, stop=False)
                    self.mm(pb[3][0:64, 0:64], tok[cs, 2, :], tok[cs, 0, :], ["rw_tok"], ["pb3"], start=False, stop=True)
                    pc_col = PI[:, st * 128 + c * 64 + 63:st * 128 + c * 64 + 64]
                    self.ts("vector", H[:], pb[3][0:64, 0:64], pc_col, ALU.mult, ["pb3", "rw_PI"], ["rw_H"])
                    self.mm(pb[2][0:64, 0:64], MT[:], Tc[:], ["rw_MT", Tk], ["pb2"])
                    self.stt("vector", Tn[:], pb[2][0:64, 0:64], pc_col, H[:], ALU.mult, ALU.add, ["pb2", "rw_PI", "rw_H"], [Tnk])
                yield
                self.cp("scalar", oo[:], pb[0][:, 0:64], ["pb0"], ["rw_oo"])
                P.op("vector", lambda e: e.reduce_sum(out=sc[:, 0:1], in_=oo[:], axis=AX.X), reads=["rw_oo"], writes=["rw_sc"])
                self.ts("vector", sc[:, 0:1], sc[:, 0:1], -1.0 / 64, ALU.mult, ["rw_sc"], ["rw_sc"])
                self.ts("vector", oo[:], oo[:], sc[:, 0:1], ALU.add, ["rw_oo", "rw_sc"], ["rw_oo"])
                self.act(o2[:], oo[:], AF.Square, ["rw_oo"], ["rw_o2", "rw_sc"], accum_out=sc[:, 1:2])
                self.act(sc[:, 1:2], sc[:, 1:2], AF.Sqrt, ["rw_sc"], ["rw_sc"], scale=1.0 / 64, bias=64e-5)
                P.op("vector", lambda e: e.reciprocal(out=sc[:, 1:2], in_=sc[:, 1:2]), reads=["rw_sc"], writes=["rw_sc"])
                self.stt("vector", oo[:], oo[:], sc[:, 1:2], lnw[:], ALU.mult, ALU.mult, ["rw_oo", "rw_sc", "lnw"], ["rw_oo"])
                self.tt("vector", oo[:], oo[:], lnb[:], ALU.add, ["rw_oo", "lnb"], ["rw_oo"])
                self.mm(pb[1][:, 64:65], prod[:, sl], self.ones64[0:64, 0:1], ["rw_prod", "ones64"], ["pb1"])
                self.mm(pb[1][:, 128:192], xg[:, sl], g2h[:], ["rw_xg", "g2h"], ["pb1"])
                self.cp("scalar", sc[:, 2:3], pb[1][:, 64:65], ["pb1"], ["rw_sc"])
                self.stt("vector", oo[:], tok[:, 0, :], sc[:, 2:3], oo[:], ALU.mult, ALU.add, ["rw_tok", "rw_sc", "rw_oo"], ["rw_oo"])
                y = yst[yi % 2]; yk = "rw_y%d" % (yi % 2); yi += 1
                self.tt("vector", y[:], pb[1][:, 128:192], oo[:], ALU.mult, ["rw_oo", "pb1"], [yk])
                P.dma(self.Ybr(0)[tk0:tk0 + 128, :], y[:], reads=[yk], writes=["Y0"], q="sync")
                yield

    def gdn(self):
        P, I = self.P, self.I
        N = 512
        names = self.names; pb = self.pb
        gp = P.sb("gp", [128, 16]); gnw = P.sb("gnw", [128, 64])
        P.dma(gp[:], I["gp"], writes=["gp"])
        P.dma(gnw[:], I["gnorm"].partition_broadcast(128), writes=["gnw"])
        self.act(gp[:, 10:11], gp[:, 8:9], AF.Exp, ["gp"], ["gp"])
        self.ts("vector", gp[:, 10:11], gp[:, 10:11], -1.0, ALU.mult, ["gp"], ["gp"])
        self.nm_alloc("gd_")
        qin = [P.sb("gd_qin%d" % i, [64, N + 3]) for i in range(2)]
        kvin = [P.sb("gd_kvin%d" % i, [128, N + 3]) for i in range(2)]
        bin_ = [P.sb("gd_bin%d" % i, [128, N]) for i in range(2)]
        ain = [P.sb("gd_ain%d" % i, [128, N]) for i in range(2)]
        q = P.sb("gd_q", [64, N]); kv = P.sb("gd_kv", [128, N]); beta = P.sb("gd_beta", [128, N]); gb_ = P.sb("gd_g", [128, N])
        t1 = P.sb("gd_t1", [128, N]); t2 = P.sb("gd_t2", [128, N])
        gtok = P.sb("gd_gtok", [128, 128]); gam = P.sb("gd_gam", [128, 128]); egam = P.sb("gd_egam", [128, 128]); gamt = P.sb("gd_gamt", [128, 1])
        BT = P.sb("gd_BT", [128, 128]); kb = P.sb("gd_kb", [64, 128]); qe = P.sb("gd_qe", [64, 128])
        DT = P.sb("gd_DT", [128, 128]); Dm = P.sb("gd_D", [128, 128])
        NT = P.sb("gd_NT", [128, 128]); Nm = P.sb("gd_N", [128, 128]); QK = P.sb("gd_QK", [128, 128])
        X0 = P.sb("gd_nmXa", [128, 128])
        ek = P.sb("gd_ek", [64, 128]); KhT = P.sb("gd_KhT", [64, 128]); Kh = P.sb("gd_Kh", [128, 64])
        RT = P.sb("gd_RT", [64, 128]); MT = P.sb("gd_MT", [64, 64]); H = P.sb("gd_H", [64, 64])
        Tst = [P.sb("gd_T%d" % i, [64, 64]) for i in range(2)]
        oo = P.sb("gd_oo", [128, 64]); o2 = P.sb("gd_o2", [128, 64]); sc = P.sb("gd_sc", [128, 4])
        gate = [P.sb("gd_gate%d" % i, [128, 64]) for i in range(2)]
        yst = [P.sb("gd_y%d" % i, [128, 64]) for i in range(2)]
        self.ms("vector", Tst[0][:], 0.0, ["gd_T0"])
        ti = 0; yi = 0
        oq, _ = names["gq"]; ok_, _ = names["gk"]; ov_, _ = names["gv"]; ob_, _ = names["gb"]; oa_, _ = names["ga"]
        ogg, _ = self.tnames["ggt"]
        for tb in range(NTB):
            t0 = tb * N
            qi = qin[tb % 2]; qk_ = "gd_qin%d" % (tb % 2); kvi = kvin[tb % 2]; kvk = "gd_kvin%d" % (tb % 2)
            bi = bin_[tb % 2]; bk = "gd_bin%d" % (tb % 2); ai = ain[tb % 2]; ak = "gd_ain%d" % (tb % 2)
            if tb == 0:
                self.ms("gpsimd", qi[:, 0:3], 0.0, [qk_]); self.ms("gpsimd", kvi[:, 0:3], 0.0, [kvk])
                P.dma(qi[:, 3:N + 3], self.UT[oq:oq + 64, 0:N], reads=["UT"], writes=[qk_])
                P.dma(kvi[0:64, 3:N + 3], self.UT[ok_:ok_ + 64, 0:N], reads=["UT"], writes=[kvk])
                P.dma(kvi[64:128, 3:N + 3], self.UT[ov_:ov_ + 64, 0:N], reads=["UT"], writes=[kvk])
            else:
                P.dma(qi[:, :], self.UT[oq:oq + 64, t0 - 3:t0 + N], reads=["UT"], writes=[qk_])
                P.dma(kvi[0:64, :], self.UT[ok_:ok_ + 64, t0 - 3:t0 + N], reads=["UT"], writes=[kvk])
                P.dma(kvi[64:128, :], self.UT[ov_:ov_ + 64, t0 - 3:t0 + N], reads=["UT"], writes=[kvk])
            P.dma(bi[:], self.UT[ob_:ob_ + 128, t0:t0 + N], reads=["UT"], writes=[bk])
            P.dma(ai[:], self.UT[oa_:oa_ + 128, t0:t0 + N], reads=["UT"], writes=[ak])
            for (src, sk, dst, dk, n, wc0) in ((qi, qk_, q, "gd_q", 64, 0), (kvi, kvk, kv, "gd_kv", 128, 4)):
                self.ts("vector", dst[0:n, :], src[0:n, 3:N + 3], gp[0:n, wc0 + 3:wc0 + 4], ALU.mult, [sk, "gp"], [dk])
                for j in range(3):
                    self.stt("vector", dst[0:n, :], src[0:n, j:N + j], gp[0:n, wc0 + j:wc0 + j + 1], dst[0:n, :], ALU.mult, ALU.add, [sk, "gp", dk], [dk])
                self.act(dst[0:n, :], dst[0:n, :], AF.Silu, [dk], [dk])
            yield
            for (dst, dk, mul) in ((q, "gd_q", 64 ** -0.5), (kv, "gd_kv", 1.0)):
                self.tt("vector", t1[0:64, :], dst[0:64, :], dst[0:64, :], ALU.mult, [dk], ["gd_t1"])
                self.mm(pb[1][0:64, :], self.ones64[0:64, :], t1[0:64, :], ["ones64", "gd_t1"], ["pb1"])
                self.act(t1[0:64, :], pb[1][0:64, :], AF.Sqrt, ["pb1"], ["gd_t1"], bias=1e-6)
                P.op("vector", lambda e: e.reciprocal(out=t1[0:64, :], in_=t1[0:64, :]), reads=["gd_t1"], writes=["gd_t1"])
                self.stt("vector", dst[0:64, :], dst[0:64, :], mul, t1[0:64, :], ALU.mult, ALU.mult, [dk, "gd_t1"], [dk])
            yield
            self.act(beta[:], bi[:], AF.Sigmoid, [bk], ["gd_beta"])
            self.ts("vector", t1[:], ai[:], gp[:, 9:10], ALU.add, [ak, "gp"], ["gd_t1"])
            self.act(t2[:], t1[:], AF.Abs, ["gd_t1"], ["gd_t2"])
            self.act(t2[:], t2[:], AF.Exp, ["gd_t2"], ["gd_t2"], scale=-1.0)
            self.act(t2[:], t2[:], AF.Ln, ["gd_t2"], ["gd_t2"], bias=1.0)
            self.ts("vector", t1[:], t1[:], 0.0, ALU.max, ["gd_t1"], ["gd_t1"])
            self.tt("vector", t1[:], t1[:], t2[:], ALU.add, ["gd_t1", "gd_t2"], ["gd_t1"])
            self.ts("vector", gb_[:], t1[:], gp[:, 10:11], ALU.mult, ["gd_t1", "gp"], ["gd_g"])
            for st in range(4):
                sl = slice(st * 128, (st + 1) * 128)
                tk0 = t0 + st * 128
                gt = gate[yi % 2]; gtk = "gd_gate%d" % (yi % 2)
                P.dma(gt[:], self.UV[tk0:tk0 + 128, ogg:ogg + 64], reads=["UV"], writes=[gtk])
                self.act(gt[:], gt[:], AF.Silu, [gtk], [gtk])
                self.tr(pb[4][:, 0:128], gb_[:, sl], ["gd_g"], ["pb4"], n=128)
                self.cp("vector", gtok[:], pb[4][:, 0:128], ["pb4"], ["gd_gtok"])
                self.mm(pb[4][:, 128:256], gtok[:], self.BTi[:], ["gd_gtok", "BTi"], ["pb4"])
                self.mm(pb[4][:, 256:257], self.BTi[:], gtok[:, 0:1], ["gd_gtok", "BTi"], ["pb4"])
                self.cp("vector", gam[:], pb[4][:, 128:256], ["pb4"], ["gd_gam"])
                self.cp("vector", gamt[:], pb[4][:, 256:257], ["pb4"], ["gd_gamt"])
                self.act(egam[:], pb[4][:, 128:256], AF.Exp, ["pb4"], ["gd_egam"])
                yield
                self.tt("vector", kb[:], kv[0:64, sl], beta[0:64, sl], ALU.mult, ["gd_kv", "gd_beta"], ["gd_kb"])
                self.tt("vector", BT[0:64, :], kb[:], egam[0:64, :], ALU.mult, ["gd_kb", "gd_egam"], ["gd_BT"])
                self.tt("vector", BT[64:128, :], kv[64:128, sl], beta[64:128, sl], ALU.mult, ["gd_kv", "gd_beta"], ["gd_BT"])
                self.tt("vector", qe[:], q[:, sl], egam[0:64, :], ALU.mult, ["gd_q", "gd_egam"], ["gd_qe"])
                self.ts("vector", DT[:], gam[:], gamt[:, 0:1], ALU.subtract, ["gd_gam", "gd_gamt"], ["gd_DT"], s2=0.0, op1=ALU.min)
                self.act(DT[:], DT[:], AF.Exp, ["gd_DT"], ["gd_DT"])
                self.ts("vector", Dm[:], gam[:], gamt[:, 0:1], ALU.subtract, ["gd_gam", "gd_gamt"], ["gd_D"], s2=0.0, op1=ALU.max)
                self.act(Dm[:], Dm[:], AF.Exp, ["gd_D"], ["gd_D"], scale=-1.0)
                yield
                self.mm(pb[5][:, 0:128], kv[0:64, sl], kb[:], ["gd_kv", "gd_kb"], ["pb5"])
                self.mm(pb[5][:, 128:256], kv[0:64, sl], q[:, sl], ["gd_kv", "gd_q"], ["pb5"])
                self.mm(pb[5][:, 256:384], kb[:], kv[0:64, sl], ["gd_kv", "gd_kb"], ["pb5"])
                self.stt("vector", NT[:], pb[5][:, 0:128], -1.0, DT[:], ALU.mult, ALU.mult, ["pb5", "gd_DT"], ["gd_NT"])
                self.tt("vector", NT[:], NT[:], self.BTe[:], ALU.mult, ["gd_NT", "BTe"], ["gd_NT"])
                self.tt("vector", QK[:], pb[5][:, 128:256], DT[:], ALU.mult, ["pb5", "gd_DT"], ["gd_QK"])
                self.tt("vector", QK[:], QK[:], self.BTi[:], ALU.mult, ["gd_QK", "BTi"], ["gd_QK"])
                self.stt("vector", Nm[:], pb[5][:, 256:384], -1.0, Dm[:], ALU.mult, ALU.mult, ["pb5", "gd_D"], ["gd_N"])
                self.tt("vector", Nm[:], Nm[:], self.BTeT[:], ALU.mult, ["gd_N", "BTeT"], ["gd_N"])
                yield
                self.tr(pb[7][:, 0:128], BT[:], ["gd_BT"], ["pb7"], n=128)
                self.cp("scalar", X0[:], pb[7][:, 0:128], ["pb7"], ["gd_nmXa"])
                yield
                yield from self.neumann_gen("gd_", X0, Nm[:], "gd_N", NT[:], "gd_NT", pb[6][:, 128:256], "pb6")
                X, Xk = self._nm_result["gd_"]
                self.mm(pb[7][0:64, 128:256], X[:, 0:64], QK[:], [Xk, "gd_QK"], ["pb7"])
                self.stt("vector", RT[:], pb[7][0:64, 128:256], -1.0, qe[:], ALU.mult, ALU.add, ["pb7", "gd_qe"], ["gd_RT"])
                yield
                for c in range(2):
                    cs = slice(c * 64, (c + 1) * 64)
                    self.act(ek[:, cs], gam[0:64, cs], AF.Exp, ["gd_gam"], ["gd_ek"], scale=-1.0, bias=gam[0:64, c * 64 + 63:c * 64 + 64])
                self.tt("vector", KhT[:], kv[0:64, sl], ek[:], ALU.mult, ["gd_kv", "gd_ek"], ["gd_KhT"])
                self.tr(pb[7][:, 256:320], KhT[:], ["gd_KhT"], ["pb7"], n=64)
                self.cp("scalar", Kh[:], pb[7][:, 256:320], ["pb7"], ["gd_Kh"])
                self.mm(pb[6][:, 0:64], QK[:], X[:, 64:128], ["gd_QK", Xk], ["pb6"], start=True, stop=False)
                for c in range(2):
                    cs = slice(c * 64, (c + 1) * 64)
                    Tc = Tst[ti % 2]; Tk = "gd_T%d" % (ti % 2); Tn = Tst[(ti + 1) % 2]; Tnk = "gd_T%d" % ((ti + 1) % 2); ti += 1
                    self.mm(pb[6][cs, 0:64], RT[:, cs], Tc[:], ["gd_RT", Tk], ["pb6"], start=False, stop=True)
                    self.mm(pb[1][0:64, 0:64], X[cs, 0:64], Kh[cs, :], [Xk, "gd_Kh"], ["pb1"])
                    egl = egam[0:64, c * 64 + 63:c * 64 + 64]
                    self.stt("vector", MT[:], self.ident[0:64, 0:64], egl, pb[1][0:64, 0:64], ALU.mult, ALU.subtract, ["ident", "gd_egam", "pb1"], ["gd_MT"])
                    self.mm(pb[3][0:64, 0:64], Kh[cs, :], X[cs, 64:128], ["gd_Kh", Xk], ["pb3"])
                    self.cp("scalar", H[:], pb[3][0:64, 0:64], ["pb3"], ["gd_H"])
                    self.mm(pb[2][0:64, 0:64], MT[:], Tc[:], ["gd_MT", Tk], ["pb2"])
                    self.tt("vector", Tn[:], pb[2][0:64, 0:64], H[:], ALU.add, ["pb2", "gd_H"], [Tnk])
                yield
                self.cp("scalar", oo[:], pb[6][:, 0:64], ["pb6"], ["gd_oo"])
                self.act(o2[:], oo[:], AF.Square, ["gd_oo"], ["gd_o2", "gd_sc"], accum_out=sc[:, 0:1])
                self.act(sc[:, 0:1], sc[:, 0:1], AF.Sqrt, ["gd_sc"], ["gd_sc"], scale=1.0 / 64, bias=1e-6)
                P.op("vector", lambda e: e.reciprocal(out=sc[:, 0:1], in_=sc[:, 0:1]), reads=["gd_sc"], writes=["gd_sc"])
                self.stt("vector", oo[:], oo[:], sc[:, 0:1], gnw[:], ALU.mult, ALU.mult, ["gd_oo", "gd_sc", "gnw"], ["gd_oo"])
                y = yst[yi % 2]; yk = "gd_y%d" % (yi % 2); yi += 1
                self.tt("vector", y[:], oo[:], gt[:], ALU.mult, ["gd_oo", gtk], [yk])
                P.dma(self.Ybr(3)[tk0:tk0 + 128, :], y[:], reads=[yk], writes=["Y3"], q="sync")
                yield

    def neumann_gen(self, pfx, X0, n0, n0k, n0t, n0tk, pX, pXk):
        Ns = self.nmN[pfx]; NTs = self.nmNT[pfx]; Xs = [X0, self.nmX[pfx]]
        Nk = [pfx + "nmN0", pfx + "nmN1"]; NTk = [pfx + "nmNT0", pfx + "nmNT1"]; Xk = [pfx + "nmXa", pfx + "nmXb"]
        pT = self.pb[2]; pN = self.pb[3]
        curN, curNT, curNk, curNTk = n0, n0t, n0k, n0tk
        xi = 0
        nr = 6
        for i in range(nr):
            self.mm(pX, curNT, Xs[xi][:], [curNTk, Xk[xi]], [pXk])
            self.tt("vector", Xs[1 - xi][:], pX, Xs[xi][:], ALU.add, [Xk[xi], pXk], [Xk[1 - xi]])
            xi = 1 - xi
            if i < nr - 1:
                self.mm(pT[:, 0:128], curN, curNT, [curNk, curNTk], ["pb2"])
                self.mm(pN[:, 0:128], curNT, curN, [curNk, curNTk], ["pb3"])
                self.cp("scalar", NTs[i % 2][:], pT[:, 0:128], ["pb2"], [NTk[i % 2]])
                self.cp("vector", Ns[i % 2][:], pN[:, 0:128], ["pb3"], [Nk[i % 2]])
                curN, curNT, curNk, curNTk = Ns[i % 2][:], NTs[i % 2][:], Nk[i % 2], NTk[i % 2]
            yield
        self._nm_result[pfx] = (Xs[xi], Xk[xi])


def make_inputs(inp, layer, core):
    b, h = core // 4, core % 4
    names, cidx, tnames, tidx = colsel(h)
    w = inp["w_in"][layer]
    d = {
        "wc": np.ascontiguousarray(w[:, cidx]),
        "wv": np.ascontiguousarray(w[:, tidx]),
        "gm": np.ascontiguousarray(inp["norm_mix"][layer].reshape(8, 128).T),
        "rope": rope_tables(),
        "dlam": np.ascontiguousarray(inp["diff_lam"][layer].reshape(1, 128)),
        "dsub": np.ascontiguousarray(inp["diff_subln"][layer].reshape(1, 64)),
        "fbias": np.ascontiguousarray(inp["fox_fbias"][layer][h].reshape(1, 1)),
    }
    hs = slice(h * 64, (h + 1) * 64)
    rp = np.zeros((128, 16), np.float32)
    mu = inp["rwkv_mu"][layer]
    rp[0:64, 0] = mu[0 + h * 64:0 + h * 64 + 64]; rp[0:64, 1] = mu[256 + h * 64:256 + h * 64 + 64]; rp[0:64, 2] = mu[512 + h * 64:512 + h * 64 + 64]
    rp[0:64, 3] = mu[768:832]; rp[0:64, 4] = mu[832:896]; rp[0:128, 5] = mu[896:1024]
    rp[0:64, 6] = inp["rwkv_w0"][layer][hs]; rp[0:64, 7] = inp["rwkv_a0"][layer][hs]
    rp[0:64, 8] = inp["rwkv_kk"][layer][hs]; rp[0:64, 9] = inp["rwkv_ka"][layer][hs]; rp[0:64, 10] = inp["rwkv_rk"][layer][h]
    d["rp"] = rp
    d["w2h"] = np.ascontiguousarray(inp["rwkv_w2"][layer][:, hs]); d["a2h"] = np.ascontiguousarray(inp["rwkv_a2"][layer][:, hs])
    d["g2h"] = np.ascontiguousarray(inp["rwkv_g2"][layer][:, hs])
    d["rln"] = np.ascontiguousarray(np.stack([inp["rwkv_ln_w"][layer][hs], inp["rwkv_ln_b"][layer][hs]]))
    gp = np.zeros((128, 16), np.float32)
    cw = inp["gdn_conv"][layer]
    gp[0:64, 0:4] = cw[h * 64:(h + 1) * 64]; gp[0:64, 4:8] = cw[256 + h * 64:256 + (h + 1) * 64]; gp[64:128, 4:8] = cw[512 + h * 64:512 + (h + 1) * 64]
    gp[:, 8] = inp["gdn_a_log"][layer][h]; gp[:, 9] = inp["gdn_dt_bias"][layer][h]
    d["gp"] = gp; d["gnorm"] = np.ascontiguousarray(inp["gdn_norm"][layer].reshape(1, 64))
    return d


D = 1024


class KB:
    def __init__(self, layer_idx, ntok=2048, tb=1024, moe=False, final=False, F=None, NE=8):
        self.layer_idx = layer_idx; self.ntok = ntok; self.tb = tb; self.moe = moe; self.final = final
        self.F = F if F is not None else (3584 if moe else 2816)
        self.NE = NE if moe else 1
        self.sbw = min(512, tb)

    def tt(self, eng, out, in0, in1, op, r, w):
        self.P.op(eng, lambda e: e.tensor_tensor(out=out, in0=in0, in1=in1, op=op), reads=r, writes=w)

    def ts(self, eng, out, in0, s1, op0, r, w, s2=None, op1=None):
        if op1 is None:
            self.P.op(eng, lambda e: e.tensor_scalar(out=out, in0=in0, scalar1=s1, scalar2=None, op0=op0), reads=r, writes=w)
        else:
            self.P.op(eng, lambda e: e.tensor_scalar(out=out, in0=in0, scalar1=s1, scalar2=s2, op0=op0, op1=op1), reads=r, writes=w)

    def stt(self, out, in0, sc, in1, op0, op1, r, w):
        self.P.op("vector", lambda e: e.scalar_tensor_tensor(out=out, in0=in0, scalar=sc, in1=in1, op0=op0, op1=op1), reads=r, writes=w)

    def act(self, out, in_, func, r, w, **kw):
        self.P.op("scalar", lambda e: e.activation(out=out, in_=in_, func=func, **kw), reads=r, writes=w)

    def cp(self, eng, out, in_, r, w):
        if eng == "scalar":
            self.P.op(eng, lambda e: e.copy(out=out, in_=in_), reads=r, writes=w)
        else:
            self.P.op(eng, lambda e: e.tensor_copy(out=out, in_=in_), reads=r, writes=w)

    def mm(self, out, lhsT, rhs, r, w, start=True, stop=True):
        self.P.op("tensor", lambda e: e.matmul(out, lhsT=lhsT, rhs=rhs, start=start, stop=stop), reads=r, writes=w)

    def tr(self, out, in_, r, w):
        self.P.op("tensor", lambda e: e.transpose(out=out, in_=in_, identity=self.ident[:]), reads=list(r) + ["ident"], writes=w)

    def declare(self, nc, sfx="", fused=False):
        NT_, F, NE = self.ntok, self.F, self.NE
        I = {}
        def inp(name, shape):
            I[name] = nc.dram_tensor(name + sfx, list(shape), F32, kind="ExternalInput").ap()
        if not fused:
            inp("x", [NT_, D]); inp("ysT", [8, 128, NT_])
        inp("pT", [2, 128, NT_])
        inp("wgate", [D, 4 * D]); inp("wbo", [4, 256, D]); inp("wout", [D, D])
        inp("norms", [128, 24])
        if self.final:
            inp("fnorm", [1, D])
        inp("fwg", [NE, D, F]); inp("fwu", [NE, D, F]); inp("fwd", [NE, F, D])
        if self.moe:
            inp("router", [D, 8])
        inp("plegate", [D, D]); inp("pleproj", [256, D])
        self.I = I
        self.fused = fused

    def emit(self, nc, P, pb, ident, xsrc_fn, out_ap, ysrc_fn=None, after_block=None):
        self.nc = nc; self.P = P; self.pb = pb; self.ident = ident
        self.xsrc_fn = xsrc_fn; self.ysrc_fn = ysrc_fn
        self.O = out_ap
        I = self.I
        self.norms = P.sb("norms", [128, 24])
        P.dma(self.norms[:], I["norms"], writes=["norms"])
        if self.final:
            self.fn = P.sb("fnorm", [128, D])
            P.dma(self.fn[:], I["fnorm"].partition_broadcast(128), writes=["fnorm"])
        TBt = self.tb // 128
        self.x = P.sb("x", [128, TBt, D])
        self.hT = P.sb("hT", [128, 8, self.tb], BF16)
        self.h32 = P.sb("h32", [128, 8, 128])
        self.sq = P.sb("sqj", [128, D]); self.ssv = P.sb("ssv", [128, 2])
        self.xn = [P.sb("xn%d" % i, [128, D]) for i in range(2)]
        self.wst = [P.sb("wst%d" % i, [128, 4096]) for i in range(3)]
        self.wbf = [P.sb("wbf%d" % i, [128, 4096], BF16) for i in range(4)]
        self.wi = 0; self.bi = 0; self.ci = 0
        for blk in range(self.ntok // self.tb):
            self.block(blk)
            if after_block is not None:
                after_block(blk)

    def build(self):
        nc = bass.Bass("TRN2", target_bir_lowering=False)
        self.declare(nc)
        I = self.I
        O = nc.dram_tensor("out", [self.ntok, D], F32, kind="ExternalOutput").ap()
        P = Prog(nc, n_chan=16)
        pb = [P.ps("pb%d" % i, [128, 512]) for i in range(8)]
        ident = P.sb("ident", [128, 128])
        P.op("gpsimd", lambda e: e.memset(ident[:], 1.0), writes=["ident"])
        P.op("gpsimd", lambda e: e.affine_select(out=ident[:], in_=ident[:], compare_op=ALU.is_equal,
                                                   fill=0.0, base=0, pattern=[[-1, 128]], channel_multiplier=1),
             reads=["ident"], writes=["ident"])
        P.phase_begin()
        self.emit(nc, P, pb, ident, lambda e, r0, n: I["x"][r0:r0 + n, :], O)
        P.phase_end()
        P.wait_all_dma("sync")
        P.emit()
        return nc

    def load_w(self, dst_view_fn, src_ap_list, nfree):
        P = self.P
        st = self.wst[self.wi % 3]; sk = "wst%d" % (self.wi % 3); self.wi += 1
        bf = self.wbf[self.bi % 4]; bk = "wbf%d" % (self.bi % 4); self.bi += 1
        for (src, view) in src_ap_list:
            P.dma(view(st), src, writes=[sk])
        eng = ("vector", "scalar")[self.ci % 2]; self.ci += 1
        self.cp(eng, bf[:, 0:nfree], st[:, 0:nfree], [sk], [bk])
        return bf, bk

    def norm_to_hT(self, ncol0, want32=None):
        P = self.P
        TBt = self.tb // 128
        for t in range(TBt):
            xn = self.xn[t % 2]; xk = "xn%d" % (t % 2)
            self.act(self.sq[:], self.x[:, t, :], AF.Square, ["x"], ["sqj", "ssv"], accum_out=self.ssv[:, 0:1])
            self.act(self.ssv[:, 0:1], self.ssv[:, 0:1], AF.Sqrt, ["ssv"], ["ssv"], scale=1.0 / D, bias=1e-6)
            P.op("vector", lambda e: e.reciprocal(out=self.ssv[:, 1:2], in_=self.ssv[:, 0:1]), reads=["ssv"], writes=["ssv"])
            self.ts("vector", xn[:], self.x[:, t, :], self.ssv[:, 1:2], ALU.mult, ["x", "ssv"], [xk])
            for half in range(2):
                pt = self.pb[half]; pk = "pb%d" % half
                for c4 in range(4):
                    c = half * 4 + c4
                    self.tr(pt[:, c4 * 128:(c4 + 1) * 128], xn[:, c * 128:(c + 1) * 128], [xk], [pk])
                gsl = self.norms[:, ncol0 + half * 4:ncol0 + half * 4 + 4]
                self.tt("vector", self.hT[:, half * 4:(half + 1) * 4, t * 128:(t + 1) * 128],
                        pt[:].rearrange("p (c t) -> p c t", c=4), gsl.unsqueeze(2).to_broadcast([128, 4, 128]), ALU.mult,
                        [pk, "norms"], ["hT"])
                if want32 is not None and want32 == t:
                    self.tt("vector", self.h32[:, half * 4:(half + 1) * 4, :],
                            pt[:].rearrange("p (c t) -> p c t", c=4), gsl.unsqueeze(2).to_broadcast([128, 4, 128]), ALU.mult,
                            [pk, "norms"], ["h32"])
            if want32 is not None and want32 == "all":
                pass

    def block(self, blk):
        P, I = self.P, self.I
        tb = self.tb; TBt = tb // 128; t0 = blk * tb; sbw = self.sbw; NSB = tb // sbw
        pb = self.pb
        x = self.x
        if blk > 0:
            P.barrier()
        for t in range(TBt):
            P.dma(x[:, t, :], (lambda e, r0=t0 + t * 128: self.xsrc_fn(e, r0, 128)), writes=["x"])
        self.norm_to_hT(0)
        if not hasattr(self, "yT"):
            self.yT = P.sb("yTact", [128, 8, tb], BF16); self.mT = P.sb("mT", [128, 8, tb], BF16)
            self.ystg = P.sb("ystg", [128, tb]); self.sg = [P.sb("sg%d" % i, [128, 512]) for i in range(2)]
            self.macc = P.sb("macc", [128, 512]); self.mtmp = P.sb("mtmp", [128, 512])
        yT, mT = self.yT, self.mT
        if self.ysrc_fn is None:
            for q in range(8):
                P.dma(self.ystg[:], I["ysT"][q, :, t0:t0 + tb], writes=["ystg"])
                self.cp("vector", yT[:, q, :], self.ystg[:], ["ystg"], ["yT"])
        else:
            for t in range(TBt):
                yt = self.xn[t % 2]; ytk = "xn%d" % (t % 2)
                ytv = yt[:].rearrange("p (b h c) -> p b h c", b=4, h=4)
                for r in range(4):
                    if getattr(self, "ysplit", False):
                        P.dma(ytv[:, 1:3, r, :], (lambda e, r=r, r0=t0 + t * 128: self.ysrc_fn(e, "A", r, r0, 128).rearrange("p (b c) -> p b c", b=2)), writes=[ytk])
                        P.dma(ytv[:, 0:4:3, r, :], (lambda e, r=r, r0=t0 + t * 128: self.ysrc_fn(e, "S", r, r0, 128).rearrange("p (b c) -> p b c", b=2)), writes=[ytk])
                    else:
                        P.dma(ytv[:, :, r, :],
                              (lambda e, r=r, r0=t0 + t * 128: self.ysrc_fn(e, r, r0, 128).rearrange("p (b c) -> p b c", b=4)), writes=[ytk])
                for half in range(2):
                    pt = self.pb[half]; pk = "pb%d" % half
                    for c4 in range(4):
                        q = half * 4 + c4
                        self.tr(pt[:, c4 * 128:(c4 + 1) * 128], yt[:, q * 128:(q + 1) * 128], [ytk], [pk])
                    self.cp("vector" if half else "scalar", yT[:, half * 4:(half + 1) * 4, t * 128:(t + 1) * 128],
                            pt[:].rearrange("p (c t) -> p c t", c=4), [pk], ["yT"])
        si = 0
        for j in range(8):
            wg, wgk = self.load_w(None, [(I["wgate"][:, b * 1024 + j * 128:b * 1024 + (j + 1) * 128].rearrange("(c p) n -> p c n", p=128),
                                          (lambda st, b=b: st[:, 0:4096].rearrange("p (c b n) -> p c b n", c=8, b=4)[:, :, b, :])) for b in range(4)], 4096)
            wb, wbk = self.load_w(None, [(I["wbo"][:, :, j * 128:(j + 1) * 128].rearrange("b (c2 p) n -> p (b c2) n", p=128),
                                          (lambda st: st[:, 0:1024].rearrange("p (q n) -> p q n", q=8)))], 1024)
            for sb_ in range(NSB):
                ts_ = slice(sb_ * sbw, (sb_ + 1) * sbw)
                for b in range(4):
                    bg = 2 + 4 * (si % 2); bu = 3 + 4 * (si % 2)
                    pg_, pu_ = pb[bg], pb[bu]; pgk, puk = "pb%d" % bg, "pb%d" % bu
                    for c in range(8):
                        self.mm(pg_[:, 0:sbw], wg[:, c * 512 + b * 128:c * 512 + (b + 1) * 128], self.hT[:, c, ts_], [wgk, "hT"], [pgk],
                                start=(c == 0), stop=(c == 7))
                    for c2 in range(2):
                        self.mm(pu_[:, 0:sbw], wb[:, (b * 2 + c2) * 128:(b * 2 + c2 + 1) * 128], yT[:, b * 2 + c2, ts_], [wbk, "yT"], [puk],
                                start=(c2 == 0), stop=(c2 == 1))
                    sg = self.sg[si % 2]; sgk = "sg%d" % (si % 2); si += 1
                    self.act(sg[:, 0:sbw], pg_[:, 0:sbw], AF.Sigmoid, [pgk], [sgk])
                    if b == 0:
                        self.tt("vector", self.macc[:, 0:sbw], pu_[:, 0:sbw], sg[:, 0:sbw], ALU.mult, [puk, sgk], ["macc"])
                    else:
                        self.tt("vector", self.mtmp[:, 0:sbw], pu_[:, 0:sbw], sg[:, 0:sbw], ALU.mult, [puk, sgk], ["mtmp"])
                        if b < 3:
                            self.tt("vector", self.macc[:, 0:sbw], self.macc[:, 0:sbw], self.mtmp[:, 0:sbw], ALU.add, ["macc", "mtmp"], ["macc"])
                        else:
                            self.tt("vector", mT[:, j, ts_], self.macc[:, 0:sbw], self.mtmp[:, 0:sbw], ALU.add, ["macc", "mtmp"], ["mT"])
        for hc in range(2):
            wo, wok = self.load_w(None, [(I["wout"][:, hc * 512:(hc + 1) * 512].rearrange("(c p) n -> p c n", p=128),
                                          (lambda st: st[:, 0:4096].rearrange("p (c n) -> p c n", c=8)))], 4096)
            for t in range(TBt):
                pz = pb[4 + t % 2]; pzk = "pb%d" % (4 + t % 2)
                for c in range(8):
                    self.mm(pz[:, :], mT[:, c, t * 128:(t + 1) * 128], wo[:, c * 512:(c + 1) * 512], ["mT", wok], [pzk], start=(c == 0), stop=(c == 7))
                self.tt("vector", x[:, t, hc * 512:(hc + 1) * 512], pz[:, :], x[:, t, hc * 512:(hc + 1) * 512], ALU.add, [pzk, "x"], ["x"])
        P.barrier()
        F = self.F; NF = F // 128
        if not hasattr(self, "actT"):
            self.actT = self.yT
            self.gs = [P.sb("gs%d" % i, [128, 512]) for i in range(2)]
            if self.moe:
                self.rt = P.sb("router", [128, 8, 8]); self.gw = P.sb("gatew", [128, TBt, 8])
                self.r1 = P.sb("r1", [128, 8]); self.r2 = P.sb("r2", [128, 8]); self.rm = P.sb("rm", [128, 4])
                self.m1 = P.sb("rmask1", [128, 8]); self.m2 = P.sb("rmask2", [128, 8])
                P.dma(self.rt[:], I["router"].rearrange("(c p) e -> p c e", p=128), writes=["router"])
        actT = self.actT
        if self.moe:
            for t in range(TBt):
                self.norm_to_hT_tile32(t)
                for c in range(8):
                    self.mm(pb[6][:, 0:8], self.h32[:, c, :], self.rt[:, c, :], ["h32", "router"], ["pb6"], start=(c == 0), stop=(c == 7))
                r1, r2, rm, m1, m2, gw = self.r1, self.r2, self.rm, self.m1, self.m2, self.gw
                self.cp("vector", r1[:], pb[6][:, 0:8], ["pb6"], ["r1"])
                P.op("vector", lambda e: e.reduce_max(out=rm[:, 0:1], in_=r1[:], axis=AX.X), reads=["r1"], writes=["rm"])
                self.ts("vector", m1[:], r1[:], rm[:, 0:1], ALU.is_equal, ["r1", "rm"], ["rmask1"])
                self.stt(r2[:], m1[:], -1e30, r1[:], ALU.mult, ALU.add, ["rmask1", "r1"], ["r2"])
                P.op("vector", lambda e: e.reduce_max(out=rm[:, 1:2], in_=r2[:], axis=AX.X), reads=["r2"], writes=["rm"])
                self.ts("vector", m2[:], r2[:], rm[:, 1:2], ALU.is_equal, ["r2", "rm"], ["rmask2"])
                self.tt("vector", rm[:, 2:3], rm[:, 1:2], rm[:, 0:1], ALU.subtract, ["rm"], ["rm"])
                self.act(rm[:, 2:3], rm[:, 2:3], AF.Exp, ["rm"], ["rm"])
                self.ts("vector", rm[:, 2:3], rm[:, 2:3], 1.0, ALU.add, ["rm"], ["rm"])
                P.op("vector", lambda e: e.reciprocal(out=rm[:, 2:3], in_=rm[:, 2:3]), reads=["rm"], writes=["rm"])
                self.ts("vector", rm[:, 3:4], rm[:, 2:3], -1.0, ALU.mult, ["rm"], ["rm"], s2=1.0, op1=ALU.add)
                self.ts("vector", m1[:], m1[:], rm[:, 2:3], ALU.mult, ["rmask1", "rm"], ["rmask1"])
                self.stt(gw[:, t, :], m2[:], rm[:, 3:4], m1[:], ALU.mult, ALU.add, ["rmask2", "rm", "rmask1"], ["gatew"])
        self.norm_to_hT(8)
        gi = 0
        for e in range(self.NE):
            for f0 in range(0, NF, 8):
                nf = min(8, NF - f0)
                for g0 in range(0, nf, 4):
                    ng = min(4, nf - g0)
                    fa = f0 + g0
                    wg_, wgk_ = self.load_w(None, [(I["fwg"][e, :, fa * 128:(fa + ng) * 128].rearrange("(c p) n -> p c n", p=128),
                                                    (lambda st, ng=ng: st[:, 0:4096].rearrange("p (c n) -> p c n", c=8)[:, :, 0:ng * 128]))], 4096)
                    wu_, wuk_ = self.load_w(None, [(I["fwu"][e, :, fa * 128:(fa + ng) * 128].rearrange("(c p) n -> p c n", p=128),
                                                    (lambda st, ng=ng: st[:, 0:4096].rearrange("p (c n) -> p c n", c=8)[:, :, 0:ng * 128]))], 4096)
                    for ii in range(ng):
                        i = g0 + ii
                        for sb_ in range(NSB):
                            ts_ = slice(sb_ * sbw, (sb_ + 1) * sbw)
                            bg = 2 + 4 * (gi % 2); bu = 3 + 4 * (gi % 2)
                            pg_, pu_ = pb[bg], pb[bu]; pgk, puk = "pb%d" % bg, "pb%d" % bu
                            for c in range(8):
                                self.mm(pg_[:, 0:sbw], wg_[:, c * 512 + ii * 128:c * 512 + (ii + 1) * 128], self.hT[:, c, ts_], [wgk_, "hT"], [pgk], start=(c == 0), stop=(c == 7))
                            for c in range(8):
                                self.mm(pu_[:, 0:sbw], wu_[:, c * 512 + ii * 128:c * 512 + (ii + 1) * 128], self.hT[:, c, ts_], [wuk_, "hT"], [puk], start=(c == 0), stop=(c == 7))
                            gs = self.gs[gi % 2]; gsk = "gs%d" % (gi % 2); gi += 1
                            self.act(gs[:, 0:sbw], pg_[:, 0:sbw], AF.Silu, [pgk], [gsk])
                            self.tt("vector", actT[:, i, ts_], pu_[:, 0:sbw], gs[:, 0:sbw], ALU.mult, [puk, gsk], ["actT"])
                for hc in range(2):
                    wd, wdk = self.load_w(None, [(I["fwd"][e, f0 * 128:(f0 + nf) * 128, hc * 512:(hc + 1) * 512].rearrange("(i p) n -> p i n", p=128),
                                                  (lambda st, nf=nf: st[:, 0:nf * 512].rearrange("p (i n) -> p i n", n=512)))], nf * 512)
                    for t in range(TBt):
                        pz = pb[4 + t % 2]; pzk = "pb%d" % (4 + t % 2)
                        for i in range(nf):
                            self.mm(pz[:, :], actT[:, i, t * 128:(t + 1) * 128], wd[:, i * 512:(i + 1) * 512], ["actT", wdk], [pzk],
                                    start=(i == 0), stop=(i == nf - 1))
                        xs = x[:, t, hc * 512:(hc + 1) * 512]
                        if self.moe:
                            self.stt(xs, pz[:, :], self.gw[:, t, e:e + 1], xs, ALU.mult, ALU.add, [pzk, "gatew", "x"], ["x"])
                        else:
                            self.tt("vector", xs, pz[:, :], xs, ALU.add, [pzk, "x"], ["x"])
        self.norm_to_hT(16)
        if not hasattr(self, "pTt"):
            self.pTt = P.sb("pTt", [128, 2, tb], BF16)
        for c2 in range(2):
            P.dma(self.ystg[:], I["pT"][c2, :, t0:t0 + tb], writes=["ystg"])
            self.cp("vector", self.pTt[:, c2, :], self.ystg[:], ["ystg"], ["pTt"])
        for hc in range(2):
            wpg, wpgk = self.load_w(None, [(I["plegate"][:, hc * 512:(hc + 1) * 512].rearrange("(c p) n -> p c n", p=128),
                                            (lambda st: st[:, 0:4096].rearrange("p (c n) -> p c n", c=8)))], 4096)
            wpp, wppk = self.load_w(None, [(I["pleproj"][:, hc * 512:(hc + 1) * 512].rearrange("(c2 p) n -> p c2 n", p=128),
                                            (lambda st: st[:, 0:1024].rearrange("p (c2 n) -> p c2 n", c2=2)))], 1024)
            for t in range(TBt):
                for c in range(8):
                    self.mm(pb[2][:, :], self.hT[:, c, t * 128:(t + 1) * 128], wpg[:, c * 512:(c + 1) * 512], ["hT", wpgk], ["pb2"], start=(c == 0), stop=(c == 7))
                for c2 in range(2):
                    self.mm(pb[3][:, :], self.pTt[:, c2, t * 128:(t + 1) * 128], wpp[:, c2 * 512:(c2 + 1) * 512], ["pTt", wppk], ["pb3"], start=(c2 == 0), stop=(c2 == 1))
                gs = self.gs[gi % 2]; gsk = "gs%d" % (gi % 2); gi += 1
                self.act(gs[:], pb[2][:, :], AF.Sigmoid, ["pb2"], [gsk])
                self.tt("vector", gs[:], pb[3][:, :], gs[:], ALU.mult, ["pb3", gsk], [gsk])
                xs = x[:, t, hc * 512:(hc + 1) * 512]
                self.tt("vector", xs, xs, gs[:], ALU.add, ["x", gsk], ["x"])
        for t in range(TBt):
            if self.final:
                xn = self.xn[t % 2]; xk = "xn%d" % (t % 2)
                self.act(self.sq[:], x[:, t, :], AF.Square, ["x"], ["sqj", "ssv"], accum_out=self.ssv[:, 0:1])
                self.act(self.ssv[:, 0:1], self.ssv[:, 0:1], AF.Sqrt, ["ssv"], ["ssv"], scale=1.0 / D, bias=1e-6)
                P.op("vector", lambda e: e.reciprocal(out=self.ssv[:, 1:2], in_=self.ssv[:, 0:1]), reads=["ssv"], writes=["ssv"])
                self.stt(xn[:], x[:, t, :], self.ssv[:, 1:2], self.fn[:], ALU.mult, ALU.mult, ["x", "ssv", "fnorm"], [xk])
                P.dma(self.O[t0 + t * 128:t0 + (t + 1) * 128, :], xn[:], reads=[xk], writes=["O"])
            else:
                P.dma(self.O[t0 + t * 128:t0 + (t + 1) * 128, :], x[:, t, :], reads=["x"], writes=["O"])

    def norm_to_hT_tile32(self, t):
        P = self.P
        xn = self.xn[t % 2]; xk = "xn%d" % (t % 2)
        self.act(self.sq[:], self.x[:, t, :], AF.Square, ["x"], ["sqj", "ssv"], accum_out=self.ssv[:, 0:1])
        self.act(self.ssv[:, 0:1], self.ssv[:, 0:1], AF.Sqrt, ["ssv"], ["ssv"], scale=1.0 / D, bias=1e-6)
        P.op("vector", lambda e: e.reciprocal(out=self.ssv[:, 1:2], in_=self.ssv[:, 0:1]), reads=["ssv"], writes=["ssv"])
        self.ts("vector", xn[:], self.x[:, t, :], self.ssv[:, 1:2], ALU.mult, ["x", "ssv"], [xk])
        for half in range(2):
            pt = self.pb[half]; pk = "pb%d" % half
            for c4 in range(4):
                c = half * 4 + c4
                self.tr(pt[:, c4 * 128:(c4 + 1) * 128], xn[:, c * 128:(c + 1) * 128], [xk], [pk])
            gsl = self.norms[:, 8 + half * 4:8 + half * 4 + 4]
            self.tt("vector", self.h32[:, half * 4:(half + 1) * 4, :],
                    pt[:].rearrange("p (c t) -> p c t", c=4), gsl.unsqueeze(2).to_broadcast([128, 4, 128]), ALU.mult,
                    [pk, "norms"], ["h32"])


def make_inputs_b(inp, layer, core, ys_full, x_full, ntok=2048, final=None, fused=False, sfx=""):
    sl = slice(core * ntok, (core + 1) * ntok)
    if not fused:
        xs = x_full.reshape(-1, D)[sl]
        ys = ys_full.reshape(-1, 4, 2, 128)[sl]
        ysT = np.ascontiguousarray(ys.transpose(1, 2, 3, 0).reshape(8, 128, ntok))
    p = inp["p"][layer].reshape(-1, 2, 128)[sl]
    pT = np.ascontiguousarray(p.transpose(1, 2, 0))
    moe = (layer % 2 == 1); j = layer // 2
    norms = np.concatenate([inp["norm_mix"][layer].reshape(8, 128).T, inp["norm_ffn"][layer].reshape(8, 128).T,
                            inp["norm_ple"][layer].reshape(8, 128).T], axis=1)
    if final is None:
        final = (layer == 1)
    d = {
        "pT": pT,
        "wgate": np.ascontiguousarray(inp["w_in"][layer][:, 3596:7692]), "wbo": inp["w_bo"][layer], "wout": inp["w_out"][layer],
        "norms": np.ascontiguousarray(norms.astype(np.float32)),
        "plegate": inp["ple_gate"][layer], "pleproj": inp["ple_proj"][layer],
    }
    if not fused:
        d["x"] = np.ascontiguousarray(xs); d["ysT"] = ysT
    if final:
        d["fnorm"] = inp["final_norm"].reshape(1, D)
    if moe:
        d["fwg"] = inp["moe_w_gate"][j]; d["fwu"] = inp["moe_w_up"][j]; d["fwd"] = inp["moe_w_down"][j]; d["router"] = inp["moe_router"][j]
    else:
        d["fwg"] = inp["ffn_w_gate"][j][None]; d["fwu"] = inp["ffn_w_up"][j][None]; d["fwd"] = inp["ffn_w_down"][j][None]
    return {k + sfx: v for k, v in d.items()}


def build_fused(do=("diff", "fox", "rwkv", "gdn"), nlayers=2):
    S_ = S
    nc = bass.Bass("TRN2", target_bir_lowering=False)
    ntok = S_ // 4; tb = min(1024, ntok)
    CRY = S_ // 4
    CRX = max(128, ntok // 8)
    NCHX = ntok // CRX
    G16 = CRY // 16
    kas = [KA(l, do=do) for l in range(nlayers)]
    kbs = [KB(l, ntok=ntok, tb=tb, moe=(l % 2 == 1), final=(l == 1)) for l in range(nlayers)]
    x_in = nc.dram_tensor("x", [S_, D], F32, kind="ExternalInput").ap()
    rope = nc.dram_tensor("rope", [2, 64, S_], F32, kind="ExternalInput").ap()
    for l in range(nlayers):
        kas[l].declare(nc, sfx="_a%d" % l)
        kbs[l].declare(nc, sfx="_b%d" % l, fused=True)
    out = nc.dram_tensor("out", [ntok, D], F32, kind="ExternalOutput").ap()
    ybufA = [nc.dram_tensor("ybufA%d" % l, [S_, 128], F32).ap() for l in range(nlayers)]
    ybufS = [nc.dram_tensor("ybufS%d" % l, [S_, 128], F32).ap() for l in range(nlayers)]
    ygA = [nc.dram_tensor("ygA%d" % l, [4 * S_, 128], F32).ap() for l in range(nlayers)]
    ygS = [nc.dram_tensor("ygS%d" % l, [4 * S_, 128], F32).ap() for l in range(nlayers)]
    xq = nc.dram_tensor("xq", [ntok, D], F32).ap()
    xgc = nc.dram_tensor("xgc", [S_, D], F32).ap()
    xq0 = nc.dram_tensor("xq0", [ntok, D], F32).ap()
    yselA = nc.dram_tensor("yselA", [4 * ntok, 128], F32).ap()
    yselS = nc.dram_tensor("yselS", [4 * ntok, 128], F32).ap()
    P = Prog(nc, n_chan=16)
    sh = KA.make_shared(nc, P)
    groups = [[0, 1, 2, 3], [4, 5, 6, 7]]

    def xg_rows(t0):
        r = t0 // ntok; w = t0 % ntok; k = w // CRX; i = w % CRX
        row = (k * 4 + r) * CRX + i
        return xgc[row:row + 128, :]

    for l in range(nlayers):
        xsrc = x_in if l == 0 else xg_rows
        YA = ybufA[l].rearrange("s (b c) -> s b c", b=2); YS = ybufS[l].rearrange("s (b c) -> s b c", b=2)
        Ymap = {0: YS[:, 0, :], 1: YA[:, 0, :], 2: YA[:, 1, :], 3: YS[:, 1, :]}

        def after_attn(l=l):
            for k in range(4):
                P.collective("AllGather", groups, ybufA[l][k * CRY:(k + 1) * CRY, :], ygA[l][k * 4 * CRY:(k + 1) * 4 * CRY, :], block=False)
        kas[l].emit(nc, P, sh, xsrc, rope, Ymap, after_attn=after_attn)
        for k in range(4):
            P.collective("AllGather", groups, ybufS[l][k * CRY:(k + 1) * CRY, :], ygS[l][k * 4 * CRY:(k + 1) * 4 * CRY, :], block=False)
        P.collective_wait()
        P.phase_begin()
        for (yg_, ysel_, key) in ((ygA[l], yselA, "yselA"), (ygS[l], yselS, "yselS")):
            ygv = yg_.rearrange("(a b) c -> a (b c)", b=32)
            P.dma(ysel_.rearrange("(a b) c -> a (b c)", b=32),
                  (lambda e, ygv=ygv: ygv[bass.ds(P.qid(e) * (4 * CRY // 32), 4 * CRY // 32), :]), writes=[key])
        if l == 0:
            P.dma(xq0, (lambda e: x_in[bass.ds(P.qid(e) * ntok, ntok), :]), writes=["xq0"])
        P.phase_end()
        P.phase_begin()
        if l == 0:
            xfn = lambda e, r0, n: xq0[r0:r0 + n, :]
        else:
            xfn = lambda e, r0, n: xq[r0:r0 + n, :]
        yfn = lambda e, grp, r, r0, n: (yselA if grp == "A" else yselS)[r * ntok + r0:r * ntok + r0 + n, :]
        kbs[l].ysplit = True
        def after_block(blk, l=l):
            if l < nlayers - 1:
                for k in range(blk * tb // CRX, (blk + 1) * tb // CRX):
                    P.collective("AllGather", groups, xq[k * CRX:(k + 1) * CRX, :], xgc[k * 4 * CRX:(k + 1) * 4 * CRX, :],
                                 reads=["O"], block=False)
        kbs[l].emit(nc, P, sh["pb"], sh["ident"], xfn, (xq if l < nlayers - 1 else out), yfn, after_block=after_block)
        P.phase_end()
        if l < nlayers - 1:
            P.collective_wait()
    P.wait_all_dma("sync")
    P.emit()
    return nc


def make_inputs_fused(inp, core, nlayers=2):
    b, h = core // 4, core % 4
    ntok = S // 4
    m = {"x": np.ascontiguousarray(inp["x"][b]), "rope": rope_tables()}
    for l in range(nlayers):
        a = make_inputs(inp, l, core)
        for k_, v in a.items():
            if k_ != "rope":
                m[k_ + "_a%d" % l] = v
        d = make_inputs_b(inp, l, 0, None, None, ntok=ntok, fused=True, sfx="_b%d" % l)
        p = inp["p"][l][b].reshape(S, 2, 128)[h * ntok:(h + 1) * ntok]
        d["pT_b%d" % l] = np.ascontiguousarray(p.transpose(1, 2, 0))
        m.update(d)
    return m


_CACHE = {}


def kernel(**inputs):
    inp = {k: np.asarray(v) for k, v in inputs.items()}
    if "nc" not in _CACHE:
        _CACHE["nc"] = build_fused()
    cores = list(range(8))
    maps = [make_inputs_fused(inp, core) for core in cores]
    res = run_bass_kernel_spmd(_CACHE["nc"], maps, core_ids=cores)
    out = np.concatenate([res.results[c]["out"] for c in cores], axis=0)
    return out.reshape(2, S, 1024).astype(np.float32)
```
